# Optimizing a Trainium2 kernel written in Bass

```python
import jax
import jax.numpy as jnp
from jax import lax
import numpy as np

D_MODEL = 1024
BATCH = 1
SEQ = 16384
DEPTH = 2

GRID_W = 64
CTX_LEN = 256
N_MOD = 6
EPS = 1e-6
NEG = -1e30

CONV_CH = 256
CONV_W = 3
DN_HEADS = 6
DN_HEAD_DIM = 64
DN_DIM = DN_HEADS * DN_HEAD_DIM
DN_CONV_W = 3
DN_CHUNK = 64
ATT_HEADS = 6
ATT_KV_HEADS = 2
ATT_GROUP = ATT_HEADS // ATT_KV_HEADS
HEAD_DIM = 64
ATT_DIM = ATT_HEADS * HEAD_DIM
ATT_KV_DIM = ATT_KV_HEADS * HEAD_DIM
WINDOW = 128
ATT_BLOCK = 128
ROPE_BASE = 10000.0
AXIS_DIM = HEAD_DIM // 2

MIX_DIM = CONV_CH + DN_DIM + ATT_DIM
IN_SPLITS = (CONV_CH, CONV_CH, CONV_CH, 3 * DN_DIM, DN_DIM, 2 * DN_HEADS, 2 * DN_HEADS, ATT_DIM, ATT_KV_DIM, ATT_KV_DIM)
IN_COLS = 3 * CONV_CH + 4 * DN_DIM + 4 * DN_HEADS + ATT_DIM + 2 * ATT_KV_DIM

D_FF = 2816
N_EXPERTS = 8
TOP_K = 2
D_FF_EXPERT = 3584
MOE_BLOCK = 128
N_DENSE = (DEPTH + 1) // 2
N_MOE = DEPTH // 2

kernel_name = 'hybrid_conv_deltanet_swa_moe_dit'

F32 = jnp.float32


def rmsnorm(x, g=None):
    xf = x.astype(F32)
    y = xf * lax.rsqrt(jnp.mean(xf * xf, axis=-1, keepdims=True) + EPS)
    if g is not None:
        y = y * g.astype(F32)
    return y.astype(x.dtype)


def modulate(h, shift, scale):
    return h * (1 + scale) + shift


def l2norm(t):
    return t * lax.rsqrt(jnp.sum(t * t, axis=-1, keepdims=True) + 1e-6)


def split_proj(p):
    idx, acc = [], 0
    for s in IN_SPLITS[:-1]:
        acc += s
        idx.append(acc)
    return jnp.split(p, idx, axis=-1)


def dwconv_centred(u, w):
    k = w.shape[0]
    return lax.conv_general_dilated(u, w[:, None, :].astype(u.dtype), window_strides=(1,),
                                    padding=[(k // 2, k // 2)], dimension_numbers=('NWC', 'WIO', 'NWC'),
                                    feature_group_count=u.shape[-1])


def axial_rope_tables(n):
    rows = n // GRID_W
    r = jnp.broadcast_to(jnp.arange(rows, dtype=F32)[:, None], (rows, GRID_W)).reshape(n)
    col = jnp.broadcast_to(jnp.arange(GRID_W, dtype=F32)[None, :], (rows, GRID_W)).reshape(n)
    inv = ROPE_BASE ** (-jnp.arange(0, AXIS_DIM, 2, dtype=F32) / AXIS_DIM)
    ang = jnp.stack([r[:, None] * inv, col[:, None] * inv], axis=1)
    return jnp.cos(ang), jnp.sin(ang)


def apply_axial_rope(x, cos, sin):
    b, n, h, d = x.shape
    xr = x.astype(F32).reshape(b, n, h, 2, 2, AXIS_DIM // 2)
    x1, x2 = xr[..., 0, :], xr[..., 1, :]
    cs, sn = cos[None, :, None], sin[None, :, None]
    out = jnp.stack([x1 * cs - x2 * sn, x2 * cs + x1 * sn], axis=-2)
    return out.reshape(b, n, h, d).astype(x.dtype)


def window_attention(q, k, v, kc, vc, sink):
    b, n = q.shape[:2]
    nb = n // ATT_BLOCK
    qb = q.astype(F32).reshape(b, nb, ATT_BLOCK, ATT_KV_HEADS, ATT_GROUP, HEAD_DIM) * HEAD_DIM ** -0.5

    def band(t):
        tp = jnp.pad(t.astype(F32), ((0, 0), (ATT_BLOCK, ATT_BLOCK), (0, 0), (0, 0)))
        tp = tp.reshape(b, nb + 2, ATT_BLOCK, ATT_KV_HEADS, HEAD_DIM)
        return jnp.concatenate([tp[:, :-2], tp[:, 1:-1], tp[:, 2:]], axis=2)

    kw, vw = band(k), band(v)
    s_loc = jnp.einsum('bnqhgd,bnkhd->bnhgqk', qb, kw)
    qpos = jnp.arange(nb)[:, None] * ATT_BLOCK + jnp.arange(ATT_BLOCK)[None]
    kpos = (jnp.arange(nb)[:, None] - 1) * ATT_BLOCK + jnp.arange(3 * ATT_BLOCK)[None]
    valid = ((jnp.abs(qpos[:, :, None] - kpos[:, None, :]) <= WINDOW)
             & (kpos[:, None, :] >= 0) & (kpos[:, None, :] < n))
    s_loc = jnp.where(valid[None, :, None, None], s_loc, NEG)
    s_ctx = jnp.einsum('bnqhgd,blhd->bnhgql', qb, kc.astype(F32))
    s_sink = sink.astype(F32).reshape(1, 1, ATT_KV_HEADS, ATT_GROUP, 1, 1)
    m = jnp.maximum(jnp.maximum(s_loc.max(-1, keepdims=True), s_ctx.max(-1, keepdims=True)), s_sink)
    p_loc = jnp.exp(s_loc - m)
    p_ctx = jnp.exp(s_ctx - m)
    denom = p_loc.sum(-1, keepdims=True) + p_ctx.sum(-1, keepdims=True) + jnp.exp(s_sink - m)
    o = (jnp.einsum('bnhgqk,bnkhd->bnhgqd', p_loc, vw)
         + jnp.einsum('bnhgql,blhd->bnhgqd', p_ctx, vc.astype(F32))) / denom
    return o.transpose(0, 1, 4, 2, 3, 5).reshape(b, n, ATT_DIM).astype(q.dtype)


def context_attention(q, k, v, sink):
    b, l = q.shape[:2]
    qg = q.astype(F32).reshape(b, l, ATT_KV_HEADS, ATT_GROUP, HEAD_DIM) * HEAD_DIM ** -0.5
    s = jnp.einsum('blhgd,bmhd->bhglm', qg, k.astype(F32))
    s_sink = jnp.broadcast_to(sink.astype(F32).reshape(1, ATT_KV_HEADS, ATT_GROUP, 1, 1), s.shape[:-1] + (1,))
    p = jax.nn.softmax(jnp.concatenate([s, s_sink], axis=-1), axis=-1)[..., :-1]
    o = jnp.einsum('bhglm,bmhd->blhgd', p, v.astype(F32))
    return o.reshape(b, l, ATT_DIM).astype(q.dtype)


def delta_prep(qkv, a, bt, conv_w, a_log, dt_bias):
    b, n, _ = qkv.shape
    qkv = jax.nn.silu(dwconv_centred(qkv, conv_w)).astype(F32).reshape(b, n, 3, DN_HEADS, DN_HEAD_DIM)
    q = l2norm(qkv[:, :, 0]) * DN_HEAD_DIM ** -0.5
    k = l2norm(qkv[:, :, 1])
    v = qkv[:, :, 2]
    a = a.astype(F32).reshape(b, n, 2, DN_HEADS)
    g = -jnp.exp(a_log.astype(F32)) * jax.nn.softplus(a + dt_bias.astype(F32))
    beta = jax.nn.sigmoid(bt.astype(F32).reshape(b, n, 2, DN_HEADS))
    return q, k, v, g, beta


def gated_delta_chunked(q, k, v, g, beta, s0):
    b, n, h, _ = q.shape
    dv = v.shape[-1]
    nc = n // DN_CHUNK

    def chunks(t):
        t = t.reshape((b, nc, DN_CHUNK, h) + t.shape[3:])
        return jnp.moveaxis(t, 3, 1)

    q, k, v, g, beta = (chunks(t) for t in (q, k, v, g, beta))
    gcum = jnp.cumsum(g, axis=-1)
    incl = jnp.tril(jnp.ones((DN_CHUNK, DN_CHUNK), bool))
    strict = jnp.tril(jnp.ones((DN_CHUNK, DN_CHUNK), bool), -1)
    diff = gcum[..., :, None] - gcum[..., None, :]
    decay = jnp.where(incl, jnp.exp(jnp.where(incl, diff, 0.0)), 0.0)
    kb = k * beta[..., None]
    a_mat = jnp.where(strict, jnp.einsum('bhcid,bhcjd->bhcij', kb, k) * decay, 0.0)
    lmat = a_mat + jnp.eye(DN_CHUNK, dtype=F32)
    rhs = jnp.concatenate([v * beta[..., None], kb * jnp.exp(gcum)[..., None]], axis=-1)
    sol = lax.linalg.triangular_solve(lmat, rhs, left_side=True, lower=True, unit_diagonal=True)
    u, w = sol[..., :dv], sol[..., dv:]
    qk = jnp.where(incl, jnp.einsum('bhcid,bhcjd->bhcij', q, k) * decay, 0.0)
    qg = q * jnp.exp(gcum)[..., None]
    kd = k * jnp.exp(gcum[..., -1:] - gcum)[..., None]
    gl = jnp.exp(gcum[..., -1])
    xs = tuple(jnp.moveaxis(t, 2, 0) for t in (qg, qk, u, w, kd, gl))

    def step(s, inp):
        qg_c, qk_c, u_c, w_c, kd_c, gl_c = inp
        v_new = u_c - jnp.einsum('bhik,bhkv->bhiv', w_c, s)
        o = jnp.einsum('bhik,bhkv->bhiv', qg_c, s) + jnp.einsum('bhij,bhjv->bhiv', qk_c, v_new)
        s = s * gl_c[..., None, None] + jnp.einsum('bhik,bhiv->bhkv', kd_c, v_new)
        return s, o

    s_fin, o = lax.scan(step, s0, xs)
    return o.transpose(1, 0, 3, 2, 4).reshape(b, n, h, dv), s_fin


def gated_delta_bidir(ctx_t, lat_t, need_ctx):
    qc, kc, vc, gc, bc = ctx_t
    qx, kx, vx, gx, bx = lat_t
    b = qx.shape[0]
    outs_x, outs_c = [], []
    for d in range(2):
        rev = (lambda t: jnp.flip(t, axis=1)) if d == 1 else (lambda t: t)
        s0 = jnp.zeros((b, DN_HEADS, DN_HEAD_DIM, DN_HEAD_DIM), F32)
        oc, s_ctx = gated_delta_chunked(rev(qc), rev(kc), rev(vc), rev(gc[:, :, d]), rev(bc[:, :, d]), s0)
        ox, _ = gated_delta_chunked(rev(qx), rev(kx), rev(vx), rev(gx[:, :, d]), rev(bx[:, :, d]), s_ctx)
        outs_x.append(rev(ox))
        outs_c.append(rev(oc))
    o_x = outs_x[0] + outs_x[1]
    o_c = outs_c[0] + outs_c[1] if need_ctx else None
    return o_x, o_c


def delta_output(o, z, norm_g):
    b, n = z.shape[:2]
    zf = z.astype(F32).reshape(b, n, DN_HEADS, DN_HEAD_DIM)
    y = rmsnorm(o, norm_g) * jax.nn.silu(zf)
    return y.reshape(b, n, DN_DIM).astype(z.dtype)


def token_mixers(px, pc, conv_w, dn_conv_w, dn_a_log, dn_dt_bias, dn_norm_g, sink, cos, sin, need_ctx):
    b, n, _ = px.shape
    l = pc.shape[1]
    xb_, xc_, xh_, xqkv, xz, xa, xbt, xq, xk, xv = split_proj(px)
    cb_, cc_, ch_, cqkv, cz, ca, cbt, cq, ck, cv = split_proj(pc)
    ya_x = xb_ * dwconv_centred(xc_ * xh_, conv_w)
    dx = delta_prep(xqkv, xa, xbt, dn_conv_w, dn_a_log, dn_dt_bias)
    dc = delta_prep(cqkv, ca, cbt, dn_conv_w, dn_a_log, dn_dt_bias)
    ob_x, ob_c = gated_delta_bidir(dc, dx, need_ctx)
    yb_x = delta_output(ob_x, xz, dn_norm_g)
    qx = apply_axial_rope(xq.reshape(b, n, ATT_HEADS, HEAD_DIM), cos, sin)
    kx = apply_axial_rope(xk.reshape(b, n, ATT_KV_HEADS, HEAD_DIM), cos, sin)
    vx = xv.reshape(b, n, ATT_KV_HEADS, HEAD_DIM)
    kc = ck.reshape(b, l, ATT_KV_HEADS, HEAD_DIM)
    vc = cv.reshape(b, l, ATT_KV_HEADS, HEAD_DIM)
    yc_x = window_attention(qx, kx, vx, kc, vc, sink)
    o_x = jnp.concatenate([ya_x, yb_x, yc_x], axis=-1)
    if not need_ctx:
        return o_x, None
    ya_c = cb_ * dwconv_centred(cc_ * ch_, conv_w)
    yb_c = delta_output(ob_c, cz, dn_norm_g)
    yc_c = context_attention(cq.reshape(b, l, ATT_HEADS, HEAD_DIM), kc, vc, sink)
    o_c = jnp.concatenate([ya_c, yb_c, yc_c], axis=-1)
    return o_x, o_c


def swiglu(h, w_gate, w_up, w_down):
    return (jax.nn.silu(h @ w_gate) * (h @ w_up)) @ w_down


def moe_swiglu(h, w_router, w_gate, w_up, w_down):
    t, d = h.shape
    logits = h.astype(F32) @ w_router.astype(F32)
    top_logit, top_e = lax.top_k(logits, TOP_K)
    gates = jax.nn.softmax(top_logit, axis=-1)
    a = t * TOP_K
    flat_e = top_e.reshape(a)
    flat_tok = jnp.arange(a, dtype=jnp.int32) // TOP_K
    flat_g = gates.reshape(a)
    order = jnp.argsort(flat_e)
    e_sorted = flat_e[order]
    counts = jnp.zeros((N_EXPERTS,), jnp.int32).at[flat_e].add(1)
    starts = jnp.cumsum(counts) - counts
    padded = (counts + MOE_BLOCK - 1) // MOE_BLOCK * MOE_BLOCK
    pad_ends = jnp.cumsum(padded)
    pad_starts = pad_ends - padded
    dest = pad_starts[e_sorted] + jnp.arange(a, dtype=jnp.int32) - starts[e_sorted]
    cap = -(-a // MOE_BLOCK) * MOE_BLOCK + N_EXPERTS * MOE_BLOCK
    n_blk = cap // MOE_BLOCK
    row_tok = jnp.zeros((cap,), jnp.int32).at[dest].set(flat_tok[order])
    row_gate = jnp.zeros((cap,), h.dtype).at[dest].set(flat_g[order].astype(h.dtype))
    blk_e = jnp.minimum(jnp.searchsorted(pad_ends, jnp.arange(n_blk, dtype=jnp.int32) * MOE_BLOCK, side='right'),
                        N_EXPERTS - 1)
    xb = h[row_tok].reshape(n_blk, MOE_BLOCK, d)

    def block_ffn(args):
        xblk, e = args
        return swiglu(xblk, w_gate[e], w_up[e], w_down[e])

    yb = lax.map(block_ffn, (xb, blk_e))
    return jax.ops.segment_sum(yb.reshape(cap, d) * row_gate[:, None], row_tok, num_segments=t)


def channel_mixer(h, layer, ffn_w_gate, ffn_w_up, ffn_w_down, moe_router, moe_w_gate, moe_w_up, moe_w_down):
    i = layer // 2
    if layer % 2 == 0:
        return swiglu(h, ffn_w_gate[i], ffn_w_up[i], ffn_w_down[i])
    return moe_swiglu(h, moe_router[i], moe_w_gate[i], moe_w_up[i], moe_w_down[i])


def setup_inputs(seed: int = 0) -> dict:
    key = jax.random.key(seed)
    ks = jax.random.split(key, 24)

    def nrm(k, shape, scale):
        return jax.random.normal(k, shape, F32) * scale

    dt = jnp.exp(jax.random.uniform(ks[11], (DEPTH, 2, DN_HEADS), F32, jnp.log(1e-3), jnp.log(1e-1)))
    return {
        'x': nrm(ks[0], (BATCH, SEQ, D_MODEL), 1.0),
        'c': nrm(ks[1], (BATCH, D_MODEL), 1.0),
        'ctx': nrm(ks[2], (BATCH, CTX_LEN, D_MODEL), 1.0),
        'c_ctx': nrm(ks[3], (D_MODEL,), 1.0),
        'w_mod': nrm(ks[4], (DEPTH, D_MODEL, N_MOD * D_MODEL), 0.5 * D_MODEL ** -0.5),
        'b_mod': nrm(ks[5], (DEPTH, N_MOD * D_MODEL), 0.02),
        'w_in': nrm(ks[6], (DEPTH, D_MODEL, IN_COLS), D_MODEL ** -0.5),
        'w_out': nrm(ks[7], (DEPTH, MIX_DIM, D_MODEL), MIX_DIM ** -0.5),
        'conv_w': nrm(ks[8], (DEPTH, CONV_W, CONV_CH), CONV_W ** -0.5),
        'dn_conv_w': nrm(ks[9], (DEPTH, DN_CONV_W, 3 * DN_DIM), DN_CONV_W ** -0.5),
        'dn_a_log': jnp.log(jax.random.uniform(ks[10], (DEPTH, 2, DN_HEADS), F32, 1.0, 16.0)),
        'dn_dt_bias': dt + jnp.log(-jnp.expm1(-dt)),
        'dn_norm_g': 1.0 + nrm(ks[12], (DEPTH, DN_HEAD_DIM), 0.02),
        'attn_sink': nrm(ks[13], (DEPTH, ATT_HEADS), 0.5),
        'ffn_w_gate': nrm(ks[14], (N_DENSE, D_MODEL, D_FF), D_MODEL ** -0.5),
        'ffn_w_up': nrm(ks[15], (N_DENSE, D_MODEL, D_FF), D_MODEL ** -0.5),
        'ffn_w_down': nrm(ks[16], (N_DENSE, D_FF, D_MODEL), D_FF ** -0.5),
        'moe_router': nrm(ks[17], (N_MOE, D_MODEL, N_EXPERTS), D_MODEL ** -0.5),
        'moe_w_gate': nrm(ks[18], (N_MOE, N_EXPERTS, D_MODEL, D_FF_EXPERT), D_MODEL ** -0.5),
        'moe_w_up': nrm(ks[19], (N_MOE, N_EXPERTS, D_MODEL, D_FF_EXPERT), D_MODEL ** -0.5),
        'moe_w_down': nrm(ks[20], (N_MOE, N_EXPERTS, D_FF_EXPERT, D_MODEL), D_FF_EXPERT ** -0.5),
        'final_norm_g': 1.0 + nrm(ks[21], (D_MODEL,), 0.02),
    }


def reference(x, c, ctx, c_ctx, w_mod, b_mod, w_in, w_out, conv_w, dn_conv_w, dn_a_log, dn_dt_bias, dn_norm_g,
              attn_sink, ffn_w_gate, ffn_w_up, ffn_w_down, moe_router, moe_w_gate, moe_w_up, moe_w_down,
              final_norm_g):
    b, n, d = x.shape
    ctx_len = ctx.shape[1]
    cos, sin = axial_rope_tables(n)
    s_c = jax.nn.silu(c)
    s_cc = jax.nn.silu(c_ctx)
    hc = ctx
    for layer in range(DEPTH):
        need_ctx = layer < DEPTH - 1
        mod_x = (s_c @ w_mod[layer] + b_mod[layer]).reshape(b, N_MOD, 1, d)
        mod_c = (s_cc @ w_mod[layer] + b_mod[layer]).reshape(N_MOD, d)
        px = modulate(rmsnorm(x), mod_x[:, 0], mod_x[:, 1]) @ w_in[layer]
        pc = modulate(rmsnorm(hc), mod_c[0], mod_c[1]) @ w_in[layer]
        o_x, o_c = token_mixers(px, pc, conv_w[layer], dn_conv_w[layer], dn_a_log[layer], dn_dt_bias[layer],
                                dn_norm_g[layer], attn_sink[layer], cos, sin, need_ctx)
        x = x + mod_x[:, 2] * (o_x @ w_out[layer])
        hx = modulate(rmsnorm(x), mod_x[:, 3], mod_x[:, 4])
        if need_ctx:
            hc = hc + mod_c[2] * (o_c @ w_out[layer])
            hcc = modulate(rmsnorm(hc), mod_c[3], mod_c[4])
            tokens = jnp.concatenate([hcc.reshape(-1, d), hx.reshape(-1, d)], axis=0)
        else:
            tokens = hx.reshape(-1, d)
        f = channel_mixer(tokens, layer, ffn_w_gate, ffn_w_up, ffn_w_down, moe_router, moe_w_gate, moe_w_up,
                          moe_w_down)
        if need_ctx:
            hc = hc + mod_c[5] * f[:b * ctx_len].reshape(b, ctx_len, d)
            f = f[b * ctx_len:]
        x = x + mod_x[:, 5] * f.reshape(b, n, d)
    return rmsnorm(x, final_norm_g)
```

```python
import numpy as np
from contextlib import ExitStack
import concourse.bass as bass
import concourse.mybir as mybir
from concourse.bass_utils import run_bass_kernel_spmd

F32 = mybir.dt.float32
BF16 = mybir.dt.bfloat16
AF = mybir.ActivationFunctionType
ALU = mybir.AluOpType
NCORES = 8

D_MODEL = 1024
SEQ = 16384
CTX = 256
EPS = 1e-6
IN_COLS = 2968


class Buf:
    __slots__ = ("name", "lw", "rd", "excl")

    def __init__(self, name, excl=False):
        self.name = name
        self.lw = None
        self.rd = {}
        self.excl = excl


class Prog:
    EPOCH = 4096
    NDMA = 24

    def __init__(self):
        self.nc = bass.Bass("TRN2", target_bir_lowering=False)
        self.es = ExitStack()
        self.q = {e: [] for e in ("sp", "pe", "dve", "act", "pool")}
        self.cnt = {e: 0 for e in self.q}
        self.seen = {e: {} for e in self.q}
        self.ndma = 0
        self.dlast = {}
        self.nt = 0

    def dram_in(self, name, shape, dt=F32):
        return self.nc.dram_tensor(name, list(shape), dt, kind="ExternalInput").ap()

    def dram_out(self, name, shape, dt=F32):
        return self.nc.dram_tensor(name, list(shape), dt, kind="ExternalOutput").ap()

    def sb(self, shape, dt=F32, name=None):
        self.nt += 1
        name = name or f"t{self.nt}"
        t = self.es.enter_context(self.nc.sbuf_tensor(name, list(shape), dt))
        return t, Buf(name)

    def psum(self, shape=(128, 512), dt=F32, name=None):
        self.nt += 1
        name = name or f"p{self.nt}"
        t = self.es.enter_context(self.nc.psum_tensor(name, list(shape), dt))
        return t, Buf(name, excl=True)

    def _deps(self, reads, writes, eng=None):
        deps = []
        for b in reads:
            if b.lw is not None:
                deps.append(b.lw)
            if b.excl:
                deps.extend(v for k_, v in b.rd.items() if k_ != eng)
        for b in writes:
            if b.lw is not None:
                deps.append(b.lw)
            deps.extend(b.rd.values())
        return deps

    def _filter(self, eng, deps):
        waits = []
        seen = self.seen[eng]
        for d in deps:
            if d[0] == "c":
                _, p, n = d
                if p == eng and eng == "pe":
                    continue
                if seen.get(p, -1) >= n:
                    continue
                seen[p] = n
            else:
                _, slot, val = d
                if seen.get(("d", slot), 0) >= val:
                    continue
                seen[("d", slot)] = val
            waits.append(d)
        return waits

    def _mark(self, tok, key, reads, writes):
        for b in reads:
            b.rd[key] = tok
        for b in writes:
            b.lw = tok
            b.rd = {}

    def op(self, eng, fn, reads=(), writes=()):
        n = self.cnt[eng]
        self.cnt[eng] += 1
        waits = self._filter(eng, self._deps(reads, writes, eng))
        tok = ("c", eng, n)
        self._mark(tok, eng, reads, writes)
        self.q[eng].append((fn, waits, tok))

    def dma(self, eng, out_ap, in_ap, reads=(), writes=()):
        j = self.ndma
        self.ndma += 1
        slot = j % self.NDMA
        val = 16 * (j // self.NDMA + 1)
        deps = self._deps(reads, writes)
        if val > 16:
            deps.append(("d", slot, val - 16))
        waits = self._filter(eng, deps)
        tok = ("d", slot, val)
        self.dlast[slot] = val
        self._mark(tok, ("d", slot), reads, writes)
        self.q[eng].append((lambda e: e.dma_start(out=out_ap, in_=in_ap), waits, tok))

    def mm(self, out, lhsT, rhs, start, stop, reads, writes):
        self.op("pe", lambda e: e.matmul(out, lhsT, rhs, start=start, stop=stop), reads, writes)

    def transpose(self, out, in_, ident, reads, writes):
        self.op("pe", lambda e: e.transpose(out, in_, ident), reads, writes)

    def act(self, out, in_, func, reads, writes, bias=None, scale=None, eng="act"):
        kw = {}
        if bias is not None:
            kw["bias"] = bias
        if scale is not None:
            kw["scale"] = scale
        self.op(eng, lambda e: e.activation(out, in_, func, **kw), reads, writes)

    def tt(self, eng, out, in0, in1, op, reads, writes):
        self.op(eng, lambda e: e.tensor_tensor(out, in0, in1, op=op), reads, writes)

    def ts(self, eng, out, in0, s1, s2, op0, op1, reads, writes):
        if s2 is None:
            self.op(eng, lambda e: e.tensor_scalar(out, in0, s1, None, op0=op0), reads, writes)
        else:
            self.op(eng, lambda e: e.tensor_scalar(out, in0, s1, s2, op0=op0, op1=op1), reads, writes)

    def stt(self, eng, out, in0, scalar, in1, op0, op1, reads, writes):
        self.op(eng, lambda e: e.scalar_tensor_tensor(out, in0, scalar, in1, op0=op0, op1=op1), reads, writes)

    def copy(self, eng, out, in_, reads, writes):
        if eng == "act":
            self.op(eng, lambda e: e.activation(out, in_, AF.Identity), reads, writes)
        else:
            self.op(eng, lambda e: e.tensor_copy(out, in_), reads, writes)

    def recip(self, out, in_, reads, writes):
        self.op("dve", lambda e: e.reciprocal(out, in_), reads, writes)

    def memset(self, eng, ap, val, writes):
        self.op(eng, lambda e: e.memset(ap, val), (), writes)

    def finish(self):
        nc = self.nc
        E = self.EPOCH
        sems = {}
        for e in ("pe", "dve", "act", "pool"):
            nep = max(1, (self.cnt[e] + E - 1) // E)
            sems[e] = [self.es.enter_context(nc.semaphore(f"s_{e}{i}")) for i in range(nep)]
        dsems = [self.es.enter_context(nc.semaphore(f"s_d{i}")) for i in range(self.NDMA)]
        fin = [("d", s, v) for s, v in sorted(self.dlast.items())]
        fin = self._filter("sp", fin)
        self.q["sp"].append((None, fin, None))

        def emit(engobj, ename):
            for fn, waits, tok in self.q[ename]:
                for w in waits:
                    if w[0] == "c":
                        engobj.wait_ge(sems[w[1]][w[2] // E], w[2] % E + 1)
                    else:
                        engobj.wait_ge(dsems[w[1]], w[2])
                if fn is None:
                    continue
                ins = fn(engobj)
                if tok[0] == "c":
                    ins.then_inc(sems[ename][tok[2] // E], 1)
                else:
                    ins.then_inc(dsems[tok[1]], 16)

        with nc.Block() as block:
            @block.sync
            def _(e):
                emit(e, "sp")

            @block.tensor
            def _(e):
                emit(e, "pe")

            @block.vector
            def _(e):
                emit(e, "dve")

            @block.scalar
            def _(e):
                emit(e, "act")

            @block.gpsimd
            def _(e):
                emit(e, "pool")
        self.es.close()
        return nc


def run(prog_nc, in_maps):
    res = run_bass_kernel_spmd(prog_nc, in_maps, core_ids=list(range(NCORES)))
    return res.results


def c_(a):
    return np.ascontiguousarray(a, dtype=np.float32)


def build_A():
    P = Prog()
    s_in = P.dram_in("s_in", [128, 8, 2])
    wm = P.dram_in("wm", [2, 1024, 6144])
    bm = P.dram_in("bm", [2, 128, 48])
    out = P.dram_out("modT", [2, 128, 2, 48])
    s_t, s_b = P.sb([128, 8, 2])
    sg_t, sg_b = P.sb([128, 8, 2])
    sb_t, sb_b = P.sb([128, 8, 2], BF16)
    bm_t, bm_b = P.sb([128, 2, 48])
    o_t, o_b = P.sb([128, 2, 2, 48])
    P.dma("sp", s_t[:], s_in[:, :, :], (), (s_b,))
    P.dma("sp", bm_t[:], bm.rearrange("l p c -> p l c"), (), (bm_b,))
    P.act(sg_t[:], s_t[:], AF.Sigmoid, (s_b,), (sg_b,))
    P.tt("dve", sb_t[:], s_t[:], sg_t[:], ALU.mult, (s_b, sg_b), (sb_b,))
    w_t, w_b = P.sb([128, 8, 6144], BF16, "wmod_sb")
    pss = [P.psum([128, 512]) for _ in range(2)]
    for l in range(2):
        ps_t, ps_b = pss[l]
        for kc in range(8):
            P.dma("pool", w_t[:, kc, :], wm[l, kc * 128:(kc + 1) * 128, :], (), (w_b,))
        for dc in range(48):
            for kc in range(8):
                P.mm(ps_t[:, dc * 2:dc * 2 + 2], w_t[:, kc, dc * 128:(dc + 1) * 128], sb_t[:, kc, :],
                     kc == 0, kc == 7, (w_b, sb_b), (ps_b,))
        for j in range(2):
            P.tt("dve", o_t[:, l, j, :], ps_t[:, j:96:2], bm_t[:, l, :], ALU.add, (ps_b, bm_b), (o_b,))
    P.dma("sp", out.rearrange("l p j c -> p l j c"), o_t[:], (o_b,), ())
    return P.finish()


def stage_A(c, c_ctx, w_mod, b_mod):
    s = np.stack([c.reshape(1024), c_ctx.reshape(1024)], axis=-1)
    s_in = c_(s.reshape(8, 128, 2).transpose(1, 0, 2))
    bm = c_(b_mod.reshape(2, 48, 128).transpose(0, 2, 1))
    nc = build_A()
    im = {"s_in": s_in, "wm": c_(w_mod), "bm": bm}
    res = run(nc, [im] * NCORES)
    return res[0]["modT"]


NCH_IN = 24
TT = 256


def build_B(ntl, ntc):
    nt = ntl + ntc
    P = Prog()
    xT = P.dram_in("xT", [8, 128, nt])
    w = P.dram_in("w", [1024, NCH_IN * 128])
    mod = P.dram_in("mod", [128, 2, 2, 8])
    out = P.dram_out("pxT", [NCH_IN, 128, nt])
    w_t, w_b = P.sb([128, 8, NCH_IN * 128], BF16, "w_sb")
    for kc in range(8):
        P.dma("pool", w_t[:, kc, :], w[kc * 128:(kc + 1) * 128, :], (), (w_b,))
    mod_t, mod_b = P.sb([128, 2, 2, 8])
    P.dma("sp", mod_t[:], mod[:, :, :, :], (), (mod_b,))
    a_t, a_b = P.sb([128, 2, 8])
    P.ts("dve", a_t[:], mod_t[:, :, 1, :], 1.0, None, ALU.add, None, (mod_b,), (a_b,))
    ones_t, ones_b = P.sb([128, 128], BF16)
    P.memset("dve", ones_t[:], 1.0, (ones_b,))
    xs = [P.sb([128, 8, TT]) for _ in range(2)]
    sqs = [P.sb([128, 8, TT], BF16) for _ in range(2)]
    hs = [P.sb([128, 8, TT], BF16) for _ in range(2)]
    rs = [P.sb([128, TT]) for _ in range(2)]
    tmp = [P.sb([128, TT]) for _ in range(2)]
    os_ = [P.sb([128, TT]) for _ in range(4)]
    pn = P.psum([128, 512])
    pp = [P.psum([128, 512]) for _ in range(4)]
    for it in range(nt // TT):
        j = 0 if it * TT < ntl else 1
        x_t, x_b = xs[it % 2]
        sq_t, sq_b = sqs[it % 2]
        h_t, h_b = hs[it % 2]
        r_t, r_b = rs[it % 2]
        P.dma("sp", x_t[:], xT[:, :, it * TT:(it + 1) * TT].rearrange("c p t -> p c t"), (), (x_b,))
        P.act(sq_t[:], x_t[:], AF.Square, (x_b,), (sq_b,))
        for kc in range(8):
            P.mm(pn[0][:, 0:TT], ones_t[:], sq_t[:, kc, :], kc == 0, kc == 7, (ones_b, sq_b), (pn[1],))
        P.act(r_t[:], pn[0][:, 0:TT], AF.Sqrt, (pn[1],), (r_b,), bias=EPS, scale=1.0 / 1024)
        P.recip(r_t[:], r_t[:], (r_b,), (r_b,))
        for kc in range(8):
            t_t, t_b = tmp[kc % 2]
            P.stt("dve", t_t[:], x_t[:, kc, :], a_t[:, j, kc:kc + 1], r_t[:], ALU.mult, ALU.mult,
                  (x_b, a_b, r_b), (t_b,))
            P.act(h_t[:, kc, :], t_t[:], AF.Identity, (t_b, mod_b), (h_b,), bias=mod_t[:, j, 0, kc:kc + 1])
        for oc in range(NCH_IN):
            ps_t, ps_b = pp[oc % 4]
            for kc in range(8):
                P.mm(ps_t[:, 0:TT], w_t[:, kc, oc * 128:(oc + 1) * 128], h_t[:, kc, :], kc == 0, kc == 7,
                     (w_b, h_b), (ps_b,))
            o_t, o_b = os_[oc % 4]
            P.copy("act" if oc % 2 else "dve", o_t[:], ps_t[:, 0:TT], (ps_b,), (o_b,))
            P.dma("sp", out[oc, :, it * TT:(it + 1) * TT], o_t[:], (o_b,), ())
    return P.finish()


def perm_w_in(w_in_l):
    z = np.zeros((1024, 104), np.float32)
    return c_(np.concatenate([w_in_l[:, 0:2304], w_in_l[:, 2328:2968], w_in_l[:, 2304:2328], z], axis=1))


def stage_B(xT_all, hcT, w_in_l, modT_l):
    ntl = SEQ // NCORES
    nc = build_B(ntl, CTX)
    w = perm_w_in(w_in_l)
    m = modT_l.reshape(128, 2, 6, 8)
    mod = c_(m[:, :, 0:2, :])
    ims = []
    for i in range(NCORES):
        xt = np.concatenate([xT_all[:, i * ntl:(i + 1) * ntl], hcT], axis=1)
        ims.append({"xT": c_(xt.reshape(8, 128, ntl + CTX)), "w": w, "mod": mod})
    res = run(nc, ims)
    px = np.concatenate([r["pxT"][:, :, :ntl].reshape(NCH_IN * 128, ntl) for r in res], axis=1)
    pc = res[0]["pxT"][:, :, ntl:].reshape(NCH_IN * 128, CTX)
    return px, pc


GRID_W = 64
NEGV = -30000.0


def rope_tables(pos):
    pos = np.asarray(pos)
    inv = 10000.0 ** (-(np.arange(0, 32, 2, dtype=np.float64)) / 32.0)
    r = (pos // GRID_W).astype(np.float64)
    col = (pos % GRID_W).astype(np.float64)
    cos = np.zeros((64, len(pos)))
    sin = np.zeros((64, len(pos)))
    for d in range(64):
        axis, half, f = d // 32, (d % 32) // 16, d % 16
        p = r if axis == 0 else col
        cos[d] = np.cos(p * inv[f])
        sin[d] = np.sin(p * inv[f]) * (-1.0 if half == 0 else 1.0)
    return cos, sin


ROPE_PERM = np.array([(d // 32) * 32 + ((d % 32) + 16) % 32 for d in range(64)])


def build_C1(ntl, need_ctx):
    nqb = ntl // 128
    nkb = nqb + 2
    nk = nkb * 128
    P = Prog()
    cv = P.dram_in("cv", [3, 2, 128, ntl + 2])
    cw = P.dram_in("cw", [128, 2, 3])
    q_in = P.dram_in("q", [2, 6, 64, ntl])
    k_in = P.dram_in("k", [2, 2, 64, nk])
    v_in = P.dram_in("v", [128, nkb, 128])
    cs_k = P.dram_in("cs_k", [2, 64, nk])
    kc_in = P.dram_in("kc", [2, 64, CTX])
    vc_in = P.dram_in("vc", [128, 2, 128])
    neg_in = P.dram_in("neg", [128, 4, 384])
    sink_in = P.dram_in("sink", [1, 2, 384])
    ident_in = P.dram_in("ident", [128, 128])
    ya = P.dram_out("yaT", [2, 128, ntl])
    yc = P.dram_out("ycT", [6, 64, ntl])
    if need_ctx:
        cvc = P.dram_in("cvc", [3, 2, 128, CTX + 2])
        qc_in = P.dram_in("qc", [6, 64, CTX])
        ya_c = P.dram_out("yaTc", [2, 128, CTX])
        yc_c = P.dram_out("ycTc", [6, 64, CTX])
    dn_in = P.dram_in("dn", [9, 128, ntl + 2])
    dnc_in = P.dram_in("dnc", [9, 128, CTX + 2])
    dcw_in = P.dram_in("dcw", [128, 9, 3])
    gab_in = P.dram_in("gab", [2, 12, ntl])
    gabc_in = P.dram_in("gabc", [2, 12, CTX])
    gpar_in = P.dram_in("gpar", [12, 2])
    bd_in = P.dram_in("bd", [128, 128])
    dn_out = P.dram_out("dno", [9, 128, ntl])
    dnc_out = P.dram_out("dnco", [9, 128, CTX])
    gb_out = P.dram_out("gbo", [2, 12, ntl])
    gbc_out = P.dram_out("gbco", [2, 12, CTX])

    cw_t, cw_b = P.sb([128, 2, 3])
    P.dma("sp", cw_t[:], cw[:, :, :], (), (cw_b,))
    ident_t, ident_b = P.sb([128, 128], BF16)
    P.dma("pool", ident_t[:], ident_in[:, :], (), (ident_b,))
    neg_t, neg_b = P.sb([128, 4, 384], BF16)
    P.dma("pool", neg_t[:], neg_in[:, :, :], (), (neg_b,))
    ones_t, ones_b = P.sb([128, 64], BF16)
    P.memset("dve", ones_t[:], 1.0, (ones_b,))
    onesf_t, onesf_b = P.sb([1, 64])
    P.memset("dve", onesf_t[:], 1.0, (onesf_b,))
    sk_t, sk_b = P.sb([1, 2, 384])
    P.dma("sp", sk_t[:], sink_in[:, :, :], (), (sk_b,))
    esk_t, esk_b = P.sb([1, 2, 384])
    P.act(esk_t[:], sk_t[:], AF.Exp, (sk_b,), (esk_b,))

    cb_t, b_b = P.sb([128, ntl + 2])
    cc_t, c_b = P.sb([128, ntl + 2])
    chh_t, h_b = P.sb([128, ntl + 2])

    def conv(src, n, dst):
        for ch in range(2):
            b_t, c_t, h_t = cb_t[:, 0:n + 2], cc_t[:, 0:n + 2], chh_t[:, 0:n + 2]
            P.dma("sp", b_t, src[0, ch, :, :], (), (b_b,))
            P.dma("sp", c_t, src[1, ch, :, :], (), (c_b,))
            P.dma("sp", h_t, src[2, ch, :, :], (), (h_b,))
            P.tt("dve", c_t, c_t, h_t, ALU.mult, (c_b, h_b), (c_b,))
            P.ts("dve", h_t[:, 0:n], c_t[:, 1:n + 1], cw_t[:, ch, 1:2], None, ALU.mult, None, (c_b, cw_b), (h_b,))
            P.stt("dve", h_t[:, 0:n], c_t[:, 0:n], cw_t[:, ch, 0:1], h_t[:, 0:n], ALU.mult, ALU.add,
                  (c_b, cw_b, h_b), (h_b,))
            P.stt("dve", h_t[:, 0:n], c_t[:, 2:n + 2], cw_t[:, ch, 2:3], h_t[:, 0:n], ALU.mult, ALU.add,
                  (c_b, cw_b, h_b), (h_b,))
            P.tt("dve", h_t[:, 0:n], h_t[:, 0:n], b_t[:, 1:n + 1], ALU.mult, (h_b, b_b), (h_b,))
            P.dma("sp", dst[ch, :, :], h_t[:, 0:n], (h_b,), ())
    conv(cv, ntl, ya)
    if need_ctx:
        conv(cvc, CTX, ya_c)

    dcw_t, dcw_b = P.sb([128, 9, 3])
    P.dma("sp", dcw_t[:], dcw_in[:, :, :], (), (dcw_b,))
    bd_t, bd_b = P.sb([128, 128])
    P.dma("sp", bd_t[:], bd_in[:, :], (), (bd_b,))
    rr_t, rr_b = P.sb([128, 512])
    pps = P.psum([128, 512])

    def dnprep(src, n, dst):
        for ch in range(9):
            x_t, y_t, s_t = cb_t[:, 0:n + 2], cc_t[:, 0:n], chh_t[:, 0:n]
            x_b, y_b, s_b = b_b, c_b, h_b
            P.dma("sp", x_t, src[ch, :, :], (), (x_b,))
            P.ts("dve", y_t, x_t[:, 1:n + 1], dcw_t[:, ch, 1:2], None, ALU.mult, None, (x_b, dcw_b), (y_b,))
            P.stt("dve", y_t, x_t[:, 0:n], dcw_t[:, ch, 0:1], y_t, ALU.mult, ALU.add, (x_b, dcw_b, y_b), (y_b,))
            P.stt("dve", y_t, x_t[:, 2:n + 2], dcw_t[:, ch, 2:3], y_t, ALU.mult, ALU.add, (x_b, dcw_b, y_b), (y_b,))
            P.act(y_t, y_t, AF.Silu, (y_b,), (y_b,))
            if ch < 6:
                P.act(s_t, y_t, AF.Square, (y_b,), (s_b,))
                for c0 in range(0, n, 512):
                    w_ = min(512, n - c0)
                    P.mm(pps[0][:, 0:w_], bd_t[:], s_t[:, c0:c0 + w_], True, True, (bd_b, s_b), (pps[1],))
                    P.act(rr_t[:, 0:w_], pps[0][:, 0:w_], AF.Sqrt, (pps[1],), (rr_b,), bias=1e-6)
                    P.recip(rr_t[:, 0:w_], rr_t[:, 0:w_], (rr_b,), (rr_b,))
                    P.stt("dve", y_t[:, c0:c0 + w_], y_t[:, c0:c0 + w_], 0.125 if ch < 3 else 1.0, rr_t[:, 0:w_],
                          ALU.mult, ALU.mult, (y_b, rr_b), (y_b,))
            P.dma("sp", dst[ch, :, :], y_t, (y_b,), ())
    dnprep(dn_in, ntl, dn_out)
    dnprep(dnc_in, CTX, dnc_out)
    gpar_t, gpar_b = P.sb([12, 2])
    P.dma("sp", gpar_t[:], gpar_in[:, :], (), (gpar_b,))
    nea_t, nea_b = P.sb([12, 1])
    P.act(nea_t[:], gpar_t[:, 0:1], AF.Exp, (gpar_b,), (nea_b,))
    P.ts("dve", nea_t[:], nea_t[:], -1.0, None, ALU.mult, None, (nea_b,), (nea_b,))

    def gates(src, n, dst):
        a_t, bt_t = cb_t[0:12, 0:n], cc_t[0:12, 0:n]
        P.dma("sp", a_t, src[0, :, :], (), (b_b,))
        P.dma("sp", bt_t, src[1, :, :], (), (c_b,))
        P.act(a_t, a_t, AF.Exp, (b_b, gpar_b), (b_b,), bias=gpar_t[:, 1:2])
        P.act(a_t, a_t, AF.Ln, (b_b,), (b_b,), bias=1.0)
        P.ts("dve", a_t, a_t, nea_t[:, 0:1], None, ALU.mult, None, (b_b, nea_b), (b_b,))
        P.act(bt_t, bt_t, AF.Sigmoid, (c_b,), (c_b,))
        P.dma("sp", dst[0, :, :], a_t, (b_b,), ())
        P.dma("sp", dst[1, :, :], bt_t, (c_b,), ())
    gates(gab_in, ntl, gb_out)
    gates(gabc_in, CTX, gbc_out)

    csk_t, csk_b = P.sb([64, 2, nk])
    P.dma("sp", csk_t[:], cs_k.rearrange("c d t -> d c t"), (), (csk_b,))
    qr_t, qr_b = P.sb([64, nqb, 2, 3, 128], BF16, "qr")
    kr_t, kr_b = P.sb([64, 2, nk], BF16, "kr")
    qa = [P.sb([64, nk]) for _ in range(2)]
    qp = [P.sb([64, nk]) for _ in range(2)]
    for h in range(6):
        a_t, a_b = qa[h % 2]
        p_t, p_b = qp[h % 2]
        P.dma("sp", a_t[:, 0:ntl], q_in[0, h, :, :], (), (a_b,))
        P.dma("sp", p_t[:, 0:ntl], q_in[1, h, :, :], (), (p_b,))
        P.tt("dve", a_t[:, 0:ntl], a_t[:, 0:ntl], csk_t[:, 0, 128:128 + ntl], ALU.mult, (a_b, csk_b), (a_b,))
        P.tt("pool", p_t[:, 0:ntl], p_t[:, 0:ntl], csk_t[:, 1, 128:128 + ntl], ALU.mult, (p_b, csk_b), (p_b,))
        P.tt("dve", qr_t[:, :, h // 3, h % 3, :], a_t[:, 0:ntl].rearrange("p (b q) -> p b q", q=128),
             p_t[:, 0:ntl].rearrange("p (b q) -> p b q", q=128), ALU.add, (a_b, p_b), (qr_b,))
    for g in range(2):
        a_t, a_b = qa[g]
        p_t, p_b = qp[g]
        P.dma("sp", a_t[:], k_in[0, g, :, :], (), (a_b,))
        P.dma("sp", p_t[:], k_in[1, g, :, :], (), (p_b,))
        P.tt("dve", a_t[:], a_t[:], csk_t[:, 0, :], ALU.mult, (a_b, csk_b), (a_b,))
        P.tt("pool", p_t[:], p_t[:], csk_t[:, 1, :], ALU.mult, (p_b, csk_b), (p_b,))
        P.tt("dve", kr_t[:, g, :], a_t[:], p_t[:], ALU.add, (a_b, p_b), (kr_b,))
    kcb_t, kcb_b = P.sb([64, 2, CTX], BF16)
    P.dma("pool", kcb_t[:], kc_in.rearrange("g d t -> d g t"), (), (kcb_b,))
    v_t, v_b = P.sb([128, nkb, 128], BF16, "v_sb")
    P.dma("pool", v_t[:], v_in[:, :, :], (), (v_b,))
    vc_t, vc_b = P.sb([128, 2, 128], BF16)
    P.dma("pool", vc_t[:], vc_in[:, :, :], (), (vc_b,))
    if need_ctx:
        qcb_t, qcb_b = P.sb([64, 2, 2, 3, 128], BF16)
        for h in range(6):
            P.dma("pool", qcb_t[:, :, h // 3, h % 3, :], qc_in[h, :, :].rearrange("d (b q) -> d b q", q=128),
                  (), (qcb_b,))

    ps_s = [P.psum([128, 512]) for _ in range(5)]
    ps_o = P.psum([128, 512])
    ps_d = P.psum([128, 512])
    pts = [P.sb([128, 384], BF16) for _ in range(5)]
    rd = [P.sb([64, 384]) for _ in range(2)]
    ob = [P.sb([64, 384]) for _ in range(2)]
    cnt = [0, 0]

    def attend(qsrc, qsrc_b, qb, g, keyspecs, dst, t0):
        pts_used = []
        for (k_ap, k_b, v_ap, v_bf, negidx) in keyspecs:
            i = cnt[0] % 5
            cnt[0] += 1
            s_t, s_b = ps_s[i]
            rhs = qsrc[:, qb, g, :, :].rearrange("d h q -> d (h q)")
            P.mm(s_t[:, 0:384], k_ap, rhs, True, negidx is None, (k_b, qsrc_b), (s_b,))
            if negidx is not None:
                P.mm(s_t[:, 0:384], ident_t[:], neg_t[:, negidx, :], False, True, (ident_b, neg_b), (s_b,))
            p_t, p_b = pts[i]
            P.act(p_t[:], s_t[:, 0:384], AF.Exp, (s_b,), (p_b,), scale=0.125)
            pts_used.append((p_t, p_b, v_ap, v_bf))
        n = len(pts_used)
        for idx, (p_t, p_b, v_ap, v_bf) in enumerate(pts_used):
            P.mm(ps_o[0][0:64, 0:384], v_ap, p_t[:], idx == 0, idx == n - 1, (v_bf, p_b), (ps_o[1],))
        for idx, (p_t, p_b, v_ap, v_bf) in enumerate(pts_used):
            P.mm(ps_d[0][0:64, 0:384], ones_t[:], p_t[:], idx == 0, False, (ones_b, p_b), (ps_d[1],))
        P.mm(ps_d[0][0:64, 0:384], onesf_t[:], esk_t[:, g, :], False, True, (onesf_b, esk_b), (ps_d[1],))
        j = cnt[1] % 2
        cnt[1] += 1
        r_t, r_b = rd[j]
        o_t, o_b = ob[j]
        P.recip(r_t[:], ps_d[0][0:64, 0:384], (ps_d[1],), (r_b,))
        P.tt("dve", o_t[:], ps_o[0][0:64, 0:384], r_t[:], ALU.mult, (ps_o[1], r_b), (o_b,))
        P.dma("sp", dst[3 * g:3 * g + 3, :, t0:t0 + 128].rearrange("h d q -> d h q"),
              o_t[:].rearrange("d (h q) -> d h q", h=3), (o_b,), ())

    for qb in range(nqb):
        for g in range(2):
            specs = []
            for off in range(3):
                kb = qb + off
                negidx = None
                if off == 0:
                    negidx = 0 if qb == 0 else 1
                elif off == 2:
                    negidx = 3 if qb == nqb - 1 else 2
                specs.append((kr_t[:, g, kb * 128:(kb + 1) * 128], kr_b, v_t[:, kb, g * 64:(g + 1) * 64], v_b, negidx))
            for cb in range(2):
                specs.append((kcb_t[:, g, cb * 128:(cb + 1) * 128], kcb_b, vc_t[:, cb, g * 64:(g + 1) * 64], vc_b, None))
            attend(qr_t, qr_b, qb, g, specs, yc, qb * 128)
    if need_ctx:
        for qb in range(2):
            for g in range(2):
                specs = [(kcb_t[:, g, cb * 128:(cb + 1) * 128], kcb_b, vc_t[:, cb, g * 64:(g + 1) * 64], vc_b, None)
                         for cb in range(2)]
                attend(qcb_t, qcb_b, qb, g, specs, yc_c, qb * 128)
    return P.finish()


R_CB, R_CC, R_CH = 0, 256, 512
R_DQ, R_DK, R_DV, R_DZ = 768, 1152, 1536, 1920
R_AQ, R_AK, R_AV = 2304, 2688, 2816
R_A, R_BT = 2944, 2956


def tri_masks():
    kk = np.arange(128)[:, None]
    qq = np.arange(128)[None, :]
    prev = np.where(qq <= kk, 0.0, NEGV)
    nxt = np.where(kk <= qq, 0.0, NEGV)
    return prev, nxt


def stage_C1(px, pc, conv_w_l, sink_l, dn_conv_w_l, dn_a_log_l, dn_dt_bias_l, need_ctx):
    ntl = SEQ // NCORES
    nc = build_C1(ntl, need_ctx)
    nkb = ntl // 128 + 2
    cw = c_(conv_w_l.T.reshape(2, 128, 3).transpose(1, 0, 2))
    prev, nxt = tri_masks()
    full = np.full((128, 128), NEGV)
    cos, sin = rope_tables(np.arange(SEQ))
    cosp = np.pad(cos, ((0, 0), (128, 128)))
    sinp = np.pad(sin, ((0, 0), (128, 128)))
    pad1 = lambda a: np.pad(a, ((0, 0), (1, 1)))
    convsrc = [pad1(px[R_CB:R_CB + 256]), pad1(px[R_CC:R_CC + 256]), pad1(px[R_CH:R_CH + 256])]
    kx = np.pad(px[R_AK:R_AK + 128], ((0, 0), (128, 128))).reshape(2, 64, SEQ + 256)
    kxp = kx[:, ROPE_PERM, :]
    vx = np.pad(px[R_AV:R_AV + 128], ((0, 0), (128, 128)))
    qx = px[R_AQ:R_AQ + 384].reshape(6, 64, SEQ)
    qxp = qx[:, ROPE_PERM, :]
    kc = c_(pc[R_AK:R_AK + 128].reshape(2, 64, CTX))
    vc = c_(pc[R_AV:R_AV + 128].T.reshape(2, 128, 128).transpose(1, 0, 2))
    sink = c_(np.repeat(sink_l.reshape(2, 3), 128, axis=1).reshape(1, 2, 384))
    ident = np.eye(128, dtype=np.float32)
    dnsrc = pad1(px[R_DQ:R_DQ + 1152])
    dnc = c_(pad1(pc[R_DQ:R_DQ + 1152]).reshape(9, 128, CTX + 2))
    dcw = c_(dn_conv_w_l.T.reshape(9, 128, 3).transpose(1, 0, 2))
    gpar = c_(np.stack([dn_a_log_l.reshape(12), dn_dt_bias_l.reshape(12)], axis=1))
    bd = c_(np.kron(np.eye(2), np.ones((64, 64))))
    gabc = c_(np.stack([pc[R_A:R_A + 12], pc[R_BT:R_BT + 12]]))
    ims = []
    for i in range(NCORES):
        t0, t1 = i * ntl, (i + 1) * ntl
        neg = np.stack([np.tile(full if i == 0 else prev, (1, 3)), np.tile(prev, (1, 3)),
                        np.tile(nxt, (1, 3)), np.tile(full if i == NCORES - 1 else nxt, (1, 3))], axis=1)
        im = {
            "cv": c_(np.stack([a[:, t0:t1 + 2].reshape(2, 128, ntl + 2) for a in convsrc])),
            "cw": cw,
            "q": c_(np.stack([qx[:, :, t0:t1], qxp[:, :, t0:t1]])),
            "k": c_(np.stack([kx[:, :, t0:t1 + 256], kxp[:, :, t0:t1 + 256]])),
            "v": c_(vx[:, t0:t1 + 256].T.reshape(nkb, 128, 128).transpose(1, 0, 2)),
            "cs_k": c_(np.stack([cosp[:, t0:t1 + 256], sinp[:, t0:t1 + 256]])),
            "kc": kc, "vc": vc, "neg": c_(neg), "sink": sink, "ident": ident,
            "dn": c_(dnsrc[:, t0:t1 + 2].reshape(9, 128, ntl + 2)), "dnc": dnc, "dcw": dcw,
            "gab": c_(np.stack([px[R_A:R_A + 12, t0:t1], px[R_BT:R_BT + 12, t0:t1]])), "gabc": gabc,
            "gpar": gpar, "bd": bd,
        }
        if need_ctx:
            im["cvc"] = c_(np.stack([pad1(pc[r:r + 256]).reshape(2, 128, CTX + 2) for r in (R_CB, R_CC, R_CH)]))
            im["qc"] = c_(pc[R_AQ:R_AQ + 384].reshape(6, 64, CTX))
        ims.append(im)
    res = run(nc, ims)
    yaT = np.concatenate([r["yaT"].reshape(256, ntl) for r in res], axis=1)
    ycT = np.concatenate([r["ycT"].reshape(384, ntl) for r in res], axis=1)
    out = {"yaT": yaT, "ycT": ycT}
    out["dn"] = np.concatenate([r["dno"].reshape(1152, ntl) for r in res], axis=1)
    out["dnc"] = res[0]["dnco"].reshape(1152, CTX)
    out["gb"] = np.concatenate([r["gbo"] for r in res], axis=2)
    out["gbc"] = res[0]["gbco"]
    if need_ctx:
        out["yaTc"] = res[0]["yaTc"].reshape(256, CTX)
        out["ycTc"] = res[0]["ycTc"].reshape(384, CTX)
    return out


def c2_consts():
    i = np.arange(128)
    same = (i[:, None] // 64) == (i[None, :] // 64)
    tri = (same & (i[:, None] <= i[None, :])).astype(np.float32)
    bd = same.astype(np.float32)
    negl = np.where(same & (i[:, None] >= i[None, :]), 0.0, NEGV).astype(np.float32)
    slm = (same & (i[:, None] > i[None, :])).astype(np.float32)
    ident = np.eye(128, dtype=np.float32)
    ones = np.ones((128, 128), np.float32)
    sel = np.zeros((128, 2, 64), np.float32)
    sel[0:64, 0, :] = 1.0
    sel[64:128, 1, :] = 1.0
    return {"tri": tri, "bd": bd, "negl": negl, "slm": slm, "ident": ident, "ones": ones,
            "sel": sel.reshape(128, 128)}


C2_STOP = 9


def build_C2(NP):
    T = NP * 128
    G = 5 if NP % 5 == 0 else (4 if NP % 4 == 0 else (2 if NP % 2 == 0 else 1))
    P = Prog()
    qk_in = P.dram_in("qk", [2, 2, 64, T])
    tok_in = P.dram_in("tok", [2, 128, NP, 192])
    gb_in = P.dram_in("gbt", [2, 128, 2, NP])
    o_out = P.dram_out("oT", [2, 64, T])
    C = {}
    for nm in ("tri", "bd", "negl", "slm", "ident", "ones", "sel"):
        d_in = P.dram_in(nm, [128, 128])
        t, b = P.sb([128, 128], F32, "c_" + nm)
        P.dma("sp", t[:], d_in[:, :], (), (b,))
        C[nm] = (t, b)
    tri_t, tri_b = C["tri"]
    ident_t, ident_b = C["ident"]
    ones_t, ones_b = C["ones"]
    negl_t, negl_b = C["negl"]
    slm_t, slm_b = C["slm"]
    bd_t, bd_b = C["bd"]
    sel_t, sel_b = C["sel"]

    banks = [P.psum([128, 512]) for _ in range(8)]

    class U:
        pass
    units = []
    for u in range(2):
        S_ = U()
        S_.u = u
        bk = banks[4 * u:4 * u + 4]
        S_.kk = (bk[0][0], 0, bk[0][1])
        S_.qk = (bk[0][0], 128, bk[0][1])
        S_.m = (bk[0][0], 256, bk[0][1])
        S_.n = (bk[0][0], 384, bk[0][1])
        S_.ng = (bk[1][0], 0, bk[1][1])
        S_.tr = (bk[1][0], 128, bk[1][1])
        S_.tr2 = (bk[1][0], 256, bk[1][1])
        S_.qe = (bk[1][0], 384, bk[1][1])
        S_.sqa = (bk[2][0], 0, bk[2][1])
        S_.sqb = (bk[2][0], 128, bk[2][1])
        S_.ap = (bk[2][0], 256, bk[2][1])
        S_.s = (bk[2][0], 384, bk[2][1])
        S_.o = [(bk[3][0], 0, bk[3][1]), (bk[3][0], 128, bk[3][1])]
        S_.pre = bk[3]
        S_.gb = P.sb([128, 2, NP])
        S_.ng_sb = P.sb([128, NP])
        S_.nb = P.sb([128, NP])
        S_.gc = P.sb([128, NP])
        S_.e = P.sb([128, NP])
        S_.kdf = P.sb([128, NP])
        S_.be = P.sb([128, NP])
        S_.GL = P.sb([64, 2, NP])
        S_.qkin = [P.sb([64, 2, G * 128]) for _ in range(2)]
        S_.tokin = [P.sb([128, G, 192]) for _ in range(2)]
        S_.R = [P.sb([128, 128]) for _ in range(2)]
        S_.Dm = [P.sb([128, 128]) for _ in range(2)]
        S_.DmT = [P.sb([128, 128]) for _ in range(2)]
        S_.DmS = [P.sb([128, 128]) for _ in range(2)]
        S_.pw = [P.sb([128, 128]) for _ in range(4)]
        S_.qkT = [P.sb([128, 128]) for _ in range(2)]
        S_.X = [P.sb([128, 128]) for _ in range(3)]
        S_.kd = [P.sb([128, 2, 64]) for _ in range(2)]
        S_.kdfm = P.sb([128, 2, NP])
        S_.nw = [P.sb([128, 64]) for _ in range(2)]
        S_.McT = [P.sb([64, 2, 64]) for _ in range(2)]
        S_.Nsb = [P.sb([64, 128]) for _ in range(2)]
        S_.dE = [P.sb([128, 128]) for _ in range(2)]
        S_.qe_sb = [P.sb([64, 128]) for _ in range(2)]
        S_.S = [P.sb([64, 64]) for _ in range(3)]
        S_.osb = [P.sb([64, 128]) for _ in range(2)]
        S_.si = 0
        S_.xi = 0
        units.append(S_)

    def ps(slot, rows=128, cols=128):
        t, c0, b = slot
        return t[0:rows, c0:c0 + cols], b

    for S_ in units:
        u = S_.u
        gb_t, gb_b = S_.gb
        P.dma("sp", gb_t[:], gb_in[u, :, :, :], (), (gb_b,))
        g_ap, beta_ap = gb_t[:, 0, :], gb_t[:, 1, :]
        P.ts("dve", S_.ng_sb[0][:], g_ap, -1.0, None, ALU.mult, None, (gb_b,), (S_.ng_sb[1],))
        P.ts("dve", S_.nb[0][:], beta_ap, -1.0, None, ALU.mult, None, (gb_b,), (S_.nb[1],))
        pre_t, pre_b = S_.pre
        P.mm(pre_t[:, 0:NP], tri_t[:], g_ap, True, True, (tri_b, gb_b), (pre_b,))
        P.copy("act", S_.gc[0][:], pre_t[:, 0:NP], (pre_b,), (S_.gc[1],))
        P.act(S_.e[0][:], S_.gc[0][:], AF.Exp, (S_.gc[1],), (S_.e[1],))
        P.tt("dve", S_.be[0][:], S_.e[0][:], beta_ap, ALU.mult, (S_.e[1], gb_b), (S_.be[1],))
        P.mm(pre_t[:, 0:NP], bd_t[:], g_ap, True, True, (bd_b, gb_b), (pre_b,))
        P.tt("dve", S_.kdf[0][:], pre_t[:, 0:NP], S_.gc[0][:], ALU.subtract, (pre_b, S_.gc[1]), (S_.kdf[1],))
        P.act(S_.kdf[0][:], S_.kdf[0][:], AF.Exp, (S_.kdf[1],), (S_.kdf[1],))
        for c in range(2):
            P.ts("dve", S_.kdfm[0][:, c, :], S_.kdf[0][:], sel_t[:, c * 64:c * 64 + 1], None, ALU.mult, None,
                 (S_.kdf[1], sel_b), (S_.kdfm[1],))
        for c in range(2):
            P.mm(pre_t[0:64, 0:NP], sel_t[:, c * 64:(c + 1) * 64], g_ap, True, True, (sel_b, gb_b), (pre_b,))
            P.act(S_.GL[0][:, c, :], pre_t[0:64, 0:NP], AF.Exp, (pre_b,), (S_.GL[1],))
        P.memset("dve", S_.S[0][0][:], 0.0, (S_.S[0][1],))

    def load_group(S_, gi):
        u = S_.u
        qk_t, qk_b = S_.qkin[gi % 2]
        tk_t, tk_b = S_.tokin[gi % 2]
        P.dma("sp", qk_t[:], qk_in[u, :, :, gi * G * 128:(gi + 1) * G * 128].rearrange("a d t -> d a t"), (), (qk_b,))
        P.dma("sp", tk_t[:], tok_in[u, :, gi * G:(gi + 1) * G, :], (), (tk_b,))

    def pre(S_, p):
        gi, pi = p // G, p % G
        qk_t, qk_b = S_.qkin[gi % 2]
        tk_t, tk_b = S_.tokin[gi % 2]
        qT = qk_t[:, 0, pi * 128:(pi + 1) * 128]
        kT = qk_t[:, 1, pi * 128:(pi + 1) * 128]
        Qt, Kt, Vt = tk_t[:, pi, 0:64], tk_t[:, pi, 64:128], tk_t[:, pi, 128:192]
        gb_t, gb_b = S_.gb
        j = p % 2
        kk_ap, kk_b = ps(S_.kk)
        qk_ap, qkp_b = ps(S_.qk)
        P.mm(kk_ap, kT, kT, True, True, (qk_b,), (kk_b,))
        P.mm(qk_ap, kT, qT, True, True, (qk_b,), (qkp_b,))
        R_t, R_b = S_.R[j]
        P.ts("pool", R_t[:], tri_t[:], S_.ng_sb[0][:, p:p + 1], None, ALU.mult, None, (tri_b, S_.ng_sb[1]), (R_b,))
        ng_ap, ng_b = ps(S_.ng)
        P.mm(ng_ap, ones_t[:], R_t[:], True, False, (ones_b, R_b), (ng_b,))
        P.mm(ng_ap, ident_t[:], negl_t[:], False, True, (ident_b, negl_b), (ng_b,))
        Dm_t, Dm_b = S_.Dm[j]
        P.act(Dm_t[:], ng_ap, AF.Exp, (ng_b, S_.gc[1]), (Dm_b,), bias=S_.gc[0][:, p:p + 1])
        tr_ap, tr_b = ps(S_.tr)
        P.transpose(tr_ap, Dm_t[:], ident_t[:], (Dm_b, ident_b), (tr_b,))
        DmT_t, DmT_b = S_.DmT[j]
        P.copy("act", DmT_t[:], tr_ap, (tr_b,), (DmT_b,))
        DmS_t, DmS_b = S_.DmS[j]
        P.tt("pool", DmS_t[:], Dm_t[:], slm_t[:], ALU.mult, (Dm_b, slm_b), (DmS_b,))
        cur_t, cur_b = S_.pw[0]
        curT_t, curT_b = S_.pw[1]
        P.stt("dve", cur_t[:], kk_ap, S_.nb[0][:, p:p + 1], DmS_t[:], ALU.mult, ALU.mult,
              (kk_b, S_.nb[1], DmS_b), (cur_b,))
        tr2_ap, tr2_b = ps(S_.tr2)
        P.transpose(tr2_ap, cur_t[:], ident_t[:], (cur_b, ident_b), (tr2_b,))
        P.copy("act", curT_t[:], tr2_ap, (tr2_b,), (curT_b,))
        qkT_t, qkT_b = S_.qkT[j]
        P.tt("dve", qkT_t[:], qk_ap, DmT_t[:], ALU.mult, (qkp_b, DmT_b), (qkT_b,))
        X_t, X_b = S_.X[S_.xi % 3]
        S_.xi += 1
        P.ts("pool", X_t[:, 0:64], Kt, S_.be[0][:, p:p + 1], None, ALU.mult, None, (tk_b, S_.be[1]), (X_b,))
        P.ts("pool", X_t[:, 64:128], Vt, gb_t[:, 1, p:p + 1], None, ALU.mult, None, (tk_b, gb_b), (X_b,))
        if C2_STOP <= 2:
            return
        pwi = 0
        for lvl in range(6):
            ap_ap, ap_b = ps(S_.ap)
            P.mm(ap_ap, curT_t[:], X_t[:], True, True, (curT_b, X_b), (ap_b,))
            Xn_t, Xn_b = S_.X[S_.xi % 3]
            S_.xi += 1
            P.tt("dve", Xn_t[:], ap_ap, X_t[:], ALU.add, (ap_b, X_b), (Xn_b,))
            X_t, X_b = Xn_t, Xn_b
            if lvl < 5:
                nxt_t, nxt_b = S_.pw[(pwi + 2) % 4]
                nxtT_t, nxtT_b = S_.pw[(pwi + 3) % 4]
                if lvl < 4:
                    sa_ap, sa_b = ps(S_.sqa)
                    P.mm(sa_ap, curT_t[:], cur_t[:], True, True, (curT_b, cur_b), (sa_b,))
                    P.copy("act", nxt_t[:], sa_ap, (sa_b,), (nxt_b,))
                sb_ap, sb_b = ps(S_.sqb)
                P.mm(sb_ap, cur_t[:], curT_t[:], True, True, (cur_b, curT_b), (sb_b,))
                P.copy("act", nxtT_t[:], sb_ap, (sb_b,), (nxtT_b,))
                cur_t, cur_b, curT_t, curT_b = nxt_t, nxt_b, nxtT_t, nxtT_b
                pwi = (pwi + 2) % 4
        if C2_STOP <= 3:
            return
        kd_t, kd_b = S_.kd[j]
        nw_t, nw_b = S_.nw[j]
        for c in range(2):
            P.ts("pool", kd_t[:, c, :], Kt, S_.kdfm[0][:, c, p:p + 1], None, ALU.mult, None, (tk_b, S_.kdfm[1]), (kd_b,))
        P.ts("pool", nw_t[:], X_t[:, 0:64], -1.0, None, ALU.mult, None, (X_b,), (nw_b,))
        if C2_STOP <= 3.3:
            return
        m_t, m0, m_b = S_.m
        n_t, n0, n_b = S_.n
        for c in range(2):
            r0, r1 = c * 64, (c + 1) * 64
            P.mm(m_t[0:64, m0 + r0:m0 + r1], nw_t[:], kd_t[:, c, :], True, True, (nw_b, kd_b), (m_b,))
            P.mm(n_t[0:64, n0 + r0:n0 + r1], kd_t[:, c, :], X_t[:, 64:128], True, True, (kd_b, X_b), (n_b,))
        if C2_STOP <= 3.4:
            return
        McT_t, McT_b = S_.McT[j]
        for c in range(2):
            P.stt("dve", McT_t[:, c, :], ident_t[0:64, 0:64], S_.GL[0][:, c, p:p + 1],
                  m_t[0:64, m0 + c * 64:m0 + (c + 1) * 64], ALU.mult, ALU.add, (ident_b, S_.GL[1], m_b), (McT_b,))
        if C2_STOP <= 3.5:
            return
        Nsb_t, Nsb_b = S_.Nsb[j]
        P.copy("act", Nsb_t[:], n_t[0:64, n0:n0 + 128], (n_b,), (Nsb_b,))
        if C2_STOP <= 3.6:
            return
        dE_t, dE_b = S_.dE[j]
        P.ts("pool", dE_t[:], ident_t[:], S_.e[0][:, p:p + 1], None, ALU.mult, None, (ident_b, S_.e[1]), (dE_b,))
        qe_ap, qe_b = ps(S_.qe, 64, 128)
        P.mm(qe_ap, Qt, dE_t[:], True, False, (tk_b, dE_b), (qe_b,))
        P.mm(qe_ap, nw_t[:], qkT_t[:], False, True, (nw_b, qkT_b), (qe_b,))
        qes_t, qes_b = S_.qe_sb[j]
        P.copy("act", qes_t[:], qe_ap, (qe_b,), (qes_b,))
        S_.cur = dict(X=(X_t, X_b), qkT=(qkT_t, qkT_b), McT=(McT_t, McT_b), Nsb=(Nsb_t, Nsb_b), qe=(qes_t, qes_b))

    def scan(S_, p):
        cur = S_.cur
        X_t, X_b = cur["X"]
        qkT_t, qkT_b = cur["qkT"]
        McT_t, McT_b = cur["McT"]
        Nsb_t, Nsb_b = cur["Nsb"]
        qes_t, qes_b = cur["qe"]
        o_ap, o_b = ps(S_.o[p % 2], 64, 128)
        ot, oc0, _ = S_.o[p % 2]
        P.mm(o_ap, X_t[:, 64:128], qkT_t[:], True, False, (X_b, qkT_b), (o_b,))
        s_ap, s_b = ps(S_.s, 64, 64)
        for c in range(2):
            St, Sb = S_.S[S_.si % 3]
            P.mm(ot[0:64, oc0 + c * 64:oc0 + (c + 1) * 64], St[:], qes_t[:, c * 64:(c + 1) * 64], False, c == 1,
                 (Sb, qes_b), (o_b,))
            P.mm(s_ap, McT_t[:, c, :], St[:], True, False, (McT_b, Sb), (s_b,))
            P.mm(s_ap, ident_t[0:64, 0:64], Nsb_t[:, c * 64:(c + 1) * 64], False, True, (ident_b, Nsb_b), (s_b,))
            S_.si += 1
            Sn, Snb = S_.S[S_.si % 3]
            P.copy("act", Sn[:], s_ap, (s_b,), (Snb,))
        os_t, os_b = S_.osb[p % 2]
        P.copy("dve", os_t[:], o_ap, (o_b,), (os_b,))
        P.dma("sp", o_out[S_.u, :, p * 128:(p + 1) * 128], os_t[:], (os_b,), ())

    for p in range(NP):
        if C2_STOP <= 1:
            break
        for S_ in units:
            if p % G == 0:
                load_group(S_, p // G)
            pre(S_, p)
        if C2_STOP <= 4:
            continue
        for S_ in units:
            scan(S_, p)
    if C2_STOP <= 4:
        for S_ in units:
            P.dma("sp", o_out[S_.u, :, 0:NP], S_.GL[0][:, 0, :], (S_.GL[1],), ())
    return P.finish()


def c2_unit_inputs(q, k, v, g, beta):
    T = q.shape[1]
    NP = T // 128
    qk = np.stack([q, k])
    tok = np.concatenate([q.T, k.T, v.T], axis=1).reshape(NP, 128, 192).transpose(1, 0, 2)
    gbt = np.stack([g.reshape(NP, 128).T, beta.reshape(NP, 128).T], axis=1)
    return c_(qk), c_(tok), c_(gbt)


def run_C2(unit_list):
    T = unit_list[0][0].shape[1]
    nc = build_C2(T // 128)
    consts = c2_consts()
    zero = tuple(np.zeros_like(a) for a in unit_list[0])
    ims = []
    for i in range(NCORES):
        us = [unit_list[2 * i + j] if 2 * i + j < len(unit_list) else zero for j in range(2)]
        parts = [c2_unit_inputs(*u_) for u_ in us]
        im = {"qk": np.stack([p_[0] for p_ in parts]), "tok": np.stack([p_[1] for p_ in parts]),
              "gbt": np.stack([p_[2] for p_ in parts])}
        im.update(consts)
        ims.append(im)
    res = run(nc, ims)
    outs = []
    for idx in range(len(unit_list)):
        outs.append(res[idx // 2]["oT"][idx % 2])
    return outs


def tok_blocks(ntl, ntc, bw=512):
    blks = [(t0, min(bw, ntl - t0), 0) for t0 in range(0, ntl, bw)]
    if ntc:
        blks += [(ntl + t0, min(bw, ntc - t0), 1) for t0 in range(0, ntc, bw)]
    return blks


def build_E1(ntl, ntc):
    nt = ntl + ntc
    P = Prog()
    xT = P.dram_in("xT", [8, 128, nt])
    ya = P.dram_in("ya", [2, 128, nt])
    yc = P.dram_in("yc", [3, 128, nt])
    of = P.dram_in("of", [3, 128, nt])
    ob = P.dram_in("ob", [3, 128, nt])
    z = P.dram_in("z", [3, 128, nt])
    w = P.dram_in("w", [1024, 1024])
    mod2 = P.dram_in("mod2", [128, 2, 8])
    dng = P.dram_in("dng", [128, 1])
    bd_in = P.dram_in("bd", [128, 128])
    out = P.dram_out("x1T", [8, 128, nt])
    w_t, w_b = P.sb([128, 8, 1024], BF16, "wout")
    for mc in range(8):
        P.dma("pool", w_t[:, mc, :], w[mc * 128:(mc + 1) * 128, :], (), (w_b,))
    m2_t, m2_b = P.sb([128, 2, 8])
    P.dma("sp", m2_t[:], mod2[:, :, :], (), (m2_b,))
    g_t, g_b = P.sb([128, 1])
    P.dma("sp", g_t[:], dng[:, :], (), (g_b,))
    bd_t, bd_b = P.sb([128, 128])
    P.dma("sp", bd_t[:], bd_in[:, :], (), (bd_b,))
    mixs = [P.sb([128, 8, 512], BF16) for _ in range(2)]
    xs = [P.sb([128, 8, 512]) for _ in range(2)]
    ofs = [P.sb([128, 512]) for _ in range(2)]
    obs = [P.sb([128, 512]) for _ in range(2)]
    zs = [P.sb([128, 512]) for _ in range(2)]
    sqs = [P.sb([128, 512]) for _ in range(2)]
    rs = [P.sb([128, 512]) for _ in range(2)]
    pn = [P.psum([128, 512]) for _ in range(2)]
    pp = [P.psum([128, 512]) for _ in range(4)]
    k = 0
    for bi, (t0, wd_, j) in enumerate(tok_blocks(ntl, ntc)):
        mix_t, mix_b = mixs[bi % 2]
        x_t, x_b = xs[bi % 2]
        P.dma("pool", mix_t[:, 0:2, 0:wd_], ya[:, :, t0:t0 + wd_].rearrange("c p t -> p c t"), (), (mix_b,))
        P.dma("pool", mix_t[:, 5:8, 0:wd_], yc[:, :, t0:t0 + wd_].rearrange("c p t -> p c t"), (), (mix_b,))
        P.dma("sp", x_t[:, :, 0:wd_], xT[:, :, t0:t0 + wd_].rearrange("c p t -> p c t"), (), (x_b,))
        for ch in range(3):
            o_t, o_b = ofs[k % 2]
            b_t, b_b = obs[k % 2]
            z_t, z_b = zs[k % 2]
            s_t, s_b = sqs[k % 2]
            r_t, r_b = rs[k % 2]
            ps_t, ps_b = pn[k % 2]
            k += 1
            P.dma("sp", o_t[:, 0:wd_], of[ch, :, t0:t0 + wd_], (), (o_b,))
            P.dma("sp", b_t[:, 0:wd_], ob[ch, :, t0:t0 + wd_], (), (b_b,))
            P.dma("sp", z_t[:, 0:wd_], z[ch, :, t0:t0 + wd_], (), (z_b,))
            P.tt("pool", o_t[:, 0:wd_], o_t[:, 0:wd_], b_t[:, 0:wd_], ALU.add, (o_b, b_b), (o_b,))
            P.act(s_t[:, 0:wd_], o_t[:, 0:wd_], AF.Square, (o_b,), (s_b,))
            P.mm(ps_t[:, 0:wd_], bd_t[:], s_t[:, 0:wd_], True, True, (bd_b, s_b), (ps_b,))
            P.act(r_t[:, 0:wd_], ps_t[:, 0:wd_], AF.Sqrt, (ps_b,), (r_b,), bias=EPS, scale=1.0 / 64)
            P.recip(r_t[:, 0:wd_], r_t[:, 0:wd_], (r_b,), (r_b,))
            P.act(z_t[:, 0:wd_], z_t[:, 0:wd_], AF.Silu, (z_b,), (z_b,))
            P.stt("dve", o_t[:, 0:wd_], o_t[:, 0:wd_], g_t[:, 0:1], r_t[:, 0:wd_], ALU.mult, ALU.mult,
                  (o_b, g_b, r_b), (o_b,))
            P.tt("dve", mix_t[:, 2 + ch, 0:wd_], o_t[:, 0:wd_], z_t[:, 0:wd_], ALU.mult, (o_b, z_b), (mix_b,))
        for dc in range(8):
            ps_t, ps_b = pp[dc % 4]
            for mc in range(8):
                P.mm(ps_t[:, 0:wd_], w_t[:, mc, dc * 128:(dc + 1) * 128], mix_t[:, mc, 0:wd_], mc == 0, mc == 7,
                     (w_b, mix_b), (ps_b,))
            P.stt("dve", x_t[:, dc, 0:wd_], ps_t[:, 0:wd_], m2_t[:, j, dc:dc + 1], x_t[:, dc, 0:wd_], ALU.mult, ALU.add,
                  (ps_b, m2_b, x_b), (x_b,))
        P.dma("sp", out[:, :, t0:t0 + wd_].rearrange("c p t -> p c t"), x_t[:, :, 0:wd_], (x_b,), ())
    return P.finish()


def build_E2(ntl, ntc, E, F, final):
    nt = ntl + ntc
    ntile = nt // 128
    FG = 256
    nfg = F // FG
    P = Prog()
    x1T = P.dram_in("x1T", [8, 128, nt])
    x1 = P.dram_in("x1", [nt, 1024])
    mod = P.dram_in("mod", [128, 2, 2, 8])
    m5 = P.dram_in("m5", [128, 2, 1024])
    wg = P.dram_in("wg", [E, 1024, F])
    wu = P.dram_in("wu", [E, 1024, F])
    wd = P.dram_in("wd", [E, F, 1024])
    out = P.dram_out("x2", [nt, 1024])
    if E > 1:
        wr = P.dram_in("wr", [128, 8, E])
    if final:
        gn = P.dram_in("gn", [128, 1024])
    mod_t, mod_b = P.sb([128, 2, 2, 8])
    P.dma("sp", mod_t[:], mod[:, :, :, :], (), (mod_b,))
    a_t, a_b = P.sb([128, 2, 8])
    P.ts("dve", a_t[:], mod_t[:, :, 1, :], 1.0, None, ALU.add, None, (mod_b,), (a_b,))
    ones_t, ones_b = P.sb([128, 128], BF16)
    P.memset("dve", ones_t[:], 1.0, (ones_b,))
    hx_t, hx_b = P.sb([128, 8, nt], BF16, "hx")
    acc_t, acc_b = P.sb([128, ntile, 1024], F32, "acc")
    P.memset("dve", acc_t[:], 0.0, (acc_b,))
    if E > 1:
        wr_t, wr_b = P.sb([128, 8, E])
        P.dma("sp", wr_t[:], wr[:, :, :], (), (wr_b,))
        lg_t, lg_b = P.sb([128, ntile, E])
        gate_t, gate_b = P.sb([128, ntile, E])
    xs = [P.sb([128, 8, TT]) for _ in range(2)]
    sqs = [P.sb([128, 8, TT], BF16) for _ in range(2)]
    hfs = [P.sb([128, 8, TT]) for _ in range(2)]
    rs = [P.sb([128, TT]) for _ in range(2)]
    pn = P.psum([128, 512])
    pr = P.psum([128, 512])
    for it, (t0, wd_, j) in enumerate(tok_blocks(ntl, ntc, TT)):
        x_t, x_b = xs[it % 2]
        sq_t, sq_b = sqs[it % 2]
        hf_t, hf_b = hfs[it % 2]
        r_t, r_b = rs[it % 2]
        P.dma("sp", x_t[:], x1T[:, :, t0:t0 + TT].rearrange("c p t -> p c t"), (), (x_b,))
        P.act(sq_t[:], x_t[:], AF.Square, (x_b,), (sq_b,))
        for kc in range(8):
            P.mm(pn[0][:, 0:TT], ones_t[:], sq_t[:, kc, :], kc == 0, kc == 7, (ones_b, sq_b), (pn[1],))
        P.act(r_t[:], pn[0][:, 0:TT], AF.Sqrt, (pn[1],), (r_b,), bias=EPS, scale=1.0 / 1024)
        P.recip(r_t[:], r_t[:], (r_b,), (r_b,))
        for kc in range(8):
            P.stt("dve", hf_t[:, kc, :], x_t[:, kc, :], a_t[:, j, kc:kc + 1], r_t[:], ALU.mult, ALU.mult,
                  (x_b, a_b, r_b), (hf_b,))
            P.act(hf_t[:, kc, :], hf_t[:, kc, :], AF.Identity, (hf_b, mod_b), (hf_b,), bias=mod_t[:, j, 0, kc:kc + 1])
        P.copy("pool", hx_t[:, :, t0:t0 + TT], hf_t[:], (hf_b,), (hx_b,))
        if E > 1:
            for sub in range(TT // 128):
                tile = t0 // 128 + sub
                for kc in range(8):
                    P.mm(pr[0][:, 0:E], hf_t[:, kc, sub * 128:(sub + 1) * 128], wr_t[:, kc, :], kc == 0, kc == 7,
                         (hf_b, wr_b), (pr[1],))
                P.copy("act", lg_t[:, tile, :], pr[0][:, 0:E], (pr[1],), (lg_b,))
    if E > 1:
        m1_t, m1_b = P.sb([128, 1])
        m2_t, m2_b = P.sb([128, 1])
        e1_t, e1_b = P.sb([128, E])
        e2_t, e2_b = P.sb([128, E])
        l2_t, l2_b = P.sb([128, E])
        g1_t, g1_b = P.sb([128, 1])
        g2_t, g2_b = P.sb([128, 1])
        for tile in range(ntile):
            l_ap = lg_t[:, tile, :]
            P.op("dve", lambda e, o=m1_t[:], i=l_ap: e.reduce_max(o, i, axis=mybir.AxisListType.X), (lg_b,), (m1_b,))
            P.ts("dve", e1_t[:], l_ap, m1_t[:, 0:1], None, ALU.is_equal, None, (lg_b, m1_b), (e1_b,))
            P.stt("dve", l2_t[:], e1_t[:], -1e9, l_ap, ALU.mult, ALU.add, (e1_b, lg_b), (l2_b,))
            P.op("dve", lambda e, o=m2_t[:], i=l2_t[:]: e.reduce_max(o, i, axis=mybir.AxisListType.X), (l2_b,), (m2_b,))
            P.ts("dve", e2_t[:], l2_t[:], m2_t[:, 0:1], None, ALU.is_equal, None, (l2_b, m2_b), (e2_b,))
            P.tt("dve", g1_t[:], m2_t[:], m1_t[:], ALU.subtract, (m2_b, m1_b), (g1_b,))
            P.act(g1_t[:], g1_t[:], AF.Exp, (g1_b,), (g1_b,))
            P.ts("dve", g1_t[:], g1_t[:], 1.0, None, ALU.add, None, (g1_b,), (g1_b,))
            P.recip(g1_t[:], g1_t[:], (g1_b,), (g1_b,))
            P.ts("dve", g2_t[:], g1_t[:], -1.0, 1.0, ALU.mult, ALU.add, (g1_b,), (g2_b,))
            P.ts("dve", e1_t[:], e1_t[:], g1_t[:, 0:1], None, ALU.mult, None, (e1_b, g1_b), (e1_b,))
            P.stt("dve", gate_t[:, tile, :], e2_t[:], g2_t[:, 0:1], e1_t[:], ALU.mult, ALU.add,
                  (e2_b, g2_b, e1_b), (gate_b,))
    wgs = [P.sb([128, 8, FG], BF16) for _ in range(2)]
    wus = [P.sb([128, 8, FG], BF16) for _ in range(2)]
    wds = [P.sb([128, 2, 1024], BF16) for _ in range(2)]
    pg = [P.psum([128, 512]) for _ in range(2)]
    pu = [P.psum([128, 512]) for _ in range(2)]
    pd = [P.psum([128, 512]) for _ in range(2)]
    sgs = [P.sb([128, 512], BF16) for _ in range(2)]
    Hs = [P.sb([128, 2, 512], BF16) for _ in range(2)]
    blocks = tok_blocks(ntl, ntc)
    it = 0
    nd = 0
    for e_ in range(E):
        for fg in range(nfg):
            f0 = fg * FG
            wg_t, wg_b = wgs[it % 2]
            wu_t, wu_b = wus[it % 2]
            wd_t, wd_b = wds[it % 2]
            it += 1
            P.dma("pool", wg_t[:], wg[e_, :, f0:f0 + FG].rearrange("(kc p) f -> p kc f", p=128), (), (wg_b,))
            P.dma("pool", wu_t[:], wu[e_, :, f0:f0 + FG].rearrange("(kc p) f -> p kc f", p=128), (), (wu_b,))
            P.dma("pool", wd_t[:], wd[e_, f0:f0 + FG, :].rearrange("(fc p) d -> p fc d", p=128), (), (wd_b,))
            for bi, (t0, wd_, j) in enumerate(blocks):
                H_t, H_b = Hs[bi % 2]
                for fc in range(2):
                    g_t, g_b = pg[fc]
                    u_t, u_b = pu[fc]
                    for kc in range(8):
                        P.mm(g_t[:, 0:wd_], wg_t[:, kc, fc * 128:(fc + 1) * 128], hx_t[:, kc, t0:t0 + wd_],
                             kc == 0, kc == 7, (wg_b, hx_b), (g_b,))
                    for kc in range(8):
                        P.mm(u_t[:, 0:wd_], wu_t[:, kc, fc * 128:(fc + 1) * 128], hx_t[:, kc, t0:t0 + wd_],
                             kc == 0, kc == 7, (wu_b, hx_b), (u_b,))
                    sg_t, sg_b = sgs[fc]
                    P.act(sg_t[:, 0:wd_], g_t[:, 0:wd_], AF.Silu, (g_b,), (sg_b,))
                    P.tt("dve", H_t[:, fc, 0:wd_], u_t[:, 0:wd_], sg_t[:, 0:wd_], ALU.mult, (u_b, sg_b), (H_b,))
                for sub in range(wd_ // 128):
                    tile = t0 // 128 + sub
                    for dh in range(2):
                        d_t, d_b = pd[nd % 2]
                        nd += 1
                        for fc in range(2):
                            P.mm(d_t[:], H_t[:, fc, sub * 128:(sub + 1) * 128], wd_t[:, fc, dh * 512:(dh + 1) * 512],
                                 fc == 0, fc == 1, (H_b, wd_b), (d_b,))
                        acc_ap = acc_t[:, tile, dh * 512:(dh + 1) * 512]
                        if E > 1:
                            P.stt("dve", acc_ap, d_t[:], gate_t[:, tile, e_:e_ + 1], acc_ap, ALU.mult, ALU.add,
                                  (d_b, gate_b, acc_b), (acc_b,))
                        else:
                            P.tt("dve", acc_ap, d_t[:], acc_ap, ALU.add, (d_b, acc_b), (acc_b,))
    m5_t, m5_b = P.sb([128, 2, 1024])
    P.dma("sp", m5_t[:], m5[:, :, :], (), (m5_b,))
    if final:
        gn_t, gn_b = P.sb([128, 1024])
        P.dma("sp", gn_t[:], gn[:, :], (), (gn_b,))
        ss_t, ss_b = P.sb([128, 1])
        tq = [P.sb([128, 1024]) for _ in range(2)]
    x1s = [P.sb([128, 1024]) for _ in range(2)]
    for tile in range(ntile):
        j = 0 if tile * 128 < ntl else 1
        x_t, x_b = x1s[tile % 2]
        P.dma("sp", x_t[:], x1[tile * 128:(tile + 1) * 128, :], (), (x_b,))
        P.tt("pool", acc_t[:, tile, :], acc_t[:, tile, :], m5_t[:, j, :], ALU.mult, (acc_b, m5_b), (acc_b,))
        P.tt("dve", x_t[:], x_t[:], acc_t[:, tile, :], ALU.add, (x_b, acc_b), (x_b,))
        if final:
            q_t, q_b = tq[tile % 2]
            P.tt("pool", q_t[:], x_t[:], x_t[:], ALU.mult, (x_b,), (q_b,))
            P.op("dve", lambda e, o=ss_t[:], i=q_t[:]: e.reduce_sum(o, i, axis=mybir.AxisListType.X), (q_b,), (ss_b,))
            P.act(ss_t[:], ss_t[:], AF.Sqrt, (ss_b,), (ss_b,), bias=EPS, scale=1.0 / 1024)
            P.recip(ss_t[:], ss_t[:], (ss_b,), (ss_b,))
            P.stt("dve", x_t[:], x_t[:], ss_t[:, 0:1], gn_t[:], ALU.mult, ALU.mult, (x_b, ss_b, gn_b), (x_b,))
        P.dma("sp", out[tile * 128:(tile + 1) * 128, :], x_t[:], (x_b,), ())
    return P.finish()


def stage_C2(c1, need_ctx):
    dn, dnc, gb, gbc = c1["dn"], c1["dnc"], c1["gb"], c1["gbc"]
    units = []
    for h in range(6):
        sl = slice(h * 64, (h + 1) * 64)
        for d in range(2):
            parts = []
            for r in (0, 384, 768):
                a_c, a_x = dnc[r:r + 384][sl], dn[r:r + 384][sl]
                if d == 1:
                    a_c, a_x = a_c[:, ::-1], a_x[:, ::-1]
                parts.append(np.concatenate([a_c, a_x], axis=1))
            gs = []
            for w_ in range(2):
                g_c, g_x = gbc[w_, d * 6 + h], gb[w_, d * 6 + h]
                if d == 1:
                    g_c, g_x = g_c[::-1], g_x[::-1]
                gs.append(np.concatenate([g_c, g_x]))
            units.append((parts[0], parts[1], parts[2], gs[0], gs[1]))
    outs = run_C2(units)
    of = np.zeros((384, SEQ), np.float32)
    ob = np.zeros((384, SEQ), np.float32)
    ofc = np.zeros((384, CTX), np.float32)
    obc = np.zeros((384, CTX), np.float32)
    for h in range(6):
        sl = slice(h * 64, (h + 1) * 64)
        f, b = outs[2 * h], outs[2 * h + 1]
        of[sl] = f[:, CTX:]
        ofc[sl] = f[:, :CTX]
        ob[sl] = b[:, CTX:][:, ::-1]
        obc[sl] = b[:, :CTX][:, ::-1]
    return of, ob, ofc, obc


def stage_E1(xT, hcT, c1, of, ob, ofc, obc, px, pc, w_out_l, modT_l, dng_l, need_ctx):
    ntl = SEQ // NCORES
    ntc = CTX if need_ctx else 0
    nc = build_E1(ntl, ntc)
    m = modT_l.reshape(128, 2, 6, 8)
    mod2 = c_(m[:, :, 2, :])
    dng = c_(np.tile(dng_l.reshape(64), 2).reshape(128, 1))
    bd = c_(np.kron(np.eye(2), np.ones((64, 64))))
    zx, zc = px[R_DZ:R_DZ + 384], pc[R_DZ:R_DZ + 384]

    def cat(a, b, i):
        sl = a[:, i * ntl:(i + 1) * ntl]
        return np.concatenate([sl, b], axis=1) if need_ctx else sl
    ims = []
    for i in range(NCORES):
        nt = ntl + ntc
        ims.append({
            "xT": c_(cat(xT, hcT, i).reshape(8, 128, nt)),
            "ya": c_(cat(c1["yaT"], c1.get("yaTc"), i).reshape(2, 128, nt)),
            "yc": c_(cat(c1["ycT"], c1.get("ycTc"), i).reshape(3, 128, nt)),
            "of": c_(cat(of, ofc, i).reshape(3, 128, nt)),
            "ob": c_(cat(ob, obc, i).reshape(3, 128, nt)),
            "z": c_(cat(zx, zc, i).reshape(3, 128, nt)),
            "w": c_(w_out_l), "mod2": mod2, "dng": dng, "bd": bd,
        })
    res = run(nc, ims)
    x1T = np.concatenate([r["x1T"][:, :, :ntl].reshape(1024, ntl) for r in res], axis=1)
    hc1T = res[0]["x1T"][:, :, ntl:].reshape(1024, CTX) if need_ctx else None
    return x1T, hc1T


def stage_E2(x1T, hc1T, modT_l, wg, wu, wd, wr, gn, need_ctx):
    ntl = SEQ // NCORES
    ntc = CTX if need_ctx else 0
    E, _, F = wg.shape
    final = gn is not None
    nc = build_E2(ntl, ntc, E, F, final)
    m = modT_l.reshape(128, 2, 6, 8)
    mod = c_(m[:, :, 3:5, :])
    m5 = np.stack([np.broadcast_to(m[:, j, 5, :].T.reshape(1, 1024), (128, 1024)) for j in range(2)], axis=1)
    wg, wu, wd = c_(wg), c_(wu), c_(wd)
    ims = []
    for i in range(NCORES):
        sl = x1T[:, i * ntl:(i + 1) * ntl]
        blk = np.concatenate([sl, hc1T], axis=1) if need_ctx else sl
        nt = ntl + ntc
        im = {"x1T": c_(blk.reshape(8, 128, nt)), "x1": c_(blk.T), "mod": mod, "m5": c_(m5),
              "wg": wg, "wu": wu, "wd": wd}
        if E > 1:
            im["wr"] = c_(wr.reshape(8, 128, E).transpose(1, 0, 2))
        if final:
            im["gn"] = c_(np.broadcast_to(gn.reshape(1, 1024), (128, 1024)))
        ims.append(im)
    res = run(nc, ims)
    x2 = np.concatenate([r["x2"][:ntl] for r in res], axis=0)
    hc2 = res[0]["x2"][ntl:] if need_ctx else None
    return x2, hc2


def kernel(x, c, ctx, c_ctx, w_mod, b_mod, w_in, w_out, conv_w, dn_conv_w, dn_a_log, dn_dt_bias, dn_norm_g,
           attn_sink, ffn_w_gate, ffn_w_up, ffn_w_down, moe_router, moe_w_gate, moe_w_up, moe_w_down,
           final_norm_g):
    f = lambda a: np.asarray(a, dtype=np.float32)
    x, c, ctx, c_ctx, w_mod, b_mod, w_in, w_out = map(f, (x, c, ctx, c_ctx, w_mod, b_mod, w_in, w_out))
    modT = stage_A(c, c_ctx, w_mod, b_mod)
    xT = np.ascontiguousarray(x[0].T)
    hcT = np.ascontiguousarray(ctx[0].T)
    x2 = None
    for l in range(2):
        need_ctx = l == 0
        px, pc = stage_B(xT, hcT, w_in[l], modT[l])
        c1 = stage_C1(px, pc, f(conv_w)[l], f(attn_sink)[l], f(dn_conv_w)[l], f(dn_a_log)[l], f(dn_dt_bias)[l],
                      need_ctx)
        of, ob, ofc, obc = stage_C2(c1, need_ctx)
        x1T, hc1T = stage_E1(xT, hcT, c1, of, ob, ofc, obc, px, pc, w_out[l], modT[l], f(dn_norm_g)[l], need_ctx)
        if l == 0:
            x2, hc2 = stage_E2(x1T, hc1T, modT[l], f(ffn_w_gate), f(ffn_w_up), f(ffn_w_down), None, None, True)
            xT = np.ascontiguousarray(x2.T)
            hcT = np.ascontiguousarray(hc2.T)
        else:
            x2, _ = stage_E2(x1T, None, modT[l], f(moe_w_gate)[0], f(moe_w_up)[0], f(moe_w_down)[0],
                             f(moe_router)[0], f(final_norm_g), False)
    return x2.reshape(1, SEQ, D_MODEL).astype(np.float32)
```

```python
import numpy as np
from contextlib import ExitStack
import concourse.bass as bass
import concourse.mybir as mybir
from concourse.bass_utils import run_bass_kernel_spmd

F32 = mybir.dt.float32
BF16 = mybir.dt.bfloat16
AF = mybir.ActivationFunctionType
ALU = mybir.AluOpType
NCORES = 8

D_MODEL = 1024
SEQ = 16384
CTX = 256
EPS = 1e-6
IN_COLS = 2968


class Buf:
    __slots__ = ("name", "lw", "rd", "excl")

    def __init__(self, name, excl=False):
        self.name = name
        self.lw = None
        self.rd = {}
        self.excl = excl


class Prog:
    EPOCH = 4096
    NDMA = 24

    def __init__(self):
        self.nc = bass.Bass("TRN2", target_bir_lowering=False)
        self.es = ExitStack()
        self.q = {e: [] for e in ("sp", "pe", "dve", "act", "pool")}
        self.cnt = {e: 0 for e in self.q}
        self.seen = {e: {} for e in self.q}
        self.ndma = 0
        self.dlast = {}
        self.nt = 0

    def dram_in(self, name, shape, dt=F32):
        return self.nc.dram_tensor(name, list(shape), dt, kind="ExternalInput").ap()

    def dram_out(self, name, shape, dt=F32):
        return self.nc.dram_tensor(name, list(shape), dt, kind="ExternalOutput").ap()

    def sb(self, shape, dt=F32, name=None):
        self.nt += 1
        name = name or f"t{self.nt}"
        t = self.es.enter_context(self.nc.sbuf_tensor(name, list(shape), dt))
        return t, Buf(name)

    def psum(self, shape=(128, 512), dt=F32, name=None):
        self.nt += 1
        name = name or f"p{self.nt}"
        t = self.es.enter_context(self.nc.psum_tensor(name, list(shape), dt))
        return t, Buf(name, excl=True)

    def _deps(self, reads, writes, eng=None):
        deps = []
        for b in reads:
            if b.lw is not None:
                deps.append(b.lw)
            if b.excl:
                deps.extend(v for k_, v in b.rd.items() if k_ != eng)
        for b in writes:
            if b.lw is not None:
                deps.append(b.lw)
            deps.extend(b.rd.values())
        return deps

    def _filter(self, eng, deps):
        waits = []
        seen = self.seen[eng]
        for d in deps:
            if d[0] == "c":
                _, p, n = d
                if p == eng and eng == "pe":
                    continue
                if seen.get(p, -1) >= n:
                    continue
                seen[p] = n
            else:
                _, slot, val = d
                if seen.get(("d", slot), 0) >= val:
                    continue
                seen[("d", slot)] = val
            waits.append(d)
        return waits

    def _mark(self, tok, key, reads, writes):
        for b in reads:
            b.rd[key] = tok
        for b in writes:
            b.lw = tok
            b.rd = {}

    def op(self, eng, fn, reads=(), writes=()):
        n = self.cnt[eng]
        self.cnt[eng] += 1
        waits = self._filter(eng, self._deps(reads, writes, eng))
        tok = ("c", eng, n)
        self._mark(tok, eng, reads, writes)
        self.q[eng].append((fn, waits, tok))

    def dma(self, eng, out_ap, in_ap, reads=(), writes=()):
        j = self.ndma
        self.ndma += 1
        slot = j % self.NDMA
        val = 16 * (j // self.NDMA + 1)
        deps = self._deps(reads, writes)
        if val > 16:
            deps.append(("d", slot, val - 16))
        waits = self._filter(eng, deps)
        tok = ("d", slot, val)
        self.dlast[slot] = val
        self._mark(tok, ("d", slot), reads, writes)
        self.q[eng].append((lambda e: e.dma_start(out=out_ap, in_=in_ap), waits, tok))

    def mm(self, out, lhsT, rhs, start, stop, reads, writes):
        self.op("pe", lambda e: e.matmul(out, lhsT, rhs, start=start, stop=stop), reads, writes)

    def transpose(self, out, in_, ident, reads, writes):
        self.op("pe", lambda e: e.transpose(out, in_, ident), reads, writes)

    def act(self, out, in_, func, reads, writes, bias=None, scale=None, eng="act"):
        kw = {}
        if bias is not None:
            kw["bias"] = bias
        if scale is not None:
            kw["scale"] = scale
        self.op(eng, lambda e: e.activation(out, in_, func, **kw), reads, writes)

    def tt(self, eng, out, in0, in1, op, reads, writes):
        self.op(eng, lambda e: e.tensor_tensor(out, in0, in1, op=op), reads, writes)

    def ts(self, eng, out, in0, s1, s2, op0, op1, reads, writes):
        if s2 is None:
            self.op(eng, lambda e: e.tensor_scalar(out, in0, s1, None, op0=op0), reads, writes)
        else:
            self.op(eng, lambda e: e.tensor_scalar(out, in0, s1, s2, op0=op0, op1=op1), reads, writes)

    def stt(self, eng, out, in0, scalar, in1, op0, op1, reads, writes):
        self.op(eng, lambda e: e.scalar_tensor_tensor(out, in0, scalar, in1, op0=op0, op1=op1), reads, writes)

    def copy(self, eng, out, in_, reads, writes):
        if eng == "act":
            self.op(eng, lambda e: e.activation(out, in_, AF.Identity), reads, writes)
        else:
            self.op(eng, lambda e: e.tensor_copy(out, in_), reads, writes)

    def recip(self, out, in_, reads, writes):
        self.op("dve", lambda e: e.reciprocal(out, in_), reads, writes)

    def memset(self, eng, ap, val, writes):
        self.op(eng, lambda e: e.memset(ap, val), (), writes)

    def finish(self):
        nc = self.nc
        E = self.EPOCH
        sems = {}
        for e in ("pe", "dve", "act", "pool"):
            nep = max(1, (self.cnt[e] + E - 1) // E)
            sems[e] = [self.es.enter_context(nc.semaphore(f"s_{e}{i}")) for i in range(nep)]
        dsems = [self.es.enter_context(nc.semaphore(f"s_d{i}")) for i in range(self.NDMA)]
        fin = [("d", s, v) for s, v in sorted(self.dlast.items())]
        fin = self._filter("sp", fin)
        self.q["sp"].append((None, fin, None))

        def emit(engobj, ename):
            for fn, waits, tok in self.q[ename]:
                for w in waits:
                    if w[0] == "c":
                        engobj.wait_ge(sems[w[1]][w[2] // E], w[2] % E + 1)
                    else:
                        engobj.wait_ge(dsems[w[1]], w[2])
                if fn is None:
                    continue
                ins = fn(engobj)
                if tok[0] == "c":
                    ins.then_inc(sems[ename][tok[2] // E], 1)
                else:
                    ins.then_inc(dsems[tok[1]], 16)

        with nc.Block() as block:
            @block.sync
            def _(e):
                emit(e, "sp")

            @block.tensor
            def _(e):
                emit(e, "pe")

            @block.vector
            def _(e):
                emit(e, "dve")

            @block.scalar
            def _(e):
                emit(e, "act")

            @block.gpsimd
            def _(e):
                emit(e, "pool")
        self.es.close()
        return nc


def run(prog_nc, in_maps):
    res = run_bass_kernel_spmd(prog_nc, in_maps, core_ids=list(range(NCORES)))
    return res.results


def c_(a):
    return np.ascontiguousarray(a, dtype=np.float32)


def build_A():
    P = Prog()
    s_in = P.dram_in("s_in", [128, 8, 2])
    wm = P.dram_in("wm", [2, 1024, 6144])
    bm = P.dram_in("bm", [2, 128, 48])
    out = P.dram_out("modT", [2, 128, 2, 48])
    s_t, s_b = P.sb([128, 8, 2])
    sg_t, sg_b = P.sb([128, 8, 2])
    sb_t, sb_b = P.sb([128, 8, 2], BF16)
    bm_t, bm_b = P.sb([128, 2, 48])
    o_t, o_b = P.sb([128, 2, 2, 48])
    P.dma("sp", s_t[:], s_in[:, :, :], (), (s_b,))
    P.dma("sp", bm_t[:], bm.rearrange("l p c -> p l c"), (), (bm_b,))
    P.act(sg_t[:], s_t[:], AF.Sigmoid, (s_b,), (sg_b,))
    P.tt("dve", sb_t[:], s_t[:], sg_t[:], ALU.mult, (s_b, sg_b), (sb_b,))
    w_t, w_b = P.sb([128, 8, 6144], BF16, "wmod_sb")
    pss = [P.psum([128, 512]) for _ in range(2)]
    for l in range(2):
        ps_t, ps_b = pss[l]
        for kc in range(8):
            P.dma("pool", w_t[:, kc, :], wm[l, kc * 128:(kc + 1) * 128, :], (), (w_b,))
        for dc in range(48):
            for kc in range(8):
                P.mm(ps_t[:, dc * 2:dc * 2 + 2], w_t[:, kc, dc * 128:(dc + 1) * 128], sb_t[:, kc, :],
                     kc == 0, kc == 7, (w_b, sb_b), (ps_b,))
        for j in range(2):
            P.tt("dve", o_t[:, l, j, :], ps_t[:, j:96:2], bm_t[:, l, :], ALU.add, (ps_b, bm_b), (o_b,))
    P.dma("sp", out.rearrange("l p j c -> p l j c"), o_t[:], (o_b,), ())
    return P.finish()


def stage_A(c, c_ctx, w_mod, b_mod):
    s = np.stack([c.reshape(1024), c_ctx.reshape(1024)], axis=-1)
    s_in = c_(s.reshape(8, 128, 2).transpose(1, 0, 2))
    bm = c_(b_mod.reshape(2, 48, 128).transpose(0, 2, 1))
    nc = build_A()
    im = {"s_in": s_in, "wm": c_(w_mod), "bm": bm}
    res = run(nc, [im] * NCORES)
    return res[0]["modT"]


NCH_IN = 24
TT = 256


def build_B(ntl, ntc):
    nt = ntl + ntc
    P = Prog()
    xT = P.dram_in("xT", [8, 128, nt])
    w = P.dram_in("w", [1024, NCH_IN * 128])
    mod = P.dram_in("mod", [128, 2, 2, 8])
    out = P.dram_out("pxT", [NCH_IN, 128, nt])
    w_t, w_b = P.sb([128, 8, NCH_IN * 128], BF16, "w_sb")
    for kc in range(8):
        P.dma("pool", w_t[:, kc, :], w[kc * 128:(kc + 1) * 128, :], (), (w_b,))
    mod_t, mod_b = P.sb([128, 2, 2, 8])
    P.dma("sp", mod_t[:], mod[:, :, :, :], (), (mod_b,))
    a_t, a_b = P.sb([128, 2, 8])
    P.ts("dve", a_t[:], mod_t[:, :, 1, :], 1.0, None, ALU.add, None, (mod_b,), (a_b,))
    ones_t, ones_b = P.sb([128, 128], BF16)
    P.memset("dve", ones_t[:], 1.0, (ones_b,))
    xs = [P.sb([128, 8, TT]) for _ in range(2)]
    sqs = [P.sb([128, 8, TT], BF16) for _ in range(2)]
    hs = [P.sb([128, 8, TT], BF16) for _ in range(2)]
    rs = [P.sb([128, TT]) for _ in range(2)]
    tmp = [P.sb([128, TT]) for _ in range(2)]
    os_ = [P.sb([128, TT]) for _ in range(4)]
    pn = P.psum([128, 512])
    pp = [P.psum([128, 512]) for _ in range(4)]
    for it in range(nt // TT):
        j = 0 if it * TT < ntl else 1
        x_t, x_b = xs[it % 2]
        sq_t, sq_b = sqs[it % 2]
        h_t, h_b = hs[it % 2]
        r_t, r_b = rs[it % 2]
        P.dma("sp", x_t[:], xT[:, :, it * TT:(it + 1) * TT].rearrange("c p t -> p c t"), (), (x_b,))
        P.act(sq_t[:], x_t[:], AF.Square, (x_b,), (sq_b,))
        for kc in range(8):
            P.mm(pn[0][:, 0:TT], ones_t[:], sq_t[:, kc, :], kc == 0, kc == 7, (ones_b, sq_b), (pn[1],))
        P.act(r_t[:], pn[0][:, 0:TT], AF.Sqrt, (pn[1],), (r_b,), bias=EPS, scale=1.0 / 1024)
        P.recip(r_t[:], r_t[:], (r_b,), (r_b,))
        for kc in range(8):
            t_t, t_b = tmp[kc % 2]
            P.stt("dve", t_t[:], x_t[:, kc, :], a_t[:, j, kc:kc + 1], r_t[:], ALU.mult, ALU.mult,
                  (x_b, a_b, r_b), (t_b,))
            P.act(h_t[:, kc, :], t_t[:], AF.Identity, (t_b, mod_b), (h_b,), bias=mod_t[:, j, 0, kc:kc + 1])
        for oc in range(NCH_IN):
            ps_t, ps_b = pp[oc % 4]
            for kc in range(8):
                P.mm(ps_t[:, 0:TT], w_t[:, kc, oc * 128:(oc + 1) * 128], h_t[:, kc, :], kc == 0, kc == 7,
                     (w_b, h_b), (ps_b,))
            o_t, o_b = os_[oc % 4]
            P.copy("act" if oc % 2 else "dve", o_t[:], ps_t[:, 0:TT], (ps_b,), (o_b,))
            P.dma("sp", out[oc, :, it * TT:(it + 1) * TT], o_t[:], (o_b,), ())
    return P.finish()


def perm_w_in(w_in_l):
    z = np.zeros((1024, 104), np.float32)
    return c_(np.concatenate([w_in_l[:, 0:2304], w_in_l[:, 2328:2968], w_in_l[:, 2304:2328], z], axis=1))


def stage_B(xT_all, hcT, w_in_l, modT_l):
    ntl = SEQ // NCORES
    nc = build_B(ntl, CTX)
    w = perm_w_in(w_in_l)
    m = modT_l.reshape(128, 2, 6, 8)
    mod = c_(m[:, :, 0:2, :])
    ims = []
    for i in range(NCORES):
        xt = np.concatenate([xT_all[:, i * ntl:(i + 1) * ntl], hcT], axis=1)
        ims.append({"xT": c_(xt.reshape(8, 128, ntl + CTX)), "w": w, "mod": mod})
    res = run(nc, ims)
    px = np.concatenate([r["pxT"][:, :, :ntl].reshape(NCH_IN * 128, ntl) for r in res], axis=1)
    pc = res[0]["pxT"][:, :, ntl:].reshape(NCH_IN * 128, CTX)
    return px, pc


GRID_W = 64
NEGV = -30000.0


def rope_tables(pos):
    pos = np.asarray(pos)
    inv = 10000.0 ** (-(np.arange(0, 32, 2, dtype=np.float64)) / 32.0)
    r = (pos // GRID_W).astype(np.float64)
    col = (pos % GRID_W).astype(np.float64)
    cos = np.zeros((64, len(pos)))
    sin = np.zeros((64, len(pos)))
    for d in range(64):
        axis, half, f = d // 32, (d % 32) // 16, d % 16
        p = r if axis == 0 else col
        cos[d] = np.cos(p * inv[f])
        sin[d] = np.sin(p * inv[f]) * (-1.0 if half == 0 else 1.0)
    return cos, sin


ROPE_PERM = np.array([(d // 32) * 32 + ((d % 32) + 16) % 32 for d in range(64)])


def build_C1(ntl, need_ctx):
    nqb = ntl // 128
    nkb = nqb + 2
    nk = nkb * 128
    P = Prog()
    cv = P.dram_in("cv", [3, 2, 128, ntl + 2])
    cw = P.dram_in("cw", [128, 2, 3])
    q_in = P.dram_in("q", [2, 6, 64, ntl])
    k_in = P.dram_in("k", [2, 2, 64, nk])
    v_in = P.dram_in("v", [128, nkb, 128])
    cs_k = P.dram_in("cs_k", [2, 64, nk])
    kc_in = P.dram_in("kc", [2, 64, CTX])
    vc_in = P.dram_in("vc", [128, 2, 128])
    neg_in = P.dram_in("neg", [128, 4, 384])
    sink_in = P.dram_in("sink", [1, 2, 384])
    ident_in = P.dram_in("ident", [128, 128])
    ya = P.dram_out("yaT", [2, 128, ntl])
    yc = P.dram_out("ycT", [6, 64, ntl])
    if need_ctx:
        cvc = P.dram_in("cvc", [3, 2, 128, CTX + 2])
        qc_in = P.dram_in("qc", [6, 64, CTX])
        ya_c = P.dram_out("yaTc", [2, 128, CTX])
        yc_c = P.dram_out("ycTc", [6, 64, CTX])
    dn_in = P.dram_in("dn", [9, 128, ntl + 2])
    dnc_in = P.dram_in("dnc", [9, 128, CTX + 2])
    dcw_in = P.dram_in("dcw", [128, 9, 3])
    gab_in = P.dram_in("gab", [2, 12, ntl])
    gabc_in = P.dram_in("gabc", [2, 12, CTX])
    gpar_in = P.dram_in("gpar", [12, 2])
    bd_in = P.dram_in("bd", [128, 128])
    dn_out = P.dram_out("dno", [9, 128, ntl])
    dnc_out = P.dram_out("dnco", [9, 128, CTX])
    gb_out = P.dram_out("gbo", [2, 12, ntl])
    gbc_out = P.dram_out("gbco", [2, 12, CTX])

    cw_t, cw_b = P.sb([128, 2, 3])
    P.dma("sp", cw_t[:], cw[:, :, :], (), (cw_b,))
    ident_t, ident_b = P.sb([128, 128], BF16)
    P.dma("pool", ident_t[:], ident_in[:, :], (), (ident_b,))
    neg_t, neg_b = P.sb([128, 4, 384], BF16)
    P.dma("pool", neg_t[:], neg_in[:, :, :], (), (neg_b,))
    ones_t, ones_b = P.sb([128, 64], BF16)
    P.memset("dve", ones_t[:], 1.0, (ones_b,))
    onesf_t, onesf_b = P.sb([1, 64])
    P.memset("dve", onesf_t[:], 1.0, (onesf_b,))
    sk_t, sk_b = P.sb([1, 2, 384])
    P.dma("sp", sk_t[:], sink_in[:, :, :], (), (sk_b,))
    esk_t, esk_b = P.sb([1, 2, 384])
    P.act(esk_t[:], sk_t[:], AF.Exp, (sk_b,), (esk_b,))

    cb_t, b_b = P.sb([128, ntl + 2])
    cc_t, c_b = P.sb([128, ntl + 2])
    chh_t, h_b = P.sb([128, ntl + 2])

    def conv(src, n, dst):
        for ch in range(2):
            b_t, c_t, h_t = cb_t[:, 0:n + 2], cc_t[:, 0:n + 2], chh_t[:, 0:n + 2]
            P.dma("sp", b_t, src[0, ch, :, :], (), (b_b,))
            P.dma("sp", c_t, src[1, ch, :, :], (), (c_b,))
            P.dma("sp", h_t, src[2, ch, :, :], (), (h_b,))
            P.tt("dve", c_t, c_t, h_t, ALU.mult, (c_b, h_b), (c_b,))
            P.ts("dve", h_t[:, 0:n], c_t[:, 1:n + 1], cw_t[:, ch, 1:2], None, ALU.mult, None, (c_b, cw_b), (h_b,))
            P.stt("dve", h_t[:, 0:n], c_t[:, 0:n], cw_t[:, ch, 0:1], h_t[:, 0:n], ALU.mult, ALU.add,
                  (c_b, cw_b, h_b), (h_b,))
            P.stt("dve", h_t[:, 0:n], c_t[:, 2:n + 2], cw_t[:, ch, 2:3], h_t[:, 0:n], ALU.mult, ALU.add,
                  (c_b, cw_b, h_b), (h_b,))
            P.tt("dve", h_t[:, 0:n], h_t[:, 0:n], b_t[:, 1:n + 1], ALU.mult, (h_b, b_b), (h_b,))
            P.dma("sp", dst[ch, :, :], h_t[:, 0:n], (h_b,), ())
    conv(cv, ntl, ya)
    if need_ctx:
        conv(cvc, CTX, ya_c)

    dcw_t, dcw_b = P.sb([128, 9, 3])
    P.dma("sp", dcw_t[:], dcw_in[:, :, :], (), (dcw_b,))
    bd_t, bd_b = P.sb([128, 128])
    P.dma("sp", bd_t[:], bd_in[:, :], (), (bd_b,))
    rr_t, rr_b = P.sb([128, 512])
    pps = P.psum([128, 512])

    def dnprep(src, n, dst):
        for ch in range(9):
            x_t, y_t, s_t = cb_t[:, 0:n + 2], cc_t[:, 0:n], chh_t[:, 0:n]
            x_b, y_b, s_b = b_b, c_b, h_b
            P.dma("sp", x_t, src[ch, :, :], (), (x_b,))
            P.ts("dve", y_t, x_t[:, 1:n + 1], dcw_t[:, ch, 1:2], None, ALU.mult, None, (x_b, dcw_b), (y_b,))
            P.stt("dve", y_t, x_t[:, 0:n], dcw_t[:, ch, 0:1], y_t, ALU.mult, ALU.add, (x_b, dcw_b, y_b), (y_b,))
            P.stt("dve", y_t, x_t[:, 2:n + 2], dcw_t[:, ch, 2:3], y_t, ALU.mult, ALU.add, (x_b, dcw_b, y_b), (y_b,))
            P.act(y_t, y_t, AF.Silu, (y_b,), (y_b,))
            if ch < 6:
                P.act(s_t, y_t, AF.Square, (y_b,), (s_b,))
                for c0 in range(0, n, 512):
                    w_ = min(512, n - c0)
                    P.mm(pps[0][:, 0:w_], bd_t[:], s_t[:, c0:c0 + w_], True, True, (bd_b, s_b), (pps[1],))
                    P.act(rr_t[:, 0:w_], pps[0][:, 0:w_], AF.Sqrt, (pps[1],), (rr_b,), bias=1e-6)
                    P.recip(rr_t[:, 0:w_], rr_t[:, 0:w_], (rr_b,), (rr_b,))
                    P.stt("dve", y_t[:, c0:c0 + w_], y_t[:, c0:c0 + w_], 0.125 if ch < 3 else 1.0, rr_t[:, 0:w_],
                          ALU.mult, ALU.mult, (y_b, rr_b), (y_b,))
            P.dma("sp", dst[ch, :, :], y_t, (y_b,), ())
    dnprep(dn_in, ntl, dn_out)
    dnprep(dnc_in, CTX, dnc_out)
    gpar_t, gpar_b = P.sb([12, 2])
    P.dma("sp", gpar_t[:], gpar_in[:, :], (), (gpar_b,))
    nea_t, nea_b = P.sb([12, 1])
    P.act(nea_t[:], gpar_t[:, 0:1], AF.Exp, (gpar_b,), (nea_b,))
    P.ts("dve", nea_t[:], nea_t[:], -1.0, None, ALU.mult, None, (nea_b,), (nea_b,))

    def gates(src, n, dst):
        a_t, bt_t = cb_t[0:12, 0:n], cc_t[0:12, 0:n]
        P.dma("sp", a_t, src[0, :, :], (), (b_b,))
        P.dma("sp", bt_t, src[1, :, :], (), (c_b,))
        P.act(a_t, a_t, AF.Exp, (b_b, gpar_b), (b_b,), bias=gpar_t[:, 1:2])
        P.act(a_t, a_t, AF.Ln, (b_b,), (b_b,), bias=1.0)
        P.ts("dve", a_t, a_t, nea_t[:, 0:1], None, ALU.mult, None, (b_b, nea_b), (b_b,))
        P.act(bt_t, bt_t, AF.Sigmoid, (c_b,), (c_b,))
        P.dma("sp", dst[0, :, :], a_t, (b_b,), ())
        P.dma("sp", dst[1, :, :], bt_t, (c_b,), ())
    gates(gab_in, ntl, gb_out)
    gates(gabc_in, CTX, gbc_out)

    csk_t, csk_b = P.sb([64, 2, nk])
    P.dma("sp", csk_t[:], cs_k.rearrange("c d t -> d c t"), (), (csk_b,))
    qr_t, qr_b = P.sb([64, nqb, 2, 3, 128], BF16, "qr")
    kr_t, kr_b = P.sb([64, 2, nk], BF16, "kr")
    qa = [P.sb([64, nk]) for _ in range(2)]
    qp = [P.sb([64, nk]) for _ in range(2)]
    for h in range(6):
        a_t, a_b = qa[h % 2]
        p_t, p_b = qp[h % 2]
        P.dma("sp", a_t[:, 0:ntl], q_in[0, h, :, :], (), (a_b,))
        P.dma("sp", p_t[:, 0:ntl], q_in[1, h, :, :], (), (p_b,))
        P.tt("dve", a_t[:, 0:ntl], a_t[:, 0:ntl], csk_t[:, 0, 128:128 + ntl], ALU.mult, (a_b, csk_b), (a_b,))
        P.tt("pool", p_t[:, 0:ntl], p_t[:, 0:ntl], csk_t[:, 1, 128:128 + ntl], ALU.mult, (p_b, csk_b), (p_b,))
        P.tt("dve", qr_t[:, :, h // 3, h % 3, :], a_t[:, 0:ntl].rearrange("p (b q) -> p b q", q=128),
             p_t[:, 0:ntl].rearrange("p (b q) -> p b q", q=128), ALU.add, (a_b, p_b), (qr_b,))
    for g in range(2):
        a_t, a_b = qa[g]
        p_t, p_b = qp[g]
        P.dma("sp", a_t[:], k_in[0, g, :, :], (), (a_b,))
        P.dma("sp", p_t[:], k_in[1, g, :, :], (), (p_b,))
        P.tt("dve", a_t[:], a_t[:], csk_t[:, 0, :], ALU.mult, (a_b, csk_b), (a_b,))
        P.tt("pool", p_t[:], p_t[:], csk_t[:, 1, :], ALU.mult, (p_b, csk_b), (p_b,))
        P.tt("dve", kr_t[:, g, :], a_t[:], p_t[:], ALU.add, (a_b, p_b), (kr_b,))
    kcb_t, kcb_b = P.sb([64, 2, CTX], BF16)
    P.dma("pool", kcb_t[:], kc_in.rearrange("g d t -> d g t"), (), (kcb_b,))
    v_t, v_b = P.sb([128, nkb, 128], BF16, "v_sb")
    P.dma("pool", v_t[:], v_in[:, :, :], (), (v_b,))
    vc_t, vc_b = P.sb([128, 2, 128], BF16)
    P.dma("pool", vc_t[:], vc_in[:, :, :], (), (vc_b,))
    if need_ctx:
        qcb_t, qcb_b = P.sb([64, 2, 2, 3, 128], BF16)
        for h in range(6):
            P.dma("pool", qcb_t[:, :, h // 3, h % 3, :], qc_in[h, :, :].rearrange("d (b q) -> d b q", q=128),
                  (), (qcb_b,))

    ps_s = [P.psum([128, 512]) for _ in range(5)]
    ps_o = P.psum([128, 512])
    ps_d = P.psum([128, 512])
    pts = [P.sb([128, 384], BF16) for _ in range(5)]
    rd = [P.sb([64, 384]) for _ in range(2)]
    ob = [P.sb([64, 384]) for _ in range(2)]
    cnt = [0, 0]

    def attend(qsrc, qsrc_b, qb, g, keyspecs, dst, t0):
        pts_used = []
        for (k_ap, k_b, v_ap, v_bf, negidx) in keyspecs:
            i = cnt[0] % 5
            cnt[0] += 1
            s_t, s_b = ps_s[i]
            rhs = qsrc[:, qb, g, :, :].rearrange("d h q -> d (h q)")
            P.mm(s_t[:, 0:384], k_ap, rhs, True, negidx is None, (k_b, qsrc_b), (s_b,))
            if negidx is not None:
                P.mm(s_t[:, 0:384], ident_t[:], neg_t[:, negidx, :], False, True, (ident_b, neg_b), (s_b,))
            p_t, p_b = pts[i]
            P.act(p_t[:], s_t[:, 0:384], AF.Exp, (s_b,), (p_b,), scale=0.125)
            pts_used.append((p_t, p_b, v_ap, v_bf))
        n = len(pts_used)
        for idx, (p_t, p_b, v_ap, v_bf) in enumerate(pts_used):
            P.mm(ps_o[0][0:64, 0:384], v_ap, p_t[:], idx == 0, idx == n - 1, (v_bf, p_b), (ps_o[1],))
        for idx, (p_t, p_b, v_ap, v_bf) in enumerate(pts_used):
            P.mm(ps_d[0][0:64, 0:384], ones_t[:], p_t[:], idx == 0, False, (ones_b, p_b), (ps_d[1],))
        P.mm(ps_d[0][0:64, 0:384], onesf_t[:], esk_t[:, g, :], False, True, (onesf_b, esk_b), (ps_d[1],))
        j = cnt[1] % 2
        cnt[1] += 1
        r_t, r_b = rd[j]
        o_t, o_b = ob[j]
        P.recip(r_t[:], ps_d[0][0:64, 0:384], (ps_d[1],), (r_b,))
        P.tt("dve", o_t[:], ps_o[0][0:64, 0:384], r_t[:], ALU.mult, (ps_o[1], r_b), (o_b,))
        P.dma("sp", dst[3 * g:3 * g + 3, :, t0:t0 + 128].rearrange("h d q -> d h q"),
              o_t[:].rearrange("d (h q) -> d h q", h=3), (o_b,), ())

    for qb in range(nqb):
        for g in range(2):
            specs = []
            for off in range(3):
                kb = qb + off
                negidx = None
                if off == 0:
                    negidx = 0 if qb == 0 else 1
                elif off == 2:
                    negidx = 3 if qb == nqb - 1 else 2
                specs.append((kr_t[:, g, kb * 128:(kb + 1) * 128], kr_b, v_t[:, kb, g * 64:(g + 1) * 64], v_b, negidx))
            for cb in range(2):
                specs.append((kcb_t[:, g, cb * 128:(cb + 1) * 128], kcb_b, vc_t[:, cb, g * 64:(g + 1) * 64], vc_b, None))
            attend(qr_t, qr_b, qb, g, specs, yc, qb * 128)
    if need_ctx:
        for qb in range(2):
            for g in range(2):
                specs = [(kcb_t[:, g, cb * 128:(cb + 1) * 128], kcb_b, vc_t[:, cb, g * 64:(g + 1) * 64], vc_b, None)
                         for cb in range(2)]
                attend(qcb_t, qcb_b, qb, g, specs, yc_c, qb * 128)
    return P.finish()


R_CB, R_CC, R_CH = 0, 256, 512
R_DQ, R_DK, R_DV, R_DZ = 768, 1152, 1536, 1920
R_AQ, R_AK, R_AV = 2304, 2688, 2816
R_A, R_BT = 2944, 2956


def tri_masks():
    kk = np.arange(128)[:, None]
    qq = np.arange(128)[None, :]
    prev = np.where(qq <= kk, 0.0, NEGV)
    nxt = np.where(kk <= qq, 0.0, NEGV)
    return prev, nxt


def stage_C1(px, pc, conv_w_l, sink_l, dn_conv_w_l, dn_a_log_l, dn_dt_bias_l, need_ctx):
    ntl = SEQ // NCORES
    nc = build_C1(ntl, need_ctx)
    nkb = ntl // 128 + 2
    cw = c_(conv_w_l.T.reshape(2, 128, 3).transpose(1, 0, 2))
    prev, nxt = tri_masks()
    full = np.full((128, 128), NEGV)
    cos, sin = rope_tables(np.arange(SEQ))
    cosp = np.pad(cos, ((0, 0), (128, 128)))
    sinp = np.pad(sin, ((0, 0), (128, 128)))
    pad1 = lambda a: np.pad(a, ((0, 0), (1, 1)))
    convsrc = [pad1(px[R_CB:R_CB + 256]), pad1(px[R_CC:R_CC + 256]), pad1(px[R_CH:R_CH + 256])]
    kx = np.pad(px[R_AK:R_AK + 128], ((0, 0), (128, 128))).reshape(2, 64, SEQ + 256)
    kxp = kx[:, ROPE_PERM, :]
    vx = np.pad(px[R_AV:R_AV + 128], ((0, 0), (128, 128)))
    qx = px[R_AQ:R_AQ + 384].reshape(6, 64, SEQ)
    qxp = qx[:, ROPE_PERM, :]
    kc = c_(pc[R_AK:R_AK + 128].reshape(2, 64, CTX))
    vc = c_(pc[R_AV:R_AV + 128].T.reshape(2, 128, 128).transpose(1, 0, 2))
    sink = c_(np.repeat(sink_l.reshape(2, 3), 128, axis=1).reshape(1, 2, 384))
    ident = np.eye(128, dtype=np.float32)
    dnsrc = pad1(px[R_DQ:R_DQ + 1152])
    dnc = c_(pad1(pc[R_DQ:R_DQ + 1152]).reshape(9, 128, CTX + 2))
    dcw = c_(dn_conv_w_l.T.reshape(9, 128, 3).transpose(1, 0, 2))
    gpar = c_(np.stack([dn_a_log_l.reshape(12), dn_dt_bias_l.reshape(12)], axis=1))
    bd = c_(np.kron(np.eye(2), np.ones((64, 64))))
    gabc = c_(np.stack([pc[R_A:R_A + 12], pc[R_BT:R_BT + 12]]))
    ims = []
    for i in range(NCORES):
        t0, t1 = i * ntl, (i + 1) * ntl
        neg = np.stack([np.tile(full if i == 0 else prev, (1, 3)), np.tile(prev, (1, 3)),
                        np.tile(nxt, (1, 3)), np.tile(full if i == NCORES - 1 else nxt, (1, 3))], axis=1)
        im = {
            "cv": c_(np.stack([a[:, t0:t1 + 2].reshape(2, 128, ntl + 2) for a in convsrc])),
            "cw": cw,
            "q": c_(np.stack([qx[:, :, t0:t1], qxp[:, :, t0:t1]])),
            "k": c_(np.stack([kx[:, :, t0:t1 + 256], kxp[:, :, t0:t1 + 256]])),
            "v": c_(vx[:, t0:t1 + 256].T.reshape(nkb, 128, 128).transpose(1, 0, 2)),
            "cs_k": c_(np.stack([cosp[:, t0:t1 + 256], sinp[:, t0:t1 + 256]])),
            "kc": kc, "vc": vc, "neg": c_(neg), "sink": sink, "ident": ident,
            "dn": c_(dnsrc[:, t0:t1 + 2].reshape(9, 128, ntl + 2)), "dnc": dnc, "dcw": dcw,
            "gab": c_(np.stack([px[R_A:R_A + 12, t0:t1], px[R_BT:R_BT + 12, t0:t1]])), "gabc": gabc,
            "gpar": gpar, "bd": bd,
        }
        if need_ctx:
            im["cvc"] = c_(np.stack([pad1(pc[r:r + 256]).reshape(2, 128, CTX + 2) for r in (R_CB, R_CC, R_CH)]))
            im["qc"] = c_(pc[R_AQ:R_AQ + 384].reshape(6, 64, CTX))
        ims.append(im)
    res = run(nc, ims)
    yaT = np.concatenate([r["yaT"].reshape(256, ntl) for r in res], axis=1)
    ycT = np.concatenate([r["ycT"].reshape(384, ntl) for r in res], axis=1)
    out = {"yaT": yaT, "ycT": ycT}
    out["dn"] = np.concatenate([r["dno"].reshape(1152, ntl) for r in res], axis=1)
    out["dnc"] = res[0]["dnco"].reshape(1152, CTX)
    out["gb"] = np.concatenate([r["gbo"] for r in res], axis=2)
    out["gbc"] = res[0]["gbco"]
    if need_ctx:
        out["yaTc"] = res[0]["yaTc"].reshape(256, CTX)
        out["ycTc"] = res[0]["ycTc"].reshape(384, CTX)
    return out


def c2_consts():
    i = np.arange(128)
    same = (i[:, None] // 64) == (i[None, :] // 64)
    tri = (same & (i[:, None] <= i[None, :])).astype(np.float32)
    bd = same.astype(np.float32)
    negl = np.where(same & (i[:, None] >= i[None, :]), 0.0, NEGV).astype(np.float32)
    slm = (same & (i[:, None] > i[None, :])).astype(np.float32)
    ident = np.eye(128, dtype=np.float32)
    ones = np.ones((128, 128), np.float32)
    sel = np.zeros((128, 2, 64), np.float32)
    sel[0:64, 0, :] = 1.0
    sel[64:128, 1, :] = 1.0
    return {"tri": tri, "bd": bd, "negl": negl, "slm": slm, "ident": ident, "ones": ones,
            "sel": sel.reshape(128, 128)}


C2_STOP = 9


def build_C2(NP):
    T = NP * 128
    G = 5 if NP % 5 == 0 else (4 if NP % 4 == 0 else (2 if NP % 2 == 0 else 1))
    P = Prog()
    qk_in = P.dram_in("qk", [2, 2, 64, T])
    tok_in = P.dram_in("tok", [2, 128, NP, 192])
    gb_in = P.dram_in("gbt", [2, 128, 2, NP])
    o_out = P.dram_out("oT", [2, 64, T])
    C = {}
    for nm in ("tri", "bd", "negl", "slm", "ident", "ones", "sel"):
        d_in = P.dram_in(nm, [128, 128])
        t, b = P.sb([128, 128], F32, "c_" + nm)
        P.dma("sp", t[:], d_in[:, :], (), (b,))
        C[nm] = (t, b)
    tri_t, tri_b = C["tri"]
    ident_t, ident_b = C["ident"]
    ones_t, ones_b = C["ones"]
    negl_t, negl_b = C["negl"]
    slm_t, slm_b = C["slm"]
    bd_t, bd_b = C["bd"]
    sel_t, sel_b = C["sel"]

    banks = [P.psum([128, 512]) for _ in range(8)]

    class U:
        pass
    units = []
    for u in range(2):
        S_ = U()
        S_.u = u
        bk = banks[4 * u:4 * u + 4]
        S_.kk = (bk[0][0], 0, bk[0][1])
        S_.qk = (bk[0][0], 128, bk[0][1])
        S_.m = (bk[0][0], 256, bk[0][1])
        S_.n = (bk[0][0], 384, bk[0][1])
        S_.ng = (bk[1][0], 0, bk[1][1])
        S_.tr = (bk[1][0], 128, bk[1][1])
        S_.tr2 = (bk[1][0], 256, bk[1][1])
        S_.qe = (bk[1][0], 384, bk[1][1])
        S_.sqa = (bk[2][0], 0, bk[2][1])
        S_.sqb = (bk[2][0], 128, bk[2][1])
        S_.ap = (bk[2][0], 256, bk[2][1])
        S_.s = (bk[2][0], 384, bk[2][1])
        S_.o = [(bk[3][0], 0, bk[3][1]), (bk[3][0], 128, bk[3][1])]
        S_.pre = bk[3]
        S_.gb = P.sb([128, 2, NP])
        S_.ng_sb = P.sb([128, NP])
        S_.nb = P.sb([128, NP])
        S_.gc = P.sb([128, NP])
        S_.e = P.sb([128, NP])
        S_.kdf = P.sb([128, NP])
        S_.be = P.sb([128, NP])
        S_.GL = P.sb([64, 2, NP])
        S_.qkin = [P.sb([64, 2, G * 128]) for _ in range(2)]
        S_.tokin = [P.sb([128, G, 192]) for _ in range(2)]
        S_.R = [P.sb([128, 128]) for _ in range(2)]
        S_.Dm = [P.sb([128, 128]) for _ in range(2)]
        S_.DmT = [P.sb([128, 128]) for _ in range(2)]
        S_.DmS = [P.sb([128, 128]) for _ in range(2)]
        S_.pw = [P.sb([128, 128]) for _ in range(4)]
        S_.qkT = [P.sb([128, 128]) for _ in range(2)]
        S_.X = [P.sb([128, 128]) for _ in range(3)]
        S_.kd = [P.sb([128, 2, 64]) for _ in range(2)]
        S_.kdfm = P.sb([128, 2, NP])
        S_.nw = [P.sb([128, 64]) for _ in range(2)]
        S_.McT = [P.sb([64, 2, 64]) for _ in range(2)]
        S_.Nsb = [P.sb([64, 128]) for _ in range(2)]
        S_.dE = [P.sb([128, 128]) for _ in range(2)]
        S_.qe_sb = [P.sb([64, 128]) for _ in range(2)]
        S_.S = [P.sb([64, 64]) for _ in range(3)]
        S_.osb = [P.sb([64, 128]) for _ in range(2)]
        S_.si = 0
        S_.xi = 0
        units.append(S_)

    def ps(slot, rows=128, cols=128):
        t, c0, b = slot
        return t[0:rows, c0:c0 + cols], b

    for S_ in units:
        u = S_.u
        gb_t, gb_b = S_.gb
        P.dma("sp", gb_t[:], gb_in[u, :, :, :], (), (gb_b,))
        g_ap, beta_ap = gb_t[:, 0, :], gb_t[:, 1, :]
        P.ts("dve", S_.ng_sb[0][:], g_ap, -1.0, None, ALU.mult, None, (gb_b,), (S_.ng_sb[1],))
        P.ts("dve", S_.nb[0][:], beta_ap, -1.0, None, ALU.mult, None, (gb_b,), (S_.nb[1],))
        pre_t, pre_b = S_.pre
        P.mm(pre_t[:, 0:NP], tri_t[:], g_ap, True, True, (tri_b, gb_b), (pre_b,))
        P.copy("act", S_.gc[0][:], pre_t[:, 0:NP], (pre_b,), (S_.gc[1],))
        P.act(S_.e[0][:], S_.gc[0][:], AF.Exp, (S_.gc[1],), (S_.e[1],))
        P.tt("dve", S_.be[0][:], S_.e[0][:], beta_ap, ALU.mult, (S_.e[1], gb_b), (S_.be[1],))
        P.mm(pre_t[:, 0:NP], bd_t[:], g_ap, True, True, (bd_b, gb_b), (pre_b,))
        P.tt("dve", S_.kdf[0][:], pre_t[:, 0:NP], S_.gc[0][:], ALU.subtract, (pre_b, S_.gc[1]), (S_.kdf[1],))
        P.act(S_.kdf[0][:], S_.kdf[0][:], AF.Exp, (S_.kdf[1],), (S_.kdf[1],))
        for c in range(2):
            P.ts("dve", S_.kdfm[0][:, c, :], S_.kdf[0][:], sel_t[:, c * 64:c * 64 + 1], None, ALU.mult, None,
                 (S_.kdf[1], sel_b), (S_.kdfm[1],))
        for c in range(2):
            P.mm(pre_t[0:64, 0:NP], sel_t[:, c * 64:(c + 1) * 64], g_ap, True, True, (sel_b, gb_b), (pre_b,))
            P.act(S_.GL[0][:, c, :], pre_t[0:64, 0:NP], AF.Exp, (pre_b,), (S_.GL[1],))
        P.memset("dve", S_.S[0][0][:], 0.0, (S_.S[0][1],))

    def load_group(S_, gi):
        u = S_.u
        qk_t, qk_b = S_.qkin[gi % 2]
        tk_t, tk_b = S_.tokin[gi % 2]
        P.dma("sp", qk_t[:], qk_in[u, :, :, gi * G * 128:(gi + 1) * G * 128].rearrange("a d t -> d a t"), (), (qk_b,))
        P.dma("sp", tk_t[:], tok_in[u, :, gi * G:(gi + 1) * G, :], (), (tk_b,))

    def pre(S_, p):
        gi, pi = p // G, p % G
        qk_t, qk_b = S_.qkin[gi % 2]
        tk_t, tk_b = S_.tokin[gi % 2]
        qT = qk_t[:, 0, pi * 128:(pi + 1) * 128]
        kT = qk_t[:, 1, pi * 128:(pi + 1) * 128]
        Qt, Kt, Vt = tk_t[:, pi, 0:64], tk_t[:, pi, 64:128], tk_t[:, pi, 128:192]
        gb_t, gb_b = S_.gb
        j = p % 2
        kk_ap, kk_b = ps(S_.kk)
        qk_ap, qkp_b = ps(S_.qk)
        P.mm(kk_ap, kT, kT, True, True, (qk_b,), (kk_b,))
        yield
        P.mm(qk_ap, kT, qT, True, True, (qk_b,), (qkp_b,))
        yield
        R_t, R_b = S_.R[j]
        P.ts("pool", R_t[:], tri_t[:], S_.ng_sb[0][:, p:p + 1], None, ALU.mult, None, (tri_b, S_.ng_sb[1]), (R_b,))
        yield
        ng_ap, ng_b = ps(S_.ng)
        P.mm(ng_ap, ones_t[:], R_t[:], True, False, (ones_b, R_b), (ng_b,))
        P.mm(ng_ap, ident_t[:], negl_t[:], False, True, (ident_b, negl_b), (ng_b,))
        yield
        Dm_t, Dm_b = S_.Dm[j]
        P.act(Dm_t[:], ng_ap, AF.Exp, (ng_b, S_.gc[1]), (Dm_b,), bias=S_.gc[0][:, p:p + 1])
        yield
        tr_ap, tr_b = ps(S_.tr)
        P.transpose(tr_ap, Dm_t[:], ident_t[:], (Dm_b, ident_b), (tr_b,))
        yield
        DmT_t, DmT_b = S_.DmT[j]
        P.copy("act", DmT_t[:], tr_ap, (tr_b,), (DmT_b,))
        yield
        DmS_t, DmS_b = S_.DmS[j]
        P.tt("pool", DmS_t[:], Dm_t[:], slm_t[:], ALU.mult, (Dm_b, slm_b), (DmS_b,))
        yield
        cur_t, cur_b = S_.pw[0]
        curT_t, curT_b = S_.pw[1]
        P.stt("dve", cur_t[:], kk_ap, S_.nb[0][:, p:p + 1], DmS_t[:], ALU.mult, ALU.mult,
              (kk_b, S_.nb[1], DmS_b), (cur_b,))
        yield
        tr2_ap, tr2_b = ps(S_.tr2)
        P.transpose(tr2_ap, cur_t[:], ident_t[:], (cur_b, ident_b), (tr2_b,))
        yield
        P.copy("act", curT_t[:], tr2_ap, (tr2_b,), (curT_b,))
        yield
        qkT_t, qkT_b = S_.qkT[j]
        P.tt("dve", qkT_t[:], qk_ap, DmT_t[:], ALU.mult, (qkp_b, DmT_b), (qkT_b,))
        yield
        X_t, X_b = S_.X[S_.xi % 3]
        S_.xi += 1
        P.ts("pool", X_t[:, 0:64], Kt, S_.be[0][:, p:p + 1], None, ALU.mult, None, (tk_b, S_.be[1]), (X_b,))
        yield
        P.ts("pool", X_t[:, 64:128], Vt, gb_t[:, 1, p:p + 1], None, ALU.mult, None, (tk_b, gb_b), (X_b,))
        yield
        pwi = 0
        for lvl in range(6):
            ap_ap, ap_b = ps(S_.ap)
            P.mm(ap_ap, curT_t[:], X_t[:], True, True, (curT_b, X_b), (ap_b,))
            yield
            Xn_t, Xn_b = S_.X[S_.xi % 3]
            S_.xi += 1
            P.tt("dve", Xn_t[:], ap_ap, X_t[:], ALU.add, (ap_b, X_b), (Xn_b,))
            yield
            X_t, X_b = Xn_t, Xn_b
            if lvl < 5:
                nxt_t, nxt_b = S_.pw[(pwi + 2) % 4]
                nxtT_t, nxtT_b = S_.pw[(pwi + 3) % 4]
                if lvl < 4:
                    sa_ap, sa_b = ps(S_.sqa)
                    P.mm(sa_ap, curT_t[:], cur_t[:], True, True, (curT_b, cur_b), (sa_b,))
                    yield
                    P.copy("act", nxt_t[:], sa_ap, (sa_b,), (nxt_b,))
                    yield
                sb_ap, sb_b = ps(S_.sqb)
                P.mm(sb_ap, cur_t[:], curT_t[:], True, True, (cur_b, curT_b), (sb_b,))
                yield
                P.copy("act", nxtT_t[:], sb_ap, (sb_b,), (nxtT_b,))
                yield
                cur_t, cur_b, curT_t, curT_b = nxt_t, nxt_b, nxtT_t, nxtT_b
                pwi = (pwi + 2) % 4
        kd_t, kd_b = S_.kd[j]
        nw_t, nw_b = S_.nw[j]
        for c in range(2):
            P.ts("pool", kd_t[:, c, :], Kt, S_.kdfm[0][:, c, p:p + 1], None, ALU.mult, None, (tk_b, S_.kdfm[1]), (kd_b,))
            yield
        P.ts("pool", nw_t[:], X_t[:, 0:64], -1.0, None, ALU.mult, None, (X_b,), (nw_b,))
        yield
        m_t, m0, m_b = S_.m
        n_t, n0, n_b = S_.n
        for c in range(2):
            r0, r1 = c * 64, (c + 1) * 64
            P.mm(m_t[0:64, m0 + r0:m0 + r1], nw_t[:], kd_t[:, c, :], True, True, (nw_b, kd_b), (m_b,))
            yield
            P.mm(n_t[0:64, n0 + r0:n0 + r1], kd_t[:, c, :], X_t[:, 64:128], True, True, (kd_b, X_b), (n_b,))
            yield
        McT_t, McT_b = S_.McT[j]
        for c in range(2):
            P.stt("dve", McT_t[:, c, :], ident_t[0:64, 0:64], S_.GL[0][:, c, p:p + 1],
                  m_t[0:64, m0 + c * 64:m0 + (c + 1) * 64], ALU.mult, ALU.add, (ident_b, S_.GL[1], m_b), (McT_b,))
            yield
        Nsb_t, Nsb_b = S_.Nsb[j]
        P.copy("act", Nsb_t[:], n_t[0:64, n0:n0 + 128], (n_b,), (Nsb_b,))
        yield
        dE_t, dE_b = S_.dE[j]
        P.ts("pool", dE_t[:], ident_t[:], S_.e[0][:, p:p + 1], None, ALU.mult, None, (ident_b, S_.e[1]), (dE_b,))
        yield
        qe_ap, qe_b = ps(S_.qe, 64, 128)
        P.mm(qe_ap, Qt, dE_t[:], True, False, (tk_b, dE_b), (qe_b,))
        P.mm(qe_ap, nw_t[:], qkT_t[:], False, True, (nw_b, qkT_b), (qe_b,))
        yield
        qes_t, qes_b = S_.qe_sb[j]
        P.copy("act", qes_t[:], qe_ap, (qe_b,), (qes_b,))
        yield
        S_.cur = dict(X=(X_t, X_b), qkT=(qkT_t, qkT_b), McT=(McT_t, McT_b), Nsb=(Nsb_t, Nsb_b), qe=(qes_t, qes_b))

    def scan(S_, p, cur):
        X_t, X_b = cur["X"]
        qkT_t, qkT_b = cur["qkT"]
        McT_t, McT_b = cur["McT"]
        Nsb_t, Nsb_b = cur["Nsb"]
        qes_t, qes_b = cur["qe"]
        o_ap, o_b = ps(S_.o[p % 2], 64, 128)
        ot, oc0, _ = S_.o[p % 2]
        P.mm(o_ap, X_t[:, 64:128], qkT_t[:], True, False, (X_b, qkT_b), (o_b,))
        yield
        s_ap, s_b = ps(S_.s, 64, 64)
        for c in range(2):
            St, Sb = S_.S[S_.si % 3]
            P.mm(ot[0:64, oc0 + c * 64:oc0 + (c + 1) * 64], St[:], qes_t[:, c * 64:(c + 1) * 64], False, c == 1,
                 (Sb, qes_b), (o_b,))
            yield
            P.mm(s_ap, McT_t[:, c, :], St[:], True, False, (McT_b, Sb), (s_b,))
            P.mm(s_ap, ident_t[0:64, 0:64], Nsb_t[:, c * 64:(c + 1) * 64], False, True, (ident_b, Nsb_b), (s_b,))
            yield
            S_.si += 1
            Sn, Snb = S_.S[S_.si % 3]
            P.copy("act", Sn[:], s_ap, (s_b,), (Snb,))
            yield
        os_t, os_b = S_.osb[p % 2]
        P.copy("dve", os_t[:], o_ap, (o_b,), (os_b,))
        yield
        P.dma("sp", o_out[S_.u, :, p * 128:(p + 1) * 128], os_t[:], (os_b,), ())

    from itertools import zip_longest
    saved = [None, None]
    for p in range(NP + 1):
        gens = []
        if p < NP:
            for S_ in units:
                if p % G == 0:
                    load_group(S_, p // G)
                gens.append(pre(S_, p))
        if p >= 1:
            for S_ in units:
                gens.append(scan(S_, p - 1, saved[S_.u]))
        for _ in zip_longest(*gens):
            pass
        saved = [S_.cur for S_ in units]
    return P.finish()


def c2_unit_inputs(q, k, v, g, beta):
    T = q.shape[1]
    NP = T // 128
    qk = np.stack([q, k])
    tok = np.concatenate([q.T, k.T, v.T], axis=1).reshape(NP, 128, 192).transpose(1, 0, 2)
    gbt = np.stack([g.reshape(NP, 128).T, beta.reshape(NP, 128).T], axis=1)
    return c_(qk), c_(tok), c_(gbt)


def run_C2(unit_list):
    T = unit_list[0][0].shape[1]
    nc = build_C2(T // 128)
    consts = c2_consts()
    zero = tuple(np.zeros_like(a) for a in unit_list[0])
    ims = []
    for i in range(NCORES):
        us = [unit_list[2 * i + j] if 2 * i + j < len(unit_list) else zero for j in range(2)]
        parts = [c2_unit_inputs(*u_) for u_ in us]
        im = {"qk": np.stack([p_[0] for p_ in parts]), "tok": np.stack([p_[1] for p_ in parts]),
              "gbt": np.stack([p_[2] for p_ in parts])}
        im.update(consts)
        ims.append(im)
    res = run(nc, ims)
    outs = []
    for idx in range(len(unit_list)):
        outs.append(res[idx // 2]["oT"][idx % 2])
    return outs


def tok_blocks(ntl, ntc, bw=512):
    blks = [(t0, min(bw, ntl - t0), 0) for t0 in range(0, ntl, bw)]
    if ntc:
        blks += [(ntl + t0, min(bw, ntc - t0), 1) for t0 in range(0, ntc, bw)]
    return blks


def build_E1(ntl, ntc):
    nt = ntl + ntc
    P = Prog()
    xT = P.dram_in("xT", [8, 128, nt])
    ya = P.dram_in("ya", [2, 128, nt])
    yc = P.dram_in("yc", [3, 128, nt])
    of = P.dram_in("of", [3, 128, nt])
    ob = P.dram_in("ob", [3, 128, nt])
    z = P.dram_in("z", [3, 128, nt])
    w = P.dram_in("w", [1024, 1024])
    mod2 = P.dram_in("mod2", [128, 2, 8])
    dng = P.dram_in("dng", [128, 1])
    bd_in = P.dram_in("bd", [128, 128])
    out = P.dram_out("x1T", [8, 128, nt])
    w_t, w_b = P.sb([128, 8, 1024], BF16, "wout")
    for mc in range(8):
        P.dma("pool", w_t[:, mc, :], w[mc * 128:(mc + 1) * 128, :], (), (w_b,))
    m2_t, m2_b = P.sb([128, 2, 8])
    P.dma("sp", m2_t[:], mod2[:, :, :], (), (m2_b,))
    g_t, g_b = P.sb([128, 1])
    P.dma("sp", g_t[:], dng[:, :], (), (g_b,))
    bd_t, bd_b = P.sb([128, 128])
    P.dma("sp", bd_t[:], bd_in[:, :], (), (bd_b,))
    mixs = [P.sb([128, 8, 512], BF16) for _ in range(2)]
    xs = [P.sb([128, 8, 512]) for _ in range(2)]
    ofs = [P.sb([128, 512]) for _ in range(2)]
    obs = [P.sb([128, 512]) for _ in range(2)]
    zs = [P.sb([128, 512]) for _ in range(2)]
    sqs = [P.sb([128, 512]) for _ in range(2)]
    rs = [P.sb([128, 512]) for _ in range(2)]
    pn = [P.psum([128, 512]) for _ in range(2)]
    pp = [P.psum([128, 512]) for _ in range(4)]
    k = 0
    for bi, (t0, wd_, j) in enumerate(tok_blocks(ntl, ntc)):
        mix_t, mix_b = mixs[bi % 2]
        x_t, x_b = xs[bi % 2]
        P.dma("pool", mix_t[:, 0:2, 0:wd_], ya[:, :, t0:t0 + wd_].rearrange("c p t -> p c t"), (), (mix_b,))
        P.dma("pool", mix_t[:, 5:8, 0:wd_], yc[:, :, t0:t0 + wd_].rearrange("c p t -> p c t"), (), (mix_b,))
        P.dma("sp", x_t[:, :, 0:wd_], xT[:, :, t0:t0 + wd_].rearrange("c p t -> p c t"), (), (x_b,))
        for ch in range(3):
            o_t, o_b = ofs[k % 2]
            b_t, b_b = obs[k % 2]
            z_t, z_b = zs[k % 2]
            s_t, s_b = sqs[k % 2]
            r_t, r_b = rs[k % 2]
            ps_t, ps_b = pn[k % 2]
            k += 1
            P.dma("sp", o_t[:, 0:wd_], of[ch, :, t0:t0 + wd_], (), (o_b,))
            P.dma("sp", b_t[:, 0:wd_], ob[ch, :, t0:t0 + wd_], (), (b_b,))
            P.dma("sp", z_t[:, 0:wd_], z[ch, :, t0:t0 + wd_], (), (z_b,))
            P.tt("pool", o_t[:, 0:wd_], o_t[:, 0:wd_], b_t[:, 0:wd_], ALU.add, (o_b, b_b), (o_b,))
            P.act(s_t[:, 0:wd_], o_t[:, 0:wd_], AF.Square, (o_b,), (s_b,))
            P.mm(ps_t[:, 0:wd_], bd_t[:], s_t[:, 0:wd_], True, True, (bd_b, s_b), (ps_b,))
            P.act(r_t[:, 0:wd_], ps_t[:, 0:wd_], AF.Sqrt, (ps_b,), (r_b,), bias=EPS, scale=1.0 / 64)
            P.recip(r_t[:, 0:wd_], r_t[:, 0:wd_], (r_b,), (r_b,))
            P.act(z_t[:, 0:wd_], z_t[:, 0:wd_], AF.Silu, (z_b,), (z_b,))
            P.stt("dve", o_t[:, 0:wd_], o_t[:, 0:wd_], g_t[:, 0:1], r_t[:, 0:wd_], ALU.mult, ALU.mult,
                  (o_b, g_b, r_b), (o_b,))
            P.tt("dve", mix_t[:, 2 + ch, 0:wd_], o_t[:, 0:wd_], z_t[:, 0:wd_], ALU.mult, (o_b, z_b), (mix_b,))
        for dc in range(8):
            ps_t, ps_b = pp[dc % 4]
            for mc in range(8):
                P.mm(ps_t[:, 0:wd_], w_t[:, mc, dc * 128:(dc + 1) * 128], mix_t[:, mc, 0:wd_], mc == 0, mc == 7,
                     (w_b, mix_b), (ps_b,))
            P.stt("dve", x_t[:, dc, 0:wd_], ps_t[:, 0:wd_], m2_t[:, j, dc:dc + 1], x_t[:, dc, 0:wd_], ALU.mult, ALU.add,
                  (ps_b, m2_b, x_b), (x_b,))
        P.dma("sp", out[:, :, t0:t0 + wd_].rearrange("c p t -> p c t"), x_t[:, :, 0:wd_], (x_b,), ())
    return P.finish()


def build_E2(ntl, ntc, E, F, final):
    nt = ntl + ntc
    ntile = nt // 128
    FG = 256
    nfg = F // FG
    P = Prog()
    x1T = P.dram_in("x1T", [8, 128, nt])
    x1 = P.dram_in("x1", [nt, 1024])
    mod = P.dram_in("mod", [128, 2, 2, 8])
    m5 = P.dram_in("m5", [128, 2, 1024])
    wg = P.dram_in("wg", [E, 1024, F])
    wu = P.dram_in("wu", [E, 1024, F])
    wd = P.dram_in("wd", [E, F, 1024])
    out = P.dram_out("x2", [nt, 1024])
    if E > 1:
        wr = P.dram_in("wr", [128, 8, E])
    if final:
        gn = P.dram_in("gn", [128, 1024])
    mod_t, mod_b = P.sb([128, 2, 2, 8])
    P.dma("sp", mod_t[:], mod[:, :, :, :], (), (mod_b,))
    a_t, a_b = P.sb([128, 2, 8])
    P.ts("dve", a_t[:], mod_t[:, :, 1, :], 1.0, None, ALU.add, None, (mod_b,), (a_b,))
    ones_t, ones_b = P.sb([128, 128], BF16)
    P.memset("dve", ones_t[:], 1.0, (ones_b,))
    hx_t, hx_b = P.sb([128, 8, nt], BF16, "hx")
    acc_t, acc_b = P.sb([128, ntile, 1024], F32, "acc")
    P.memset("dve", acc_t[:], 0.0, (acc_b,))
    if E > 1:
        wr_t, wr_b = P.sb([128, 8, E])
        P.dma("sp", wr_t[:], wr[:, :, :], (), (wr_b,))
        lg_t, lg_b = P.sb([128, ntile, E])
        gate_t, gate_b = P.sb([128, ntile, E])
    xs = [P.sb([128, 8, TT]) for _ in range(2)]
    sqs = [P.sb([128, 8, TT], BF16) for _ in range(2)]
    hfs = [P.sb([128, 8, TT]) for _ in range(2)]
    rs = [P.sb([128, TT]) for _ in range(2)]
    pn = P.psum([128, 512])
    pr = P.psum([128, 512])
    for it, (t0, wd_, j) in enumerate(tok_blocks(ntl, ntc, TT)):
        x_t, x_b = xs[it % 2]
        sq_t, sq_b = sqs[it % 2]
        hf_t, hf_b = hfs[it % 2]
        r_t, r_b = rs[it % 2]
        P.dma("sp", x_t[:], x1T[:, :, t0:t0 + TT].rearrange("c p t -> p c t"), (), (x_b,))
        P.act(sq_t[:], x_t[:], AF.Square, (x_b,), (sq_b,))
        for kc in range(8):
            P.mm(pn[0][:, 0:TT], ones_t[:], sq_t[:, kc, :], kc == 0, kc == 7, (ones_b, sq_b), (pn[1],))
        P.act(r_t[:], pn[0][:, 0:TT], AF.Sqrt, (pn[1],), (r_b,), bias=EPS, scale=1.0 / 1024)
        P.recip(r_t[:], r_t[:], (r_b,), (r_b,))
        for kc in range(8):
            P.stt("dve", hf_t[:, kc, :], x_t[:, kc, :], a_t[:, j, kc:kc + 1], r_t[:], ALU.mult, ALU.mult,
                  (x_b, a_b, r_b), (hf_b,))
            P.act(hf_t[:, kc, :], hf_t[:, kc, :], AF.Identity, (hf_b, mod_b), (hf_b,), bias=mod_t[:, j, 0, kc:kc + 1])
        P.copy("pool", hx_t[:, :, t0:t0 + TT], hf_t[:], (hf_b,), (hx_b,))
        if E > 1:
            for sub in range(TT // 128):
                tile = t0 // 128 + sub
                for kc in range(8):
                    P.mm(pr[0][:, 0:E], hf_t[:, kc, sub * 128:(sub + 1) * 128], wr_t[:, kc, :], kc == 0, kc == 7,
                         (hf_b, wr_b), (pr[1],))
                P.copy("act", lg_t[:, tile, :], pr[0][:, 0:E], (pr[1],), (lg_b,))
    if E > 1:
        m1_t, m1_b = P.sb([128, 1])
        m2_t, m2_b = P.sb([128, 1])
        e1_t, e1_b = P.sb([128, E])
        e2_t, e2_b = P.sb([128, E])
        l2_t, l2_b = P.sb([128, E])
        g1_t, g1_b = P.sb([128, 1])
        g2_t, g2_b = P.sb([128, 1])
        for tile in range(ntile):
            l_ap = lg_t[:, tile, :]
            P.op("dve", lambda e, o=m1_t[:], i=l_ap: e.reduce_max(o, i, axis=mybir.AxisListType.X), (lg_b,), (m1_b,))
            P.ts("dve", e1_t[:], l_ap, m1_t[:, 0:1], None, ALU.is_equal, None, (lg_b, m1_b), (e1_b,))
            P.stt("dve", l2_t[:], e1_t[:], -1e9, l_ap, ALU.mult, ALU.add, (e1_b, lg_b), (l2_b,))
            P.op("dve", lambda e, o=m2_t[:], i=l2_t[:]: e.reduce_max(o, i, axis=mybir.AxisListType.X), (l2_b,), (m2_b,))
            P.ts("dve", e2_t[:], l2_t[:], m2_t[:, 0:1], None, ALU.is_equal, None, (l2_b, m2_b), (e2_b,))
            P.tt("dve", g1_t[:], m2_t[:], m1_t[:], ALU.subtract, (m2_b, m1_b), (g1_b,))
            P.act(g1_t[:], g1_t[:], AF.Exp, (g1_b,), (g1_b,))
            P.ts("dve", g1_t[:], g1_t[:], 1.0, None, ALU.add, None, (g1_b,), (g1_b,))
            P.recip(g1_t[:], g1_t[:], (g1_b,), (g1_b,))
            P.ts("dve", g2_t[:], g1_t[:], -1.0, 1.0, ALU.mult, ALU.add, (g1_b,), (g2_b,))
            P.ts("dve", e1_t[:], e1_t[:], g1_t[:, 0:1], None, ALU.mult, None, (e1_b, g1_b), (e1_b,))
            P.stt("dve", gate_t[:, tile, :], e2_t[:], g2_t[:, 0:1], e1_t[:], ALU.mult, ALU.add,
                  (e2_b, g2_b, e1_b), (gate_b,))
    wgs = [P.sb([128, 8, FG], BF16) for _ in range(2)]
    wus = [P.sb([128, 8, FG], BF16) for _ in range(2)]
    wds = [P.sb([128, 2, 1024], BF16) for _ in range(2)]
    pg = [P.psum([128, 512]) for _ in range(2)]
    pu = [P.psum([128, 512]) for _ in range(2)]
    pd = [P.psum([128, 512]) for _ in range(2)]
    sgs = [P.sb([128, 512], BF16) for _ in range(2)]
    Hs = [P.sb([128, 2, 512], BF16) for _ in range(2)]
    blocks = tok_blocks(ntl, ntc)
    jobs = [(e_, fg, bi) for e_ in range(E) for fg in range(nfg) for bi in range(len(blocks))]
    nd = [0]

    def load_w(it):
        if it >= E * nfg:
            return
        e_, fg = it // nfg, it % nfg
        f0 = fg * FG
        wg_t, wg_b = wgs[it % 2]
        wu_t, wu_b = wus[it % 2]
        wd_t, wd_b = wds[it % 2]
        P.dma("pool", wg_t[:], wg[e_, :, f0:f0 + FG].rearrange("(kc p) f -> p kc f", p=128), (), (wg_b,))
        P.dma("pool", wu_t[:], wu[e_, :, f0:f0 + FG].rearrange("(kc p) f -> p kc f", p=128), (), (wu_b,))
        P.dma("pool", wd_t[:], wd[e_, f0:f0 + FG, :].rearrange("(fc p) d -> p fc d", p=128), (), (wd_b,))

    def GU(k):
        e_, fg, bi = jobs[k]
        it = e_ * nfg + fg
        if bi == 0 and it == 0:
            load_w(0)
        if bi == 1:
            load_w(it + 1)
        wg_t, wg_b = wgs[it % 2]
        wu_t, wu_b = wus[it % 2]
        t0, wd_, j = blocks[bi]
        H_t, H_b = Hs[k % 2]
        for fc in range(2):
            g_t, g_b = pg[fc]
            u_t, u_b = pu[fc]
            for kc in range(8):
                P.mm(g_t[:, 0:wd_], wg_t[:, kc, fc * 128:(fc + 1) * 128], hx_t[:, kc, t0:t0 + wd_],
                     kc == 0, kc == 7, (wg_b, hx_b), (g_b,))
            for kc in range(8):
                P.mm(u_t[:, 0:wd_], wu_t[:, kc, fc * 128:(fc + 1) * 128], hx_t[:, kc, t0:t0 + wd_],
                     kc == 0, kc == 7, (wu_b, hx_b), (u_b,))
            sg_t, sg_b = sgs[fc]
            P.act(sg_t[:, 0:wd_], g_t[:, 0:wd_], AF.Silu, (g_b,), (sg_b,))
            P.tt("dve", H_t[:, fc, 0:wd_], u_t[:, 0:wd_], sg_t[:, 0:wd_], ALU.mult, (u_b, sg_b), (H_b,))

    def DOWN(k):
        e_, fg, bi = jobs[k]
        it = e_ * nfg + fg
        wd_t, wd_b = wds[it % 2]
        t0, wd_, j = blocks[bi]
        H_t, H_b = Hs[k % 2]
        for sub in range(wd_ // 128):
            tile = t0 // 128 + sub
            for dh in range(2):
                d_t, d_b = pd[nd[0] % 2]
                nd[0] += 1
                for fc in range(2):
                    P.mm(d_t[:], H_t[:, fc, sub * 128:(sub + 1) * 128], wd_t[:, fc, dh * 512:(dh + 1) * 512],
                         fc == 0, fc == 1, (H_b, wd_b), (d_b,))
                acc_ap = acc_t[:, tile, dh * 512:(dh + 1) * 512]
                if E > 1:
                    P.stt("dve", acc_ap, d_t[:], gate_t[:, tile, e_:e_ + 1], acc_ap, ALU.mult, ALU.add,
                          (d_b, gate_b, acc_b), (acc_b,))
                else:
                    P.tt("dve", acc_ap, d_t[:], acc_ap, ALU.add, (d_b, acc_b), (acc_b,))

    for k in range(len(jobs) + 1):
        if k < len(jobs):
            GU(k)
        if k >= 1:
            DOWN(k - 1)
    m5_t, m5_b = P.sb([128, 2, 1024])
    P.dma("sp", m5_t[:], m5[:, :, :], (), (m5_b,))
    if final:
        gn_t, gn_b = P.sb([128, 1024])
        P.dma("sp", gn_t[:], gn[:, :], (), (gn_b,))
        ss_t, ss_b = P.sb([128, 1])
        tq = [P.sb([128, 1024]) for _ in range(2)]
    x1s = [P.sb([128, 1024]) for _ in range(2)]
    for tile in range(ntile):
        j = 0 if tile * 128 < ntl else 1
        x_t, x_b = x1s[tile % 2]
        P.dma("sp", x_t[:], x1[tile * 128:(tile + 1) * 128, :], (), (x_b,))
        P.tt("pool", acc_t[:, tile, :], acc_t[:, tile, :], m5_t[:, j, :], ALU.mult, (acc_b, m5_b), (acc_b,))
        P.tt("dve", x_t[:], x_t[:], acc_t[:, tile, :], ALU.add, (x_b, acc_b), (x_b,))
        if final:
            q_t, q_b = tq[tile % 2]
            P.tt("pool", q_t[:], x_t[:], x_t[:], ALU.mult, (x_b,), (q_b,))
            P.op("dve", lambda e, o=ss_t[:], i=q_t[:]: e.reduce_sum(o, i, axis=mybir.AxisListType.X), (q_b,), (ss_b,))
            P.act(ss_t[:], ss_t[:], AF.Sqrt, (ss_b,), (ss_b,), bias=EPS, scale=1.0 / 1024)
            P.recip(ss_t[:], ss_t[:], (ss_b,), (ss_b,))
            P.stt("dve", x_t[:], x_t[:], ss_t[:, 0:1], gn_t[:], ALU.mult, ALU.mult, (x_b, ss_b, gn_b), (x_b,))
        P.dma("sp", out[tile * 128:(tile + 1) * 128, :], x_t[:], (x_b,), ())
    return P.finish()


def stage_C2(c1, need_ctx):
    dn, dnc, gb, gbc = c1["dn"], c1["dnc"], c1["gb"], c1["gbc"]
    units = []
    for h in range(6):
        sl = slice(h * 64, (h + 1) * 64)
        for d in range(2):
            parts = []
            for r in (0, 384, 768):
                a_c, a_x = dnc[r:r + 384][sl], dn[r:r + 384][sl]
                if d == 1:
                    a_c, a_x = a_c[:, ::-1], a_x[:, ::-1]
                parts.append(np.concatenate([a_c, a_x], axis=1))
            gs = []
            for w_ in range(2):
                g_c, g_x = gbc[w_, d * 6 + h], gb[w_, d * 6 + h]
                if d == 1:
                    g_c, g_x = g_c[::-1], g_x[::-1]
                gs.append(np.concatenate([g_c, g_x]))
            units.append((parts[0], parts[1], parts[2], gs[0], gs[1]))
    outs = run_C2(units)
    of = np.zeros((384, SEQ), np.float32)
    ob = np.zeros((384, SEQ), np.float32)
    ofc = np.zeros((384, CTX), np.float32)
    obc = np.zeros((384, CTX), np.float32)
    for h in range(6):
        sl = slice(h * 64, (h + 1) * 64)
        f, b = outs[2 * h], outs[2 * h + 1]
        of[sl] = f[:, CTX:]
        ofc[sl] = f[:, :CTX]
        ob[sl] = b[:, CTX:][:, ::-1]
        obc[sl] = b[:, :CTX][:, ::-1]
    return of, ob, ofc, obc


def stage_E1(xT, hcT, c1, of, ob, ofc, obc, px, pc, w_out_l, modT_l, dng_l, need_ctx):
    ntl = SEQ // NCORES
    ntc = CTX if need_ctx else 0
    nc = build_E1(ntl, ntc)
    m = modT_l.reshape(128, 2, 6, 8)
    mod2 = c_(m[:, :, 2, :])
    dng = c_(np.tile(dng_l.reshape(64), 2).reshape(128, 1))
    bd = c_(np.kron(np.eye(2), np.ones((64, 64))))
    zx, zc = px[R_DZ:R_DZ + 384], pc[R_DZ:R_DZ + 384]

    def cat(a, b, i):
        sl = a[:, i * ntl:(i + 1) * ntl]
        return np.concatenate([sl, b], axis=1) if need_ctx else sl
    ims = []
    for i in range(NCORES):
        nt = ntl + ntc
        ims.append({
            "xT": c_(cat(xT, hcT, i).reshape(8, 128, nt)),
            "ya": c_(cat(c1["yaT"], c1.get("yaTc"), i).reshape(2, 128, nt)),
            "yc": c_(cat(c1["ycT"], c1.get("ycTc"), i).reshape(3, 128, nt)),
            "of": c_(cat(of, ofc, i).reshape(3, 128, nt)),
            "ob": c_(cat(ob, obc, i).reshape(3, 128, nt)),
            "z": c_(cat(zx, zc, i).reshape(3, 128, nt)),
            "w": c_(w_out_l), "mod2": mod2, "dng": dng, "bd": bd,
        })
    res = run(nc, ims)
    x1T = np.concatenate([r["x1T"][:, :, :ntl].reshape(1024, ntl) for r in res], axis=1)
    hc1T = res[0]["x1T"][:, :, ntl:].reshape(1024, CTX) if need_ctx else None
    return x1T, hc1T


def stage_E2(x1T, hc1T, modT_l, wg, wu, wd, wr, gn, need_ctx):
    ntl = SEQ // NCORES
    ntc = CTX if need_ctx else 0
    E, _, F = wg.shape
    final = gn is not None
    nc = build_E2(ntl, ntc, E, F, final)
    m = modT_l.reshape(128, 2, 6, 8)
    mod = c_(m[:, :, 3:5, :])
    m5 = np.stack([np.broadcast_to(m[:, j, 5, :].T.reshape(1, 1024), (128, 1024)) for j in range(2)], axis=1)
    wg, wu, wd = c_(wg), c_(wu), c_(wd)
    ims = []
    for i in range(NCORES):
        sl = x1T[:, i * ntl:(i + 1) * ntl]
        blk = np.concatenate([sl, hc1T], axis=1) if need_ctx else sl
        nt = ntl + ntc
        im = {"x1T": c_(blk.reshape(8, 128, nt)), "x1": c_(blk.T), "mod": mod, "m5": c_(m5),
              "wg": wg, "wu": wu, "wd": wd}
        if E > 1:
            im["wr"] = c_(wr.reshape(8, 128, E).transpose(1, 0, 2))
        if final:
            im["gn"] = c_(np.broadcast_to(gn.reshape(1, 1024), (128, 1024)))
        ims.append(im)
    res = run(nc, ims)
    x2 = np.concatenate([r["x2"][:ntl] for r in res], axis=0)
    hc2 = res[0]["x2"][ntl:] if need_ctx else None
    return x2, hc2


def kernel(x, c, ctx, c_ctx, w_mod, b_mod, w_in, w_out, conv_w, dn_conv_w, dn_a_log, dn_dt_bias, dn_norm_g,
           attn_sink, ffn_w_gate, ffn_w_up, ffn_w_down, moe_router, moe_w_gate, moe_w_up, moe_w_down,
           final_norm_g):
    f = lambda a: np.asarray(a, dtype=np.float32)
    x, c, ctx, c_ctx, w_mod, b_mod, w_in, w_out = map(f, (x, c, ctx, c_ctx, w_mod, b_mod, w_in, w_out))
    modT = stage_A(c, c_ctx, w_mod, b_mod)
    xT = np.ascontiguousarray(x[0].T)
    hcT = np.ascontiguousarray(ctx[0].T)
    x2 = None
    for l in range(2):
        need_ctx = l == 0
        px, pc = stage_B(xT, hcT, w_in[l], modT[l])
        c1 = stage_C1(px, pc, f(conv_w)[l], f(attn_sink)[l], f(dn_conv_w)[l], f(dn_a_log)[l], f(dn_dt_bias)[l],
                      need_ctx)
        of, ob, ofc, obc = stage_C2(c1, need_ctx)
        x1T, hc1T = stage_E1(xT, hcT, c1, of, ob, ofc, obc, px, pc, w_out[l], modT[l], f(dn_norm_g)[l], need_ctx)
        if l == 0:
            x2, hc2 = stage_E2(x1T, hc1T, modT[l], f(ffn_w_gate), f(ffn_w_up), f(ffn_w_down), None, None, True)
            xT = np.ascontiguousarray(x2.T)
            hcT = np.ascontiguousarray(hc2.T)
        else:
            x2, _ = stage_E2(x1T, None, modT[l], f(moe_w_gate)[0], f(moe_w_up)[0], f(moe_w_down)[0],
                             f(moe_router)[0], f(final_norm_g), False)
    return x2.reshape(1, SEQ, D_MODEL).astype(np.float32)
```

```python
import numpy as np
from contextlib import ExitStack
import concourse.bass as bass
import concourse.mybir as mybir
from concourse.bass_utils import run_bass_kernel_spmd

F32 = mybir.dt.float32
BF16 = mybir.dt.bfloat16
AF = mybir.ActivationFunctionType
ALU = mybir.AluOpType
NCORES = 8

D_MODEL = 1024
SEQ = 16384
CTX = 256
EPS = 1e-6
IN_COLS = 2968


class Buf:
    __slots__ = ("name", "lw", "rd", "excl")

    def __init__(self, name, excl=False):
        self.name = name
        self.lw = None
        self.rd = {}
        self.excl = excl


class Prog:
    EPOCH = 4096
    NDMA = 24

    def __init__(self):
        self.nc = bass.Bass("TRN2", target_bir_lowering=False)
        self.es = ExitStack()
        self.q = {e: [] for e in ("sp", "pe", "dve", "act", "pool")}
        self.cnt = {e: 0 for e in self.q}
        self.seen = {e: {} for e in self.q}
        self.ndma = {"sp": 0, "pool": 0}
        self.dlast = {}
        self.nt = 0

    def dram_in(self, name, shape, dt=F32):
        return self.nc.dram_tensor(name, list(shape), dt, kind="ExternalInput").ap()

    def dram_out(self, name, shape, dt=F32):
        return self.nc.dram_tensor(name, list(shape), dt, kind="ExternalOutput").ap()

    def sb(self, shape, dt=F32, name=None):
        self.nt += 1
        name = name or f"t{self.nt}"
        t = self.es.enter_context(self.nc.sbuf_tensor(name, list(shape), dt))
        return t, Buf(name)

    def psum(self, shape=(128, 512), dt=F32, name=None):
        self.nt += 1
        name = name or f"p{self.nt}"
        t = self.es.enter_context(self.nc.psum_tensor(name, list(shape), dt))
        return t, Buf(name, excl=True)

    def _deps(self, reads, writes, eng=None):
        deps = []
        for b in reads:
            if b.lw is not None:
                deps.append(b.lw)
            if b.excl:
                deps.extend(v for k_, v in b.rd.items() if k_ != eng)
        for b in writes:
            if b.lw is not None:
                deps.append(b.lw)
            deps.extend(b.rd.values())
        return deps

    def _filter(self, eng, deps):
        waits = []
        seen = self.seen[eng]
        for d in deps:
            if d[0] == "c":
                _, p, n = d
                if p == eng and eng == "pe":
                    continue
                if seen.get(p, -1) >= n:
                    continue
                seen[p] = n
            else:
                _, slot, val = d
                if seen.get(("d", slot), 0) >= val:
                    continue
                seen[("d", slot)] = val
            waits.append(d)
        return waits

    def _mark(self, tok, key, reads, writes):
        for b in reads:
            b.rd[key] = tok
        for b in writes:
            b.lw = tok
            b.rd = {}

    def op(self, eng, fn, reads=(), writes=()):
        n = self.cnt[eng]
        self.cnt[eng] += 1
        waits = self._filter(eng, self._deps(reads, writes, eng))
        tok = ("c", eng, n)
        self._mark(tok, eng, reads, writes)
        self.q[eng].append((fn, waits, tok))

    def dma(self, eng, out_ap, in_ap, reads=(), writes=()):
        j = self.ndma[eng]
        self.ndma[eng] += 1
        slot = (j % self.NDMA) + (self.NDMA if eng == "pool" else 0)
        val = 16 * (j // self.NDMA + 1)
        deps = self._deps(reads, writes)
        if val > 16:
            deps.append(("d", slot, val - 16))
        waits = self._filter(eng, deps)
        tok = ("d", slot, val)
        self.dlast[slot] = val
        self._mark(tok, ("d", slot), reads, writes)
        self.q[eng].append((lambda e: e.dma_start(out=out_ap, in_=in_ap), waits, tok))

    def mm(self, out, lhsT, rhs, start, stop, reads, writes):
        self.op("pe", lambda e: e.matmul(out, lhsT, rhs, start=start, stop=stop), reads, writes)

    def transpose(self, out, in_, ident, reads, writes):
        self.op("pe", lambda e: e.transpose(out, in_, ident), reads, writes)

    def act(self, out, in_, func, reads, writes, bias=None, scale=None, eng="act"):
        kw = {}
        if bias is not None:
            kw["bias"] = bias
        if scale is not None:
            kw["scale"] = scale
        self.op(eng, lambda e: e.activation(out, in_, func, **kw), reads, writes)

    def tt(self, eng, out, in0, in1, op, reads, writes):
        self.op(eng, lambda e: e.tensor_tensor(out, in0, in1, op=op), reads, writes)

    def ts(self, eng, out, in0, s1, s2, op0, op1, reads, writes):
        if s2 is None:
            self.op(eng, lambda e: e.tensor_scalar(out, in0, s1, None, op0=op0), reads, writes)
        else:
            self.op(eng, lambda e: e.tensor_scalar(out, in0, s1, s2, op0=op0, op1=op1), reads, writes)

    def stt(self, eng, out, in0, scalar, in1, op0, op1, reads, writes):
        self.op(eng, lambda e: e.scalar_tensor_tensor(out, in0, scalar, in1, op0=op0, op1=op1), reads, writes)

    def copy(self, eng, out, in_, reads, writes):
        if eng == "act":
            self.op(eng, lambda e: e.activation(out, in_, AF.Identity), reads, writes)
        else:
            self.op(eng, lambda e: e.tensor_copy(out, in_), reads, writes)

    def recip(self, out, in_, reads, writes):
        self.op("dve", lambda e: e.reciprocal(out, in_), reads, writes)

    def memset(self, eng, ap, val, writes):
        self.op(eng, lambda e: e.memset(ap, val), (), writes)

    def finish(self):
        nc = self.nc
        E = self.EPOCH
        sems = {}
        for e in ("pe", "dve", "act", "pool"):
            nep = max(1, (self.cnt[e] + E - 1) // E)
            sems[e] = [self.es.enter_context(nc.semaphore(f"s_{e}{i}")) for i in range(nep)]
        dsems = [self.es.enter_context(nc.semaphore(f"s_d{i}")) for i in range(2 * self.NDMA)]
        fin = [("d", s, v) for s, v in sorted(self.dlast.items())]
        fin = self._filter("sp", fin)
        self.q["sp"].append((None, fin, None))

        def emit(engobj, ename):
            for fn, waits, tok in self.q[ename]:
                for w in waits:
                    if w[0] == "c":
                        engobj.wait_ge(sems[w[1]][w[2] // E], w[2] % E + 1)
                    else:
                        engobj.wait_ge(dsems[w[1]], w[2])
                if fn is None:
                    continue
                ins = fn(engobj)
                if tok[0] == "c":
                    ins.then_inc(sems[ename][tok[2] // E], 1)
                else:
                    ins.then_inc(dsems[tok[1]], 16)

        with nc.Block() as block:
            @block.sync
            def _(e):
                emit(e, "sp")

            @block.tensor
            def _(e):
                emit(e, "pe")

            @block.vector
            def _(e):
                emit(e, "dve")

            @block.scalar
            def _(e):
                emit(e, "act")

            @block.gpsimd
            def _(e):
                emit(e, "pool")
        self.es.close()
        return nc


def run(prog_nc, in_maps):
    res = run_bass_kernel_spmd(prog_nc, in_maps, core_ids=list(range(NCORES)))
    return res.results


def c_(a):
    return np.ascontiguousarray(a, dtype=np.float32)


def build_A():
    P = Prog()
    s_in = P.dram_in("s_in", [128, 8, 2])
    wm = P.dram_in("wm", [2, 1024, 6144])
    bm = P.dram_in("bm", [2, 128, 48])
    out = P.dram_out("modT", [2, 128, 2, 48])
    s_t, s_b = P.sb([128, 8, 2])
    sg_t, sg_b = P.sb([128, 8, 2])
    sb_t, sb_b = P.sb([128, 8, 2], BF16)
    bm_t, bm_b = P.sb([128, 2, 48])
    o_t, o_b = P.sb([128, 2, 2, 48])
    P.dma("sp", s_t[:], s_in[:, :, :], (), (s_b,))
    P.dma("sp", bm_t[:], bm.rearrange("l p c -> p l c"), (), (bm_b,))
    P.act(sg_t[:], s_t[:], AF.Sigmoid, (s_b,), (sg_b,))
    P.tt("dve", sb_t[:], s_t[:], sg_t[:], ALU.mult, (s_b, sg_b), (sb_b,))
    w_t, w_b = P.sb([128, 8, 6144], BF16, "wmod_sb")
    pss = [P.psum([128, 512]) for _ in range(2)]
    for l in range(2):
        ps_t, ps_b = pss[l]
        for kc in range(8):
            P.dma("pool", w_t[:, kc, :], wm[l, kc * 128:(kc + 1) * 128, :], (), (w_b,))
        for dc in range(48):
            for kc in range(8):
                P.mm(ps_t[:, dc * 2:dc * 2 + 2], w_t[:, kc, dc * 128:(dc + 1) * 128], sb_t[:, kc, :],
                     kc == 0, kc == 7, (w_b, sb_b), (ps_b,))
        for j in range(2):
            P.tt("dve", o_t[:, l, j, :], ps_t[:, j:96:2], bm_t[:, l, :], ALU.add, (ps_b, bm_b), (o_b,))
    P.dma("sp", out.rearrange("l p j c -> p l j c"), o_t[:], (o_b,), ())
    return P.finish()


def stage_A(c, c_ctx, w_mod, b_mod):
    s = np.stack([c.reshape(1024), c_ctx.reshape(1024)], axis=-1)
    s_in = c_(s.reshape(8, 128, 2).transpose(1, 0, 2))
    bm = c_(b_mod.reshape(2, 48, 128).transpose(0, 2, 1))
    nc = build_A()
    im = {"s_in": s_in, "wm": c_(w_mod), "bm": bm}
    res = run(nc, [im] * NCORES)
    return res[0]["modT"]


NCH_IN = 24
TT = 256


def build_B(ntl, ntc):
    nt = ntl + ntc
    P = Prog()
    xT = P.dram_in("xT", [8, 128, nt])
    w = P.dram_in("w", [1024, NCH_IN * 128])
    mod = P.dram_in("mod", [128, 2, 2, 8])
    out = P.dram_out("pxT", [NCH_IN, 128, nt])
    w_t, w_b = P.sb([128, 8, NCH_IN * 128], BF16, "w_sb")
    for kc in range(8):
        P.dma("pool", w_t[:, kc, :], w[kc * 128:(kc + 1) * 128, :], (), (w_b,))
    mod_t, mod_b = P.sb([128, 2, 2, 8])
    P.dma("sp", mod_t[:], mod[:, :, :, :], (), (mod_b,))
    a_t, a_b = P.sb([128, 2, 8])
    P.ts("dve", a_t[:], mod_t[:, :, 1, :], 1.0, None, ALU.add, None, (mod_b,), (a_b,))
    ones_t, ones_b = P.sb([128, 128], BF16)
    P.memset("dve", ones_t[:], 1.0, (ones_b,))
    xs = [P.sb([128, 8, TT]) for _ in range(2)]
    sqs = [P.sb([128, 8, TT], BF16) for _ in range(2)]
    hs = [P.sb([128, 8, TT], BF16) for _ in range(2)]
    rs = [P.sb([128, TT]) for _ in range(2)]
    tmp = [P.sb([128, TT]) for _ in range(2)]
    os_ = [P.sb([128, TT]) for _ in range(4)]
    pn = P.psum([128, 512])
    pp = [P.psum([128, 512]) for _ in range(4)]
    for it in range(nt // TT):
        j = 0 if it * TT < ntl else 1
        x_t, x_b = xs[it % 2]
        sq_t, sq_b = sqs[it % 2]
        h_t, h_b = hs[it % 2]
        r_t, r_b = rs[it % 2]
        P.dma("sp", x_t[:], xT[:, :, it * TT:(it + 1) * TT].rearrange("c p t -> p c t"), (), (x_b,))
        P.act(sq_t[:], x_t[:], AF.Square, (x_b,), (sq_b,))
        for kc in range(8):
            P.mm(pn[0][:, 0:TT], ones_t[:], sq_t[:, kc, :], kc == 0, kc == 7, (ones_b, sq_b), (pn[1],))
        P.act(r_t[:], pn[0][:, 0:TT], AF.Sqrt, (pn[1],), (r_b,), bias=EPS, scale=1.0 / 1024)
        P.recip(r_t[:], r_t[:], (r_b,), (r_b,))
        for kc in range(8):
            t_t, t_b = tmp[kc % 2]
            P.stt("dve", t_t[:], x_t[:, kc, :], a_t[:, j, kc:kc + 1], r_t[:], ALU.mult, ALU.mult,
                  (x_b, a_b, r_b), (t_b,))
            P.act(h_t[:, kc, :], t_t[:], AF.Identity, (t_b, mod_b), (h_b,), bias=mod_t[:, j, 0, kc:kc + 1])
        for oc in range(NCH_IN):
            ps_t, ps_b = pp[oc % 4]
            for kc in range(8):
                P.mm(ps_t[:, 0:TT], w_t[:, kc, oc * 128:(oc + 1) * 128], h_t[:, kc, :], kc == 0, kc == 7,
                     (w_b, h_b), (ps_b,))
            o_t, o_b = os_[oc % 4]
            P.copy("act" if oc % 2 else "dve", o_t[:], ps_t[:, 0:TT], (ps_b,), (o_b,))
            P.dma("sp", out[oc, :, it * TT:(it + 1) * TT], o_t[:], (o_b,), ())
    return P.finish()


def perm_w_in(w_in_l):
    z = np.zeros((1024, 104), np.float32)
    return c_(np.concatenate([w_in_l[:, 0:2304], w_in_l[:, 2328:2968], w_in_l[:, 2304:2328], z], axis=1))


def stage_B(xT_all, hcT, w_in_l, modT_l):
    ntl = SEQ // NCORES
    nc = build_B(ntl, CTX)
    w = perm_w_in(w_in_l)
    m = modT_l.reshape(128, 2, 6, 8)
    mod = c_(m[:, :, 0:2, :])
    ims = []
    for i in range(NCORES):
        xt = np.concatenate([xT_all[:, i * ntl:(i + 1) * ntl], hcT], axis=1)
        ims.append({"xT": c_(xt.reshape(8, 128, ntl + CTX)), "w": w, "mod": mod})
    res = run(nc, ims)
    px = np.concatenate([r["pxT"][:, :, :ntl].reshape(NCH_IN * 128, ntl) for r in res], axis=1)
    pc = res[0]["pxT"][:, :, ntl:].reshape(NCH_IN * 128, CTX)
    return px, pc


GRID_W = 64
NEGV = -30000.0


def rope_tables(pos):
    pos = np.asarray(pos)
    inv = 10000.0 ** (-(np.arange(0, 32, 2, dtype=np.float64)) / 32.0)
    r = (pos // GRID_W).astype(np.float64)
    col = (pos % GRID_W).astype(np.float64)
    cos = np.zeros((64, len(pos)))
    sin = np.zeros((64, len(pos)))
    for d in range(64):
        axis, half, f = d // 32, (d % 32) // 16, d % 16
        p = r if axis == 0 else col
        cos[d] = np.cos(p * inv[f])
        sin[d] = np.sin(p * inv[f]) * (-1.0 if half == 0 else 1.0)
    return cos, sin


ROPE_PERM = np.array([(d // 32) * 32 + ((d % 32) + 16) % 32 for d in range(64)])


def build_C1(ntl, need_ctx):
    nqb = ntl // 128
    nkb = nqb + 2
    nk = nkb * 128
    P = Prog()
    cv = P.dram_in("cv", [3, 2, 128, ntl + 2])
    cw = P.dram_in("cw", [128, 2, 3])
    q_in = P.dram_in("q", [2, 6, 64, ntl])
    k_in = P.dram_in("k", [2, 2, 64, nk])
    v_in = P.dram_in("v", [128, nkb, 128])
    cs_k = P.dram_in("cs_k", [2, 64, nk])
    kc_in = P.dram_in("kc", [2, 64, CTX])
    vc_in = P.dram_in("vc", [128, 2, 128])
    neg_in = P.dram_in("neg", [128, 4, 384])
    sink_in = P.dram_in("sink", [1, 2, 384])
    ident_in = P.dram_in("ident", [128, 128])
    ya = P.dram_out("yaT", [2, 128, ntl])
    yc = P.dram_out("ycT", [6, 64, ntl])
    if need_ctx:
        cvc = P.dram_in("cvc", [3, 2, 128, CTX + 2])
        qc_in = P.dram_in("qc", [6, 64, CTX])
        ya_c = P.dram_out("yaTc", [2, 128, CTX])
        yc_c = P.dram_out("ycTc", [6, 64, CTX])
    dn_in = P.dram_in("dn", [9, 128, ntl + 2])
    dnc_in = P.dram_in("dnc", [9, 128, CTX + 2])
    dcw_in = P.dram_in("dcw", [128, 9, 3])
    gab_in = P.dram_in("gab", [2, 12, ntl])
    gabc_in = P.dram_in("gabc", [2, 12, CTX])
    gpar_in = P.dram_in("gpar", [12, 2])
    bd_in = P.dram_in("bd", [128, 128])
    dn_out = P.dram_out("dno", [9, 128, ntl])
    dnc_out = P.dram_out("dnco", [9, 128, CTX])
    gb_out = P.dram_out("gbo", [2, 12, ntl])
    gbc_out = P.dram_out("gbco", [2, 12, CTX])

    cw_t, cw_b = P.sb([128, 2, 3])
    P.dma("sp", cw_t[:], cw[:, :, :], (), (cw_b,))
    ident_t, ident_b = P.sb([128, 128], BF16)
    P.dma("pool", ident_t[:], ident_in[:, :], (), (ident_b,))
    neg_t, neg_b = P.sb([128, 4, 384], BF16)
    P.dma("pool", neg_t[:], neg_in[:, :, :], (), (neg_b,))
    ones_t, ones_b = P.sb([128, 64], BF16)
    P.memset("dve", ones_t[:], 1.0, (ones_b,))
    onesf_t, onesf_b = P.sb([1, 64])
    P.memset("dve", onesf_t[:], 1.0, (onesf_b,))
    sk_t, sk_b = P.sb([1, 2, 384])
    P.dma("sp", sk_t[:], sink_in[:, :, :], (), (sk_b,))
    esk_t, esk_b = P.sb([1, 2, 384])
    P.act(esk_t[:], sk_t[:], AF.Exp, (sk_b,), (esk_b,))

    cb_t, b_b = P.sb([128, ntl + 2])
    cc_t, c_b = P.sb([128, ntl + 2])
    chh_t, h_b = P.sb([128, ntl + 2])

    def conv(src, n, dst):
        for ch in range(2):
            b_t, c_t, h_t = cb_t[:, 0:n + 2], cc_t[:, 0:n + 2], chh_t[:, 0:n + 2]
            P.dma("sp", b_t, src[0, ch, :, :], (), (b_b,))
            P.dma("sp", c_t, src[1, ch, :, :], (), (c_b,))
            P.dma("sp", h_t, src[2, ch, :, :], (), (h_b,))
            P.tt("dve", c_t, c_t, h_t, ALU.mult, (c_b, h_b), (c_b,))
            P.ts("dve", h_t[:, 0:n], c_t[:, 1:n + 1], cw_t[:, ch, 1:2], None, ALU.mult, None, (c_b, cw_b), (h_b,))
            P.stt("dve", h_t[:, 0:n], c_t[:, 0:n], cw_t[:, ch, 0:1], h_t[:, 0:n], ALU.mult, ALU.add,
                  (c_b, cw_b, h_b), (h_b,))
            P.stt("dve", h_t[:, 0:n], c_t[:, 2:n + 2], cw_t[:, ch, 2:3], h_t[:, 0:n], ALU.mult, ALU.add,
                  (c_b, cw_b, h_b), (h_b,))
            P.tt("dve", h_t[:, 0:n], h_t[:, 0:n], b_t[:, 1:n + 1], ALU.mult, (h_b, b_b), (h_b,))
            P.dma("sp", dst[ch, :, :], h_t[:, 0:n], (h_b,), ())
    conv(cv, ntl, ya)
    if need_ctx:
        conv(cvc, CTX, ya_c)

    dcw_t, dcw_b = P.sb([128, 9, 3])
    P.dma("sp", dcw_t[:], dcw_in[:, :, :], (), (dcw_b,))
    bd_t, bd_b = P.sb([128, 128])
    P.dma("sp", bd_t[:], bd_in[:, :], (), (bd_b,))
    rr_t, rr_b = P.sb([128, 512])
    pps = P.psum([128, 512])

    def dnprep(src, n, dst):
        for ch in range(9):
            x_t, y_t, s_t = cb_t[:, 0:n + 2], cc_t[:, 0:n], chh_t[:, 0:n]
            x_b, y_b, s_b = b_b, c_b, h_b
            P.dma("sp", x_t, src[ch, :, :], (), (x_b,))
            P.ts("dve", y_t, x_t[:, 1:n + 1], dcw_t[:, ch, 1:2], None, ALU.mult, None, (x_b, dcw_b), (y_b,))
            P.stt("dve", y_t, x_t[:, 0:n], dcw_t[:, ch, 0:1], y_t, ALU.mult, ALU.add, (x_b, dcw_b, y_b), (y_b,))
            P.stt("dve", y_t, x_t[:, 2:n + 2], dcw_t[:, ch, 2:3], y_t, ALU.mult, ALU.add, (x_b, dcw_b, y_b), (y_b,))
            P.act(y_t, y_t, AF.Silu, (y_b,), (y_b,))
            if ch < 6:
                P.act(s_t, y_t, AF.Square, (y_b,), (s_b,))
                for c0 in range(0, n, 512):
                    w_ = min(512, n - c0)
                    P.mm(pps[0][:, 0:w_], bd_t[:], s_t[:, c0:c0 + w_], True, True, (bd_b, s_b), (pps[1],))
                    P.act(rr_t[:, 0:w_], pps[0][:, 0:w_], AF.Sqrt, (pps[1],), (rr_b,), bias=1e-6)
                    P.recip(rr_t[:, 0:w_], rr_t[:, 0:w_], (rr_b,), (rr_b,))
                    P.stt("dve", y_t[:, c0:c0 + w_], y_t[:, c0:c0 + w_], 0.125 if ch < 3 else 1.0, rr_t[:, 0:w_],
                          ALU.mult, ALU.mult, (y_b, rr_b), (y_b,))
            P.dma("sp", dst[ch, :, :], y_t, (y_b,), ())
    dnprep(dn_in, ntl, dn_out)
    dnprep(dnc_in, CTX, dnc_out)
    gpar_t, gpar_b = P.sb([12, 2])
    P.dma("sp", gpar_t[:], gpar_in[:, :], (), (gpar_b,))
    nea_t, nea_b = P.sb([12, 1])
    P.act(nea_t[:], gpar_t[:, 0:1], AF.Exp, (gpar_b,), (nea_b,))
    P.ts("dve", nea_t[:], nea_t[:], -1.0, None, ALU.mult, None, (nea_b,), (nea_b,))

    def gates(src, n, dst):
        a_t, bt_t = cb_t[0:12, 0:n], cc_t[0:12, 0:n]
        P.dma("sp", a_t, src[0, :, :], (), (b_b,))
        P.dma("sp", bt_t, src[1, :, :], (), (c_b,))
        P.act(a_t, a_t, AF.Exp, (b_b, gpar_b), (b_b,), bias=gpar_t[:, 1:2])
        P.act(a_t, a_t, AF.Ln, (b_b,), (b_b,), bias=1.0)
        P.ts("dve", a_t, a_t, nea_t[:, 0:1], None, ALU.mult, None, (b_b, nea_b), (b_b,))
        P.act(bt_t, bt_t, AF.Sigmoid, (c_b,), (c_b,))
        P.dma("sp", dst[0, :, :], a_t, (b_b,), ())
        P.dma("sp", dst[1, :, :], bt_t, (c_b,), ())
    gates(gab_in, ntl, gb_out)
    gates(gabc_in, CTX, gbc_out)

    csk_t, csk_b = P.sb([64, 2, nk])
    P.dma("sp", csk_t[:], cs_k.rearrange("c d t -> d c t"), (), (csk_b,))
    qr_t, qr_b = P.sb([64, nqb, 2, 3, 128], BF16, "qr")
    kr_t, kr_b = P.sb([64, 2, nk], BF16, "kr")
    qa = [P.sb([64, nk]) for _ in range(2)]
    qp = [P.sb([64, nk]) for _ in range(2)]
    for h in range(6):
        a_t, a_b = qa[h % 2]
        p_t, p_b = qp[h % 2]
        P.dma("sp", a_t[:, 0:ntl], q_in[0, h, :, :], (), (a_b,))
        P.dma("sp", p_t[:, 0:ntl], q_in[1, h, :, :], (), (p_b,))
        P.tt("dve", a_t[:, 0:ntl], a_t[:, 0:ntl], csk_t[:, 0, 128:128 + ntl], ALU.mult, (a_b, csk_b), (a_b,))
        P.tt("pool", p_t[:, 0:ntl], p_t[:, 0:ntl], csk_t[:, 1, 128:128 + ntl], ALU.mult, (p_b, csk_b), (p_b,))
        P.tt("dve", qr_t[:, :, h // 3, h % 3, :], a_t[:, 0:ntl].rearrange("p (b q) -> p b q", q=128),
             p_t[:, 0:ntl].rearrange("p (b q) -> p b q", q=128), ALU.add, (a_b, p_b), (qr_b,))
    for g in range(2):
        a_t, a_b = qa[g]
        p_t, p_b = qp[g]
        P.dma("sp", a_t[:], k_in[0, g, :, :], (), (a_b,))
        P.dma("sp", p_t[:], k_in[1, g, :, :], (), (p_b,))
        P.tt("dve", a_t[:], a_t[:], csk_t[:, 0, :], ALU.mult, (a_b, csk_b), (a_b,))
        P.tt("pool", p_t[:], p_t[:], csk_t[:, 1, :], ALU.mult, (p_b, csk_b), (p_b,))
        P.tt("dve", kr_t[:, g, :], a_t[:], p_t[:], ALU.add, (a_b, p_b), (kr_b,))
    kcb_t, kcb_b = P.sb([64, 2, CTX], BF16)
    P.dma("pool", kcb_t[:], kc_in.rearrange("g d t -> d g t"), (), (kcb_b,))
    v_t, v_b = P.sb([128, nkb, 128], BF16, "v_sb")
    P.dma("pool", v_t[:], v_in[:, :, :], (), (v_b,))
    vc_t, vc_b = P.sb([128, 2, 128], BF16)
    P.dma("pool", vc_t[:], vc_in[:, :, :], (), (vc_b,))
    if need_ctx:
        qcb_t, qcb_b = P.sb([64, 2, 2, 3, 128], BF16)
        for h in range(6):
            P.dma("pool", qcb_t[:, :, h // 3, h % 3, :], qc_in[h, :, :].rearrange("d (b q) -> d b q", q=128),
                  (), (qcb_b,))

    ps_s = [P.psum([128, 512]) for _ in range(5)]
    ps_o = P.psum([128, 512])
    ps_d = P.psum([128, 512])
    pts = [P.sb([128, 384], BF16) for _ in range(5)]
    rd = [P.sb([64, 384]) for _ in range(2)]
    ob = [P.sb([64, 384]) for _ in range(2)]
    cnt = [0, 0]

    def attend(qsrc, qsrc_b, qb, g, keyspecs, dst, t0):
        pts_used = []
        for (k_ap, k_b, v_ap, v_bf, negidx) in keyspecs:
            i = cnt[0] % 5
            cnt[0] += 1
            s_t, s_b = ps_s[i]
            rhs = qsrc[:, qb, g, :, :].rearrange("d h q -> d (h q)")
            P.mm(s_t[:, 0:384], k_ap, rhs, True, negidx is None, (k_b, qsrc_b), (s_b,))
            if negidx is not None:
                P.mm(s_t[:, 0:384], ident_t[:], neg_t[:, negidx, :], False, True, (ident_b, neg_b), (s_b,))
            p_t, p_b = pts[i]
            P.act(p_t[:], s_t[:, 0:384], AF.Exp, (s_b,), (p_b,), scale=0.125)
            pts_used.append((p_t, p_b, v_ap, v_bf))
        n = len(pts_used)
        for idx, (p_t, p_b, v_ap, v_bf) in enumerate(pts_used):
            P.mm(ps_o[0][0:64, 0:384], v_ap, p_t[:], idx == 0, idx == n - 1, (v_bf, p_b), (ps_o[1],))
        for idx, (p_t, p_b, v_ap, v_bf) in enumerate(pts_used):
            P.mm(ps_d[0][0:64, 0:384], ones_t[:], p_t[:], idx == 0, False, (ones_b, p_b), (ps_d[1],))
        P.mm(ps_d[0][0:64, 0:384], onesf_t[:], esk_t[:, g, :], False, True, (onesf_b, esk_b), (ps_d[1],))
        j = cnt[1] % 2
        cnt[1] += 1
        r_t, r_b = rd[j]
        o_t, o_b = ob[j]
        P.recip(r_t[:], ps_d[0][0:64, 0:384], (ps_d[1],), (r_b,))
        P.tt("dve", o_t[:], ps_o[0][0:64, 0:384], r_t[:], ALU.mult, (ps_o[1], r_b), (o_b,))
        P.dma("sp", dst[3 * g:3 * g + 3, :, t0:t0 + 128].rearrange("h d q -> d h q"),
              o_t[:].rearrange("d (h q) -> d h q", h=3), (o_b,), ())

    for qb in range(nqb):
        for g in range(2):
            specs = []
            for off in range(3):
                kb = qb + off
                negidx = None
                if off == 0:
                    negidx = 0 if qb == 0 else 1
                elif off == 2:
                    negidx = 3 if qb == nqb - 1 else 2
                specs.append((kr_t[:, g, kb * 128:(kb + 1) * 128], kr_b, v_t[:, kb, g * 64:(g + 1) * 64], v_b, negidx))
            for cb in range(2):
                specs.append((kcb_t[:, g, cb * 128:(cb + 1) * 128], kcb_b, vc_t[:, cb, g * 64:(g + 1) * 64], vc_b, None))
            attend(qr_t, qr_b, qb, g, specs, yc, qb * 128)
    if need_ctx:
        for qb in range(2):
            for g in range(2):
                specs = [(kcb_t[:, g, cb * 128:(cb + 1) * 128], kcb_b, vc_t[:, cb, g * 64:(g + 1) * 64], vc_b, None)
                         for cb in range(2)]
                attend(qcb_t, qcb_b, qb, g, specs, yc_c, qb * 128)
    return P.finish()


R_CB, R_CC, R_CH = 0, 256, 512
R_DQ, R_DK, R_DV, R_DZ = 768, 1152, 1536, 1920
R_AQ, R_AK, R_AV = 2304, 2688, 2816
R_A, R_BT = 2944, 2956


def tri_masks():
    kk = np.arange(128)[:, None]
    qq = np.arange(128)[None, :]
    prev = np.where(qq <= kk, 0.0, NEGV)
    nxt = np.where(kk <= qq, 0.0, NEGV)
    return prev, nxt


def stage_C1(px, pc, conv_w_l, sink_l, dn_conv_w_l, dn_a_log_l, dn_dt_bias_l, need_ctx):
    ntl = SEQ // NCORES
    nc = build_C1(ntl, need_ctx)
    nkb = ntl // 128 + 2
    cw = c_(conv_w_l.T.reshape(2, 128, 3).transpose(1, 0, 2))
    prev, nxt = tri_masks()
    full = np.full((128, 128), NEGV)
    cos, sin = rope_tables(np.arange(SEQ))
    cosp = np.pad(cos, ((0, 0), (128, 128)))
    sinp = np.pad(sin, ((0, 0), (128, 128)))
    pad1 = lambda a: np.pad(a, ((0, 0), (1, 1)))
    convsrc = [pad1(px[R_CB:R_CB + 256]), pad1(px[R_CC:R_CC + 256]), pad1(px[R_CH:R_CH + 256])]
    kx = np.pad(px[R_AK:R_AK + 128], ((0, 0), (128, 128))).reshape(2, 64, SEQ + 256)
    kxp = kx[:, ROPE_PERM, :]
    vx = np.pad(px[R_AV:R_AV + 128], ((0, 0), (128, 128)))
    qx = px[R_AQ:R_AQ + 384].reshape(6, 64, SEQ)
    qxp = qx[:, ROPE_PERM, :]
    kc = c_(pc[R_AK:R_AK + 128].reshape(2, 64, CTX))
    vc = c_(pc[R_AV:R_AV + 128].T.reshape(2, 128, 128).transpose(1, 0, 2))
    sink = c_(np.repeat(sink_l.reshape(2, 3), 128, axis=1).reshape(1, 2, 384))
    ident = np.eye(128, dtype=np.float32)
    dnsrc = pad1(px[R_DQ:R_DQ + 1152])
    dnc = c_(pad1(pc[R_DQ:R_DQ + 1152]).reshape(9, 128, CTX + 2))
    dcw = c_(dn_conv_w_l.T.reshape(9, 128, 3).transpose(1, 0, 2))
    gpar = c_(np.stack([dn_a_log_l.reshape(12), dn_dt_bias_l.reshape(12)], axis=1))
    bd = c_(np.kron(np.eye(2), np.ones((64, 64))))
    gabc = c_(np.stack([pc[R_A:R_A + 12], pc[R_BT:R_BT + 12]]))
    ims = []
    for i in range(NCORES):
        t0, t1 = i * ntl, (i + 1) * ntl
        neg = np.stack([np.tile(full if i == 0 else prev, (1, 3)), np.tile(prev, (1, 3)),
                        np.tile(nxt, (1, 3)), np.tile(full if i == NCORES - 1 else nxt, (1, 3))], axis=1)
        im = {
            "cv": c_(np.stack([a[:, t0:t1 + 2].reshape(2, 128, ntl + 2) for a in convsrc])),
            "cw": cw,
            "q": c_(np.stack([qx[:, :, t0:t1], qxp[:, :, t0:t1]])),
            "k": c_(np.stack([kx[:, :, t0:t1 + 256], kxp[:, :, t0:t1 + 256]])),
            "v": c_(vx[:, t0:t1 + 256].T.reshape(nkb, 128, 128).transpose(1, 0, 2)),
            "cs_k": c_(np.stack([cosp[:, t0:t1 + 256], sinp[:, t0:t1 + 256]])),
            "kc": kc, "vc": vc, "neg": c_(neg), "sink": sink, "ident": ident,
            "dn": c_(dnsrc[:, t0:t1 + 2].reshape(9, 128, ntl + 2)), "dnc": dnc, "dcw": dcw,
            "gab": c_(np.stack([px[R_A:R_A + 12, t0:t1], px[R_BT:R_BT + 12, t0:t1]])), "gabc": gabc,
            "gpar": gpar, "bd": bd,
        }
        if need_ctx:
            im["cvc"] = c_(np.stack([pad1(pc[r:r + 256]).reshape(2, 128, CTX + 2) for r in (R_CB, R_CC, R_CH)]))
            im["qc"] = c_(pc[R_AQ:R_AQ + 384].reshape(6, 64, CTX))
        ims.append(im)
    res = run(nc, ims)
    yaT = np.concatenate([r["yaT"].reshape(256, ntl) for r in res], axis=1)
    ycT = np.concatenate([r["ycT"].reshape(384, ntl) for r in res], axis=1)
    out = {"yaT": yaT, "ycT": ycT}
    out["dn"] = np.concatenate([r["dno"].reshape(1152, ntl) for r in res], axis=1)
    out["dnc"] = res[0]["dnco"].reshape(1152, CTX)
    out["gb"] = np.concatenate([r["gbo"] for r in res], axis=2)
    out["gbc"] = res[0]["gbco"]
    if need_ctx:
        out["yaTc"] = res[0]["yaTc"].reshape(256, CTX)
        out["ycTc"] = res[0]["ycTc"].reshape(384, CTX)
    return out


def c2_consts():
    i = np.arange(128)
    same = (i[:, None] // 64) == (i[None, :] // 64)
    tri = (same & (i[:, None] <= i[None, :])).astype(np.float32)
    bd = same.astype(np.float32)
    negl = np.where(same & (i[:, None] >= i[None, :]), 0.0, NEGV).astype(np.float32)
    slm = (same & (i[:, None] > i[None, :])).astype(np.float32)
    ident = np.eye(128, dtype=np.float32)
    ones = np.ones((128, 128), np.float32)
    sel = np.zeros((128, 2, 64), np.float32)
    sel[0:64, 0, :] = 1.0
    sel[64:128, 1, :] = 1.0
    return {"tri": tri, "bd": bd, "negl": negl, "slm": slm, "ident": ident, "ones": ones,
            "sel": sel.reshape(128, 128)}


C2_STOP = 9


def build_C2(NP):
    T = NP * 128
    G = 5 if NP % 5 == 0 else (4 if NP % 4 == 0 else (2 if NP % 2 == 0 else 1))
    P = Prog()
    qk_in = P.dram_in("qk", [2, 2, 64, T])
    tok_in = P.dram_in("tok", [2, 128, NP, 192])
    gb_in = P.dram_in("gbt", [2, 128, 2, NP])
    o_out = P.dram_out("oT", [2, 64, T])
    C = {}
    for nm in ("tri", "bd", "negl", "slm", "ident", "ones", "sel"):
        d_in = P.dram_in(nm, [128, 128])
        t, b = P.sb([128, 128], F32, "c_" + nm)
        P.dma("sp", t[:], d_in[:, :], (), (b,))
        C[nm] = (t, b)
    tri_t, tri_b = C["tri"]
    ident_t, ident_b = C["ident"]
    ones_t, ones_b = C["ones"]
    negl_t, negl_b = C["negl"]
    slm_t, slm_b = C["slm"]
    bd_t, bd_b = C["bd"]
    sel_t, sel_b = C["sel"]

    banks = [P.psum([128, 512]) for _ in range(8)]

    class U:
        pass
    units = []
    for u in range(2):
        S_ = U()
        S_.u = u
        bk = banks[4 * u:4 * u + 4]
        S_.kk = (bk[0][0], 0, bk[0][1])
        S_.qk = (bk[0][0], 128, bk[0][1])
        S_.m = (bk[0][0], 256, bk[0][1])
        S_.n = (bk[0][0], 384, bk[0][1])
        S_.ng = (bk[1][0], 0, bk[1][1])
        S_.tr = (bk[1][0], 128, bk[1][1])
        S_.tr2 = (bk[1][0], 256, bk[1][1])
        S_.qe = (bk[1][0], 384, bk[1][1])
        S_.sqa = (bk[2][0], 0, bk[2][1])
        S_.sqb = (bk[2][0], 128, bk[2][1])
        S_.ap = (bk[2][0], 256, bk[2][1])
        S_.s = (bk[2][0], 384, bk[2][1])
        S_.o = [(bk[3][0], 0, bk[3][1]), (bk[3][0], 128, bk[3][1])]
        S_.pre = bk[3]
        S_.gb = P.sb([128, 2, NP])
        S_.ng_sb = P.sb([128, NP])
        S_.nb = P.sb([128, NP])
        S_.gc = P.sb([128, NP])
        S_.e = P.sb([128, NP])
        S_.kdf = P.sb([128, NP])
        S_.be = P.sb([128, NP])
        S_.GL = P.sb([64, 2, NP])
        S_.qkin = [P.sb([64, 2, G * 128]) for _ in range(2)]
        S_.tokin = [P.sb([128, G, 192]) for _ in range(2)]
        S_.R = [P.sb([128, 128]) for _ in range(2)]
        S_.Dm = [P.sb([128, 128]) for _ in range(2)]
        S_.DmT = [P.sb([128, 128]) for _ in range(2)]
        S_.DmS = [P.sb([128, 128]) for _ in range(2)]
        S_.pw = [P.sb([128, 128]) for _ in range(4)]
        S_.qkT = [P.sb([128, 128]) for _ in range(2)]
        S_.X = [P.sb([128, 128]) for _ in range(3)]
        S_.kd = [P.sb([128, 2, 64]) for _ in range(2)]
        S_.kdfm = P.sb([128, 2, NP])
        S_.nw = [P.sb([128, 64]) for _ in range(2)]
        S_.McT = [P.sb([64, 2, 64]) for _ in range(2)]
        S_.Nsb = [P.sb([64, 128]) for _ in range(2)]
        S_.dE = [P.sb([128, 128]) for _ in range(2)]
        S_.qe_sb = [P.sb([64, 128]) for _ in range(2)]
        S_.S = [P.sb([64, 64]) for _ in range(3)]
        S_.osb = [P.sb([64, 128]) for _ in range(2)]
        S_.si = 0
        S_.xi = 0
        units.append(S_)

    def ps(slot, rows=128, cols=128):
        t, c0, b = slot
        return t[0:rows, c0:c0 + cols], b

    for S_ in units:
        u = S_.u
        gb_t, gb_b = S_.gb
        P.dma("sp", gb_t[:], gb_in[u, :, :, :], (), (gb_b,))
        g_ap, beta_ap = gb_t[:, 0, :], gb_t[:, 1, :]
        P.ts("dve", S_.ng_sb[0][:], g_ap, -1.0, None, ALU.mult, None, (gb_b,), (S_.ng_sb[1],))
        P.ts("dve", S_.nb[0][:], beta_ap, -1.0, None, ALU.mult, None, (gb_b,), (S_.nb[1],))
        pre_t, pre_b = S_.pre
        P.mm(pre_t[:, 0:NP], tri_t[:], g_ap, True, True, (tri_b, gb_b), (pre_b,))
        P.copy("act", S_.gc[0][:], pre_t[:, 0:NP], (pre_b,), (S_.gc[1],))
        P.act(S_.e[0][:], S_.gc[0][:], AF.Exp, (S_.gc[1],), (S_.e[1],))
        P.tt("dve", S_.be[0][:], S_.e[0][:], beta_ap, ALU.mult, (S_.e[1], gb_b), (S_.be[1],))
        P.mm(pre_t[:, 0:NP], bd_t[:], g_ap, True, True, (bd_b, gb_b), (pre_b,))
        P.tt("dve", S_.kdf[0][:], pre_t[:, 0:NP], S_.gc[0][:], ALU.subtract, (pre_b, S_.gc[1]), (S_.kdf[1],))
        P.act(S_.kdf[0][:], S_.kdf[0][:], AF.Exp, (S_.kdf[1],), (S_.kdf[1],))
        for c in range(2):
            P.ts("dve", S_.kdfm[0][:, c, :], S_.kdf[0][:], sel_t[:, c * 64:c * 64 + 1], None, ALU.mult, None,
                 (S_.kdf[1], sel_b), (S_.kdfm[1],))
        for c in range(2):
            P.mm(pre_t[0:64, 0:NP], sel_t[:, c * 64:(c + 1) * 64], g_ap, True, True, (sel_b, gb_b), (pre_b,))
            P.act(S_.GL[0][:, c, :], pre_t[0:64, 0:NP], AF.Exp, (pre_b,), (S_.GL[1],))
        P.memset("dve", S_.S[0][0][:], 0.0, (S_.S[0][1],))

    def load_group(S_, gi):
        u = S_.u
        qk_t, qk_b = S_.qkin[gi % 2]
        tk_t, tk_b = S_.tokin[gi % 2]
        P.dma("sp", qk_t[:], qk_in[u, :, :, gi * G * 128:(gi + 1) * G * 128].rearrange("a d t -> d a t"), (), (qk_b,))
        P.dma("sp", tk_t[:], tok_in[u, :, gi * G:(gi + 1) * G, :], (), (tk_b,))

    def pre(S_, p):
        gi, pi = p // G, p % G
        qk_t, qk_b = S_.qkin[gi % 2]
        tk_t, tk_b = S_.tokin[gi % 2]
        qT = qk_t[:, 0, pi * 128:(pi + 1) * 128]
        kT = qk_t[:, 1, pi * 128:(pi + 1) * 128]
        Qt, Kt, Vt = tk_t[:, pi, 0:64], tk_t[:, pi, 64:128], tk_t[:, pi, 128:192]
        gb_t, gb_b = S_.gb
        j = p % 2
        kk_ap, kk_b = ps(S_.kk)
        qk_ap, qkp_b = ps(S_.qk)
        P.mm(kk_ap, kT, kT, True, True, (qk_b,), (kk_b,))
        yield
        P.mm(qk_ap, kT, qT, True, True, (qk_b,), (qkp_b,))
        yield
        R_t, R_b = S_.R[j]
        P.ts("pool", R_t[:], tri_t[:], S_.ng_sb[0][:, p:p + 1], None, ALU.mult, None, (tri_b, S_.ng_sb[1]), (R_b,))
        yield
        ng_ap, ng_b = ps(S_.ng)
        P.mm(ng_ap, ones_t[:], R_t[:], True, False, (ones_b, R_b), (ng_b,))
        P.mm(ng_ap, ident_t[:], negl_t[:], False, True, (ident_b, negl_b), (ng_b,))
        yield
        Dm_t, Dm_b = S_.Dm[j]
        P.act(Dm_t[:], ng_ap, AF.Exp, (ng_b, S_.gc[1]), (Dm_b,), bias=S_.gc[0][:, p:p + 1])
        yield
        tr_ap, tr_b = ps(S_.tr)
        P.transpose(tr_ap, Dm_t[:], ident_t[:], (Dm_b, ident_b), (tr_b,))
        yield
        DmT_t, DmT_b = S_.DmT[j]
        P.copy("act", DmT_t[:], tr_ap, (tr_b,), (DmT_b,))
        yield
        DmS_t, DmS_b = S_.DmS[j]
        P.tt("pool", DmS_t[:], Dm_t[:], slm_t[:], ALU.mult, (Dm_b, slm_b), (DmS_b,))
        yield
        cur_t, cur_b = S_.pw[0]
        curT_t, curT_b = S_.pw[1]
        P.stt("dve", cur_t[:], kk_ap, S_.nb[0][:, p:p + 1], DmS_t[:], ALU.mult, ALU.mult,
              (kk_b, S_.nb[1], DmS_b), (cur_b,))
        yield
        tr2_ap, tr2_b = ps(S_.tr2)
        P.transpose(tr2_ap, cur_t[:], ident_t[:], (cur_b, ident_b), (tr2_b,))
        yield
        P.copy("act", curT_t[:], tr2_ap, (tr2_b,), (curT_b,))
        yield
        qkT_t, qkT_b = S_.qkT[j]
        P.tt("dve", qkT_t[:], qk_ap, DmT_t[:], ALU.mult, (qkp_b, DmT_b), (qkT_b,))
        yield
        X_t, X_b = S_.X[S_.xi % 3]
        S_.xi += 1
        P.ts("pool", X_t[:, 0:64], Kt, S_.be[0][:, p:p + 1], None, ALU.mult, None, (tk_b, S_.be[1]), (X_b,))
        yield
        P.ts("pool", X_t[:, 64:128], Vt, gb_t[:, 1, p:p + 1], None, ALU.mult, None, (tk_b, gb_b), (X_b,))
        yield
        pwi = 0
        for lvl in range(6):
            ap_ap, ap_b = ps(S_.ap)
            P.mm(ap_ap, curT_t[:], X_t[:], True, True, (curT_b, X_b), (ap_b,))
            yield
            Xn_t, Xn_b = S_.X[S_.xi % 3]
            S_.xi += 1
            P.tt("dve", Xn_t[:], ap_ap, X_t[:], ALU.add, (ap_b, X_b), (Xn_b,))
            yield
            X_t, X_b = Xn_t, Xn_b
            if lvl < 5:
                nxt_t, nxt_b = S_.pw[(pwi + 2) % 4]
                nxtT_t, nxtT_b = S_.pw[(pwi + 3) % 4]
                if lvl < 4:
                    sa_ap, sa_b = ps(S_.sqa)
                    P.mm(sa_ap, curT_t[:], cur_t[:], True, True, (curT_b, cur_b), (sa_b,))
                    yield
                    P.copy("act", nxt_t[:], sa_ap, (sa_b,), (nxt_b,))
                    yield
                sb_ap, sb_b = ps(S_.sqb)
                P.mm(sb_ap, cur_t[:], curT_t[:], True, True, (cur_b, curT_b), (sb_b,))
                yield
                P.copy("act", nxtT_t[:], sb_ap, (sb_b,), (nxtT_b,))
                yield
                cur_t, cur_b, curT_t, curT_b = nxt_t, nxt_b, nxtT_t, nxtT_b
                pwi = (pwi + 2) % 4
        kd_t, kd_b = S_.kd[j]
        nw_t, nw_b = S_.nw[j]
        for c in range(2):
            P.ts("pool", kd_t[:, c, :], Kt, S_.kdfm[0][:, c, p:p + 1], None, ALU.mult, None, (tk_b, S_.kdfm[1]), (kd_b,))
            yield
        P.ts("pool", nw_t[:], X_t[:, 0:64], -1.0, None, ALU.mult, None, (X_b,), (nw_b,))
        yield
        m_t, m0, m_b = S_.m
        n_t, n0, n_b = S_.n
        for c in range(2):
            r0, r1 = c * 64, (c + 1) * 64
            P.mm(m_t[0:64, m0 + r0:m0 + r1], nw_t[:], kd_t[:, c, :], True, True, (nw_b, kd_b), (m_b,))
            yield
            P.mm(n_t[0:64, n0 + r0:n0 + r1], kd_t[:, c, :], X_t[:, 64:128], True, True, (kd_b, X_b), (n_b,))
            yield
        McT_t, McT_b = S_.McT[j]
        for c in range(2):
            P.stt("dve", McT_t[:, c, :], ident_t[0:64, 0:64], S_.GL[0][:, c, p:p + 1],
                  m_t[0:64, m0 + c * 64:m0 + (c + 1) * 64], ALU.mult, ALU.add, (ident_b, S_.GL[1], m_b), (McT_b,))
            yield
        Nsb_t, Nsb_b = S_.Nsb[j]
        P.copy("act", Nsb_t[:], n_t[0:64, n0:n0 + 128], (n_b,), (Nsb_b,))
        yield
        dE_t, dE_b = S_.dE[j]
        P.ts("pool", dE_t[:], ident_t[:], S_.e[0][:, p:p + 1], None, ALU.mult, None, (ident_b, S_.e[1]), (dE_b,))
        yield
        qe_ap, qe_b = ps(S_.qe, 64, 128)
        P.mm(qe_ap, Qt, dE_t[:], True, False, (tk_b, dE_b), (qe_b,))
        P.mm(qe_ap, nw_t[:], qkT_t[:], False, True, (nw_b, qkT_b), (qe_b,))
        yield
        qes_t, qes_b = S_.qe_sb[j]
        P.copy("act", qes_t[:], qe_ap, (qe_b,), (qes_b,))
        yield
        S_.cur = dict(X=(X_t, X_b), qkT=(qkT_t, qkT_b), McT=(McT_t, McT_b), Nsb=(Nsb_t, Nsb_b), qe=(qes_t, qes_b))

    def scan(S_, p, cur):
        X_t, X_b = cur["X"]
        qkT_t, qkT_b = cur["qkT"]
        McT_t, McT_b = cur["McT"]
        Nsb_t, Nsb_b = cur["Nsb"]
        qes_t, qes_b = cur["qe"]
        o_ap, o_b = ps(S_.o[p % 2], 64, 128)
        ot, oc0, _ = S_.o[p % 2]
        P.mm(o_ap, X_t[:, 64:128], qkT_t[:], True, False, (X_b, qkT_b), (o_b,))
        yield
        s_ap, s_b = ps(S_.s, 64, 64)
        for c in range(2):
            St, Sb = S_.S[S_.si % 3]
            P.mm(ot[0:64, oc0 + c * 64:oc0 + (c + 1) * 64], St[:], qes_t[:, c * 64:(c + 1) * 64], False, c == 1,
                 (Sb, qes_b), (o_b,))
            yield
            P.mm(s_ap, McT_t[:, c, :], St[:], True, False, (McT_b, Sb), (s_b,))
            P.mm(s_ap, ident_t[0:64, 0:64], Nsb_t[:, c * 64:(c + 1) * 64], False, True, (ident_b, Nsb_b), (s_b,))
            yield
            S_.si += 1
            Sn, Snb = S_.S[S_.si % 3]
            P.copy("act", Sn[:], s_ap, (s_b,), (Snb,))
            yield
        os_t, os_b = S_.osb[p % 2]
        P.copy("dve", os_t[:], o_ap, (o_b,), (os_b,))
        yield
        P.dma("sp", o_out[S_.u, :, p * 128:(p + 1) * 128], os_t[:], (os_b,), ())

    from itertools import zip_longest
    saved = [None, None]
    for p in range(NP + 1):
        gens = []
        if p < NP:
            for S_ in units:
                if p % G == 0:
                    load_group(S_, p // G)
                gens.append(pre(S_, p))
        if p >= 1:
            for S_ in units:
                gens.append(scan(S_, p - 1, saved[S_.u]))
        for _ in zip_longest(*gens):
            pass
        saved = [S_.cur for S_ in units]
    return P.finish()


def c2_unit_inputs(q, k, v, g, beta):
    T = q.shape[1]
    NP = T // 128
    qk = np.stack([q, k])
    tok = np.concatenate([q.T, k.T, v.T], axis=1).reshape(NP, 128, 192).transpose(1, 0, 2)
    gbt = np.stack([g.reshape(NP, 128).T, beta.reshape(NP, 128).T], axis=1)
    return c_(qk), c_(tok), c_(gbt)


def run_C2(unit_list):
    T = unit_list[0][0].shape[1]
    nc = build_C2(T // 128)
    consts = c2_consts()
    zero = tuple(np.zeros_like(a) for a in unit_list[0])
    ims = []
    for i in range(NCORES):
        us = [unit_list[2 * i + j] if 2 * i + j < len(unit_list) else zero for j in range(2)]
        parts = [c2_unit_inputs(*u_) for u_ in us]
        im = {"qk": np.stack([p_[0] for p_ in parts]), "tok": np.stack([p_[1] for p_ in parts]),
              "gbt": np.stack([p_[2] for p_ in parts])}
        im.update(consts)
        ims.append(im)
    res = run(nc, ims)
    outs = []
    for idx in range(len(unit_list)):
        outs.append(res[idx // 2]["oT"][idx % 2])
    return outs


def tok_blocks(ntl, ntc, bw=512):
    blks = [(t0, min(bw, ntl - t0), 0) for t0 in range(0, ntl, bw)]
    if ntc:
        blks += [(ntl + t0, min(bw, ntc - t0), 1) for t0 in range(0, ntc, bw)]
    return blks


def build_E1(ntl, ntc):
    nt = ntl + ntc
    P = Prog()
    xT = P.dram_in("xT", [8, 128, nt])
    ya = P.dram_in("ya", [2, 128, nt])
    yc = P.dram_in("yc", [3, 128, nt])
    of = P.dram_in("of", [3, 128, nt])
    ob = P.dram_in("ob", [3, 128, nt])
    z = P.dram_in("z", [3, 128, nt])
    w = P.dram_in("w", [1024, 1024])
    mod2 = P.dram_in("mod2", [128, 2, 8])
    dng = P.dram_in("dng", [128, 1])
    bd_in = P.dram_in("bd", [128, 128])
    out = P.dram_out("x1T", [8, 128, nt])
    w_t, w_b = P.sb([128, 8, 1024], BF16, "wout")
    for mc in range(8):
        P.dma("pool", w_t[:, mc, :], w[mc * 128:(mc + 1) * 128, :], (), (w_b,))
    m2_t, m2_b = P.sb([128, 2, 8])
    P.dma("sp", m2_t[:], mod2[:, :, :], (), (m2_b,))
    g_t, g_b = P.sb([128, 1])
    P.dma("sp", g_t[:], dng[:, :], (), (g_b,))
    bd_t, bd_b = P.sb([128, 128])
    P.dma("sp", bd_t[:], bd_in[:, :], (), (bd_b,))
    mixs = [P.sb([128, 8, 512], BF16) for _ in range(2)]
    xs = [P.sb([128, 8, 512]) for _ in range(2)]
    ofs = [P.sb([128, 512]) for _ in range(2)]
    obs = [P.sb([128, 512]) for _ in range(2)]
    zs = [P.sb([128, 512]) for _ in range(2)]
    sqs = [P.sb([128, 512]) for _ in range(2)]
    rs = [P.sb([128, 512]) for _ in range(2)]
    pn = [P.psum([128, 512]) for _ in range(2)]
    pp = [P.psum([128, 512]) for _ in range(4)]
    k = 0
    for bi, (t0, wd_, j) in enumerate(tok_blocks(ntl, ntc)):
        mix_t, mix_b = mixs[bi % 2]
        x_t, x_b = xs[bi % 2]
        P.dma("pool", mix_t[:, 0:2, 0:wd_], ya[:, :, t0:t0 + wd_].rearrange("c p t -> p c t"), (), (mix_b,))
        P.dma("pool", mix_t[:, 5:8, 0:wd_], yc[:, :, t0:t0 + wd_].rearrange("c p t -> p c t"), (), (mix_b,))
        P.dma("sp", x_t[:, :, 0:wd_], xT[:, :, t0:t0 + wd_].rearrange("c p t -> p c t"), (), (x_b,))
        for ch in range(3):
            o_t, o_b = ofs[k % 2]
            b_t, b_b = obs[k % 2]
            z_t, z_b = zs[k % 2]
            s_t, s_b = sqs[k % 2]
            r_t, r_b = rs[k % 2]
            ps_t, ps_b = pn[k % 2]
            k += 1
            P.dma("sp", o_t[:, 0:wd_], of[ch, :, t0:t0 + wd_], (), (o_b,))
            P.dma("sp", b_t[:, 0:wd_], ob[ch, :, t0:t0 + wd_], (), (b_b,))
            P.dma("sp", z_t[:, 0:wd_], z[ch, :, t0:t0 + wd_], (), (z_b,))
            P.tt("pool", o_t[:, 0:wd_], o_t[:, 0:wd_], b_t[:, 0:wd_], ALU.add, (o_b, b_b), (o_b,))
            P.act(s_t[:, 0:wd_], o_t[:, 0:wd_], AF.Square, (o_b,), (s_b,))
            P.mm(ps_t[:, 0:wd_], bd_t[:], s_t[:, 0:wd_], True, True, (bd_b, s_b), (ps_b,))
            P.act(r_t[:, 0:wd_], ps_t[:, 0:wd_], AF.Sqrt, (ps_b,), (r_b,), bias=EPS, scale=1.0 / 64)
            P.recip(r_t[:, 0:wd_], r_t[:, 0:wd_], (r_b,), (r_b,))
            P.act(z_t[:, 0:wd_], z_t[:, 0:wd_], AF.Silu, (z_b,), (z_b,))
            P.stt("dve", o_t[:, 0:wd_], o_t[:, 0:wd_], g_t[:, 0:1], r_t[:, 0:wd_], ALU.mult, ALU.mult,
                  (o_b, g_b, r_b), (o_b,))
            P.tt("dve", mix_t[:, 2 + ch, 0:wd_], o_t[:, 0:wd_], z_t[:, 0:wd_], ALU.mult, (o_b, z_b), (mix_b,))
        for dc in range(8):
            ps_t, ps_b = pp[dc % 4]
            for mc in range(8):
                P.mm(ps_t[:, 0:wd_], w_t[:, mc, dc * 128:(dc + 1) * 128], mix_t[:, mc, 0:wd_], mc == 0, mc == 7,
                     (w_b, mix_b), (ps_b,))
            P.stt("dve", x_t[:, dc, 0:wd_], ps_t[:, 0:wd_], m2_t[:, j, dc:dc + 1], x_t[:, dc, 0:wd_], ALU.mult, ALU.add,
                  (ps_b, m2_b, x_b), (x_b,))
        P.dma("sp", out[:, :, t0:t0 + wd_].rearrange("c p t -> p c t"), x_t[:, :, 0:wd_], (x_b,), ())
    return P.finish()


def build_E2(ntl, ntc, E, F, final):
    nt = ntl + ntc
    ntile = nt // 128
    FG = 256
    nfg = F // FG
    P = Prog()
    x1T = P.dram_in("x1T", [8, 128, nt])
    x1 = P.dram_in("x1", [nt, 1024])
    mod = P.dram_in("mod", [128, 2, 2, 8])
    m5 = P.dram_in("m5", [128, 2, 1024])
    wg = P.dram_in("wg", [E, 1024, F])
    wu = P.dram_in("wu", [E, 1024, F])
    wd = P.dram_in("wd", [E, F, 1024])
    out = P.dram_out("x2", [nt, 1024])
    if E > 1:
        wr = P.dram_in("wr", [128, 8, E])
    if final:
        gn = P.dram_in("gn", [128, 1024])
    mod_t, mod_b = P.sb([128, 2, 2, 8])
    P.dma("sp", mod_t[:], mod[:, :, :, :], (), (mod_b,))
    a_t, a_b = P.sb([128, 2, 8])
    P.ts("dve", a_t[:], mod_t[:, :, 1, :], 1.0, None, ALU.add, None, (mod_b,), (a_b,))
    ones_t, ones_b = P.sb([128, 128], BF16)
    P.memset("dve", ones_t[:], 1.0, (ones_b,))
    hx_t, hx_b = P.sb([128, 8, nt], BF16, "hx")
    acc_t, acc_b = P.sb([128, ntile, 1024], F32, "acc")
    P.memset("dve", acc_t[:], 0.0, (acc_b,))
    if E > 1:
        wr_t, wr_b = P.sb([128, 8, E])
        P.dma("sp", wr_t[:], wr[:, :, :], (), (wr_b,))
        lg_t, lg_b = P.sb([128, ntile, E])
        gate_t, gate_b = P.sb([128, ntile, E])
    xs = [P.sb([128, 8, TT]) for _ in range(2)]
    sqs = [P.sb([128, 8, TT], BF16) for _ in range(2)]
    hfs = [P.sb([128, 8, TT]) for _ in range(2)]
    rs = [P.sb([128, TT]) for _ in range(2)]
    pn = P.psum([128, 512])
    pr = P.psum([128, 512])
    for it, (t0, wd_, j) in enumerate(tok_blocks(ntl, ntc, TT)):
        x_t, x_b = xs[it % 2]
        sq_t, sq_b = sqs[it % 2]
        hf_t, hf_b = hfs[it % 2]
        r_t, r_b = rs[it % 2]
        P.dma("sp", x_t[:], x1T[:, :, t0:t0 + TT].rearrange("c p t -> p c t"), (), (x_b,))
        P.act(sq_t[:], x_t[:], AF.Square, (x_b,), (sq_b,))
        for kc in range(8):
            P.mm(pn[0][:, 0:TT], ones_t[:], sq_t[:, kc, :], kc == 0, kc == 7, (ones_b, sq_b), (pn[1],))
        P.act(r_t[:], pn[0][:, 0:TT], AF.Sqrt, (pn[1],), (r_b,), bias=EPS, scale=1.0 / 1024)
        P.recip(r_t[:], r_t[:], (r_b,), (r_b,))
        for kc in range(8):
            P.stt("dve", hf_t[:, kc, :], x_t[:, kc, :], a_t[:, j, kc:kc + 1], r_t[:], ALU.mult, ALU.mult,
                  (x_b, a_b, r_b), (hf_b,))
            P.act(hf_t[:, kc, :], hf_t[:, kc, :], AF.Identity, (hf_b, mod_b), (hf_b,), bias=mod_t[:, j, 0, kc:kc + 1])
        P.copy("pool", hx_t[:, :, t0:t0 + TT], hf_t[:], (hf_b,), (hx_b,))
        if E > 1:
            for sub in range(TT // 128):
                tile = t0 // 128 + sub
                for kc in range(8):
                    P.mm(pr[0][:, 0:E], hf_t[:, kc, sub * 128:(sub + 1) * 128], wr_t[:, kc, :], kc == 0, kc == 7,
                         (hf_b, wr_b), (pr[1],))
                P.copy("act", lg_t[:, tile, :], pr[0][:, 0:E], (pr[1],), (lg_b,))
    if E > 1:
        m1_t, m1_b = P.sb([128, 1])
        m2_t, m2_b = P.sb([128, 1])
        e1_t, e1_b = P.sb([128, E])
        e2_t, e2_b = P.sb([128, E])
        l2_t, l2_b = P.sb([128, E])
        g1_t, g1_b = P.sb([128, 1])
        g2_t, g2_b = P.sb([128, 1])
        for tile in range(ntile):
            l_ap = lg_t[:, tile, :]
            P.op("dve", lambda e, o=m1_t[:], i=l_ap: e.reduce_max(o, i, axis=mybir.AxisListType.X), (lg_b,), (m1_b,))
            P.ts("dve", e1_t[:], l_ap, m1_t[:, 0:1], None, ALU.is_equal, None, (lg_b, m1_b), (e1_b,))
            P.stt("dve", l2_t[:], e1_t[:], -1e9, l_ap, ALU.mult, ALU.add, (e1_b, lg_b), (l2_b,))
            P.op("dve", lambda e, o=m2_t[:], i=l2_t[:]: e.reduce_max(o, i, axis=mybir.AxisListType.X), (l2_b,), (m2_b,))
            P.ts("dve", e2_t[:], l2_t[:], m2_t[:, 0:1], None, ALU.is_equal, None, (l2_b, m2_b), (e2_b,))
            P.tt("dve", g1_t[:], m2_t[:], m1_t[:], ALU.subtract, (m2_b, m1_b), (g1_b,))
            P.act(g1_t[:], g1_t[:], AF.Exp, (g1_b,), (g1_b,))
            P.ts("dve", g1_t[:], g1_t[:], 1.0, None, ALU.add, None, (g1_b,), (g1_b,))
            P.recip(g1_t[:], g1_t[:], (g1_b,), (g1_b,))
            P.ts("dve", g2_t[:], g1_t[:], -1.0, 1.0, ALU.mult, ALU.add, (g1_b,), (g2_b,))
            P.ts("dve", e1_t[:], e1_t[:], g1_t[:, 0:1], None, ALU.mult, None, (e1_b, g1_b), (e1_b,))
            P.stt("dve", gate_t[:, tile, :], e2_t[:], g2_t[:, 0:1], e1_t[:], ALU.mult, ALU.add,
                  (e2_b, g2_b, e1_b), (gate_b,))
    wgs = [P.sb([128, 8, FG], BF16) for _ in range(2)]
    wus = [P.sb([128, 8, FG], BF16) for _ in range(2)]
    wds = [P.sb([128, 2, 1024], BF16) for _ in range(2)]
    pg = [P.psum([128, 512]) for _ in range(2)]
    pu = [P.psum([128, 512]) for _ in range(2)]
    pd = [P.psum([128, 512]) for _ in range(2)]
    sgs = [P.sb([128, 512], BF16) for _ in range(2)]
    Hs = [P.sb([128, 2, 512], BF16) for _ in range(2)]
    blocks = tok_blocks(ntl, ntc)
    jobs = [(e_, fg, bi) for e_ in range(E) for fg in range(nfg) for bi in range(len(blocks))]
    nd = [0]

    def load_w(it):
        if it >= E * nfg:
            return
        e_, fg = it // nfg, it % nfg
        f0 = fg * FG
        wg_t, wg_b = wgs[it % 2]
        wu_t, wu_b = wus[it % 2]
        wd_t, wd_b = wds[it % 2]
        P.dma("pool", wg_t[:], wg[e_, :, f0:f0 + FG].rearrange("(kc p) f -> p kc f", p=128), (), (wg_b,))
        P.dma("pool", wu_t[:], wu[e_, :, f0:f0 + FG].rearrange("(kc p) f -> p kc f", p=128), (), (wu_b,))
        P.dma("pool", wd_t[:], wd[e_, f0:f0 + FG, :].rearrange("(fc p) d -> p fc d", p=128), (), (wd_b,))

    def GU(k):
        e_, fg, bi = jobs[k]
        it = e_ * nfg + fg
        if bi == 0 and it == 0:
            load_w(0)
        if bi == 1:
            load_w(it + 1)
        wg_t, wg_b = wgs[it % 2]
        wu_t, wu_b = wus[it % 2]
        t0, wd_, j = blocks[bi]
        H_t, H_b = Hs[k % 2]
        for fc in range(2):
            g_t, g_b = pg[fc]
            u_t, u_b = pu[fc]
            for kc in range(8):
                P.mm(g_t[:, 0:wd_], wg_t[:, kc, fc * 128:(fc + 1) * 128], hx_t[:, kc, t0:t0 + wd_],
                     kc == 0, kc == 7, (wg_b, hx_b), (g_b,))
            for kc in range(8):
                P.mm(u_t[:, 0:wd_], wu_t[:, kc, fc * 128:(fc + 1) * 128], hx_t[:, kc, t0:t0 + wd_],
                     kc == 0, kc == 7, (wu_b, hx_b), (u_b,))
            sg_t, sg_b = sgs[fc]
            P.act(sg_t[:, 0:wd_], g_t[:, 0:wd_], AF.Silu, (g_b,), (sg_b,))
            P.tt("dve", H_t[:, fc, 0:wd_], u_t[:, 0:wd_], sg_t[:, 0:wd_], ALU.mult, (u_b, sg_b), (H_b,))

    def DOWN(k):
        e_, fg, bi = jobs[k]
        it = e_ * nfg + fg
        wd_t, wd_b = wds[it % 2]
        t0, wd_, j = blocks[bi]
        H_t, H_b = Hs[k % 2]
        for sub in range(wd_ // 128):
            tile = t0 // 128 + sub
            for dh in range(2):
                d_t, d_b = pd[nd[0] % 2]
                nd[0] += 1
                for fc in range(2):
                    P.mm(d_t[:], H_t[:, fc, sub * 128:(sub + 1) * 128], wd_t[:, fc, dh * 512:(dh + 1) * 512],
                         fc == 0, fc == 1, (H_b, wd_b), (d_b,))
                acc_ap = acc_t[:, tile, dh * 512:(dh + 1) * 512]
                if E > 1:
                    P.stt("dve", acc_ap, d_t[:], gate_t[:, tile, e_:e_ + 1], acc_ap, ALU.mult, ALU.add,
                          (d_b, gate_b, acc_b), (acc_b,))
                else:
                    P.tt("dve", acc_ap, d_t[:], acc_ap, ALU.add, (d_b, acc_b), (acc_b,))

    for k in range(len(jobs) + 1):
        if k < len(jobs):
            GU(k)
        if k >= 1:
            DOWN(k - 1)
    m5_t, m5_b = P.sb([128, 2, 1024])
    P.dma("sp", m5_t[:], m5[:, :, :], (), (m5_b,))
    if final:
        gn_t, gn_b = P.sb([128, 1024])
        P.dma("sp", gn_t[:], gn[:, :], (), (gn_b,))
        ss_t, ss_b = P.sb([128, 1])
        tq = [P.sb([128, 1024]) for _ in range(2)]
    x1s = [P.sb([128, 1024]) for _ in range(2)]
    for tile in range(ntile):
        j = 0 if tile * 128 < ntl else 1
        x_t, x_b = x1s[tile % 2]
        P.dma("sp", x_t[:], x1[tile * 128:(tile + 1) * 128, :], (), (x_b,))
        P.tt("pool", acc_t[:, tile, :], acc_t[:, tile, :], m5_t[:, j, :], ALU.mult, (acc_b, m5_b), (acc_b,))
        P.tt("dve", x_t[:], x_t[:], acc_t[:, tile, :], ALU.add, (x_b, acc_b), (x_b,))
        if final:
            q_t, q_b = tq[tile % 2]
            P.tt("pool", q_t[:], x_t[:], x_t[:], ALU.mult, (x_b,), (q_b,))
            P.op("dve", lambda e, o=ss_t[:], i=q_t[:]: e.reduce_sum(o, i, axis=mybir.AxisListType.X), (q_b,), (ss_b,))
            P.act(ss_t[:], ss_t[:], AF.Sqrt, (ss_b,), (ss_b,), bias=EPS, scale=1.0 / 1024)
            P.recip(ss_t[:], ss_t[:], (ss_b,), (ss_b,))
            P.stt("dve", x_t[:], x_t[:], ss_t[:, 0:1], gn_t[:], ALU.mult, ALU.mult, (x_b, ss_b, gn_b), (x_b,))
        P.dma("sp", out[tile * 128:(tile + 1) * 128, :], x_t[:], (x_b,), ())
    return P.finish()


def stage_C2(c1, need_ctx):
    dn, dnc, gb, gbc = c1["dn"], c1["dnc"], c1["gb"], c1["gbc"]
    units = []
    for h in range(6):
        sl = slice(h * 64, (h + 1) * 64)
        for d in range(2):
            parts = []
            for r in (0, 384, 768):
                a_c, a_x = dnc[r:r + 384][sl], dn[r:r + 384][sl]
                if d == 1:
                    a_c, a_x = a_c[:, ::-1], a_x[:, ::-1]
                parts.append(np.concatenate([a_c, a_x], axis=1))
            gs = []
            for w_ in range(2):
                g_c, g_x = gbc[w_, d * 6 + h], gb[w_, d * 6 + h]
                if d == 1:
                    g_c, g_x = g_c[::-1], g_x[::-1]
                gs.append(np.concatenate([g_c, g_x]))
            units.append((parts[0], parts[1], parts[2], gs[0], gs[1]))
    outs = run_C2(units)
    of = np.zeros((384, SEQ), np.float32)
    ob = np.zeros((384, SEQ), np.float32)
    ofc = np.zeros((384, CTX), np.float32)
    obc = np.zeros((384, CTX), np.float32)
    for h in range(6):
        sl = slice(h * 64, (h + 1) * 64)
        f, b = outs[2 * h], outs[2 * h + 1]
        of[sl] = f[:, CTX:]
        ofc[sl] = f[:, :CTX]
        ob[sl] = b[:, CTX:][:, ::-1]
        obc[sl] = b[:, :CTX][:, ::-1]
    return of, ob, ofc, obc


def stage_E1(xT, hcT, c1, of, ob, ofc, obc, px, pc, w_out_l, modT_l, dng_l, need_ctx):
    ntl = SEQ // NCORES
    ntc = CTX if need_ctx else 0
    nc = build_E1(ntl, ntc)
    m = modT_l.reshape(128, 2, 6, 8)
    mod2 = c_(m[:, :, 2, :])
    dng = c_(np.tile(dng_l.reshape(64), 2).reshape(128, 1))
    bd = c_(np.kron(np.eye(2), np.ones((64, 64))))
    zx, zc = px[R_DZ:R_DZ + 384], pc[R_DZ:R_DZ + 384]

    def cat(a, b, i):
        sl = a[:, i * ntl:(i + 1) * ntl]
        return np.concatenate([sl, b], axis=1) if need_ctx else sl
    ims = []
    for i in range(NCORES):
        nt = ntl + ntc
        ims.append({
            "xT": c_(cat(xT, hcT, i).reshape(8, 128, nt)),
            "ya": c_(cat(c1["yaT"], c1.get("yaTc"), i).reshape(2, 128, nt)),
            "yc": c_(cat(c1["ycT"], c1.get("ycTc"), i).reshape(3, 128, nt)),
            "of": c_(cat(of, ofc, i).reshape(3, 128, nt)),
            "ob": c_(cat(ob, obc, i).reshape(3, 128, nt)),
            "z": c_(cat(zx, zc, i).reshape(3, 128, nt)),
            "w": c_(w_out_l), "mod2": mod2, "dng": dng, "bd": bd,
        })
    res = run(nc, ims)
    x1T = np.concatenate([r["x1T"][:, :, :ntl].reshape(1024, ntl) for r in res], axis=1)
    hc1T = res[0]["x1T"][:, :, ntl:].reshape(1024, CTX) if need_ctx else None
    return x1T, hc1T


def stage_E2(x1T, hc1T, modT_l, wg, wu, wd, wr, gn, need_ctx):
    ntl = SEQ // NCORES
    ntc = CTX if need_ctx else 0
    E, _, F = wg.shape
    final = gn is not None
    nc = build_E2(ntl, ntc, E, F, final)
    m = modT_l.reshape(128, 2, 6, 8)
    mod = c_(m[:, :, 3:5, :])
    m5 = np.stack([np.broadcast_to(m[:, j, 5, :].T.reshape(1, 1024), (128, 1024)) for j in range(2)], axis=1)
    wg, wu, wd = c_(wg), c_(wu), c_(wd)
    ims = []
    for i in range(NCORES):
        sl = x1T[:, i * ntl:(i + 1) * ntl]
        blk = np.concatenate([sl, hc1T], axis=1) if need_ctx else sl
        nt = ntl + ntc
        im = {"x1T": c_(blk.reshape(8, 128, nt)), "x1": c_(blk.T), "mod": mod, "m5": c_(m5),
              "wg": wg, "wu": wu, "wd": wd}
        if E > 1:
            im["wr"] = c_(wr.reshape(8, 128, E).transpose(1, 0, 2))
        if final:
            im["gn"] = c_(np.broadcast_to(gn.reshape(1, 1024), (128, 1024)))
        ims.append(im)
    res = run(nc, ims)
    x2 = np.concatenate([r["x2"][:ntl] for r in res], axis=0)
    hc2 = res[0]["x2"][ntl:] if need_ctx else None
    return x2, hc2


def kernel(x, c, ctx, c_ctx, w_mod, b_mod, w_in, w_out, conv_w, dn_conv_w, dn_a_log, dn_dt_bias, dn_norm_g,
           attn_sink, ffn_w_gate, ffn_w_up, ffn_w_down, moe_router, moe_w_gate, moe_w_up, moe_w_down,
           final_norm_g):
    f = lambda a: np.asarray(a, dtype=np.float32)
    x, c, ctx, c_ctx, w_mod, b_mod, w_in, w_out = map(f, (x, c, ctx, c_ctx, w_mod, b_mod, w_in, w_out))
    modT = stage_A(c, c_ctx, w_mod, b_mod)
    xT = np.ascontiguousarray(x[0].T)
    hcT = np.ascontiguousarray(ctx[0].T)
    x2 = None
    for l in range(2):
        need_ctx = l == 0
        px, pc = stage_B(xT, hcT, w_in[l], modT[l])
        c1 = stage_C1(px, pc, f(conv_w)[l], f(attn_sink)[l], f(dn_conv_w)[l], f(dn_a_log)[l], f(dn_dt_bias)[l],
                      need_ctx)
        of, ob, ofc, obc = stage_C2(c1, need_ctx)
        x1T, hc1T = stage_E1(xT, hcT, c1, of, ob, ofc, obc, px, pc, w_out[l], modT[l], f(dn_norm_g)[l], need_ctx)
        if l == 0:
            x2, hc2 = stage_E2(x1T, hc1T, modT[l], f(ffn_w_gate), f(ffn_w_up), f(ffn_w_down), None, None, True)
            xT = np.ascontiguousarray(x2.T)
            hcT = np.ascontiguousarray(hc2.T)
        else:
            x2, _ = stage_E2(x1T, None, modT[l], f(moe_w_gate)[0], f(moe_w_up)[0], f(moe_w_down)[0],
                             f(moe_router)[0], f(final_norm_g), False)
    return x2.reshape(1, SEQ, D_MODEL).astype(np.float32)
```

```python
import numpy as np
from contextlib import ExitStack
import concourse.bass as bass
import concourse.mybir as mybir
from concourse.bass_utils import run_bass_kernel_spmd

F32 = mybir.dt.float32
BF16 = mybir.dt.bfloat16
AF = mybir.ActivationFunctionType
ALU = mybir.AluOpType
NCORES = 8

D_MODEL = 1024
SEQ = 16384
CTX = 256
EPS = 1e-6
IN_COLS = 2968


class Buf:
    __slots__ = ("name", "lw", "rd", "excl", "dw")

    def __init__(self, name, excl=False):
        self.name = name
        self.lw = None
        self.dw = []
        self.rd = {}
        self.excl = excl


class Prog:
    EPOCH = 4096
    NDMA = 24

    def __init__(self):
        self.nc = bass.Bass("TRN2", target_bir_lowering=False)
        self.es = ExitStack()
        self.q = {e: [] for e in ("sp", "pe", "dve", "act", "pool")}
        self.cnt = {e: 0 for e in self.q}
        self.seen = {e: {} for e in self.q}
        self.ndma = {"sp": 0, "pool": 0}
        self.dlast = {}
        self.nt = 0

    def dram_in(self, name, shape, dt=F32):
        return self.nc.dram_tensor(name, list(shape), dt, kind="ExternalInput").ap()

    def dram_out(self, name, shape, dt=F32):
        return self.nc.dram_tensor(name, list(shape), dt, kind="ExternalOutput").ap()

    def sb(self, shape, dt=F32, name=None):
        self.nt += 1
        name = name or f"t{self.nt}"
        t = self.es.enter_context(self.nc.sbuf_tensor(name, list(shape), dt))
        return t, Buf(name)

    def psum(self, shape=(128, 512), dt=F32, name=None):
        self.nt += 1
        name = name or f"p{self.nt}"
        t = self.es.enter_context(self.nc.psum_tensor(name, list(shape), dt))
        return t, Buf(name, excl=True)

    def _deps(self, reads, writes, eng=None):
        deps = []
        for b in reads:
            if b.lw is not None:
                deps.append(b.lw)
            deps.extend(b.dw)
            if b.excl:
                deps.extend(v for k_, v in b.rd.items() if k_ != eng)
        for b in writes:
            if b.lw is not None:
                deps.append(b.lw)
            deps.extend(b.dw)
            deps.extend(b.rd.values())
        return deps

    def _filter(self, eng, deps):
        waits = []
        seen = self.seen[eng]
        for d in deps:
            if d[0] == "c":
                _, p, n = d
                if p == eng and eng == "pe":
                    continue
                if seen.get(p, -1) >= n:
                    continue
                seen[p] = n
            else:
                _, slot, val = d
                if seen.get(("d", slot), 0) >= val:
                    continue
                seen[("d", slot)] = val
            waits.append(d)
        return waits

    def _mark(self, tok, key, reads, writes):
        for b in reads:
            b.rd[key] = tok
        for b in writes:
            if tok[0] == "d":
                if b.rd:
                    b.dw = []
                b.dw.append(tok)
            else:
                b.lw = tok
                b.dw = []
            b.rd = {}

    def op(self, eng, fn, reads=(), writes=()):
        n = self.cnt[eng]
        self.cnt[eng] += 1
        waits = self._filter(eng, self._deps(reads, writes, eng))
        tok = ("c", eng, n)
        self._mark(tok, eng, reads, writes)
        self.q[eng].append((fn, waits, tok))

    def dma(self, eng, out_ap, in_ap, reads=(), writes=()):
        j = self.ndma[eng]
        self.ndma[eng] += 1
        slot = (j % self.NDMA) + (self.NDMA if eng == "pool" else 0)
        val = 16 * (j // self.NDMA + 1)
        deps = self._deps(reads, writes)
        if val > 16:
            deps.append(("d", slot, val - 16))
        waits = self._filter(eng, deps)
        tok = ("d", slot, val)
        self.dlast[slot] = val
        self._mark(tok, ("d", slot), reads, writes)
        self.q[eng].append((lambda e: e.dma_start(out=out_ap, in_=in_ap), waits, tok))

    def mm(self, out, lhsT, rhs, start, stop, reads, writes):
        self.op("pe", lambda e: e.matmul(out, lhsT, rhs, start=start, stop=stop), reads, writes)

    def transpose(self, out, in_, ident, reads, writes):
        self.op("pe", lambda e: e.transpose(out, in_, ident), reads, writes)

    def act(self, out, in_, func, reads, writes, bias=None, scale=None, eng="act"):
        kw = {}
        if bias is not None:
            kw["bias"] = bias
        if scale is not None:
            kw["scale"] = scale
        self.op(eng, lambda e: e.activation(out, in_, func, **kw), reads, writes)

    def tt(self, eng, out, in0, in1, op, reads, writes):
        self.op(eng, lambda e: e.tensor_tensor(out, in0, in1, op=op), reads, writes)

    def ts(self, eng, out, in0, s1, s2, op0, op1, reads, writes):
        if s2 is None:
            self.op(eng, lambda e: e.tensor_scalar(out, in0, s1, None, op0=op0), reads, writes)
        else:
            self.op(eng, lambda e: e.tensor_scalar(out, in0, s1, s2, op0=op0, op1=op1), reads, writes)

    def stt(self, eng, out, in0, scalar, in1, op0, op1, reads, writes):
        self.op(eng, lambda e: e.scalar_tensor_tensor(out, in0, scalar, in1, op0=op0, op1=op1), reads, writes)

    def copy(self, eng, out, in_, reads, writes):
        if eng == "act":
            self.op(eng, lambda e: e.activation(out, in_, AF.Identity), reads, writes)
        else:
            self.op(eng, lambda e: e.tensor_copy(out, in_), reads, writes)

    def recip(self, out, in_, reads, writes):
        self.op("dve", lambda e: e.reciprocal(out, in_), reads, writes)

    def memset(self, eng, ap, val, writes):
        self.op(eng, lambda e: e.memset(ap, val), (), writes)

    def finish(self):
        nc = self.nc
        E = self.EPOCH
        sems = {}
        for e in ("pe", "dve", "act", "pool"):
            nep = max(1, (self.cnt[e] + E - 1) // E)
            sems[e] = [self.es.enter_context(nc.semaphore(f"s_{e}{i}")) for i in range(nep)]
        dsems = [self.es.enter_context(nc.semaphore(f"s_d{i}")) for i in range(2 * self.NDMA)]
        fin = [("d", s, v) for s, v in sorted(self.dlast.items())]
        fin = self._filter("sp", fin)
        self.q["sp"].append((None, fin, None))

        def emit(engobj, ename):
            for fn, waits, tok in self.q[ename]:
                for w in waits:
                    if w[0] == "c":
                        engobj.wait_ge(sems[w[1]][w[2] // E], w[2] % E + 1)
                    else:
                        engobj.wait_ge(dsems[w[1]], w[2])
                if fn is None:
                    continue
                ins = fn(engobj)
                if tok[0] == "c":
                    ins.then_inc(sems[ename][tok[2] // E], 1)
                else:
                    ins.then_inc(dsems[tok[1]], 16)

        with nc.Block() as block:
            @block.sync
            def _(e):
                emit(e, "sp")

            @block.tensor
            def _(e):
                emit(e, "pe")

            @block.vector
            def _(e):
                emit(e, "dve")

            @block.scalar
            def _(e):
                emit(e, "act")

            @block.gpsimd
            def _(e):
                emit(e, "pool")
        self.es.close()
        return nc


def run(prog_nc, in_maps):
    res = run_bass_kernel_spmd(prog_nc, in_maps, core_ids=list(range(NCORES)))
    return res.results


def c_(a):
    return np.ascontiguousarray(a, dtype=np.float32)


def build_A():
    ND = 48 // NCORES
    P = Prog()
    s_in = P.dram_in("s_in", [128, 8, 2])
    wm = P.dram_in("wm", [2, 1024, ND * 128])
    bm = P.dram_in("bm", [2, 128, ND])
    out = P.dram_out("modT", [2, 128, 2, ND])
    s_t, s_b = P.sb([128, 8, 2])
    sg_t, sg_b = P.sb([128, 8, 2])
    sb_t, sb_b = P.sb([128, 8, 2], BF16)
    bm_t, bm_b = P.sb([128, 2, ND])
    o_t, o_b = P.sb([128, 2, 2, ND])
    P.dma("sp", s_t[:], s_in[:, :, :], (), (s_b,))
    P.dma("sp", bm_t[:], bm.rearrange("l p c -> p l c"), (), (bm_b,))
    P.act(sg_t[:], s_t[:], AF.Sigmoid, (s_b,), (sg_b,))
    P.tt("dve", sb_t[:], s_t[:], sg_t[:], ALU.mult, (s_b, sg_b), (sb_b,))
    ws = [P.sb([128, 8, ND * 128], BF16) for _ in range(2)]
    pss = [P.psum([128, 512]) for _ in range(2)]
    for l in range(2):
        w_t, w_b = ws[l]
        for kc in range(8):
            P.dma("pool", w_t[:, kc, :], wm[l, kc * 128:(kc + 1) * 128, :], (), (w_b,))
    for l in range(2):
        w_t, w_b = ws[l]
        ps_t, ps_b = pss[l]
        for dc in range(ND):
            for kc in range(8):
                P.mm(ps_t[:, dc * 2:dc * 2 + 2], w_t[:, kc, dc * 128:(dc + 1) * 128], sb_t[:, kc, :],
                     kc == 0, kc == 7, (w_b, sb_b), (ps_b,))
        for j in range(2):
            P.tt("dve", o_t[:, l, j, :], ps_t[:, j:2 * ND:2], bm_t[:, l, :], ALU.add, (ps_b, bm_b), (o_b,))
    P.dma("sp", out.rearrange("l p j c -> p l j c"), o_t[:], (o_b,), ())
    return P.finish()


def stage_A(c, c_ctx, w_mod, b_mod):
    ND = 48 // NCORES
    s = np.stack([c.reshape(1024), c_ctx.reshape(1024)], axis=-1)
    s_in = c_(s.reshape(8, 128, 2).transpose(1, 0, 2))
    bm = b_mod.reshape(2, 48, 128).transpose(0, 2, 1)
    nc = build_A()
    ims = [{"s_in": s_in, "wm": c_(w_mod[:, :, i * ND * 128:(i + 1) * ND * 128]),
            "bm": c_(bm[:, :, i * ND:(i + 1) * ND])} for i in range(NCORES)]
    res = run(nc, ims)
    return np.concatenate([r["modT"] for r in res], axis=3)


NCH_IN = 24
TT = 256


def build_B(ntl, ntc):
    nt = ntl + ntc
    P = Prog()
    xT = P.dram_in("xT", [8, 128, nt])
    w = P.dram_in("w", [1024, NCH_IN * 128])
    mod = P.dram_in("mod", [128, 2, 2, 8])
    out = P.dram_out("pxT", [NCH_IN, 128, nt])
    w_t, w_b = P.sb([128, 8, NCH_IN * 128], BF16, "w_sb")
    for kc in range(8):
        P.dma("pool", w_t[:, kc, :], w[kc * 128:(kc + 1) * 128, :], (), (w_b,))
    mod_t, mod_b = P.sb([128, 2, 2, 8])
    P.dma("sp", mod_t[:], mod[:, :, :, :], (), (mod_b,))
    a_t, a_b = P.sb([128, 2, 8])
    P.ts("dve", a_t[:], mod_t[:, :, 1, :], 1.0, None, ALU.add, None, (mod_b,), (a_b,))
    ones_t, ones_b = P.sb([128, 128], BF16)
    P.memset("dve", ones_t[:], 1.0, (ones_b,))
    xs = [P.sb([128, 8, TT]) for _ in range(2)]
    sqs = [P.sb([128, 8, TT], BF16) for _ in range(2)]
    hs = [P.sb([128, 8, TT], BF16) for _ in range(2)]
    rs = [P.sb([128, TT]) for _ in range(2)]
    tmp = [P.sb([128, TT]) for _ in range(2)]
    os_ = [P.sb([128, TT]) for _ in range(4)]
    pn = P.psum([128, 512])
    pp = [P.psum([128, 512]) for _ in range(4)]
    for it in range(nt // TT):
        j = 0 if it * TT < ntl else 1
        x_t, x_b = xs[it % 2]
        sq_t, sq_b = sqs[it % 2]
        h_t, h_b = hs[it % 2]
        r_t, r_b = rs[it % 2]
        P.dma("sp", x_t[:], xT[:, :, it * TT:(it + 1) * TT].rearrange("c p t -> p c t"), (), (x_b,))
        P.act(sq_t[:], x_t[:], AF.Square, (x_b,), (sq_b,))
        for kc in range(8):
            P.mm(pn[0][:, 0:TT], ones_t[:], sq_t[:, kc, :], kc == 0, kc == 7, (ones_b, sq_b), (pn[1],))
        P.act(r_t[:], pn[0][:, 0:TT], AF.Sqrt, (pn[1],), (r_b,), bias=EPS, scale=1.0 / 1024)
        P.recip(r_t[:], r_t[:], (r_b,), (r_b,))
        for kc in range(8):
            t_t, t_b = tmp[kc % 2]
            P.stt("dve", t_t[:], x_t[:, kc, :], a_t[:, j, kc:kc + 1], r_t[:], ALU.mult, ALU.mult,
                  (x_b, a_b, r_b), (t_b,))
            P.act(h_t[:, kc, :], t_t[:], AF.Identity, (t_b, mod_b), (h_b,), bias=mod_t[:, j, 0, kc:kc + 1])
        for oc in range(NCH_IN):
            ps_t, ps_b = pp[oc % 4]
            for kc in range(8):
                P.mm(ps_t[:, 0:TT], w_t[:, kc, oc * 128:(oc + 1) * 128], h_t[:, kc, :], kc == 0, kc == 7,
                     (w_b, h_b), (ps_b,))
            o_t, o_b = os_[oc % 4]
            P.copy("act" if oc % 2 else "dve", o_t[:], ps_t[:, 0:TT], (ps_b,), (o_b,))
            P.dma("sp", out[oc, :, it * TT:(it + 1) * TT], o_t[:], (o_b,), ())
    return P.finish()


def perm_w_in(w_in_l):
    z = np.zeros((1024, 104), np.float32)
    return c_(np.concatenate([w_in_l[:, 0:2304], w_in_l[:, 2328:2968], w_in_l[:, 2304:2328], z], axis=1))


def stage_B(xT_all, hcT, w_in_l, modT_l):
    ntl = SEQ // NCORES
    nc = build_B(ntl, CTX)
    w = perm_w_in(w_in_l)
    m = modT_l.reshape(128, 2, 6, 8)
    mod = c_(m[:, :, 0:2, :])
    ims = []
    for i in range(NCORES):
        xt = np.concatenate([xT_all[:, i * ntl:(i + 1) * ntl], hcT], axis=1)
        ims.append({"xT": c_(xt.reshape(8, 128, ntl + CTX)), "w": w, "mod": mod})
    res = run(nc, ims)
    px = np.concatenate([r["pxT"][:, :, :ntl].reshape(NCH_IN * 128, ntl) for r in res], axis=1)
    pc = res[0]["pxT"][:, :, ntl:].reshape(NCH_IN * 128, CTX)
    return px, pc


GRID_W = 64
NEGV = -30000.0


def rope_tables(pos):
    pos = np.asarray(pos)
    inv = 10000.0 ** (-(np.arange(0, 32, 2, dtype=np.float64)) / 32.0)
    r = (pos // GRID_W).astype(np.float64)
    col = (pos % GRID_W).astype(np.float64)
    cos = np.zeros((64, len(pos)))
    sin = np.zeros((64, len(pos)))
    for d in range(64):
        axis, half, f = d // 32, (d % 32) // 16, d % 16
        p = r if axis == 0 else col
        cos[d] = np.cos(p * inv[f])
        sin[d] = np.sin(p * inv[f]) * (-1.0 if half == 0 else 1.0)
    return cos, sin


ROPE_PERM = np.array([(d // 32) * 32 + ((d % 32) + 16) % 32 for d in range(64)])


def build_C1(ntl, need_ctx):
    nqb = ntl // 128
    nkb = nqb + 2
    nk = nkb * 128
    P = Prog()
    cv = P.dram_in("cv", [3, 2, 128, ntl + 2])
    cw = P.dram_in("cw", [128, 2, 3])
    q_in = P.dram_in("q", [2, 6, 64, ntl])
    k_in = P.dram_in("k", [2, 2, 64, nk])
    v_in = P.dram_in("v", [128, nkb, 128])
    cs_k = P.dram_in("cs_k", [2, 64, nk])
    kc_in = P.dram_in("kc", [2, 64, CTX])
    vc_in = P.dram_in("vc", [128, 2, 128])
    neg_in = P.dram_in("neg", [128, 4, 384])
    sink_in = P.dram_in("sink", [1, 2, 384])
    ident_in = P.dram_in("ident", [128, 128])
    ya = P.dram_out("yaT", [2, 128, ntl])
    yc = P.dram_out("ycT", [6, 64, ntl])
    if need_ctx:
        cvc = P.dram_in("cvc", [3, 2, 128, CTX + 2])
        qc_in = P.dram_in("qc", [6, 64, CTX])
        ya_c = P.dram_out("yaTc", [2, 128, CTX])
        yc_c = P.dram_out("ycTc", [6, 64, CTX])
    dn_in = P.dram_in("dn", [9, 128, ntl + 2])
    dnc_in = P.dram_in("dnc", [9, 128, CTX + 2])
    dcw_in = P.dram_in("dcw", [128, 9, 3])
    gab_in = P.dram_in("gab", [2, 12, ntl])
    gabc_in = P.dram_in("gabc", [2, 12, CTX])
    gpar_in = P.dram_in("gpar", [12, 2])
    bd_in = P.dram_in("bd", [128, 128])
    dn_out = P.dram_out("dno", [9, 128, ntl])
    dnc_out = P.dram_out("dnco", [9, 128, CTX])
    gb_out = P.dram_out("gbo", [2, 12, ntl])
    gbc_out = P.dram_out("gbco", [2, 12, CTX])

    cw_t, cw_b = P.sb([128, 2, 3])
    P.dma("sp", cw_t[:], cw[:, :, :], (), (cw_b,))
    ident_t, ident_b = P.sb([128, 128], BF16)
    P.dma("pool", ident_t[:], ident_in[:, :], (), (ident_b,))
    neg_t, neg_b = P.sb([128, 4, 384], BF16)
    P.dma("pool", neg_t[:], neg_in[:, :, :], (), (neg_b,))
    ones_t, ones_b = P.sb([128, 64], BF16)
    P.memset("dve", ones_t[:], 1.0, (ones_b,))
    onesf_t, onesf_b = P.sb([1, 64])
    P.memset("dve", onesf_t[:], 1.0, (onesf_b,))
    sk_t, sk_b = P.sb([1, 2, 384])
    P.dma("sp", sk_t[:], sink_in[:, :, :], (), (sk_b,))
    esk_t, esk_b = P.sb([1, 2, 384])
    P.act(esk_t[:], sk_t[:], AF.Exp, (sk_b,), (esk_b,))

    cb_t, b_b = P.sb([128, ntl + 2])
    cc_t, c_b = P.sb([128, ntl + 2])
    chh_t, h_b = P.sb([128, ntl + 2])

    def conv(src, n, dst):
        for ch in range(2):
            b_t, c_t, h_t = cb_t[:, 0:n + 2], cc_t[:, 0:n + 2], chh_t[:, 0:n + 2]
            P.dma("sp", b_t, src[0, ch, :, :], (), (b_b,))
            P.dma("sp", c_t, src[1, ch, :, :], (), (c_b,))
            P.dma("sp", h_t, src[2, ch, :, :], (), (h_b,))
            P.tt("dve", c_t, c_t, h_t, ALU.mult, (c_b, h_b), (c_b,))
            P.ts("dve", h_t[:, 0:n], c_t[:, 1:n + 1], cw_t[:, ch, 1:2], None, ALU.mult, None, (c_b, cw_b), (h_b,))
            P.stt("dve", h_t[:, 0:n], c_t[:, 0:n], cw_t[:, ch, 0:1], h_t[:, 0:n], ALU.mult, ALU.add,
                  (c_b, cw_b, h_b), (h_b,))
            P.stt("dve", h_t[:, 0:n], c_t[:, 2:n + 2], cw_t[:, ch, 2:3], h_t[:, 0:n], ALU.mult, ALU.add,
                  (c_b, cw_b, h_b), (h_b,))
            P.tt("dve", h_t[:, 0:n], h_t[:, 0:n], b_t[:, 1:n + 1], ALU.mult, (h_b, b_b), (h_b,))
            P.dma("sp", dst[ch, :, :], h_t[:, 0:n], (h_b,), ())
    conv(cv, ntl, ya)
    if need_ctx:
        conv(cvc, CTX, ya_c)

    dcw_t, dcw_b = P.sb([128, 9, 3])
    P.dma("sp", dcw_t[:], dcw_in[:, :, :], (), (dcw_b,))
    bd_t, bd_b = P.sb([128, 128])
    P.dma("sp", bd_t[:], bd_in[:, :], (), (bd_b,))
    rr_t, rr_b = P.sb([128, 512])
    pps = P.psum([128, 512])

    def dnprep(src, n, dst):
        for ch in range(9):
            x_t, y_t, s_t = cb_t[:, 0:n + 2], cc_t[:, 0:n], chh_t[:, 0:n]
            x_b, y_b, s_b = b_b, c_b, h_b
            P.dma("sp", x_t, src[ch, :, :], (), (x_b,))
            P.ts("dve", y_t, x_t[:, 1:n + 1], dcw_t[:, ch, 1:2], None, ALU.mult, None, (x_b, dcw_b), (y_b,))
            P.stt("dve", y_t, x_t[:, 0:n], dcw_t[:, ch, 0:1], y_t, ALU.mult, ALU.add, (x_b, dcw_b, y_b), (y_b,))
            P.stt("dve", y_t, x_t[:, 2:n + 2], dcw_t[:, ch, 2:3], y_t, ALU.mult, ALU.add, (x_b, dcw_b, y_b), (y_b,))
            P.act(y_t, y_t, AF.Silu, (y_b,), (y_b,))
            if ch < 6:
                P.act(s_t, y_t, AF.Square, (y_b,), (s_b,))
                for c0 in range(0, n, 512):
                    w_ = min(512, n - c0)
                    P.mm(pps[0][:, 0:w_], bd_t[:], s_t[:, c0:c0 + w_], True, True, (bd_b, s_b), (pps[1],))
                    P.act(rr_t[:, 0:w_], pps[0][:, 0:w_], AF.Sqrt, (pps[1],), (rr_b,), bias=1e-6)
                    P.recip(rr_t[:, 0:w_], rr_t[:, 0:w_], (rr_b,), (rr_b,))
                    P.stt("dve", y_t[:, c0:c0 + w_], y_t[:, c0:c0 + w_], 0.125 if ch < 3 else 1.0, rr_t[:, 0:w_],
                          ALU.mult, ALU.mult, (y_b, rr_b), (y_b,))
            P.dma("sp", dst[ch, :, :], y_t, (y_b,), ())
    dnprep(dn_in, ntl, dn_out)
    dnprep(dnc_in, CTX, dnc_out)
    gpar_t, gpar_b = P.sb([12, 2])
    P.dma("sp", gpar_t[:], gpar_in[:, :], (), (gpar_b,))
    nea_t, nea_b = P.sb([12, 1])
    P.act(nea_t[:], gpar_t[:, 0:1], AF.Exp, (gpar_b,), (nea_b,))
    P.ts("dve", nea_t[:], nea_t[:], -1.0, None, ALU.mult, None, (nea_b,), (nea_b,))

    def gates(src, n, dst):
        a_t, bt_t = cb_t[0:12, 0:n], cc_t[0:12, 0:n]
        P.dma("sp", a_t, src[0, :, :], (), (b_b,))
        P.dma("sp", bt_t, src[1, :, :], (), (c_b,))
        P.act(a_t, a_t, AF.Exp, (b_b, gpar_b), (b_b,), bias=gpar_t[:, 1:2])
        P.act(a_t, a_t, AF.Ln, (b_b,), (b_b,), bias=1.0)
        P.ts("dve", a_t, a_t, nea_t[:, 0:1], None, ALU.mult, None, (b_b, nea_b), (b_b,))
        P.act(bt_t, bt_t, AF.Sigmoid, (c_b,), (c_b,))
        P.dma("sp", dst[0, :, :], a_t, (b_b,), ())
        P.dma("sp", dst[1, :, :], bt_t, (c_b,), ())
    gates(gab_in, ntl, gb_out)
    gates(gabc_in, CTX, gbc_out)

    csk_t, csk_b = P.sb([64, 2, nk])
    P.dma("sp", csk_t[:], cs_k.rearrange("c d t -> d c t"), (), (csk_b,))
    qr_t, qr_b = P.sb([64, nqb, 2, 3, 128], BF16, "qr")
    kr_t, kr_b = P.sb([64, 2, nk], BF16, "kr")
    qa = [P.sb([64, nk]) for _ in range(2)]
    qp = [P.sb([64, nk]) for _ in range(2)]
    for h in range(6):
        a_t, a_b = qa[h % 2]
        p_t, p_b = qp[h % 2]
        P.dma("sp", a_t[:, 0:ntl], q_in[0, h, :, :], (), (a_b,))
        P.dma("sp", p_t[:, 0:ntl], q_in[1, h, :, :], (), (p_b,))
        P.tt("dve", a_t[:, 0:ntl], a_t[:, 0:ntl], csk_t[:, 0, 128:128 + ntl], ALU.mult, (a_b, csk_b), (a_b,))
        P.tt("pool", p_t[:, 0:ntl], p_t[:, 0:ntl], csk_t[:, 1, 128:128 + ntl], ALU.mult, (p_b, csk_b), (p_b,))
        P.tt("dve", qr_t[:, :, h // 3, h % 3, :], a_t[:, 0:ntl].rearrange("p (b q) -> p b q", q=128),
             p_t[:, 0:ntl].rearrange("p (b q) -> p b q", q=128), ALU.add, (a_b, p_b), (qr_b,))
    for g in range(2):
        a_t, a_b = qa[g]
        p_t, p_b = qp[g]
        P.dma("sp", a_t[:], k_in[0, g, :, :], (), (a_b,))
        P.dma("sp", p_t[:], k_in[1, g, :, :], (), (p_b,))
        P.tt("dve", a_t[:], a_t[:], csk_t[:, 0, :], ALU.mult, (a_b, csk_b), (a_b,))
        P.tt("pool", p_t[:], p_t[:], csk_t[:, 1, :], ALU.mult, (p_b, csk_b), (p_b,))
        P.tt("dve", kr_t[:, g, :], a_t[:], p_t[:], ALU.add, (a_b, p_b), (kr_b,))
    kcb_t, kcb_b = P.sb([64, 2, CTX], BF16)
    P.dma("pool", kcb_t[:], kc_in.rearrange("g d t -> d g t"), (), (kcb_b,))
    v_t, v_b = P.sb([128, nkb, 128], BF16, "v_sb")
    P.dma("pool", v_t[:], v_in[:, :, :], (), (v_b,))
    vc_t, vc_b = P.sb([128, 2, 128], BF16)
    P.dma("pool", vc_t[:], vc_in[:, :, :], (), (vc_b,))
    if need_ctx:
        qcb_t, qcb_b = P.sb([64, 2, 2, 3, 128], BF16)
        for h in range(6):
            P.dma("pool", qcb_t[:, :, h // 3, h % 3, :], qc_in[h, :, :].rearrange("d (b q) -> d b q", q=128),
                  (), (qcb_b,))

    ps_s = [P.psum([128, 512]) for _ in range(5)]
    ps_o = P.psum([128, 512])
    ps_d = P.psum([128, 512])
    pts = [P.sb([128, 384], BF16) for _ in range(5)]
    rd = [P.sb([64, 384]) for _ in range(2)]
    ob = [P.sb([64, 384]) for _ in range(2)]
    cnt = [0, 0]

    def attend(qsrc, qsrc_b, qb, g, keyspecs, dst, t0):
        pts_used = []
        for (k_ap, k_b, v_ap, v_bf, negidx) in keyspecs:
            i = cnt[0] % 5
            cnt[0] += 1
            s_t, s_b = ps_s[i]
            rhs = qsrc[:, qb, g, :, :].rearrange("d h q -> d (h q)")
            P.mm(s_t[:, 0:384], k_ap, rhs, True, negidx is None, (k_b, qsrc_b), (s_b,))
            if negidx is not None:
                P.mm(s_t[:, 0:384], ident_t[:], neg_t[:, negidx, :], False, True, (ident_b, neg_b), (s_b,))
            p_t, p_b = pts[i]
            P.act(p_t[:], s_t[:, 0:384], AF.Exp, (s_b,), (p_b,), scale=0.125)
            pts_used.append((p_t, p_b, v_ap, v_bf))
        n = len(pts_used)
        for idx, (p_t, p_b, v_ap, v_bf) in enumerate(pts_used):
            P.mm(ps_o[0][0:64, 0:384], v_ap, p_t[:], idx == 0, idx == n - 1, (v_bf, p_b), (ps_o[1],))
        for idx, (p_t, p_b, v_ap, v_bf) in enumerate(pts_used):
            P.mm(ps_d[0][0:64, 0:384], ones_t[:], p_t[:], idx == 0, False, (ones_b, p_b), (ps_d[1],))
        P.mm(ps_d[0][0:64, 0:384], onesf_t[:], esk_t[:, g, :], False, True, (onesf_b, esk_b), (ps_d[1],))
        j = cnt[1] % 2
        cnt[1] += 1
        r_t, r_b = rd[j]
        o_t, o_b = ob[j]
        P.recip(r_t[:], ps_d[0][0:64, 0:384], (ps_d[1],), (r_b,))
        P.tt("dve", o_t[:], ps_o[0][0:64, 0:384], r_t[:], ALU.mult, (ps_o[1], r_b), (o_b,))
        P.dma("sp", dst[3 * g:3 * g + 3, :, t0:t0 + 128].rearrange("h d q -> d h q"),
              o_t[:].rearrange("d (h q) -> d h q", h=3), (o_b,), ())

    for qb in range(nqb):
        for g in range(2):
            specs = []
            for off in range(3):
                kb = qb + off
                negidx = None
                if off == 0:
                    negidx = 0 if qb == 0 else 1
                elif off == 2:
                    negidx = 3 if qb == nqb - 1 else 2
                specs.append((kr_t[:, g, kb * 128:(kb + 1) * 128], kr_b, v_t[:, kb, g * 64:(g + 1) * 64], v_b, negidx))
            for cb in range(2):
                specs.append((kcb_t[:, g, cb * 128:(cb + 1) * 128], kcb_b, vc_t[:, cb, g * 64:(g + 1) * 64], vc_b, None))
            attend(qr_t, qr_b, qb, g, specs, yc, qb * 128)
    if need_ctx:
        for qb in range(2):
            for g in range(2):
                specs = [(kcb_t[:, g, cb * 128:(cb + 1) * 128], kcb_b, vc_t[:, cb, g * 64:(g + 1) * 64], vc_b, None)
                         for cb in range(2)]
                attend(qcb_t, qcb_b, qb, g, specs, yc_c, qb * 128)
    return P.finish()


R_CB, R_CC, R_CH = 0, 256, 512
R_DQ, R_DK, R_DV, R_DZ = 768, 1152, 1536, 1920
R_AQ, R_AK, R_AV = 2304, 2688, 2816
R_A, R_BT = 2944, 2956


def tri_masks():
    kk = np.arange(128)[:, None]
    qq = np.arange(128)[None, :]
    prev = np.where(qq <= kk, 0.0, NEGV)
    nxt = np.where(kk <= qq, 0.0, NEGV)
    return prev, nxt


def stage_C1(px, pc, conv_w_l, sink_l, dn_conv_w_l, dn_a_log_l, dn_dt_bias_l, need_ctx):
    ntl = SEQ // NCORES
    nc = build_C1(ntl, need_ctx)
    nkb = ntl // 128 + 2
    cw = c_(conv_w_l.T.reshape(2, 128, 3).transpose(1, 0, 2))
    prev, nxt = tri_masks()
    full = np.full((128, 128), NEGV)
    cos, sin = rope_tables(np.arange(SEQ))
    cosp = np.pad(cos, ((0, 0), (128, 128)))
    sinp = np.pad(sin, ((0, 0), (128, 128)))
    pad1 = lambda a: np.pad(a, ((0, 0), (1, 1)))
    convsrc = [pad1(px[R_CB:R_CB + 256]), pad1(px[R_CC:R_CC + 256]), pad1(px[R_CH:R_CH + 256])]
    kx = np.pad(px[R_AK:R_AK + 128], ((0, 0), (128, 128))).reshape(2, 64, SEQ + 256)
    kxp = kx[:, ROPE_PERM, :]
    vx = np.pad(px[R_AV:R_AV + 128], ((0, 0), (128, 128)))
    qx = px[R_AQ:R_AQ + 384].reshape(6, 64, SEQ)
    qxp = qx[:, ROPE_PERM, :]
    kc = c_(pc[R_AK:R_AK + 128].reshape(2, 64, CTX))
    vc = c_(pc[R_AV:R_AV + 128].T.reshape(2, 128, 128).transpose(1, 0, 2))
    sink = c_(np.repeat(sink_l.reshape(2, 3), 128, axis=1).reshape(1, 2, 384))
    ident = np.eye(128, dtype=np.float32)
    dnsrc = pad1(px[R_DQ:R_DQ + 1152])
    dnc = c_(pad1(pc[R_DQ:R_DQ + 1152]).reshape(9, 128, CTX + 2))
    dcw = c_(dn_conv_w_l.T.reshape(9, 128, 3).transpose(1, 0, 2))
    gpar = c_(np.stack([dn_a_log_l.reshape(12), dn_dt_bias_l.reshape(12)], axis=1))
    bd = c_(np.kron(np.eye(2), np.ones((64, 64))))
    gabc = c_(np.stack([pc[R_A:R_A + 12], pc[R_BT:R_BT + 12]]))
    ims = []
    for i in range(NCORES):
        t0, t1 = i * ntl, (i + 1) * ntl
        neg = np.stack([np.tile(full if i == 0 else prev, (1, 3)), np.tile(prev, (1, 3)),
                        np.tile(nxt, (1, 3)), np.tile(full if i == NCORES - 1 else nxt, (1, 3))], axis=1)
        im = {
            "cv": c_(np.stack([a[:, t0:t1 + 2].reshape(2, 128, ntl + 2) for a in convsrc])),
            "cw": cw,
            "q": c_(np.stack([qx[:, :, t0:t1], qxp[:, :, t0:t1]])),
            "k": c_(np.stack([kx[:, :, t0:t1 + 256], kxp[:, :, t0:t1 + 256]])),
            "v": c_(vx[:, t0:t1 + 256].T.reshape(nkb, 128, 128).transpose(1, 0, 2)),
            "cs_k": c_(np.stack([cosp[:, t0:t1 + 256], sinp[:, t0:t1 + 256]])),
            "kc": kc, "vc": vc, "neg": c_(neg), "sink": sink, "ident": ident,
            "dn": c_(dnsrc[:, t0:t1 + 2].reshape(9, 128, ntl + 2)), "dnc": dnc, "dcw": dcw,
            "gab": c_(np.stack([px[R_A:R_A + 12, t0:t1], px[R_BT:R_BT + 12, t0:t1]])), "gabc": gabc,
            "gpar": gpar, "bd": bd,
        }
        if need_ctx:
            im["cvc"] = c_(np.stack([pad1(pc[r:r + 256]).reshape(2, 128, CTX + 2) for r in (R_CB, R_CC, R_CH)]))
            im["qc"] = c_(pc[R_AQ:R_AQ + 384].reshape(6, 64, CTX))
        ims.append(im)
    res = run(nc, ims)
    yaT = np.concatenate([r["yaT"].reshape(256, ntl) for r in res], axis=1)
    ycT = np.concatenate([r["ycT"].reshape(384, ntl) for r in res], axis=1)
    out = {"yaT": yaT, "ycT": ycT}
    out["dn"] = np.concatenate([r["dno"].reshape(1152, ntl) for r in res], axis=1)
    out["dnc"] = res[0]["dnco"].reshape(1152, CTX)
    out["gb"] = np.concatenate([r["gbo"] for r in res], axis=2)
    out["gbc"] = res[0]["gbco"]
    if need_ctx:
        out["yaTc"] = res[0]["yaTc"].reshape(256, CTX)
        out["ycTc"] = res[0]["ycTc"].reshape(384, CTX)
    return out


def c2_consts():
    i = np.arange(128)
    same = (i[:, None] // 64) == (i[None, :] // 64)
    tri = (same & (i[:, None] <= i[None, :])).astype(np.float32)
    bd = same.astype(np.float32)
    negl = np.where(same & (i[:, None] >= i[None, :]), 0.0, NEGV).astype(np.float32)
    slm = (same & (i[:, None] > i[None, :])).astype(np.float32)
    ident = np.eye(128, dtype=np.float32)
    ones = np.ones((128, 128), np.float32)
    sel = np.zeros((128, 2, 64), np.float32)
    sel[0:64, 0, :] = 1.0
    sel[64:128, 1, :] = 1.0
    return {"tri": tri, "bd": bd, "negl": negl, "slm": slm, "ident": ident, "ones": ones,
            "sel": sel.reshape(128, 128)}


C2_STOP = 9


def build_C2(NP, mode="full"):
    T = NP * 128
    G = 5 if NP % 5 == 0 else (4 if NP % 4 == 0 else (2 if NP % 2 == 0 else 1))
    P = Prog()
    qk_in = P.dram_in("qk", [2, 2, 64, T])
    tok_in = P.dram_in("tok", [2, 128, NP, 192])
    gb_in = P.dram_in("gbt", [2, 128, 2, NP])
    if mode == "full":
        o_out = P.dram_out("oT", [2, 64, T])
    else:
        pa_out = P.dram_out("pa", [2, NP, 64, 384])
        pb_out = P.dram_out("pb", [2, NP, 128, 192])
    C = {}
    for nm in ("tri", "bd", "negl", "slm", "ident", "ones", "sel"):
        d_in = P.dram_in(nm, [128, 128])
        t, b = P.sb([128, 128], F32, "c_" + nm)
        P.dma("sp", t[:], d_in[:, :], (), (b,))
        C[nm] = (t, b)
    tri_t, tri_b = C["tri"]
    ident_t, ident_b = C["ident"]
    ones_t, ones_b = C["ones"]
    negl_t, negl_b = C["negl"]
    slm_t, slm_b = C["slm"]
    bd_t, bd_b = C["bd"]
    sel_t, sel_b = C["sel"]

    banks = [P.psum([128, 512]) for _ in range(8)]

    class U:
        pass
    units = []
    for u in range(2):
        S_ = U()
        S_.u = u
        bk = banks[4 * u:4 * u + 4]
        S_.kk = (bk[0][0], 0, bk[0][1])
        S_.qk = (bk[0][0], 128, bk[0][1])
        S_.m = (bk[0][0], 256, bk[0][1])
        S_.n = (bk[0][0], 384, bk[0][1])
        S_.ng = (bk[1][0], 0, bk[1][1])
        S_.tr = (bk[1][0], 128, bk[1][1])
        S_.tr2 = (bk[1][0], 256, bk[1][1])
        S_.qe = (bk[1][0], 384, bk[1][1])
        S_.sqa = (bk[2][0], 0, bk[2][1])
        S_.sqb = (bk[2][0], 128, bk[2][1])
        S_.ap = (bk[2][0], 256, bk[2][1])
        S_.s = (bk[2][0], 384, bk[2][1])
        S_.o = [(bk[3][0], 0, bk[3][1]), (bk[3][0], 128, bk[3][1])]
        if mode == "pre":
            S_.ap = (bk[1][0], 384, bk[1][1])
            S_.qe = (bk[3][0], 256, bk[3][1])
            S_.sqa = (bk[3][0], 0, bk[3][1])
            S_.sqb = (bk[2][0], 128, bk[2][1])
            S_.n = (bk[2][0], 256, bk[2][1])
        S_.pre = bk[3]
        S_.gb = P.sb([128, 2, NP])
        S_.ng_sb = P.sb([128, NP])
        S_.nb = P.sb([128, NP])
        S_.gc = P.sb([128, NP])
        S_.e = P.sb([128, NP])
        S_.kdf = P.sb([128, NP])
        S_.be = P.sb([128, NP])
        S_.GL = P.sb([64, 2, NP])
        S_.qkin = [P.sb([64, 2, G * 128]) for _ in range(2)]
        S_.tokin = [P.sb([128, G, 192]) for _ in range(2)]
        S_.R = [P.sb([128, 128]) for _ in range(2)]
        S_.Dm = [P.sb([128, 128]) for _ in range(2)]
        S_.DmT = [P.sb([128, 128]) for _ in range(2)]
        S_.DmS = [P.sb([128, 128]) for _ in range(2)]
        S_.pw = [P.sb([128, 128]) for _ in range(4)]
        S_.qkT = [P.sb([128, 128]) for _ in range(2)]
        S_.X = [P.sb([128, 128]) for _ in range(3)]
        S_.kd = [P.sb([128, 2, 64]) for _ in range(2)]
        S_.kdfm = P.sb([128, 2, NP])
        S_.nw = [P.sb([128, 64]) for _ in range(2)]
        S_.McT = [P.sb([64, 2, 64]) for _ in range(2)]
        S_.Nsb = [P.sb([64, 128]) for _ in range(2)]
        S_.dE = [P.sb([128, 128]) for _ in range(2)]
        S_.qe_sb = [P.sb([64, 128]) for _ in range(2)]
        S_.S = [P.sb([64, 64]) for _ in range(3)]
        S_.osb = [P.sb([64, 128]) for _ in range(2)]
        S_.si = 0
        S_.xi = 0
        units.append(S_)

    def ps(slot, rows=128, cols=128):
        t, c0, b = slot
        return t[0:rows, c0:c0 + cols], b

    for S_ in units:
        u = S_.u
        gb_t, gb_b = S_.gb
        P.dma("sp", gb_t[:], gb_in[u, :, :, :], (), (gb_b,))
        g_ap, beta_ap = gb_t[:, 0, :], gb_t[:, 1, :]
        P.ts("dve", S_.ng_sb[0][:], g_ap, -1.0, None, ALU.mult, None, (gb_b,), (S_.ng_sb[1],))
        P.ts("dve", S_.nb[0][:], beta_ap, -1.0, None, ALU.mult, None, (gb_b,), (S_.nb[1],))
        pre_t, pre_b = S_.pre
        P.mm(pre_t[:, 0:NP], tri_t[:], g_ap, True, True, (tri_b, gb_b), (pre_b,))
        P.copy("act", S_.gc[0][:], pre_t[:, 0:NP], (pre_b,), (S_.gc[1],))
        P.act(S_.e[0][:], S_.gc[0][:], AF.Exp, (S_.gc[1],), (S_.e[1],))
        P.tt("dve", S_.be[0][:], S_.e[0][:], beta_ap, ALU.mult, (S_.e[1], gb_b), (S_.be[1],))
        P.mm(pre_t[:, 0:NP], bd_t[:], g_ap, True, True, (bd_b, gb_b), (pre_b,))
        P.tt("dve", S_.kdf[0][:], pre_t[:, 0:NP], S_.gc[0][:], ALU.subtract, (pre_b, S_.gc[1]), (S_.kdf[1],))
        P.act(S_.kdf[0][:], S_.kdf[0][:], AF.Exp, (S_.kdf[1],), (S_.kdf[1],))
        for c in range(2):
            P.ts("dve", S_.kdfm[0][:, c, :], S_.kdf[0][:], sel_t[:, c * 64:c * 64 + 1], None, ALU.mult, None,
                 (S_.kdf[1], sel_b), (S_.kdfm[1],))
        for c in range(2):
            P.mm(pre_t[0:64, 0:NP], sel_t[:, c * 64:(c + 1) * 64], g_ap, True, True, (sel_b, gb_b), (pre_b,))
            P.act(S_.GL[0][:, c, :], pre_t[0:64, 0:NP], AF.Exp, (pre_b,), (S_.GL[1],))
        P.memset("dve", S_.S[0][0][:], 0.0, (S_.S[0][1],))

    def load_group(S_, gi):
        u = S_.u
        qk_t, qk_b = S_.qkin[gi % 2]
        tk_t, tk_b = S_.tokin[gi % 2]
        if gi * G >= NP:
            return
        P.dma("pool", qk_t[:], qk_in[u, :, :, gi * G * 128:(gi + 1) * G * 128].rearrange("a d t -> d a t"), (), (qk_b,))
        P.dma("pool", tk_t[:], tok_in[u, :, gi * G:(gi + 1) * G, :], (), (tk_b,))

    def pre(S_, p):
        gi, pi = p // G, p % G
        qk_t, qk_b = S_.qkin[gi % 2]
        tk_t, tk_b = S_.tokin[gi % 2]
        qT = qk_t[:, 0, pi * 128:(pi + 1) * 128]
        kT = qk_t[:, 1, pi * 128:(pi + 1) * 128]
        Qt, Kt, Vt = tk_t[:, pi, 0:64], tk_t[:, pi, 64:128], tk_t[:, pi, 128:192]
        gb_t, gb_b = S_.gb
        j = p % 2
        kk_ap, kk_b = ps(S_.kk)
        qk_ap, qkp_b = ps(S_.qk)
        P.mm(kk_ap, kT, kT, True, True, (qk_b,), (kk_b,))
        yield
        P.mm(qk_ap, kT, qT, True, True, (qk_b,), (qkp_b,))
        yield
        R_t, R_b = S_.R[j]
        P.ts("pool", R_t[:], tri_t[:], S_.ng_sb[0][:, p:p + 1], None, ALU.mult, None, (tri_b, S_.ng_sb[1]), (R_b,))
        yield
        ng_ap, ng_b = ps(S_.ng)
        P.mm(ng_ap, ones_t[:], R_t[:], True, False, (ones_b, R_b), (ng_b,))
        P.mm(ng_ap, ident_t[:], negl_t[:], False, True, (ident_b, negl_b), (ng_b,))
        yield
        Dm_t, Dm_b = S_.Dm[j]
        P.act(Dm_t[:], ng_ap, AF.Exp, (ng_b, S_.gc[1]), (Dm_b,), bias=S_.gc[0][:, p:p + 1])
        yield
        tr_ap, tr_b = ps(S_.tr)
        P.transpose(tr_ap, Dm_t[:], ident_t[:], (Dm_b, ident_b), (tr_b,))
        yield
        DmT_t, DmT_b = S_.DmT[j]
        P.copy("act", DmT_t[:], tr_ap, (tr_b,), (DmT_b,))
        yield
        DmS_t, DmS_b = S_.DmS[j]
        P.tt("pool", DmS_t[:], Dm_t[:], slm_t[:], ALU.mult, (Dm_b, slm_b), (DmS_b,))
        yield
        cur_t, cur_b = S_.pw[0]
        curT_t, curT_b = S_.pw[1]
        P.stt("dve", cur_t[:], kk_ap, S_.nb[0][:, p:p + 1], DmS_t[:], ALU.mult, ALU.mult,
              (kk_b, S_.nb[1], DmS_b), (cur_b,))
        yield
        tr2_ap, tr2_b = ps(S_.tr2)
        P.transpose(tr2_ap, cur_t[:], ident_t[:], (cur_b, ident_b), (tr2_b,))
        yield
        P.copy("act", curT_t[:], tr2_ap, (tr2_b,), (curT_b,))
        yield
        qkT_t, qkT_b = S_.qkT[j]
        P.tt("dve", qkT_t[:], qk_ap, DmT_t[:], ALU.mult, (qkp_b, DmT_b), (qkT_b,))
        yield
        X_t, X_b = S_.X[S_.xi % 3]
        S_.xi += 1
        P.act(X_t[:, 0:64], Kt, AF.Copy, (tk_b, S_.be[1]), (X_b,), scale=S_.be[0][:, p:p + 1])
        yield
        P.act(X_t[:, 64:128], Vt, AF.Copy, (tk_b, gb_b), (X_b,), scale=gb_t[:, 1, p:p + 1])
        yield
        pwi = 0
        for lvl in range(6):
            if lvl < 5:
                nxt_t, nxt_b = S_.pw[(pwi + 2) % 4]
                nxtT_t, nxtT_b = S_.pw[(pwi + 3) % 4]
                sb_ap, sb_b = ps(S_.sqb)
                P.mm(sb_ap, cur_t[:], curT_t[:], True, True, (cur_b, curT_b), (sb_b,))
                yield
                P.copy("act", nxtT_t[:], sb_ap, (sb_b,), (nxtT_b,))
                yield
                if lvl < 4:
                    sa_ap, sa_b = ps(S_.sqa)
                    P.mm(sa_ap, curT_t[:], cur_t[:], True, True, (curT_b, cur_b), (sa_b,))
                    yield
                    P.copy("act", nxt_t[:], sa_ap, (sa_b,), (nxt_b,))
                    yield
            ap_ap, ap_b = ps(S_.ap)
            P.mm(ap_ap, curT_t[:], X_t[:], True, True, (curT_b, X_b), (ap_b,))
            yield
            Xn_t, Xn_b = S_.X[S_.xi % 3]
            S_.xi += 1
            P.tt("dve", Xn_t[:], ap_ap, X_t[:], ALU.add, (ap_b, X_b), (Xn_b,))
            yield
            X_t, X_b = Xn_t, Xn_b
            if lvl < 5:
                cur_t, cur_b, curT_t, curT_b = nxt_t, nxt_b, nxtT_t, nxtT_b
                pwi = (pwi + 2) % 4
        kd_t, kd_b = S_.kd[j]
        nw_t, nw_b = S_.nw[j]
        for c in range(2):
            P.ts("pool", kd_t[:, c, :], Kt, S_.kdfm[0][:, c, p:p + 1], None, ALU.mult, None, (tk_b, S_.kdfm[1]), (kd_b,))
            yield
        P.ts("dve", nw_t[:], X_t[:, 0:64], -1.0, None, ALU.mult, None, (X_b,), (nw_b,))
        yield
        m_t, m0, m_b = S_.m
        n_t, n0, n_b = S_.n
        for c in range(2):
            r0, r1 = c * 64, (c + 1) * 64
            P.mm(m_t[0:64, m0 + r0:m0 + r1], nw_t[:], kd_t[:, c, :], True, True, (nw_b, kd_b), (m_b,))
            yield
            P.mm(n_t[0:64, n0 + r0:n0 + r1], kd_t[:, c, :], X_t[:, 64:128], True, True, (kd_b, X_b), (n_b,))
            yield
        McT_t, McT_b = S_.McT[j]
        for c in range(2):
            P.stt("dve", McT_t[:, c, :], ident_t[0:64, 0:64], S_.GL[0][:, c, p:p + 1],
                  m_t[0:64, m0 + c * 64:m0 + (c + 1) * 64], ALU.mult, ALU.add, (ident_b, S_.GL[1], m_b), (McT_b,))
            yield
        Nsb_t, Nsb_b = S_.Nsb[j]
        P.copy("act", Nsb_t[:], n_t[0:64, n0:n0 + 128], (n_b,), (Nsb_b,))
        yield
        dE_t, dE_b = S_.dE[j]
        P.ts("dve", dE_t[:], ident_t[:], S_.e[0][:, p:p + 1], None, ALU.mult, None, (ident_b, S_.e[1]), (dE_b,))
        yield
        qe_ap, qe_b = ps(S_.qe, 64, 128)
        P.mm(qe_ap, Qt, dE_t[:], True, False, (tk_b, dE_b), (qe_b,))
        P.mm(qe_ap, nw_t[:], qkT_t[:], False, True, (nw_b, qkT_b), (qe_b,))
        yield
        qes_t, qes_b = S_.qe_sb[j]
        P.copy("act", qes_t[:], qe_ap, (qe_b,), (qes_b,))
        yield
        S_.cur = dict(X=(X_t, X_b), qkT=(qkT_t, qkT_b), McT=(McT_t, McT_b), Nsb=(Nsb_t, Nsb_b), qe=(qes_t, qes_b))
        if mode == "pre":
            u_ = S_.u
            P.dma("sp", pa_out[u_, p, :, 0:128], McT_t[:].rearrange("d c k -> d (c k)"), (McT_b,), ())
            P.dma("sp", pa_out[u_, p, :, 128:256], Nsb_t[:], (Nsb_b,), ())
            P.dma("sp", pa_out[u_, p, :, 256:384], qes_t[:], (qes_b,), ())
            P.dma("sp", pb_out[u_, p, :, 0:128], qkT_t[:], (qkT_b,), ())
            P.dma("sp", pb_out[u_, p, :, 128:192], X_t[:, 64:128], (X_b,), ())

    def scan(S_, p, cur):
        X_t, X_b = cur["X"]
        qkT_t, qkT_b = cur["qkT"]
        McT_t, McT_b = cur["McT"]
        Nsb_t, Nsb_b = cur["Nsb"]
        qes_t, qes_b = cur["qe"]
        o_ap, o_b = ps(S_.o[p % 2], 64, 128)
        ot, oc0, _ = S_.o[p % 2]
        P.mm(o_ap, X_t[:, 64:128], qkT_t[:], True, False, (X_b, qkT_b), (o_b,))
        yield
        s_ap, s_b = ps(S_.s, 64, 64)
        for c in range(2):
            St, Sb = S_.S[S_.si % 3]
            P.mm(ot[0:64, oc0 + c * 64:oc0 + (c + 1) * 64], St[:], qes_t[:, c * 64:(c + 1) * 64], False, c == 1,
                 (Sb, qes_b), (o_b,))
            yield
            P.mm(s_ap, McT_t[:, c, :], St[:], True, False, (McT_b, Sb), (s_b,))
            P.mm(s_ap, ident_t[0:64, 0:64], Nsb_t[:, c * 64:(c + 1) * 64], False, True, (ident_b, Nsb_b), (s_b,))
            yield
            S_.si += 1
            Sn, Snb = S_.S[S_.si % 3]
            P.copy("act", Sn[:], s_ap, (s_b,), (Snb,))
            yield
        os_t, os_b = S_.osb[p % 2]
        P.copy("dve", os_t[:], o_ap, (o_b,), (os_b,))
        yield
        P.dma("sp", o_out[S_.u, :, p * 128:(p + 1) * 128], os_t[:], (os_b,), ())

    from itertools import zip_longest
    saved = [None, None]
    for p in range(NP + 1):
        gens = []
        if p < NP:
            for S_ in units:
                if p == 0:
                    load_group(S_, 0)
                if p % G == 0:
                    load_group(S_, p // G + 1)
                gens.append(pre(S_, p))
        if p >= 1 and mode == "full":
            for S_ in units:
                gens.append(scan(S_, p - 1, saved[S_.u]))
        for _ in zip_longest(*gens):
            pass
        saved = [S_.cur for S_ in units]
    return P.finish()


def build_C2b(NP):
    T = NP * 128
    G = 5 if NP % 5 == 0 else (2 if NP % 2 == 0 else 1)
    P = Prog()
    pa_in = P.dram_in("pa", [2, NP, 64, 384])
    pb_in = P.dram_in("pb", [2, NP, 128, 192])
    id_in = P.dram_in("ident", [128, 128])
    o_out = P.dram_out("oT", [2, 64, T])
    ident_t, ident_b = P.sb([128, 128])
    P.dma("sp", ident_t[:], id_in[:, :], (), (ident_b,))
    banks = [P.psum([128, 512]) for _ in range(4)]
    st = []
    for u in range(2):
        d = dict(u=u, o=banks[2 * u], s=banks[2 * u + 1],
                 pa=[P.sb([64, G, 384]) for _ in range(2)], pb=[P.sb([128, G, 192]) for _ in range(2)],
                 S=[P.sb([64, 64]) for _ in range(3)], osb=[P.sb([64, 128]) for _ in range(2)], si=0)
        P.memset("dve", d["S"][0][0][:], 0.0, (d["S"][0][1],))
        st.append(d)

    def scan(d, p):
        gi, pi = p // G, p % G
        pa_t, pa_b = d["pa"][gi % 2]
        pb_t, pb_b = d["pb"][gi % 2]
        if pi == 0:
            for g2 in ([0, 1] if gi == 0 else [gi + 1]):
                if g2 * G < NP:
                    a2_t, a2_b = d["pa"][g2 % 2]
                    b2_t, b2_b = d["pb"][g2 % 2]
                    P.dma("pool", a2_t[:], pa_in[d["u"], g2 * G:(g2 + 1) * G, :, :].rearrange("g d c -> d g c"), (), (a2_b,))
                    P.dma("pool", b2_t[:], pb_in[d["u"], g2 * G:(g2 + 1) * G, :, :].rearrange("g j c -> j g c"), (), (b2_b,))
        ot, o_b = d["o"]
        oc0 = (p % 2) * 128
        P.mm(ot[0:64, oc0:oc0 + 128], pb_t[:, pi, 128:192], pb_t[:, pi, 0:128], True, False, (pb_b,), (o_b,))
        yield
        s_t, s_b = d["s"]
        for c in range(2):
            St, Sb = d["S"][d["si"] % 3]
            P.mm(ot[0:64, oc0 + c * 64:oc0 + (c + 1) * 64], St[:], pa_t[:, pi, 256 + c * 64:256 + (c + 1) * 64],
                 False, c == 1, (Sb, pa_b), (o_b,))
            yield
            P.mm(s_t[0:64, 0:64], pa_t[:, pi, c * 64:(c + 1) * 64], St[:], True, False, (pa_b, Sb), (s_b,))
            P.mm(s_t[0:64, 0:64], ident_t[0:64, 0:64], pa_t[:, pi, 128 + c * 64:128 + (c + 1) * 64], False, True,
                 (ident_b, pa_b), (s_b,))
            yield
            d["si"] += 1
            Sn, Snb = d["S"][d["si"] % 3]
            P.copy("act" if d["u"] == 0 else "dve", Sn[:], s_t[0:64, 0:64], (s_b,), (Snb,))
            yield
        os_t, os_b = d["osb"][p % 2]
        P.copy("dve" if d["u"] == 0 else "act", os_t[:], ot[0:64, oc0:oc0 + 128], (o_b,), (os_b,))
        yield
        P.dma("sp", o_out[d["u"], :, p * 128:(p + 1) * 128], os_t[:], (os_b,), ())

    from itertools import zip_longest
    for p in range(NP):
        for _ in zip_longest(*[scan(d, p) for d in st]):
            pass
    return P.finish()


def c2_unit_inputs(q, k, v, g, beta):
    T = q.shape[1]
    NP = T // 128
    qk = np.stack([q, k])
    tok = np.concatenate([q.T, k.T, v.T], axis=1).reshape(NP, 128, 192).transpose(1, 0, 2)
    gbt = np.stack([g.reshape(NP, 128).T, beta.reshape(NP, 128).T], axis=1)
    return c_(qk), c_(tok), c_(gbt)


def run_C2(unit_list):
    nu = len(unit_list)
    T = unit_list[0][0].shape[1]
    NPU = T // 128
    total = nu * NPU
    NS = 2 * NCORES
    per = -(-total // NS)
    padc = NS * per * 128 - total * 128
    cat2 = lambda idx: np.pad(np.concatenate([u_[idx] for u_ in unit_list], axis=1), ((0, 0), (0, padc)))
    cat1 = lambda idx: np.pad(np.concatenate([u_[idx] for u_ in unit_list]), (0, padc))
    Q, K_, V, Gg, Bb = cat2(0), cat2(1), cat2(2), cat1(3), cat1(4)
    consts = c2_consts()
    nc = build_C2(per, mode="pre")
    ims = []
    for i in range(NCORES):
        parts = []
        for j in range(2):
            s0 = (2 * i + j) * per * 128
            sl = slice(s0, s0 + per * 128)
            parts.append(c2_unit_inputs(Q[:, sl], K_[:, sl], V[:, sl], Gg[sl], Bb[sl]))
        im = {"qk": np.stack([p_[0] for p_ in parts]), "tok": np.stack([p_[1] for p_ in parts]),
              "gbt": np.stack([p_[2] for p_ in parts])}
        im.update(consts)
        ims.append(im)
    res = run(nc, ims)
    pa = np.concatenate([r["pa"].reshape(2 * per, 64, 384) for r in res], axis=0)
    pb = np.concatenate([r["pb"].reshape(2 * per, 128, 192) for r in res], axis=0)
    nc2 = build_C2b(NPU)
    ident = np.eye(128, dtype=np.float32)
    za, zb = np.zeros((NPU, 64, 384), np.float32), np.zeros((NPU, 128, 192), np.float32)
    ims = []
    for i in range(NCORES):
        ua = [pa[(2 * i + j) * NPU:(2 * i + j + 1) * NPU] if 2 * i + j < nu else za for j in range(2)]
        ub = [pb[(2 * i + j) * NPU:(2 * i + j + 1) * NPU] if 2 * i + j < nu else zb for j in range(2)]
        ims.append({"pa": c_(np.stack(ua)), "pb": c_(np.stack(ub)), "ident": ident})
    res = run(nc2, ims)
    return [res[idx // 2]["oT"][idx % 2] for idx in range(nu)]


def tok_blocks(ntl, ntc, bw=512):
    blks = [(t0, min(bw, ntl - t0), 0) for t0 in range(0, ntl, bw)]
    if ntc:
        blks += [(ntl + t0, min(bw, ntc - t0), 1) for t0 in range(0, ntc, bw)]
    return blks


def build_E1(ntl, ntc):
    nt = ntl + ntc
    P = Prog()
    xT = P.dram_in("xT", [8, 128, nt])
    ya = P.dram_in("ya", [2, 128, nt])
    yc = P.dram_in("yc", [3, 128, nt])
    of = P.dram_in("of", [3, 128, nt])
    ob = P.dram_in("ob", [3, 128, nt])
    z = P.dram_in("z", [3, 128, nt])
    w = P.dram_in("w", [1024, 1024])
    mod2 = P.dram_in("mod2", [128, 2, 8])
    dng = P.dram_in("dng", [128, 1])
    bd_in = P.dram_in("bd", [128, 128])
    out = P.dram_out("x1T", [8, 128, nt])
    w_t, w_b = P.sb([128, 8, 1024], BF16, "wout")
    for mc in range(8):
        P.dma("pool", w_t[:, mc, :], w[mc * 128:(mc + 1) * 128, :], (), (w_b,))
    m2_t, m2_b = P.sb([128, 2, 8])
    P.dma("sp", m2_t[:], mod2[:, :, :], (), (m2_b,))
    g_t, g_b = P.sb([128, 1])
    P.dma("sp", g_t[:], dng[:, :], (), (g_b,))
    bd_t, bd_b = P.sb([128, 128])
    P.dma("sp", bd_t[:], bd_in[:, :], (), (bd_b,))
    mixs = [P.sb([128, 8, 512], BF16) for _ in range(2)]
    xs = [P.sb([128, 8, 512]) for _ in range(2)]
    ofs = [P.sb([128, 512]) for _ in range(2)]
    obs = [P.sb([128, 512]) for _ in range(2)]
    zs = [P.sb([128, 512]) for _ in range(2)]
    sqs = [P.sb([128, 512]) for _ in range(2)]
    rs = [P.sb([128, 512]) for _ in range(2)]
    pn = [P.psum([128, 512]) for _ in range(2)]
    pp = [P.psum([128, 512]) for _ in range(4)]
    k = 0
    for bi, (t0, wd_, j) in enumerate(tok_blocks(ntl, ntc)):
        mix_t, mix_b = mixs[bi % 2]
        x_t, x_b = xs[bi % 2]
        P.dma("pool", mix_t[:, 0:2, 0:wd_], ya[:, :, t0:t0 + wd_].rearrange("c p t -> p c t"), (), (mix_b,))
        P.dma("pool", mix_t[:, 5:8, 0:wd_], yc[:, :, t0:t0 + wd_].rearrange("c p t -> p c t"), (), (mix_b,))
        P.dma("sp", x_t[:, :, 0:wd_], xT[:, :, t0:t0 + wd_].rearrange("c p t -> p c t"), (), (x_b,))
        for ch in range(3):
            o_t, o_b = ofs[k % 2]
            b_t, b_b = obs[k % 2]
            z_t, z_b = zs[k % 2]
            s_t, s_b = sqs[k % 2]
            r_t, r_b = rs[k % 2]
            ps_t, ps_b = pn[k % 2]
            k += 1
            P.dma("sp", o_t[:, 0:wd_], of[ch, :, t0:t0 + wd_], (), (o_b,))
            P.dma("sp", b_t[:, 0:wd_], ob[ch, :, t0:t0 + wd_], (), (b_b,))
            P.dma("sp", z_t[:, 0:wd_], z[ch, :, t0:t0 + wd_], (), (z_b,))
            P.tt("pool", o_t[:, 0:wd_], o_t[:, 0:wd_], b_t[:, 0:wd_], ALU.add, (o_b, b_b), (o_b,))
            P.act(s_t[:, 0:wd_], o_t[:, 0:wd_], AF.Square, (o_b,), (s_b,))
            P.mm(ps_t[:, 0:wd_], bd_t[:], s_t[:, 0:wd_], True, True, (bd_b, s_b), (ps_b,))
            P.act(r_t[:, 0:wd_], ps_t[:, 0:wd_], AF.Sqrt, (ps_b,), (r_b,), bias=EPS, scale=1.0 / 64)
            P.recip(r_t[:, 0:wd_], r_t[:, 0:wd_], (r_b,), (r_b,))
            P.act(z_t[:, 0:wd_], z_t[:, 0:wd_], AF.Silu, (z_b,), (z_b,))
            P.stt("dve", o_t[:, 0:wd_], o_t[:, 0:wd_], g_t[:, 0:1], r_t[:, 0:wd_], ALU.mult, ALU.mult,
                  (o_b, g_b, r_b), (o_b,))
            P.tt("dve", mix_t[:, 2 + ch, 0:wd_], o_t[:, 0:wd_], z_t[:, 0:wd_], ALU.mult, (o_b, z_b), (mix_b,))
        for dc in range(8):
            ps_t, ps_b = pp[dc % 4]
            for mc in range(8):
                P.mm(ps_t[:, 0:wd_], w_t[:, mc, dc * 128:(dc + 1) * 128], mix_t[:, mc, 0:wd_], mc == 0, mc == 7,
                     (w_b, mix_b), (ps_b,))
            P.stt("dve", x_t[:, dc, 0:wd_], ps_t[:, 0:wd_], m2_t[:, j, dc:dc + 1], x_t[:, dc, 0:wd_], ALU.mult, ALU.add,
                  (ps_b, m2_b, x_b), (x_b,))
        P.dma("sp", out[:, :, t0:t0 + wd_].rearrange("c p t -> p c t"), x_t[:, :, 0:wd_], (x_b,), ())
    return P.finish()


def build_E2(ntl, ntc, E, F, final):
    nt = ntl + ntc
    ntile = nt // 128
    FG = 256
    nfg = F // FG
    P = Prog()
    x1T = P.dram_in("x1T", [8, 128, nt])
    x1 = P.dram_in("x1", [nt, 1024])
    mod = P.dram_in("mod", [128, 2, 2, 8])
    m5 = P.dram_in("m5", [128, 2, 1024])
    wg = P.dram_in("wg", [E, 1024, F])
    wu = P.dram_in("wu", [E, 1024, F])
    wd = P.dram_in("wd", [E, F, 1024])
    out = P.dram_out("x2", [nt, 1024])
    if E > 1:
        wr = P.dram_in("wr", [128, 8, E])
    if final:
        gn = P.dram_in("gn", [128, 1024])
    mod_t, mod_b = P.sb([128, 2, 2, 8])
    P.dma("sp", mod_t[:], mod[:, :, :, :], (), (mod_b,))
    a_t, a_b = P.sb([128, 2, 8])
    P.ts("dve", a_t[:], mod_t[:, :, 1, :], 1.0, None, ALU.add, None, (mod_b,), (a_b,))
    ones_t, ones_b = P.sb([128, 128], BF16)
    P.memset("dve", ones_t[:], 1.0, (ones_b,))
    hx_t, hx_b = P.sb([128, 8, nt], BF16, "hx")
    acc_t, acc_b = P.sb([128, ntile, 1024], F32, "acc")
    P.memset("dve", acc_t[:], 0.0, (acc_b,))
    if E > 1:
        wr_t, wr_b = P.sb([128, 8, E])
        P.dma("sp", wr_t[:], wr[:, :, :], (), (wr_b,))
        lg_t, lg_b = P.sb([128, ntile, E])
        gate_t, gate_b = P.sb([128, ntile, E])
    xs = [P.sb([128, 8, TT]) for _ in range(2)]
    sqs = [P.sb([128, 8, TT], BF16) for _ in range(2)]
    hfs = [P.sb([128, 8, TT]) for _ in range(2)]
    rs = [P.sb([128, TT]) for _ in range(2)]
    pn = P.psum([128, 512])
    pr = P.psum([128, 512])
    for it, (t0, wd_, j) in enumerate(tok_blocks(ntl, ntc, TT)):
        x_t, x_b = xs[it % 2]
        sq_t, sq_b = sqs[it % 2]
        hf_t, hf_b = hfs[it % 2]
        r_t, r_b = rs[it % 2]
        P.dma("sp", x_t[:], x1T[:, :, t0:t0 + TT].rearrange("c p t -> p c t"), (), (x_b,))
        P.act(sq_t[:], x_t[:], AF.Square, (x_b,), (sq_b,))
        for kc in range(8):
            P.mm(pn[0][:, 0:TT], ones_t[:], sq_t[:, kc, :], kc == 0, kc == 7, (ones_b, sq_b), (pn[1],))
        P.act(r_t[:], pn[0][:, 0:TT], AF.Sqrt, (pn[1],), (r_b,), bias=EPS, scale=1.0 / 1024)
        P.recip(r_t[:], r_t[:], (r_b,), (r_b,))
        for kc in range(8):
            P.stt("dve", hf_t[:, kc, :], x_t[:, kc, :], a_t[:, j, kc:kc + 1], r_t[:], ALU.mult, ALU.mult,
                  (x_b, a_b, r_b), (hf_b,))
            P.act(hf_t[:, kc, :], hf_t[:, kc, :], AF.Identity, (hf_b, mod_b), (hf_b,), bias=mod_t[:, j, 0, kc:kc + 1])
        P.copy("pool", hx_t[:, :, t0:t0 + TT], hf_t[:], (hf_b,), (hx_b,))
        if E > 1:
            for sub in range(TT // 128):
                tile = t0 // 128 + sub
                for kc in range(8):
                    P.mm(pr[0][:, 0:E], hf_t[:, kc, sub * 128:(sub + 1) * 128], wr_t[:, kc, :], kc == 0, kc == 7,
                         (hf_b, wr_b), (pr[1],))
                P.copy("act", lg_t[:, tile, :], pr[0][:, 0:E], (pr[1],), (lg_b,))
    if E > 1:
        m1_t, m1_b = P.sb([128, 1])
        m2_t, m2_b = P.sb([128, 1])
        e1_t, e1_b = P.sb([128, E])
        e2_t, e2_b = P.sb([128, E])
        l2_t, l2_b = P.sb([128, E])
        g1_t, g1_b = P.sb([128, 1])
        g2_t, g2_b = P.sb([128, 1])
        for tile in range(ntile):
            l_ap = lg_t[:, tile, :]
            P.op("dve", lambda e, o=m1_t[:], i=l_ap: e.reduce_max(o, i, axis=mybir.AxisListType.X), (lg_b,), (m1_b,))
            P.ts("dve", e1_t[:], l_ap, m1_t[:, 0:1], None, ALU.is_equal, None, (lg_b, m1_b), (e1_b,))
            P.stt("dve", l2_t[:], e1_t[:], -1e9, l_ap, ALU.mult, ALU.add, (e1_b, lg_b), (l2_b,))
            P.op("dve", lambda e, o=m2_t[:], i=l2_t[:]: e.reduce_max(o, i, axis=mybir.AxisListType.X), (l2_b,), (m2_b,))
            P.ts("dve", e2_t[:], l2_t[:], m2_t[:, 0:1], None, ALU.is_equal, None, (l2_b, m2_b), (e2_b,))
            P.tt("dve", g1_t[:], m2_t[:], m1_t[:], ALU.subtract, (m2_b, m1_b), (g1_b,))
            P.act(g1_t[:], g1_t[:], AF.Exp, (g1_b,), (g1_b,))
            P.ts("dve", g1_t[:], g1_t[:], 1.0, None, ALU.add, None, (g1_b,), (g1_b,))
            P.recip(g1_t[:], g1_t[:], (g1_b,), (g1_b,))
            P.ts("dve", g2_t[:], g1_t[:], -1.0, 1.0, ALU.mult, ALU.add, (g1_b,), (g2_b,))
            P.ts("dve", e1_t[:], e1_t[:], g1_t[:, 0:1], None, ALU.mult, None, (e1_b, g1_b), (e1_b,))
            P.stt("dve", gate_t[:, tile, :], e2_t[:], g2_t[:, 0:1], e1_t[:], ALU.mult, ALU.add,
                  (e2_b, g2_b, e1_b), (gate_b,))
    wgs = [P.sb([128, 8, FG], BF16) for _ in range(2)]
    wus = [P.sb([128, 8, FG], BF16) for _ in range(2)]
    wds = [P.sb([128, 2, 1024], BF16) for _ in range(2)]
    pg = [P.psum([128, 512]) for _ in range(2)]
    pu = [P.psum([128, 512]) for _ in range(2)]
    pd = [P.psum([128, 512]) for _ in range(2)]
    sgs = [P.sb([128, 512], BF16) for _ in range(2)]
    Hs = [P.sb([128, 2, 512], BF16) for _ in range(2)]
    blocks = tok_blocks(ntl, ntc)
    jobs = [(e_, fg, bi) for e_ in range(E) for fg in range(nfg) for bi in range(len(blocks))]
    nd = [0]

    def load_w(it):
        if it >= E * nfg:
            return
        e_, fg = it // nfg, it % nfg
        f0 = fg * FG
        wg_t, wg_b = wgs[it % 2]
        wu_t, wu_b = wus[it % 2]
        wd_t, wd_b = wds[it % 2]
        P.dma("pool", wg_t[:], wg[e_, :, f0:f0 + FG].rearrange("(kc p) f -> p kc f", p=128), (), (wg_b,))
        P.dma("pool", wu_t[:], wu[e_, :, f0:f0 + FG].rearrange("(kc p) f -> p kc f", p=128), (), (wu_b,))
        P.dma("pool", wd_t[:], wd[e_, f0:f0 + FG, :].rearrange("(fc p) d -> p fc d", p=128), (), (wd_b,))

    def GU(k):
        e_, fg, bi = jobs[k]
        it = e_ * nfg + fg
        if bi == 0 and it == 0:
            load_w(0)
        if bi == 1:
            load_w(it + 1)
        wg_t, wg_b = wgs[it % 2]
        wu_t, wu_b = wus[it % 2]
        t0, wd_, j = blocks[bi]
        H_t, H_b = Hs[k % 2]
        for fc in range(2):
            g_t, g_b = pg[fc]
            u_t, u_b = pu[fc]
            for kc in range(8):
                P.mm(g_t[:, 0:wd_], wg_t[:, kc, fc * 128:(fc + 1) * 128], hx_t[:, kc, t0:t0 + wd_],
                     kc == 0, kc == 7, (wg_b, hx_b), (g_b,))
            for kc in range(8):
                P.mm(u_t[:, 0:wd_], wu_t[:, kc, fc * 128:(fc + 1) * 128], hx_t[:, kc, t0:t0 + wd_],
                     kc == 0, kc == 7, (wu_b, hx_b), (u_b,))
            sg_t, sg_b = sgs[fc]
            P.act(sg_t[:, 0:wd_], g_t[:, 0:wd_], AF.Silu, (g_b,), (sg_b,))
            P.tt("dve", H_t[:, fc, 0:wd_], u_t[:, 0:wd_], sg_t[:, 0:wd_], ALU.mult, (u_b, sg_b), (H_b,))

    def DOWN(k):
        e_, fg, bi = jobs[k]
        it = e_ * nfg + fg
        wd_t, wd_b = wds[it % 2]
        t0, wd_, j = blocks[bi]
        H_t, H_b = Hs[k % 2]
        for sub in range(wd_ // 128):
            tile = t0 // 128 + sub
            for dh in range(2):
                d_t, d_b = pd[nd[0] % 2]
                nd[0] += 1
                for fc in range(2):
                    P.mm(d_t[:], H_t[:, fc, sub * 128:(sub + 1) * 128], wd_t[:, fc, dh * 512:(dh + 1) * 512],
                         fc == 0, fc == 1, (H_b, wd_b), (d_b,))
                acc_ap = acc_t[:, tile, dh * 512:(dh + 1) * 512]
                if E > 1:
                    P.stt("dve", acc_ap, d_t[:], gate_t[:, tile, e_:e_ + 1], acc_ap, ALU.mult, ALU.add,
                          (d_b, gate_b, acc_b), (acc_b,))
                else:
                    P.tt("dve", acc_ap, d_t[:], acc_ap, ALU.add, (d_b, acc_b), (acc_b,))

    for k in range(len(jobs) + 1):
        if k < len(jobs):
            GU(k)
        if k >= 1:
            DOWN(k - 1)
    m5_t, m5_b = P.sb([128, 2, 1024])
    P.dma("sp", m5_t[:], m5[:, :, :], (), (m5_b,))
    if final:
        gn_t, gn_b = P.sb([128, 1024])
        P.dma("sp", gn_t[:], gn[:, :], (), (gn_b,))
        ss_t, ss_b = P.sb([128, 1])
        tq = [P.sb([128, 1024]) for _ in range(2)]
    x1s = [P.sb([128, 1024]) for _ in range(2)]
    for tile in range(ntile):
        j = 0 if tile * 128 < ntl else 1
        x_t, x_b = x1s[tile % 2]
        P.dma("sp", x_t[:], x1[tile * 128:(tile + 1) * 128, :], (), (x_b,))
        P.tt("pool", acc_t[:, tile, :], acc_t[:, tile, :], m5_t[:, j, :], ALU.mult, (acc_b, m5_b), (acc_b,))
        P.tt("dve", x_t[:], x_t[:], acc_t[:, tile, :], ALU.add, (x_b, acc_b), (x_b,))
        if final:
            q_t, q_b = tq[tile % 2]
            P.tt("pool", q_t[:], x_t[:], x_t[:], ALU.mult, (x_b,), (q_b,))
            P.op("dve", lambda e, o=ss_t[:], i=q_t[:]: e.reduce_sum(o, i, axis=mybir.AxisListType.X), (q_b,), (ss_b,))
            P.act(ss_t[:], ss_t[:], AF.Sqrt, (ss_b,), (ss_b,), bias=EPS, scale=1.0 / 1024)
            P.recip(ss_t[:], ss_t[:], (ss_b,), (ss_b,))
            P.stt("dve", x_t[:], x_t[:], ss_t[:, 0:1], gn_t[:], ALU.mult, ALU.mult, (x_b, ss_b, gn_b), (x_b,))
        P.dma("sp", out[tile * 128:(tile + 1) * 128, :], x_t[:], (x_b,), ())
    return P.finish()


def stage_C2(c1, need_ctx):
    dn, dnc, gb, gbc = c1["dn"], c1["dnc"], c1["gb"], c1["gbc"]
    units = []
    for h in range(6):
        sl = slice(h * 64, (h + 1) * 64)
        for d in range(2):
            parts = []
            for r in (0, 384, 768):
                a_c, a_x = dnc[r:r + 384][sl], dn[r:r + 384][sl]
                if d == 1:
                    a_c, a_x = a_c[:, ::-1], a_x[:, ::-1]
                parts.append(np.concatenate([a_c, a_x], axis=1))
            gs = []
            for w_ in range(2):
                g_c, g_x = gbc[w_, d * 6 + h], gb[w_, d * 6 + h]
                if d == 1:
                    g_c, g_x = g_c[::-1], g_x[::-1]
                gs.append(np.concatenate([g_c, g_x]))
            units.append((parts[0], parts[1], parts[2], gs[0], gs[1]))
    outs = run_C2(units)
    of = np.zeros((384, SEQ), np.float32)
    ob = np.zeros((384, SEQ), np.float32)
    ofc = np.zeros((384, CTX), np.float32)
    obc = np.zeros((384, CTX), np.float32)
    for h in range(6):
        sl = slice(h * 64, (h + 1) * 64)
        f, b = outs[2 * h], outs[2 * h + 1]
        of[sl] = f[:, CTX:]
        ofc[sl] = f[:, :CTX]
        ob[sl] = b[:, CTX:][:, ::-1]
        obc[sl] = b[:, :CTX][:, ::-1]
    return of, ob, ofc, obc


def stage_E1(xT, hcT, c1, of, ob, ofc, obc, px, pc, w_out_l, modT_l, dng_l, need_ctx):
    ntl = SEQ // NCORES
    ntc = CTX if need_ctx else 0
    nc = build_E1(ntl, ntc)
    m = modT_l.reshape(128, 2, 6, 8)
    mod2 = c_(m[:, :, 2, :])
    dng = c_(np.tile(dng_l.reshape(64), 2).reshape(128, 1))
    bd = c_(np.kron(np.eye(2), np.ones((64, 64))))
    zx, zc = px[R_DZ:R_DZ + 384], pc[R_DZ:R_DZ + 384]

    def cat(a, b, i):
        sl = a[:, i * ntl:(i + 1) * ntl]
        return np.concatenate([sl, b], axis=1) if need_ctx else sl
    ims = []
    for i in range(NCORES):
        nt = ntl + ntc
        ims.append({
            "xT": c_(cat(xT, hcT, i).reshape(8, 128, nt)),
            "ya": c_(cat(c1["yaT"], c1.get("yaTc"), i).reshape(2, 128, nt)),
            "yc": c_(cat(c1["ycT"], c1.get("ycTc"), i).reshape(3, 128, nt)),
            "of": c_(cat(of, ofc, i).reshape(3, 128, nt)),
            "ob": c_(cat(ob, obc, i).reshape(3, 128, nt)),
            "z": c_(cat(zx, zc, i).reshape(3, 128, nt)),
            "w": c_(w_out_l), "mod2": mod2, "dng": dng, "bd": bd,
        })
    res = run(nc, ims)
    x1T = np.concatenate([r["x1T"][:, :, :ntl].reshape(1024, ntl) for r in res], axis=1)
    hc1T = res[0]["x1T"][:, :, ntl:].reshape(1024, CTX) if need_ctx else None
    return x1T, hc1T


def stage_E2(x1T, hc1T, modT_l, wg, wu, wd, wr, gn, need_ctx):
    ntl = SEQ // NCORES
    ntc = CTX if need_ctx else 0
    E, _, F = wg.shape
    final = gn is not None
    nc = build_E2(ntl, ntc, E, F, final)
    m = modT_l.reshape(128, 2, 6, 8)
    mod = c_(m[:, :, 3:5, :])
    m5 = np.stack([np.broadcast_to(m[:, j, 5, :].T.reshape(1, 1024), (128, 1024)) for j in range(2)], axis=1)
    wg, wu, wd = c_(wg), c_(wu), c_(wd)
    ims = []
    for i in range(NCORES):
        sl = x1T[:, i * ntl:(i + 1) * ntl]
        blk = np.concatenate([sl, hc1T], axis=1) if need_ctx else sl
        nt = ntl + ntc
        im = {"x1T": c_(blk.reshape(8, 128, nt)), "x1": c_(blk.T), "mod": mod, "m5": c_(m5),
              "wg": wg, "wu": wu, "wd": wd}
        if E > 1:
            im["wr"] = c_(wr.reshape(8, 128, E).transpose(1, 0, 2))
        if final:
            im["gn"] = c_(np.broadcast_to(gn.reshape(1, 1024), (128, 1024)))
        ims.append(im)
    res = run(nc, ims)
    x2 = np.concatenate([r["x2"][:ntl] for r in res], axis=0)
    hc2 = res[0]["x2"][ntl:] if need_ctx else None
    return x2, hc2


def kernel(x, c, ctx, c_ctx, w_mod, b_mod, w_in, w_out, conv_w, dn_conv_w, dn_a_log, dn_dt_bias, dn_norm_g,
           attn_sink, ffn_w_gate, ffn_w_up, ffn_w_down, moe_router, moe_w_gate, moe_w_up, moe_w_down,
           final_norm_g):
    f = lambda a: np.asarray(a, dtype=np.float32)
    x, c, ctx, c_ctx, w_mod, b_mod, w_in, w_out = map(f, (x, c, ctx, c_ctx, w_mod, b_mod, w_in, w_out))
    modT = stage_A(c, c_ctx, w_mod, b_mod)
    xT = np.ascontiguousarray(x[0].T)
    hcT = np.ascontiguousarray(ctx[0].T)
    x2 = None
    for l in range(2):
        need_ctx = l == 0
        px, pc = stage_B(xT, hcT, w_in[l], modT[l])
        c1 = stage_C1(px, pc, f(conv_w)[l], f(attn_sink)[l], f(dn_conv_w)[l], f(dn_a_log)[l], f(dn_dt_bias)[l],
                      need_ctx)
        of, ob, ofc, obc = stage_C2(c1, need_ctx)
        x1T, hc1T = stage_E1(xT, hcT, c1, of, ob, ofc, obc, px, pc, w_out[l], modT[l], f(dn_norm_g)[l], need_ctx)
        if l == 0:
            x2, hc2 = stage_E2(x1T, hc1T, modT[l], f(ffn_w_gate), f(ffn_w_up), f(ffn_w_down), None, None, True)
            xT = np.ascontiguousarray(x2.T)
            hcT = np.ascontiguousarray(hc2.T)
        else:
            x2, _ = stage_E2(x1T, None, modT[l], f(moe_w_gate)[0], f(moe_w_up)[0], f(moe_w_down)[0],
                             f(moe_router)[0], f(final_norm_g), False)
    return x2.reshape(1, SEQ, D_MODEL).astype(np.float32)
```

```python
import numpy as np
from contextlib import ExitStack
import concourse.bass as bass
import concourse.mybir as mybir
from concourse.bass_utils import run_bass_kernel_spmd

F32 = mybir.dt.float32
BF16 = mybir.dt.bfloat16
AF = mybir.ActivationFunctionType
ALU = mybir.AluOpType
NCORES = 8

D_MODEL = 1024
SEQ = 16384
CTX = 256
EPS = 1e-6
IN_COLS = 2968


class Buf:
    __slots__ = ("name", "lw", "rd", "excl", "dw")

    def __init__(self, name, excl=False):
        self.name = name
        self.lw = None
        self.dw = []
        self.rd = {}
        self.excl = excl


class Prog:
    EPOCH = 4096
    NDMA = 24

    def __init__(self):
        self.nc = bass.Bass("TRN2", target_bir_lowering=False)
        self.es = ExitStack()
        self.q = {e: [] for e in ("sp", "pe", "dve", "act", "pool")}
        self.cnt = {e: 0 for e in self.q}
        self.seen = {e: {} for e in self.q}
        self.ndma = {"sp": 0, "pool": 0}
        self.dlast = {}
        self.nt = 0

    def dram_in(self, name, shape, dt=F32):
        return self.nc.dram_tensor(name, list(shape), dt, kind="ExternalInput").ap()

    def dram_out(self, name, shape, dt=F32):
        return self.nc.dram_tensor(name, list(shape), dt, kind="ExternalOutput").ap()

    def sb(self, shape, dt=F32, name=None):
        self.nt += 1
        name = name or f"t{self.nt}"
        t = self.es.enter_context(self.nc.sbuf_tensor(name, list(shape), dt))
        return t, Buf(name)

    def psum(self, shape=(128, 512), dt=F32, name=None):
        self.nt += 1
        name = name or f"p{self.nt}"
        t = self.es.enter_context(self.nc.psum_tensor(name, list(shape), dt))
        return t, Buf(name, excl=True)

    def _deps(self, reads, writes, eng=None):
        deps = []
        for b in reads:
            if b.lw is not None:
                deps.append(b.lw)
            deps.extend(b.dw)
            if b.excl:
                deps.extend(v for k_, v in b.rd.items() if k_ != eng)
        for b in writes:
            if b.lw is not None:
                deps.append(b.lw)
            deps.extend(b.dw)
            deps.extend(b.rd.values())
        return deps

    def _filter(self, eng, deps):
        waits = []
        seen = self.seen[eng]
        for d in deps:
            if d[0] == "c":
                _, p, n = d
                if p == eng and eng == "pe":
                    continue
                if seen.get(p, -1) >= n:
                    continue
                seen[p] = n
            else:
                _, slot, val = d
                if seen.get(("d", slot), 0) >= val:
                    continue
                seen[("d", slot)] = val
            waits.append(d)
        return waits

    def _mark(self, tok, key, reads, writes):
        for b in reads:
            b.rd[key] = tok
        for b in writes:
            if tok[0] == "d":
                if b.rd:
                    b.dw = []
                b.dw.append(tok)
            else:
                b.lw = tok
                b.dw = []
            b.rd = {}

    def op(self, eng, fn, reads=(), writes=()):
        n = self.cnt[eng]
        self.cnt[eng] += 1
        waits = self._filter(eng, self._deps(reads, writes, eng))
        tok = ("c", eng, n)
        self._mark(tok, eng, reads, writes)
        self.q[eng].append((fn, waits, tok))

    def dma(self, eng, out_ap, in_ap, reads=(), writes=()):
        j = self.ndma[eng]
        self.ndma[eng] += 1
        slot = (j % self.NDMA) + (self.NDMA if eng == "pool" else 0)
        val = 16 * (j // self.NDMA + 1)
        deps = self._deps(reads, writes)
        if val > 16:
            deps.append(("d", slot, val - 16))
        waits = self._filter(eng, deps)
        tok = ("d", slot, val)
        self.dlast[slot] = val
        self._mark(tok, ("d", slot), reads, writes)
        self.q[eng].append((lambda e: e.dma_start(out=out_ap, in_=in_ap), waits, tok))

    def mm(self, out, lhsT, rhs, start, stop, reads, writes):
        self.op("pe", lambda e: e.matmul(out, lhsT, rhs, start=start, stop=stop), reads, writes)

    def transpose(self, out, in_, ident, reads, writes):
        self.op("pe", lambda e: e.transpose(out, in_, ident), reads, writes)

    def act(self, out, in_, func, reads, writes, bias=None, scale=None, eng="act"):
        kw = {}
        if bias is not None:
            kw["bias"] = bias
        if scale is not None:
            kw["scale"] = scale
        self.op(eng, lambda e: e.activation(out, in_, func, **kw), reads, writes)

    def tt(self, eng, out, in0, in1, op, reads, writes):
        self.op(eng, lambda e: e.tensor_tensor(out, in0, in1, op=op), reads, writes)

    def ts(self, eng, out, in0, s1, s2, op0, op1, reads, writes):
        if s2 is None:
            self.op(eng, lambda e: e.tensor_scalar(out, in0, s1, None, op0=op0), reads, writes)
        else:
            self.op(eng, lambda e: e.tensor_scalar(out, in0, s1, s2, op0=op0, op1=op1), reads, writes)

    def stt(self, eng, out, in0, scalar, in1, op0, op1, reads, writes):
        self.op(eng, lambda e: e.scalar_tensor_tensor(out, in0, scalar, in1, op0=op0, op1=op1), reads, writes)

    def copy(self, eng, out, in_, reads, writes):
        if eng == "act":
            self.op(eng, lambda e: e.activation(out, in_, AF.Identity), reads, writes)
        else:
            self.op(eng, lambda e: e.tensor_copy(out, in_), reads, writes)

    def recip(self, out, in_, reads, writes):
        self.op("dve", lambda e: e.reciprocal(out, in_), reads, writes)

    def memset(self, eng, ap, val, writes):
        self.op(eng, lambda e: e.memset(ap, val), (), writes)

    def finish(self):
        nc = self.nc
        E = self.EPOCH
        sems = {}
        for e in ("pe", "dve", "act", "pool"):
            nep = max(1, (self.cnt[e] + E - 1) // E)
            sems[e] = [self.es.enter_context(nc.semaphore(f"s_{e}{i}")) for i in range(nep)]
        dsems = [self.es.enter_context(nc.semaphore(f"s_d{i}")) for i in range(2 * self.NDMA)]
        fin = [("d", s, v) for s, v in sorted(self.dlast.items())]
        fin = self._filter("sp", fin)
        self.q["sp"].append((None, fin, None))

        def emit(engobj, ename):
            for fn, waits, tok in self.q[ename]:
                for w in waits:
                    if w[0] == "c":
                        engobj.wait_ge(sems[w[1]][w[2] // E], w[2] % E + 1)
                    else:
                        engobj.wait_ge(dsems[w[1]], w[2])
                if fn is None:
                    continue
                ins = fn(engobj)
                if tok[0] == "c":
                    ins.then_inc(sems[ename][tok[2] // E], 1)
                else:
                    ins.then_inc(dsems[tok[1]], 16)

        with nc.Block() as block:
            @block.sync
            def _(e):
                emit(e, "sp")

            @block.tensor
            def _(e):
                emit(e, "pe")

            @block.vector
            def _(e):
                emit(e, "dve")

            @block.scalar
            def _(e):
                emit(e, "act")

            @block.gpsimd
            def _(e):
                emit(e, "pool")
        self.es.close()
        return nc


def run(prog_nc, in_maps):
    res = run_bass_kernel_spmd(prog_nc, in_maps, core_ids=list(range(NCORES)))
    return res.results


def c_(a):
    return np.ascontiguousarray(a, dtype=np.float32)


def build_A():
    ND = 48 // NCORES
    P = Prog()
    s_in = P.dram_in("s_in", [128, 8, 2])
    wm = P.dram_in("wm", [2, 1024, ND * 128])
    bm = P.dram_in("bm", [2, 128, ND])
    out = P.dram_out("modT", [2, 128, 2, ND])
    s_t, s_b = P.sb([128, 8, 2])
    sg_t, sg_b = P.sb([128, 8, 2])
    sb_t, sb_b = P.sb([128, 8, 2], BF16)
    bm_t, bm_b = P.sb([128, 2, ND])
    o_t, o_b = P.sb([128, 2, 2, ND])
    P.dma("sp", s_t[:], s_in[:, :, :], (), (s_b,))
    P.dma("sp", bm_t[:], bm.rearrange("l p c -> p l c"), (), (bm_b,))
    P.act(sg_t[:], s_t[:], AF.Sigmoid, (s_b,), (sg_b,))
    P.tt("dve", sb_t[:], s_t[:], sg_t[:], ALU.mult, (s_b, sg_b), (sb_b,))
    ws = [P.sb([128, 8, ND * 128], BF16) for _ in range(2)]
    pss = [P.psum([128, 512]) for _ in range(2)]
    for l in range(2):
        w_t, w_b = ws[l]
        for kc in range(8):
            P.dma("pool", w_t[:, kc, :], wm[l, kc * 128:(kc + 1) * 128, :], (), (w_b,))
    for l in range(2):
        w_t, w_b = ws[l]
        ps_t, ps_b = pss[l]
        for dc in range(ND):
            for kc in range(8):
                P.mm(ps_t[:, dc * 2:dc * 2 + 2], w_t[:, kc, dc * 128:(dc + 1) * 128], sb_t[:, kc, :],
                     kc == 0, kc == 7, (w_b, sb_b), (ps_b,))
        for j in range(2):
            P.tt("dve", o_t[:, l, j, :], ps_t[:, j:2 * ND:2], bm_t[:, l, :], ALU.add, (ps_b, bm_b), (o_b,))
    P.dma("sp", out.rearrange("l p j c -> p l j c"), o_t[:], (o_b,), ())
    return P.finish()


def stage_A(c, c_ctx, w_mod, b_mod):
    ND = 48 // NCORES
    s = np.stack([c.reshape(1024), c_ctx.reshape(1024)], axis=-1)
    s_in = c_(s.reshape(8, 128, 2).transpose(1, 0, 2))
    bm = b_mod.reshape(2, 48, 128).transpose(0, 2, 1)
    nc = build_A()
    ims = [{"s_in": s_in, "wm": c_(w_mod[:, :, i * ND * 128:(i + 1) * ND * 128]),
            "bm": c_(bm[:, :, i * ND:(i + 1) * ND])} for i in range(NCORES)]
    res = run(nc, ims)
    return np.concatenate([r["modT"] for r in res], axis=3)


NCH_IN = 24
TT = 256


def build_B(ntl, ntc):
    nt = ntl + ntc
    P = Prog()
    xT = P.dram_in("xT", [8, 128, nt])
    w = P.dram_in("w", [1024, NCH_IN * 128])
    mod = P.dram_in("mod", [128, 2, 2, 8])
    out = P.dram_out("pxT", [NCH_IN, 128, nt])
    w_t, w_b = P.sb([128, 8, NCH_IN * 128], BF16, "w_sb")
    for kc in range(8):
        P.dma("pool", w_t[:, kc, :], w[kc * 128:(kc + 1) * 128, :], (), (w_b,))
    mod_t, mod_b = P.sb([128, 2, 2, 8])
    P.dma("sp", mod_t[:], mod[:, :, :, :], (), (mod_b,))
    a_t, a_b = P.sb([128, 2, 8])
    P.ts("dve", a_t[:], mod_t[:, :, 1, :], 1.0, None, ALU.add, None, (mod_b,), (a_b,))
    ones_t, ones_b = P.sb([128, 128], BF16)
    P.memset("dve", ones_t[:], 1.0, (ones_b,))
    xs = [P.sb([128, 8, TT]) for _ in range(2)]
    sqs = [P.sb([128, 8, TT], BF16) for _ in range(2)]
    hs = [P.sb([128, 8, TT], BF16) for _ in range(2)]
    rs = [P.sb([128, TT]) for _ in range(2)]
    tmp = [P.sb([128, TT]) for _ in range(2)]
    os_ = [P.sb([128, TT]) for _ in range(4)]
    pn = P.psum([128, 512])
    pp = [P.psum([128, 512]) for _ in range(4)]
    for it in range(nt // TT):
        j = 0 if it * TT < ntl else 1
        x_t, x_b = xs[it % 2]
        sq_t, sq_b = sqs[it % 2]
        h_t, h_b = hs[it % 2]
        r_t, r_b = rs[it % 2]
        P.dma("pool", x_t[:], xT[:, :, it * TT:(it + 1) * TT].rearrange("c p t -> p c t"), (), (x_b,))
        P.act(sq_t[:], x_t[:], AF.Square, (x_b,), (sq_b,))
        for kc in range(8):
            P.mm(pn[0][:, 0:TT], ones_t[:], sq_t[:, kc, :], kc == 0, kc == 7, (ones_b, sq_b), (pn[1],))
        P.act(r_t[:], pn[0][:, 0:TT], AF.Sqrt, (pn[1],), (r_b,), bias=EPS, scale=1.0 / 1024)
        P.recip(r_t[:], r_t[:], (r_b,), (r_b,))
        for kc in range(8):
            t_t, t_b = tmp[kc % 2]
            P.stt("dve", t_t[:], x_t[:, kc, :], a_t[:, j, kc:kc + 1], r_t[:], ALU.mult, ALU.mult,
                  (x_b, a_b, r_b), (t_b,))
            P.act(h_t[:, kc, :], t_t[:], AF.Identity, (t_b, mod_b), (h_b,), bias=mod_t[:, j, 0, kc:kc + 1])
        for oc in range(NCH_IN):
            ps_t, ps_b = pp[oc % 4]
            for kc in range(8):
                P.mm(ps_t[:, 0:TT], w_t[:, kc, oc * 128:(oc + 1) * 128], h_t[:, kc, :], kc == 0, kc == 7,
                     (w_b, h_b), (ps_b,))
            o_t, o_b = os_[oc % 4]
            P.copy("act" if oc % 2 else "dve", o_t[:], ps_t[:, 0:TT], (ps_b,), (o_b,))
            P.dma("sp", out[oc, :, it * TT:(it + 1) * TT], o_t[:], (o_b,), ())
    return P.finish()


def perm_w_in(w_in_l):
    z = np.zeros((1024, 104), np.float32)
    return c_(np.concatenate([w_in_l[:, 0:2304], w_in_l[:, 2328:2968], w_in_l[:, 2304:2328], z], axis=1))


def stage_B(xT_all, hcT, w_in_l, modT_l):
    ntl = SEQ // NCORES
    nc = build_B(ntl, CTX)
    w = perm_w_in(w_in_l)
    m = modT_l.reshape(128, 2, 6, 8)
    mod = c_(m[:, :, 0:2, :])
    ims = []
    for i in range(NCORES):
        xt = np.concatenate([xT_all[:, i * ntl:(i + 1) * ntl], hcT], axis=1)
        ims.append({"xT": c_(xt.reshape(8, 128, ntl + CTX)), "w": w, "mod": mod})
    res = run(nc, ims)
    px = np.concatenate([r["pxT"][:, :, :ntl].reshape(NCH_IN * 128, ntl) for r in res], axis=1)
    pc = res[0]["pxT"][:, :, ntl:].reshape(NCH_IN * 128, CTX)
    return px, pc


GRID_W = 64
NEGV = -30000.0


def rope_tables(pos):
    pos = np.asarray(pos)
    inv = 10000.0 ** (-(np.arange(0, 32, 2, dtype=np.float64)) / 32.0)
    r = (pos // GRID_W).astype(np.float64)
    col = (pos % GRID_W).astype(np.float64)
    cos = np.zeros((64, len(pos)))
    sin = np.zeros((64, len(pos)))
    for d in range(64):
        axis, half, f = d // 32, (d % 32) // 16, d % 16
        p = r if axis == 0 else col
        cos[d] = np.cos(p * inv[f])
        sin[d] = np.sin(p * inv[f]) * (-1.0 if half == 0 else 1.0)
    return cos, sin


ROPE_PERM = np.array([(d // 32) * 32 + ((d % 32) + 16) % 32 for d in range(64)])


def build_C1(ntl, need_ctx):
    nqb = ntl // 128
    nkb = nqb + 2
    nk = nkb * 128
    P = Prog()
    cv = P.dram_in("cv", [3, 2, 128, ntl + 2])
    cw = P.dram_in("cw", [128, 2, 3])
    q_in = P.dram_in("q", [2, 6, 64, ntl])
    k_in = P.dram_in("k", [2, 2, 64, nk])
    v_in = P.dram_in("v", [128, nkb, 128])
    cs_k = P.dram_in("cs_k", [2, 64, nk])
    kc_in = P.dram_in("kc", [2, 64, CTX])
    vc_in = P.dram_in("vc", [128, 2, 128])
    neg_in = P.dram_in("neg", [128, 4, 384])
    sink_in = P.dram_in("sink", [1, 2, 384])
    ident_in = P.dram_in("ident", [128, 128])
    ya = P.dram_out("yaT", [2, 128, ntl])
    yc = P.dram_out("ycT", [6, 64, ntl])
    if need_ctx:
        cvc = P.dram_in("cvc", [3, 2, 128, CTX + 2])
        qc_in = P.dram_in("qc", [6, 64, CTX])
        ya_c = P.dram_out("yaTc", [2, 128, CTX])
        yc_c = P.dram_out("ycTc", [6, 64, CTX])
    dn_in = P.dram_in("dn", [9, 128, ntl + 2])
    dnc_in = P.dram_in("dnc", [9, 128, CTX + 2])
    dcw_in = P.dram_in("dcw", [128, 9, 3])
    gab_in = P.dram_in("gab", [2, 12, ntl])
    gabc_in = P.dram_in("gabc", [2, 12, CTX])
    gpar_in = P.dram_in("gpar", [12, 2])
    bd_in = P.dram_in("bd", [128, 128])
    dn_out = P.dram_out("dno", [9, 128, ntl])
    dnc_out = P.dram_out("dnco", [9, 128, CTX])
    gb_out = P.dram_out("gbo", [2, 12, ntl])
    gbc_out = P.dram_out("gbco", [2, 12, CTX])

    cw_t, cw_b = P.sb([128, 2, 3])
    P.dma("sp", cw_t[:], cw[:, :, :], (), (cw_b,))
    ident_t, ident_b = P.sb([128, 128], BF16)
    P.dma("pool", ident_t[:], ident_in[:, :], (), (ident_b,))
    neg_t, neg_b = P.sb([128, 4, 384], BF16)
    P.dma("pool", neg_t[:], neg_in[:, :, :], (), (neg_b,))
    ones_t, ones_b = P.sb([128, 64], BF16)
    P.memset("dve", ones_t[:], 1.0, (ones_b,))
    onesf_t, onesf_b = P.sb([1, 64])
    P.memset("dve", onesf_t[:], 1.0, (onesf_b,))
    sk_t, sk_b = P.sb([1, 2, 384])
    P.dma("sp", sk_t[:], sink_in[:, :, :], (), (sk_b,))
    esk_t, esk_b = P.sb([1, 2, 384])
    P.act(esk_t[:], sk_t[:], AF.Exp, (sk_b,), (esk_b,))

    cb_t, b_b = P.sb([128, ntl + 2])
    cc_t, c_b = P.sb([128, ntl + 2])
    chh_t, h_b = P.sb([128, ntl + 2])

    def conv(src, n, dst):
        for ch in range(2):
            b_t, c_t, h_t = cb_t[:, 0:n + 2], cc_t[:, 0:n + 2], chh_t[:, 0:n + 2]
            P.dma("sp", b_t, src[0, ch, :, :], (), (b_b,))
            P.dma("sp", c_t, src[1, ch, :, :], (), (c_b,))
            P.dma("sp", h_t, src[2, ch, :, :], (), (h_b,))
            P.tt("dve", c_t, c_t, h_t, ALU.mult, (c_b, h_b), (c_b,))
            P.ts("dve", h_t[:, 0:n], c_t[:, 1:n + 1], cw_t[:, ch, 1:2], None, ALU.mult, None, (c_b, cw_b), (h_b,))
            P.stt("dve", h_t[:, 0:n], c_t[:, 0:n], cw_t[:, ch, 0:1], h_t[:, 0:n], ALU.mult, ALU.add,
                  (c_b, cw_b, h_b), (h_b,))
            P.stt("dve", h_t[:, 0:n], c_t[:, 2:n + 2], cw_t[:, ch, 2:3], h_t[:, 0:n], ALU.mult, ALU.add,
                  (c_b, cw_b, h_b), (h_b,))
            P.tt("dve", h_t[:, 0:n], h_t[:, 0:n], b_t[:, 1:n + 1], ALU.mult, (h_b, b_b), (h_b,))
            P.dma("sp", dst[ch, :, :], h_t[:, 0:n], (h_b,), ())
    conv(cv, ntl, ya)
    if need_ctx:
        conv(cvc, CTX, ya_c)

    dcw_t, dcw_b = P.sb([128, 9, 3])
    P.dma("sp", dcw_t[:], dcw_in[:, :, :], (), (dcw_b,))
    bd_t, bd_b = P.sb([128, 128])
    P.dma("sp", bd_t[:], bd_in[:, :], (), (bd_b,))
    rr_t, rr_b = P.sb([128, 512])
    pps = P.psum([128, 512])

    def dnprep(src, n, dst):
        for ch in range(9):
            x_t, y_t, s_t = cb_t[:, 0:n + 2], cc_t[:, 0:n], chh_t[:, 0:n]
            x_b, y_b, s_b = b_b, c_b, h_b
            P.dma("sp", x_t, src[ch, :, :], (), (x_b,))
            P.ts("dve", y_t, x_t[:, 1:n + 1], dcw_t[:, ch, 1:2], None, ALU.mult, None, (x_b, dcw_b), (y_b,))
            P.stt("dve", y_t, x_t[:, 0:n], dcw_t[:, ch, 0:1], y_t, ALU.mult, ALU.add, (x_b, dcw_b, y_b), (y_b,))
            P.stt("dve", y_t, x_t[:, 2:n + 2], dcw_t[:, ch, 2:3], y_t, ALU.mult, ALU.add, (x_b, dcw_b, y_b), (y_b,))
            P.act(y_t, y_t, AF.Silu, (y_b,), (y_b,))
            if ch < 6:
                P.act(s_t, y_t, AF.Square, (y_b,), (s_b,))
                for c0 in range(0, n, 512):
                    w_ = min(512, n - c0)
                    P.mm(pps[0][:, 0:w_], bd_t[:], s_t[:, c0:c0 + w_], True, True, (bd_b, s_b), (pps[1],))
                    P.act(rr_t[:, 0:w_], pps[0][:, 0:w_], AF.Sqrt, (pps[1],), (rr_b,), bias=1e-6)
                    P.recip(rr_t[:, 0:w_], rr_t[:, 0:w_], (rr_b,), (rr_b,))
                    P.stt("dve", y_t[:, c0:c0 + w_], y_t[:, c0:c0 + w_], 0.125 if ch < 3 else 1.0, rr_t[:, 0:w_],
                          ALU.mult, ALU.mult, (y_b, rr_b), (y_b,))
            P.dma("sp", dst[ch, :, :], y_t, (y_b,), ())
    dnprep(dn_in, ntl, dn_out)
    dnprep(dnc_in, CTX, dnc_out)
    gpar_t, gpar_b = P.sb([12, 2])
    P.dma("sp", gpar_t[:], gpar_in[:, :], (), (gpar_b,))
    nea_t, nea_b = P.sb([12, 1])
    P.act(nea_t[:], gpar_t[:, 0:1], AF.Exp, (gpar_b,), (nea_b,))
    P.ts("dve", nea_t[:], nea_t[:], -1.0, None, ALU.mult, None, (nea_b,), (nea_b,))

    def gates(src, n, dst):
        a_t, bt_t = cb_t[0:12, 0:n], cc_t[0:12, 0:n]
        P.dma("sp", a_t, src[0, :, :], (), (b_b,))
        P.dma("sp", bt_t, src[1, :, :], (), (c_b,))
        P.act(a_t, a_t, AF.Exp, (b_b, gpar_b), (b_b,), bias=gpar_t[:, 1:2])
        P.act(a_t, a_t, AF.Ln, (b_b,), (b_b,), bias=1.0)
        P.ts("dve", a_t, a_t, nea_t[:, 0:1], None, ALU.mult, None, (b_b, nea_b), (b_b,))
        P.act(bt_t, bt_t, AF.Sigmoid, (c_b,), (c_b,))
        P.dma("sp", dst[0, :, :], a_t, (b_b,), ())
        P.dma("sp", dst[1, :, :], bt_t, (c_b,), ())
    gates(gab_in, ntl, gb_out)
    gates(gabc_in, CTX, gbc_out)

    csk_t, csk_b = P.sb([64, 2, nk])
    P.dma("sp", csk_t[:], cs_k.rearrange("c d t -> d c t"), (), (csk_b,))
    qr_t, qr_b = P.sb([64, nqb, 2, 3, 128], BF16, "qr")
    kr_t, kr_b = P.sb([64, 2, nk], BF16, "kr")
    qa = [P.sb([64, nk]) for _ in range(2)]
    qp = [P.sb([64, nk]) for _ in range(2)]
    for h in range(6):
        a_t, a_b = qa[h % 2]
        p_t, p_b = qp[h % 2]
        P.dma("sp", a_t[:, 0:ntl], q_in[0, h, :, :], (), (a_b,))
        P.dma("sp", p_t[:, 0:ntl], q_in[1, h, :, :], (), (p_b,))
        P.tt("dve", a_t[:, 0:ntl], a_t[:, 0:ntl], csk_t[:, 0, 128:128 + ntl], ALU.mult, (a_b, csk_b), (a_b,))
        P.tt("pool", p_t[:, 0:ntl], p_t[:, 0:ntl], csk_t[:, 1, 128:128 + ntl], ALU.mult, (p_b, csk_b), (p_b,))
        P.tt("dve", qr_t[:, :, h // 3, h % 3, :], a_t[:, 0:ntl].rearrange("p (b q) -> p b q", q=128),
             p_t[:, 0:ntl].rearrange("p (b q) -> p b q", q=128), ALU.add, (a_b, p_b), (qr_b,))
    for g in range(2):
        a_t, a_b = qa[g]
        p_t, p_b = qp[g]
        P.dma("sp", a_t[:], k_in[0, g, :, :], (), (a_b,))
        P.dma("sp", p_t[:], k_in[1, g, :, :], (), (p_b,))
        P.tt("dve", a_t[:], a_t[:], csk_t[:, 0, :], ALU.mult, (a_b, csk_b), (a_b,))
        P.tt("pool", p_t[:], p_t[:], csk_t[:, 1, :], ALU.mult, (p_b, csk_b), (p_b,))
        P.tt("dve", kr_t[:, g, :], a_t[:], p_t[:], ALU.add, (a_b, p_b), (kr_b,))
    kcb_t, kcb_b = P.sb([64, 2, CTX], BF16)
    P.dma("pool", kcb_t[:], kc_in.rearrange("g d t -> d g t"), (), (kcb_b,))
    v_t, v_b = P.sb([128, nkb, 128], BF16, "v_sb")
    P.dma("pool", v_t[:], v_in[:, :, :], (), (v_b,))
    vc_t, vc_b = P.sb([128, 2, 128], BF16)
    P.dma("pool", vc_t[:], vc_in[:, :, :], (), (vc_b,))
    if need_ctx:
        qcb_t, qcb_b = P.sb([64, 2, 2, 3, 128], BF16)
        for h in range(6):
            P.dma("pool", qcb_t[:, :, h // 3, h % 3, :], qc_in[h, :, :].rearrange("d (b q) -> d b q", q=128),
                  (), (qcb_b,))

    ps_s = [P.psum([128, 512]) for _ in range(5)]
    ps_o = P.psum([128, 512])
    ps_d = P.psum([128, 512])
    pts = [P.sb([128, 384], BF16) for _ in range(5)]
    rd = [P.sb([64, 384]) for _ in range(2)]
    ob = [P.sb([64, 384]) for _ in range(2)]
    cnt = [0, 0]

    def attend(qsrc, qsrc_b, qb, g, keyspecs, dst, t0):
        pts_used = []
        for (k_ap, k_b, v_ap, v_bf, negidx) in keyspecs:
            i = cnt[0] % 5
            cnt[0] += 1
            s_t, s_b = ps_s[i]
            rhs = qsrc[:, qb, g, :, :].rearrange("d h q -> d (h q)")
            P.mm(s_t[:, 0:384], k_ap, rhs, True, negidx is None, (k_b, qsrc_b), (s_b,))
            if negidx is not None:
                P.mm(s_t[:, 0:384], ident_t[:], neg_t[:, negidx, :], False, True, (ident_b, neg_b), (s_b,))
            p_t, p_b = pts[i]
            P.act(p_t[:], s_t[:, 0:384], AF.Exp, (s_b,), (p_b,), scale=0.125)
            pts_used.append((p_t, p_b, v_ap, v_bf))
        n = len(pts_used)
        for idx, (p_t, p_b, v_ap, v_bf) in enumerate(pts_used):
            P.mm(ps_o[0][0:64, 0:384], v_ap, p_t[:], idx == 0, idx == n - 1, (v_bf, p_b), (ps_o[1],))
        for idx, (p_t, p_b, v_ap, v_bf) in enumerate(pts_used):
            P.mm(ps_d[0][0:64, 0:384], ones_t[:], p_t[:], idx == 0, False, (ones_b, p_b), (ps_d[1],))
        P.mm(ps_d[0][0:64, 0:384], onesf_t[:], esk_t[:, g, :], False, True, (onesf_b, esk_b), (ps_d[1],))
        j = cnt[1] % 2
        cnt[1] += 1
        r_t, r_b = rd[j]
        o_t, o_b = ob[j]
        P.recip(r_t[:], ps_d[0][0:64, 0:384], (ps_d[1],), (r_b,))
        P.tt("dve", o_t[:], ps_o[0][0:64, 0:384], r_t[:], ALU.mult, (ps_o[1], r_b), (o_b,))
        P.dma("sp", dst[3 * g:3 * g + 3, :, t0:t0 + 128].rearrange("h d q -> d h q"),
              o_t[:].rearrange("d (h q) -> d h q", h=3), (o_b,), ())

    for qb in range(nqb):
        for g in range(2):
            specs = []
            for off in range(3):
                kb = qb + off
                negidx = None
                if off == 0:
                    negidx = 0 if qb == 0 else 1
                elif off == 2:
                    negidx = 3 if qb == nqb - 1 else 2
                specs.append((kr_t[:, g, kb * 128:(kb + 1) * 128], kr_b, v_t[:, kb, g * 64:(g + 1) * 64], v_b, negidx))
            for cb in range(2):
                specs.append((kcb_t[:, g, cb * 128:(cb + 1) * 128], kcb_b, vc_t[:, cb, g * 64:(g + 1) * 64], vc_b, None))
            attend(qr_t, qr_b, qb, g, specs, yc, qb * 128)
    if need_ctx:
        for qb in range(2):
            for g in range(2):
                specs = [(kcb_t[:, g, cb * 128:(cb + 1) * 128], kcb_b, vc_t[:, cb, g * 64:(g + 1) * 64], vc_b, None)
                         for cb in range(2)]
                attend(qcb_t, qcb_b, qb, g, specs, yc_c, qb * 128)
    return P.finish()


R_CB, R_CC, R_CH = 0, 256, 512
R_DQ, R_DK, R_DV, R_DZ = 768, 1152, 1536, 1920
R_AQ, R_AK, R_AV = 2304, 2688, 2816
R_A, R_BT = 2944, 2956


def tri_masks():
    kk = np.arange(128)[:, None]
    qq = np.arange(128)[None, :]
    prev = np.where(qq <= kk, 0.0, NEGV)
    nxt = np.where(kk <= qq, 0.0, NEGV)
    return prev, nxt


def stage_C1(px, pc, conv_w_l, sink_l, dn_conv_w_l, dn_a_log_l, dn_dt_bias_l, need_ctx):
    ntl = SEQ // NCORES
    nc = build_C1(ntl, need_ctx)
    nkb = ntl // 128 + 2
    cw = c_(conv_w_l.T.reshape(2, 128, 3).transpose(1, 0, 2))
    prev, nxt = tri_masks()
    full = np.full((128, 128), NEGV)
    cos, sin = rope_tables(np.arange(SEQ))
    cosp = np.pad(cos, ((0, 0), (128, 128)))
    sinp = np.pad(sin, ((0, 0), (128, 128)))
    pad1 = lambda a: np.pad(a, ((0, 0), (1, 1)))
    convsrc = [pad1(px[R_CB:R_CB + 256]), pad1(px[R_CC:R_CC + 256]), pad1(px[R_CH:R_CH + 256])]
    kx = np.pad(px[R_AK:R_AK + 128], ((0, 0), (128, 128))).reshape(2, 64, SEQ + 256)
    kxp = kx[:, ROPE_PERM, :]
    vx = np.pad(px[R_AV:R_AV + 128], ((0, 0), (128, 128)))
    qx = px[R_AQ:R_AQ + 384].reshape(6, 64, SEQ)
    qxp = qx[:, ROPE_PERM, :]
    kc = c_(pc[R_AK:R_AK + 128].reshape(2, 64, CTX))
    vc = c_(pc[R_AV:R_AV + 128].T.reshape(2, 128, 128).transpose(1, 0, 2))
    sink = c_(np.repeat(sink_l.reshape(2, 3), 128, axis=1).reshape(1, 2, 384))
    ident = np.eye(128, dtype=np.float32)
    dnsrc = pad1(px[R_DQ:R_DQ + 1152])
    dnc = c_(pad1(pc[R_DQ:R_DQ + 1152]).reshape(9, 128, CTX + 2))
    dcw = c_(dn_conv_w_l.T.reshape(9, 128, 3).transpose(1, 0, 2))
    gpar = c_(np.stack([dn_a_log_l.reshape(12), dn_dt_bias_l.reshape(12)], axis=1))
    bd = c_(np.kron(np.eye(2), np.ones((64, 64))))
    gabc = c_(np.stack([pc[R_A:R_A + 12], pc[R_BT:R_BT + 12]]))
    ims = []
    for i in range(NCORES):
        t0, t1 = i * ntl, (i + 1) * ntl
        neg = np.stack([np.tile(full if i == 0 else prev, (1, 3)), np.tile(prev, (1, 3)),
                        np.tile(nxt, (1, 3)), np.tile(full if i == NCORES - 1 else nxt, (1, 3))], axis=1)
        im = {
            "cv": c_(np.stack([a[:, t0:t1 + 2].reshape(2, 128, ntl + 2) for a in convsrc])),
            "cw": cw,
            "q": c_(np.stack([qx[:, :, t0:t1], qxp[:, :, t0:t1]])),
            "k": c_(np.stack([kx[:, :, t0:t1 + 256], kxp[:, :, t0:t1 + 256]])),
            "v": c_(vx[:, t0:t1 + 256].T.reshape(nkb, 128, 128).transpose(1, 0, 2)),
            "cs_k": c_(np.stack([cosp[:, t0:t1 + 256], sinp[:, t0:t1 + 256]])),
            "kc": kc, "vc": vc, "neg": c_(neg), "sink": sink, "ident": ident,
            "dn": c_(dnsrc[:, t0:t1 + 2].reshape(9, 128, ntl + 2)), "dnc": dnc, "dcw": dcw,
            "gab": c_(np.stack([px[R_A:R_A + 12, t0:t1], px[R_BT:R_BT + 12, t0:t1]])), "gabc": gabc,
            "gpar": gpar, "bd": bd,
        }
        if need_ctx:
            im["cvc"] = c_(np.stack([pad1(pc[r:r + 256]).reshape(2, 128, CTX + 2) for r in (R_CB, R_CC, R_CH)]))
            im["qc"] = c_(pc[R_AQ:R_AQ + 384].reshape(6, 64, CTX))
        ims.append(im)
    res = run(nc, ims)
    yaT = np.concatenate([r["yaT"].reshape(256, ntl) for r in res], axis=1)
    ycT = np.concatenate([r["ycT"].reshape(384, ntl) for r in res], axis=1)
    out = {"yaT": yaT, "ycT": ycT}
    out["dn"] = np.concatenate([r["dno"].reshape(1152, ntl) for r in res], axis=1)
    out["dnc"] = res[0]["dnco"].reshape(1152, CTX)
    out["gb"] = np.concatenate([r["gbo"] for r in res], axis=2)
    out["gbc"] = res[0]["gbco"]
    if need_ctx:
        out["yaTc"] = res[0]["yaTc"].reshape(256, CTX)
        out["ycTc"] = res[0]["ycTc"].reshape(384, CTX)
    return out


def c2_consts():
    i = np.arange(128)
    same = (i[:, None] // 64) == (i[None, :] // 64)
    tri = (same & (i[:, None] <= i[None, :])).astype(np.float32)
    bd = same.astype(np.float32)
    negl = np.where(same & (i[:, None] >= i[None, :]), 0.0, NEGV).astype(np.float32)
    slm = (same & (i[:, None] > i[None, :])).astype(np.float32)
    ident = np.eye(128, dtype=np.float32)
    ones = np.ones((128, 128), np.float32)
    sel = np.zeros((128, 2, 64), np.float32)
    sel[0:64, 0, :] = 1.0
    sel[64:128, 1, :] = 1.0
    return {"tri": tri, "bd": bd, "negl": negl, "slm": slm, "ident": ident, "ones": ones,
            "sel": sel.reshape(128, 128)}


C2_STOP = 9


def build_C2(NP, mode="full"):
    T = NP * 128
    G = 5 if NP % 5 == 0 else (4 if NP % 4 == 0 else (2 if NP % 2 == 0 else 1))
    P = Prog()
    qk_in = P.dram_in("qk", [2, 2, 64, T])
    tok_in = P.dram_in("tok", [2, 128, NP, 192])
    gb_in = P.dram_in("gbt", [2, 128, 2, NP])
    if mode == "full":
        o_out = P.dram_out("oT", [2, 64, T])
    else:
        pa_out = P.dram_out("pa", [2, NP, 64, 384])
        pb_out = P.dram_out("pb", [2, NP, 128, 192])
    C = {}
    for nm in ("tri", "bd", "negl", "slm", "ident", "ones", "sel"):
        d_in = P.dram_in(nm, [128, 128])
        t, b = P.sb([128, 128], F32, "c_" + nm)
        P.dma("sp", t[:], d_in[:, :], (), (b,))
        C[nm] = (t, b)
    tri_t, tri_b = C["tri"]
    ident_t, ident_b = C["ident"]
    ones_t, ones_b = C["ones"]
    negl_t, negl_b = C["negl"]
    slm_t, slm_b = C["slm"]
    bd_t, bd_b = C["bd"]
    sel_t, sel_b = C["sel"]

    banks = [P.psum([128, 512]) for _ in range(8)]

    class U:
        pass
    units = []
    for u in range(2):
        S_ = U()
        S_.u = u
        bk = banks[4 * u:4 * u + 4]
        S_.kk = (bk[0][0], 0, bk[0][1])
        S_.qk = (bk[0][0], 128, bk[0][1])
        S_.m = (bk[0][0], 256, bk[0][1])
        S_.n = (bk[0][0], 384, bk[0][1])
        S_.ng = (bk[1][0], 0, bk[1][1])
        S_.tr = (bk[1][0], 128, bk[1][1])
        S_.tr2 = (bk[1][0], 256, bk[1][1])
        S_.qe = (bk[1][0], 384, bk[1][1])
        S_.sqa = (bk[2][0], 0, bk[2][1])
        S_.sqb = (bk[2][0], 128, bk[2][1])
        S_.ap = (bk[2][0], 256, bk[2][1])
        S_.s = (bk[2][0], 384, bk[2][1])
        S_.o = [(bk[3][0], 0, bk[3][1]), (bk[3][0], 128, bk[3][1])]
        if mode == "pre":
            S_.ap = (bk[1][0], 384, bk[1][1])
            S_.qe = (bk[3][0], 256, bk[3][1])
            S_.sqa = (bk[3][0], 0, bk[3][1])
            S_.sqb = (bk[2][0], 128, bk[2][1])
            S_.n = (bk[2][0], 256, bk[2][1])
        S_.pre = bk[3]
        S_.gb = P.sb([128, 2, NP])
        S_.ng_sb = P.sb([128, NP])
        S_.nb = P.sb([128, NP])
        S_.gc = P.sb([128, NP])
        S_.e = P.sb([128, NP])
        S_.kdf = P.sb([128, NP])
        S_.be = P.sb([128, NP])
        S_.GL = P.sb([64, 2, NP])
        S_.qkin = [P.sb([64, 2, G * 128]) for _ in range(2)]
        S_.tokin = [P.sb([128, G, 192]) for _ in range(2)]
        S_.R = [P.sb([128, 128]) for _ in range(2)]
        S_.Dm = [P.sb([128, 128]) for _ in range(2)]
        S_.DmT = [P.sb([128, 128]) for _ in range(2)]
        S_.DmS = [P.sb([128, 128]) for _ in range(2)]
        S_.pw = [P.sb([128, 128]) for _ in range(4)]
        S_.qkT = [P.sb([128, 128]) for _ in range(2)]
        S_.X = [P.sb([128, 128]) for _ in range(3)]
        S_.kd = [P.sb([128, 2, 64]) for _ in range(2)]
        S_.kdfm = P.sb([128, 2, NP])
        S_.nw = [P.sb([128, 64]) for _ in range(2)]
        S_.McT = [P.sb([64, 2, 64]) for _ in range(2)]
        S_.Nsb = [P.sb([64, 128]) for _ in range(2)]
        S_.dE = [P.sb([128, 128]) for _ in range(2)]
        S_.qe_sb = [P.sb([64, 128]) for _ in range(2)]
        S_.S = [P.sb([64, 64]) for _ in range(3)]
        S_.osb = [P.sb([64, 128]) for _ in range(2)]
        S_.si = 0
        S_.xi = 0
        units.append(S_)

    def ps(slot, rows=128, cols=128):
        t, c0, b = slot
        return t[0:rows, c0:c0 + cols], b

    for S_ in units:
        u = S_.u
        gb_t, gb_b = S_.gb
        P.dma("sp", gb_t[:], gb_in[u, :, :, :], (), (gb_b,))
        g_ap, beta_ap = gb_t[:, 0, :], gb_t[:, 1, :]
        P.ts("dve", S_.ng_sb[0][:], g_ap, -1.0, None, ALU.mult, None, (gb_b,), (S_.ng_sb[1],))
        P.ts("dve", S_.nb[0][:], beta_ap, -1.0, None, ALU.mult, None, (gb_b,), (S_.nb[1],))
        pre_t, pre_b = S_.pre
        P.mm(pre_t[:, 0:NP], tri_t[:], g_ap, True, True, (tri_b, gb_b), (pre_b,))
        P.copy("act", S_.gc[0][:], pre_t[:, 0:NP], (pre_b,), (S_.gc[1],))
        P.act(S_.e[0][:], S_.gc[0][:], AF.Exp, (S_.gc[1],), (S_.e[1],))
        P.tt("dve", S_.be[0][:], S_.e[0][:], beta_ap, ALU.mult, (S_.e[1], gb_b), (S_.be[1],))
        P.mm(pre_t[:, 0:NP], bd_t[:], g_ap, True, True, (bd_b, gb_b), (pre_b,))
        P.tt("dve", S_.kdf[0][:], pre_t[:, 0:NP], S_.gc[0][:], ALU.subtract, (pre_b, S_.gc[1]), (S_.kdf[1],))
        P.act(S_.kdf[0][:], S_.kdf[0][:], AF.Exp, (S_.kdf[1],), (S_.kdf[1],))
        for c in range(2):
            P.ts("dve", S_.kdfm[0][:, c, :], S_.kdf[0][:], sel_t[:, c * 64:c * 64 + 1], None, ALU.mult, None,
                 (S_.kdf[1], sel_b), (S_.kdfm[1],))
        for c in range(2):
            P.mm(pre_t[0:64, 0:NP], sel_t[:, c * 64:(c + 1) * 64], g_ap, True, True, (sel_b, gb_b), (pre_b,))
            P.act(S_.GL[0][:, c, :], pre_t[0:64, 0:NP], AF.Exp, (pre_b,), (S_.GL[1],))
        P.memset("dve", S_.S[0][0][:], 0.0, (S_.S[0][1],))

    def load_group(S_, gi):
        u = S_.u
        qk_t, qk_b = S_.qkin[gi % 2]
        tk_t, tk_b = S_.tokin[gi % 2]
        if gi * G >= NP:
            return
        P.dma("pool", qk_t[:], qk_in[u, :, :, gi * G * 128:(gi + 1) * G * 128].rearrange("a d t -> d a t"), (), (qk_b,))
        P.dma("pool", tk_t[:], tok_in[u, :, gi * G:(gi + 1) * G, :], (), (tk_b,))

    def pre(S_, p):
        gi, pi = p // G, p % G
        qk_t, qk_b = S_.qkin[gi % 2]
        tk_t, tk_b = S_.tokin[gi % 2]
        qT = qk_t[:, 0, pi * 128:(pi + 1) * 128]
        kT = qk_t[:, 1, pi * 128:(pi + 1) * 128]
        Qt, Kt, Vt = tk_t[:, pi, 0:64], tk_t[:, pi, 64:128], tk_t[:, pi, 128:192]
        gb_t, gb_b = S_.gb
        j = p % 2
        kk_ap, kk_b = ps(S_.kk)
        qk_ap, qkp_b = ps(S_.qk)
        P.mm(kk_ap, kT, kT, True, True, (qk_b,), (kk_b,))
        yield
        P.mm(qk_ap, kT, qT, True, True, (qk_b,), (qkp_b,))
        yield
        R_t, R_b = S_.R[j]
        P.ts("pool", R_t[:], tri_t[:], S_.ng_sb[0][:, p:p + 1], None, ALU.mult, None, (tri_b, S_.ng_sb[1]), (R_b,))
        yield
        ng_ap, ng_b = ps(S_.ng)
        P.mm(ng_ap, ones_t[:], R_t[:], True, False, (ones_b, R_b), (ng_b,))
        P.mm(ng_ap, ident_t[:], negl_t[:], False, True, (ident_b, negl_b), (ng_b,))
        yield
        Dm_t, Dm_b = S_.Dm[j]
        P.act(Dm_t[:], ng_ap, AF.Exp, (ng_b, S_.gc[1]), (Dm_b,), bias=S_.gc[0][:, p:p + 1])
        yield
        tr_ap, tr_b = ps(S_.tr)
        P.transpose(tr_ap, Dm_t[:], ident_t[:], (Dm_b, ident_b), (tr_b,))
        yield
        DmT_t, DmT_b = S_.DmT[j]
        P.copy("act", DmT_t[:], tr_ap, (tr_b,), (DmT_b,))
        yield
        DmS_t, DmS_b = S_.DmS[j]
        P.tt("pool", DmS_t[:], Dm_t[:], slm_t[:], ALU.mult, (Dm_b, slm_b), (DmS_b,))
        yield
        cur_t, cur_b = S_.pw[0]
        curT_t, curT_b = S_.pw[1]
        P.stt("dve", cur_t[:], kk_ap, S_.nb[0][:, p:p + 1], DmS_t[:], ALU.mult, ALU.mult,
              (kk_b, S_.nb[1], DmS_b), (cur_b,))
        yield
        tr2_ap, tr2_b = ps(S_.tr2)
        P.transpose(tr2_ap, cur_t[:], ident_t[:], (cur_b, ident_b), (tr2_b,))
        yield
        P.copy("act", curT_t[:], tr2_ap, (tr2_b,), (curT_b,))
        yield
        qkT_t, qkT_b = S_.qkT[j]
        P.tt("dve", qkT_t[:], qk_ap, DmT_t[:], ALU.mult, (qkp_b, DmT_b), (qkT_b,))
        yield
        X_t, X_b = S_.X[S_.xi % 3]
        S_.xi += 1
        P.act(X_t[:, 0:64], Kt, AF.Copy, (tk_b, S_.be[1]), (X_b,), scale=S_.be[0][:, p:p + 1])
        yield
        P.act(X_t[:, 64:128], Vt, AF.Copy, (tk_b, gb_b), (X_b,), scale=gb_t[:, 1, p:p + 1])
        yield
        pwi = 0
        for lvl in range(6):
            if lvl < 5:
                nxt_t, nxt_b = S_.pw[(pwi + 2) % 4]
                nxtT_t, nxtT_b = S_.pw[(pwi + 3) % 4]
                sb_ap, sb_b = ps(S_.sqb)
                P.mm(sb_ap, cur_t[:], curT_t[:], True, True, (cur_b, curT_b), (sb_b,))
                yield
                P.copy("act", nxtT_t[:], sb_ap, (sb_b,), (nxtT_b,))
                yield
                if lvl < 4:
                    sa_ap, sa_b = ps(S_.sqa)
                    P.mm(sa_ap, curT_t[:], cur_t[:], True, True, (curT_b, cur_b), (sa_b,))
                    yield
                    P.copy("act", nxt_t[:], sa_ap, (sa_b,), (nxt_b,))
                    yield
            ap_ap, ap_b = ps(S_.ap)
            P.mm(ap_ap, curT_t[:], X_t[:], True, True, (curT_b, X_b), (ap_b,))
            yield
            Xn_t, Xn_b = S_.X[S_.xi % 3]
            S_.xi += 1
            P.tt("dve", Xn_t[:], ap_ap, X_t[:], ALU.add, (ap_b, X_b), (Xn_b,))
            yield
            X_t, X_b = Xn_t, Xn_b
            if lvl < 5:
                cur_t, cur_b, curT_t, curT_b = nxt_t, nxt_b, nxtT_t, nxtT_b
                pwi = (pwi + 2) % 4
        kd_t, kd_b = S_.kd[j]
        nw_t, nw_b = S_.nw[j]
        for c in range(2):
            P.ts("pool", kd_t[:, c, :], Kt, S_.kdfm[0][:, c, p:p + 1], None, ALU.mult, None, (tk_b, S_.kdfm[1]), (kd_b,))
            yield
        P.ts("dve", nw_t[:], X_t[:, 0:64], -1.0, None, ALU.mult, None, (X_b,), (nw_b,))
        yield
        m_t, m0, m_b = S_.m
        n_t, n0, n_b = S_.n
        for c in range(2):
            r0, r1 = c * 64, (c + 1) * 64
            P.mm(m_t[0:64, m0 + r0:m0 + r1], nw_t[:], kd_t[:, c, :], True, True, (nw_b, kd_b), (m_b,))
            yield
            P.mm(n_t[0:64, n0 + r0:n0 + r1], kd_t[:, c, :], X_t[:, 64:128], True, True, (kd_b, X_b), (n_b,))
            yield
        McT_t, McT_b = S_.McT[j]
        for c in range(2):
            P.stt("dve", McT_t[:, c, :], ident_t[0:64, 0:64], S_.GL[0][:, c, p:p + 1],
                  m_t[0:64, m0 + c * 64:m0 + (c + 1) * 64], ALU.mult, ALU.add, (ident_b, S_.GL[1], m_b), (McT_b,))
            yield
        Nsb_t, Nsb_b = S_.Nsb[j]
        P.copy("act", Nsb_t[:], n_t[0:64, n0:n0 + 128], (n_b,), (Nsb_b,))
        yield
        dE_t, dE_b = S_.dE[j]
        P.ts("dve", dE_t[:], ident_t[:], S_.e[0][:, p:p + 1], None, ALU.mult, None, (ident_b, S_.e[1]), (dE_b,))
        yield
        qe_ap, qe_b = ps(S_.qe, 64, 128)
        P.mm(qe_ap, Qt, dE_t[:], True, False, (tk_b, dE_b), (qe_b,))
        P.mm(qe_ap, nw_t[:], qkT_t[:], False, True, (nw_b, qkT_b), (qe_b,))
        yield
        qes_t, qes_b = S_.qe_sb[j]
        P.copy("act", qes_t[:], qe_ap, (qe_b,), (qes_b,))
        yield
        S_.cur = dict(X=(X_t, X_b), qkT=(qkT_t, qkT_b), McT=(McT_t, McT_b), Nsb=(Nsb_t, Nsb_b), qe=(qes_t, qes_b))
        if mode == "pre":
            u_ = S_.u
            P.dma("sp", pa_out[u_, p, :, 0:128], McT_t[:].rearrange("d c k -> d (c k)"), (McT_b,), ())
            P.dma("sp", pa_out[u_, p, :, 128:256], Nsb_t[:], (Nsb_b,), ())
            P.dma("sp", pa_out[u_, p, :, 256:384], qes_t[:], (qes_b,), ())
            P.dma("sp", pb_out[u_, p, :, 0:128], qkT_t[:], (qkT_b,), ())
            P.dma("sp", pb_out[u_, p, :, 128:192], X_t[:, 64:128], (X_b,), ())

    def scan(S_, p, cur):
        X_t, X_b = cur["X"]
        qkT_t, qkT_b = cur["qkT"]
        McT_t, McT_b = cur["McT"]
        Nsb_t, Nsb_b = cur["Nsb"]
        qes_t, qes_b = cur["qe"]
        o_ap, o_b = ps(S_.o[p % 2], 64, 128)
        ot, oc0, _ = S_.o[p % 2]
        P.mm(o_ap, X_t[:, 64:128], qkT_t[:], True, False, (X_b, qkT_b), (o_b,))
        yield
        s_ap, s_b = ps(S_.s, 64, 64)
        for c in range(2):
            St, Sb = S_.S[S_.si % 3]
            P.mm(ot[0:64, oc0 + c * 64:oc0 + (c + 1) * 64], St[:], qes_t[:, c * 64:(c + 1) * 64], False, c == 1,
                 (Sb, qes_b), (o_b,))
            yield
            P.mm(s_ap, McT_t[:, c, :], St[:], True, False, (McT_b, Sb), (s_b,))
            P.mm(s_ap, ident_t[0:64, 0:64], Nsb_t[:, c * 64:(c + 1) * 64], False, True, (ident_b, Nsb_b), (s_b,))
            yield
            S_.si += 1
            Sn, Snb = S_.S[S_.si % 3]
            P.copy("act", Sn[:], s_ap, (s_b,), (Snb,))
            yield
        os_t, os_b = S_.osb[p % 2]
        P.copy("dve", os_t[:], o_ap, (o_b,), (os_b,))
        yield
        P.dma("sp", o_out[S_.u, :, p * 128:(p + 1) * 128], os_t[:], (os_b,), ())

    from itertools import zip_longest
    saved = [None, None]
    for p in range(NP + 1):
        gens = []
        if p < NP:
            for S_ in units:
                if p == 0:
                    load_group(S_, 0)
                if p % G == 0:
                    load_group(S_, p // G + 1)
                gens.append(pre(S_, p))
        if p >= 1 and mode == "full":
            for S_ in units:
                gens.append(scan(S_, p - 1, saved[S_.u]))
        for _ in zip_longest(*gens):
            pass
        saved = [S_.cur for S_ in units]
    return P.finish()


def build_C2b(NP):
    T = NP * 128
    G = 5 if NP % 5 == 0 else (2 if NP % 2 == 0 else 1)
    P = Prog()
    pa_in = P.dram_in("pa", [2, NP, 64, 384])
    pb_in = P.dram_in("pb", [2, NP, 128, 192])
    id_in = P.dram_in("ident", [128, 128])
    o_out = P.dram_out("oT", [2, 64, T])
    ident_t, ident_b = P.sb([128, 128])
    P.dma("sp", ident_t[:], id_in[:, :], (), (ident_b,))
    banks = [P.psum([128, 512]) for _ in range(4)]
    st = []
    for u in range(2):
        d = dict(u=u, o=banks[2 * u], s=banks[2 * u + 1],
                 pa=[P.sb([64, G, 384]) for _ in range(2)], pb=[P.sb([128, G, 192]) for _ in range(2)],
                 S=[P.sb([64, 64]) for _ in range(3)], osb=[P.sb([64, 128]) for _ in range(2)], si=0)
        P.memset("dve", d["S"][0][0][:], 0.0, (d["S"][0][1],))
        st.append(d)

    def scan(d, p):
        gi, pi = p // G, p % G
        pa_t, pa_b = d["pa"][gi % 2]
        pb_t, pb_b = d["pb"][gi % 2]
        if pi == 0:
            for g2 in ([0, 1] if gi == 0 else [gi + 1]):
                if g2 * G < NP:
                    a2_t, a2_b = d["pa"][g2 % 2]
                    b2_t, b2_b = d["pb"][g2 % 2]
                    P.dma("pool", a2_t[:], pa_in[d["u"], g2 * G:(g2 + 1) * G, :, :].rearrange("g d c -> d g c"), (), (a2_b,))
                    P.dma("pool", b2_t[:], pb_in[d["u"], g2 * G:(g2 + 1) * G, :, :].rearrange("g j c -> j g c"), (), (b2_b,))
        ot, o_b = d["o"]
        oc0 = (p % 2) * 128
        P.mm(ot[0:64, oc0:oc0 + 128], pb_t[:, pi, 128:192], pb_t[:, pi, 0:128], True, False, (pb_b,), (o_b,))
        yield
        s_t, s_b = d["s"]
        for c in range(2):
            St, Sb = d["S"][d["si"] % 3]
            P.mm(ot[0:64, oc0 + c * 64:oc0 + (c + 1) * 64], St[:], pa_t[:, pi, 256 + c * 64:256 + (c + 1) * 64],
                 False, c == 1, (Sb, pa_b), (o_b,))
            yield
            P.mm(s_t[0:64, 0:64], pa_t[:, pi, c * 64:(c + 1) * 64], St[:], True, False, (pa_b, Sb), (s_b,))
            P.mm(s_t[0:64, 0:64], ident_t[0:64, 0:64], pa_t[:, pi, 128 + c * 64:128 + (c + 1) * 64], False, True,
                 (ident_b, pa_b), (s_b,))
            yield
            d["si"] += 1
            Sn, Snb = d["S"][d["si"] % 3]
            P.copy("act" if d["u"] == 0 else "dve", Sn[:], s_t[0:64, 0:64], (s_b,), (Snb,))
            yield
        os_t, os_b = d["osb"][p % 2]
        P.copy("dve" if d["u"] == 0 else "act", os_t[:], ot[0:64, oc0:oc0 + 128], (o_b,), (os_b,))
        yield
        P.dma("sp", o_out[d["u"], :, p * 128:(p + 1) * 128], os_t[:], (os_b,), ())

    from itertools import zip_longest
    for p in range(NP):
        for _ in zip_longest(*[scan(d, p) for d in st]):
            pass
    return P.finish()


def c2_unit_inputs(q, k, v, g, beta):
    T = q.shape[1]
    NP = T // 128
    qk = np.stack([q, k])
    tok = np.concatenate([q.T, k.T, v.T], axis=1).reshape(NP, 128, 192).transpose(1, 0, 2)
    gbt = np.stack([g.reshape(NP, 128).T, beta.reshape(NP, 128).T], axis=1)
    return c_(qk), c_(tok), c_(gbt)


def run_C2(unit_list):
    nu = len(unit_list)
    T = unit_list[0][0].shape[1]
    NPU = T // 128
    total = nu * NPU
    NS = 2 * NCORES
    per = -(-total // NS)
    padc = NS * per * 128 - total * 128
    cat2 = lambda idx: np.pad(np.concatenate([u_[idx] for u_ in unit_list], axis=1), ((0, 0), (0, padc)))
    cat1 = lambda idx: np.pad(np.concatenate([u_[idx] for u_ in unit_list]), (0, padc))
    Q, K_, V, Gg, Bb = cat2(0), cat2(1), cat2(2), cat1(3), cat1(4)
    consts = c2_consts()
    nc = build_C2(per, mode="pre")
    ims = []
    for i in range(NCORES):
        parts = []
        for j in range(2):
            s0 = (2 * i + j) * per * 128
            sl = slice(s0, s0 + per * 128)
            parts.append(c2_unit_inputs(Q[:, sl], K_[:, sl], V[:, sl], Gg[sl], Bb[sl]))
        im = {"qk": np.stack([p_[0] for p_ in parts]), "tok": np.stack([p_[1] for p_ in parts]),
              "gbt": np.stack([p_[2] for p_ in parts])}
        im.update(consts)
        ims.append(im)
    res = run(nc, ims)
    pa = np.concatenate([r["pa"].reshape(2 * per, 64, 384) for r in res], axis=0)
    pb = np.concatenate([r["pb"].reshape(2 * per, 128, 192) for r in res], axis=0)
    nc2 = build_C2b(NPU)
    ident = np.eye(128, dtype=np.float32)
    za, zb = np.zeros((NPU, 64, 384), np.float32), np.zeros((NPU, 128, 192), np.float32)
    ims = []
    for i in range(NCORES):
        ua = [pa[(2 * i + j) * NPU:(2 * i + j + 1) * NPU] if 2 * i + j < nu else za for j in range(2)]
        ub = [pb[(2 * i + j) * NPU:(2 * i + j + 1) * NPU] if 2 * i + j < nu else zb for j in range(2)]
        ims.append({"pa": c_(np.stack(ua)), "pb": c_(np.stack(ub)), "ident": ident})
    res = run(nc2, ims)
    return [res[idx // 2]["oT"][idx % 2] for idx in range(nu)]


def tok_blocks(ntl, ntc, bw=512):
    blks = [(t0, min(bw, ntl - t0), 0) for t0 in range(0, ntl, bw)]
    if ntc:
        blks += [(ntl + t0, min(bw, ntc - t0), 1) for t0 in range(0, ntc, bw)]
    return blks


def build_E1(ntl, ntc):
    nt = ntl + ntc
    P = Prog()
    xT = P.dram_in("xT", [8, 128, nt])
    ya = P.dram_in("ya", [2, 128, nt])
    yc = P.dram_in("yc", [3, 128, nt])
    of = P.dram_in("of", [3, 128, nt])
    ob = P.dram_in("ob", [3, 128, nt])
    z = P.dram_in("z", [3, 128, nt])
    w = P.dram_in("w", [1024, 1024])
    mod2 = P.dram_in("mod2", [128, 2, 8])
    dng = P.dram_in("dng", [128, 1])
    bd_in = P.dram_in("bd", [128, 128])
    out = P.dram_out("x1T", [8, 128, nt])
    w_t, w_b = P.sb([128, 8, 1024], BF16, "wout")
    for mc in range(8):
        P.dma("pool", w_t[:, mc, :], w[mc * 128:(mc + 1) * 128, :], (), (w_b,))
    m2_t, m2_b = P.sb([128, 2, 8])
    P.dma("sp", m2_t[:], mod2[:, :, :], (), (m2_b,))
    g_t, g_b = P.sb([128, 1])
    P.dma("sp", g_t[:], dng[:, :], (), (g_b,))
    bd_t, bd_b = P.sb([128, 128])
    P.dma("sp", bd_t[:], bd_in[:, :], (), (bd_b,))
    mixs = [P.sb([128, 8, 512], BF16) for _ in range(2)]
    xs = [P.sb([128, 8, 512]) for _ in range(2)]
    ofs = [P.sb([128, 512]) for _ in range(2)]
    obs = [P.sb([128, 512]) for _ in range(2)]
    zs = [P.sb([128, 512]) for _ in range(2)]
    sqs = [P.sb([128, 512]) for _ in range(2)]
    rs = [P.sb([128, 512]) for _ in range(2)]
    pn = [P.psum([128, 512]) for _ in range(2)]
    pp = [P.psum([128, 512]) for _ in range(4)]
    k = 0
    for bi, (t0, wd_, j) in enumerate(tok_blocks(ntl, ntc)):
        mix_t, mix_b = mixs[bi % 2]
        x_t, x_b = xs[bi % 2]
        P.dma("pool", mix_t[:, 0:2, 0:wd_], ya[:, :, t0:t0 + wd_].rearrange("c p t -> p c t"), (), (mix_b,))
        P.dma("pool", mix_t[:, 5:8, 0:wd_], yc[:, :, t0:t0 + wd_].rearrange("c p t -> p c t"), (), (mix_b,))
        P.dma("pool", x_t[:, :, 0:wd_], xT[:, :, t0:t0 + wd_].rearrange("c p t -> p c t"), (), (x_b,))
        for ch in range(3):
            o_t, o_b = ofs[k % 2]
            b_t, b_b = obs[k % 2]
            z_t, z_b = zs[k % 2]
            s_t, s_b = sqs[k % 2]
            r_t, r_b = rs[k % 2]
            ps_t, ps_b = pn[k % 2]
            k += 1
            P.dma("pool", o_t[:, 0:wd_], of[ch, :, t0:t0 + wd_], (), (o_b,))
            P.dma("pool", b_t[:, 0:wd_], ob[ch, :, t0:t0 + wd_], (), (b_b,))
            P.dma("pool", z_t[:, 0:wd_], z[ch, :, t0:t0 + wd_], (), (z_b,))
            P.tt("dve", o_t[:, 0:wd_], o_t[:, 0:wd_], b_t[:, 0:wd_], ALU.add, (o_b, b_b), (o_b,))
            P.act(s_t[:, 0:wd_], o_t[:, 0:wd_], AF.Square, (o_b,), (s_b,))
            P.mm(ps_t[:, 0:wd_], bd_t[:], s_t[:, 0:wd_], True, True, (bd_b, s_b), (ps_b,))
            P.act(r_t[:, 0:wd_], ps_t[:, 0:wd_], AF.Sqrt, (ps_b,), (r_b,), bias=EPS, scale=1.0 / 64)
            P.recip(r_t[:, 0:wd_], r_t[:, 0:wd_], (r_b,), (r_b,))
            P.act(z_t[:, 0:wd_], z_t[:, 0:wd_], AF.Silu, (z_b,), (z_b,))
            P.stt("dve", o_t[:, 0:wd_], o_t[:, 0:wd_], g_t[:, 0:1], r_t[:, 0:wd_], ALU.mult, ALU.mult,
                  (o_b, g_b, r_b), (o_b,))
            P.tt("dve", mix_t[:, 2 + ch, 0:wd_], o_t[:, 0:wd_], z_t[:, 0:wd_], ALU.mult, (o_b, z_b), (mix_b,))
        for dc in range(8):
            ps_t, ps_b = pp[dc % 4]
            for mc in range(8):
                P.mm(ps_t[:, 0:wd_], w_t[:, mc, dc * 128:(dc + 1) * 128], mix_t[:, mc, 0:wd_], mc == 0, mc == 7,
                     (w_b, mix_b), (ps_b,))
            P.stt("dve", x_t[:, dc, 0:wd_], ps_t[:, 0:wd_], m2_t[:, j, dc:dc + 1], x_t[:, dc, 0:wd_], ALU.mult, ALU.add,
                  (ps_b, m2_b, x_b), (x_b,))
        P.dma("sp", out[:, :, t0:t0 + wd_].rearrange("c p t -> p c t"), x_t[:, :, 0:wd_], (x_b,), ())
    return P.finish()


def build_E2(ntl, ntc, E, F, final):
    nt = ntl + ntc
    ntile = nt // 128
    FG = 256
    nfg = F // FG
    P = Prog()
    x1T = P.dram_in("x1T", [8, 128, nt])
    x1 = P.dram_in("x1", [nt, 1024])
    mod = P.dram_in("mod", [128, 2, 2, 8])
    m5 = P.dram_in("m5", [128, 2, 1024])
    wg = P.dram_in("wg", [E, 1024, F])
    wu = P.dram_in("wu", [E, 1024, F])
    wd = P.dram_in("wd", [E, F, 1024])
    out = P.dram_out("x2", [nt, 1024])
    if E > 1:
        wr = P.dram_in("wr", [128, 8, E])
    if final:
        gn = P.dram_in("gn", [128, 1024])
    mod_t, mod_b = P.sb([128, 2, 2, 8])
    P.dma("sp", mod_t[:], mod[:, :, :, :], (), (mod_b,))
    a_t, a_b = P.sb([128, 2, 8])
    P.ts("dve", a_t[:], mod_t[:, :, 1, :], 1.0, None, ALU.add, None, (mod_b,), (a_b,))
    ones_t, ones_b = P.sb([128, 128], BF16)
    P.memset("dve", ones_t[:], 1.0, (ones_b,))
    hx_t, hx_b = P.sb([128, 8, nt], BF16, "hx")
    acc_t, acc_b = P.sb([128, ntile, 1024], F32, "acc")
    P.memset("dve", acc_t[:], 0.0, (acc_b,))
    if E > 1:
        wr_t, wr_b = P.sb([128, 8, E])
        P.dma("sp", wr_t[:], wr[:, :, :], (), (wr_b,))
        lg_t, lg_b = P.sb([128, ntile, E])
        gate_t, gate_b = P.sb([128, ntile, E])
    xs = [P.sb([128, 8, TT]) for _ in range(2)]
    sqs = [P.sb([128, 8, TT], BF16) for _ in range(2)]
    hfs = [P.sb([128, 8, TT]) for _ in range(2)]
    rs = [P.sb([128, TT]) for _ in range(2)]
    pn = P.psum([128, 512])
    pr = P.psum([128, 512])
    for it, (t0, wd_, j) in enumerate(tok_blocks(ntl, ntc, TT)):
        x_t, x_b = xs[it % 2]
        sq_t, sq_b = sqs[it % 2]
        hf_t, hf_b = hfs[it % 2]
        r_t, r_b = rs[it % 2]
        P.dma("sp", x_t[:], x1T[:, :, t0:t0 + TT].rearrange("c p t -> p c t"), (), (x_b,))
        P.act(sq_t[:], x_t[:], AF.Square, (x_b,), (sq_b,))
        for kc in range(8):
            P.mm(pn[0][:, 0:TT], ones_t[:], sq_t[:, kc, :], kc == 0, kc == 7, (ones_b, sq_b), (pn[1],))
        P.act(r_t[:], pn[0][:, 0:TT], AF.Sqrt, (pn[1],), (r_b,), bias=EPS, scale=1.0 / 1024)
        P.recip(r_t[:], r_t[:], (r_b,), (r_b,))
        for kc in range(8):
            P.stt("dve", hf_t[:, kc, :], x_t[:, kc, :], a_t[:, j, kc:kc + 1], r_t[:], ALU.mult, ALU.mult,
                  (x_b, a_b, r_b), (hf_b,))
            P.act(hf_t[:, kc, :], hf_t[:, kc, :], AF.Identity, (hf_b, mod_b), (hf_b,), bias=mod_t[:, j, 0, kc:kc + 1])
        P.copy("pool", hx_t[:, :, t0:t0 + TT], hf_t[:], (hf_b,), (hx_b,))
        if E > 1:
            for sub in range(TT // 128):
                tile = t0 // 128 + sub
                for kc in range(8):
                    P.mm(pr[0][:, 0:E], hf_t[:, kc, sub * 128:(sub + 1) * 128], wr_t[:, kc, :], kc == 0, kc == 7,
                         (hf_b, wr_b), (pr[1],))
                P.copy("act", lg_t[:, tile, :], pr[0][:, 0:E], (pr[1],), (lg_b,))
    if E > 1:
        m1_t, m1_b = P.sb([128, 1])
        m2_t, m2_b = P.sb([128, 1])
        e1_t, e1_b = P.sb([128, E])
        e2_t, e2_b = P.sb([128, E])
        l2_t, l2_b = P.sb([128, E])
        g1_t, g1_b = P.sb([128, 1])
        g2_t, g2_b = P.sb([128, 1])
        for tile in range(ntile):
            l_ap = lg_t[:, tile, :]
            P.op("dve", lambda e, o=m1_t[:], i=l_ap: e.reduce_max(o, i, axis=mybir.AxisListType.X), (lg_b,), (m1_b,))
            P.ts("dve", e1_t[:], l_ap, m1_t[:, 0:1], None, ALU.is_equal, None, (lg_b, m1_b), (e1_b,))
            P.stt("dve", l2_t[:], e1_t[:], -1e9, l_ap, ALU.mult, ALU.add, (e1_b, lg_b), (l2_b,))
            P.op("dve", lambda e, o=m2_t[:], i=l2_t[:]: e.reduce_max(o, i, axis=mybir.AxisListType.X), (l2_b,), (m2_b,))
            P.ts("dve", e2_t[:], l2_t[:], m2_t[:, 0:1], None, ALU.is_equal, None, (l2_b, m2_b), (e2_b,))
            P.tt("dve", g1_t[:], m2_t[:], m1_t[:], ALU.subtract, (m2_b, m1_b), (g1_b,))
            P.act(g1_t[:], g1_t[:], AF.Exp, (g1_b,), (g1_b,))
            P.ts("dve", g1_t[:], g1_t[:], 1.0, None, ALU.add, None, (g1_b,), (g1_b,))
            P.recip(g1_t[:], g1_t[:], (g1_b,), (g1_b,))
            P.ts("dve", g2_t[:], g1_t[:], -1.0, 1.0, ALU.mult, ALU.add, (g1_b,), (g2_b,))
            P.ts("dve", e1_t[:], e1_t[:], g1_t[:, 0:1], None, ALU.mult, None, (e1_b, g1_b), (e1_b,))
            P.stt("dve", gate_t[:, tile, :], e2_t[:], g2_t[:, 0:1], e1_t[:], ALU.mult, ALU.add,
                  (e2_b, g2_b, e1_b), (gate_b,))
    wgs = [P.sb([128, 8, FG], BF16) for _ in range(2)]
    wus = [P.sb([128, 8, FG], BF16) for _ in range(2)]
    wds = [P.sb([128, 2, 1024], BF16) for _ in range(2)]
    pg = [P.psum([128, 512]) for _ in range(2)]
    pu = [P.psum([128, 512]) for _ in range(2)]
    pd = [P.psum([128, 512]) for _ in range(2)]
    sgs = [P.sb([128, 512], BF16) for _ in range(2)]
    Hs = [P.sb([128, 2, 512], BF16) for _ in range(2)]
    blocks = tok_blocks(ntl, ntc)
    jobs = [(e_, fg, bi) for e_ in range(E) for fg in range(nfg) for bi in range(len(blocks))]
    nd = [0]

    def load_w(it):
        if it >= E * nfg:
            return
        e_, fg = it // nfg, it % nfg
        f0 = fg * FG
        wg_t, wg_b = wgs[it % 2]
        wu_t, wu_b = wus[it % 2]
        wd_t, wd_b = wds[it % 2]
        P.dma("pool", wg_t[:], wg[e_, :, f0:f0 + FG].rearrange("(kc p) f -> p kc f", p=128), (), (wg_b,))
        P.dma("pool", wu_t[:], wu[e_, :, f0:f0 + FG].rearrange("(kc p) f -> p kc f", p=128), (), (wu_b,))
        P.dma("pool", wd_t[:], wd[e_, f0:f0 + FG, :].rearrange("(fc p) d -> p fc d", p=128), (), (wd_b,))

    pd4 = pd + [pn, pr]

    def GU(k):
        e_, fg, bi = jobs[k]
        it = e_ * nfg + fg
        if bi == 0 and it == 0:
            load_w(0)
        if bi == 1:
            load_w(it + 1)
        wg_t, wg_b = wgs[it % 2]
        wu_t, wu_b = wus[it % 2]
        t0, wd_, j = blocks[bi]
        H_t, H_b = Hs[k % 2]
        for fc in range(2):
            g_t, g_b = pg[fc]
            u_t, u_b = pu[fc]
            for kc in range(8):
                P.mm(g_t[:, 0:wd_], wg_t[:, kc, fc * 128:(fc + 1) * 128], hx_t[:, kc, t0:t0 + wd_],
                     kc == 0, kc == 7, (wg_b, hx_b), (g_b,))
                if kc == 3:
                    yield
            yield
            for kc in range(8):
                P.mm(u_t[:, 0:wd_], wu_t[:, kc, fc * 128:(fc + 1) * 128], hx_t[:, kc, t0:t0 + wd_],
                     kc == 0, kc == 7, (wu_b, hx_b), (u_b,))
                if kc == 3:
                    yield
            sg_t, sg_b = sgs[fc]
            P.act(sg_t[:, 0:wd_], g_t[:, 0:wd_], AF.Silu, (g_b,), (sg_b,))
            yield
            P.tt("dve", H_t[:, fc, 0:wd_], u_t[:, 0:wd_], sg_t[:, 0:wd_], ALU.mult, (u_b, sg_b), (H_b,))
            yield

    def DOWN(k):
        e_, fg, bi = jobs[k]
        it = e_ * nfg + fg
        wd_t, wd_b = wds[it % 2]
        t0, wd_, j = blocks[bi]
        H_t, H_b = Hs[k % 2]
        for sub in range(wd_ // 128):
            tile = t0 // 128 + sub
            for dh in range(2):
                d_t, d_b = pd4[nd[0] % 4]
                nd[0] += 1
                for fc in range(2):
                    P.mm(d_t[:], H_t[:, fc, sub * 128:(sub + 1) * 128], wd_t[:, fc, dh * 512:(dh + 1) * 512],
                         fc == 0, fc == 1, (H_b, wd_b), (d_b,))
                acc_ap = acc_t[:, tile, dh * 512:(dh + 1) * 512]
                if E > 1:
                    P.stt("dve", acc_ap, d_t[:], gate_t[:, tile, e_:e_ + 1], acc_ap, ALU.mult, ALU.add,
                          (d_b, gate_b, acc_b), (acc_b,))
                else:
                    P.tt("dve", acc_ap, d_t[:], acc_ap, ALU.add, (d_b, acc_b), (acc_b,))
                yield

    from itertools import zip_longest
    for k in range(len(jobs) + 1):
        gens = []
        if k < len(jobs):
            gens.append(GU(k))
        if k >= 1:
            gens.append(DOWN(k - 1))
        for _ in zip_longest(*gens):
            pass
    m5_t, m5_b = P.sb([128, 2, 1024])
    P.dma("sp", m5_t[:], m5[:, :, :], (), (m5_b,))
    if final:
        gn_t, gn_b = P.sb([128, 1024])
        P.dma("sp", gn_t[:], gn[:, :], (), (gn_b,))
        ss_t, ss_b = P.sb([128, 1])
        tq = [P.sb([128, 1024]) for _ in range(2)]
    x1s = [P.sb([128, 1024]) for _ in range(2)]
    for tile in range(ntile):
        j = 0 if tile * 128 < ntl else 1
        x_t, x_b = x1s[tile % 2]
        P.dma("sp", x_t[:], x1[tile * 128:(tile + 1) * 128, :], (), (x_b,))
        P.tt("pool", acc_t[:, tile, :], acc_t[:, tile, :], m5_t[:, j, :], ALU.mult, (acc_b, m5_b), (acc_b,))
        P.tt("dve", x_t[:], x_t[:], acc_t[:, tile, :], ALU.add, (x_b, acc_b), (x_b,))
        if final:
            q_t, q_b = tq[tile % 2]
            P.tt("pool", q_t[:], x_t[:], x_t[:], ALU.mult, (x_b,), (q_b,))
            P.op("dve", lambda e, o=ss_t[:], i=q_t[:]: e.reduce_sum(o, i, axis=mybir.AxisListType.X), (q_b,), (ss_b,))
            P.act(ss_t[:], ss_t[:], AF.Sqrt, (ss_b,), (ss_b,), bias=EPS, scale=1.0 / 1024)
            P.recip(ss_t[:], ss_t[:], (ss_b,), (ss_b,))
            P.stt("dve", x_t[:], x_t[:], ss_t[:, 0:1], gn_t[:], ALU.mult, ALU.mult, (x_b, ss_b, gn_b), (x_b,))
        P.dma("sp", out[tile * 128:(tile + 1) * 128, :], x_t[:], (x_b,), ())
    return P.finish()


def stage_C2(c1, need_ctx):
    dn, dnc, gb, gbc = c1["dn"], c1["dnc"], c1["gb"], c1["gbc"]
    units = []
    for h in range(6):
        sl = slice(h * 64, (h + 1) * 64)
        for d in range(2):
            parts = []
            for r in (0, 384, 768):
                a_c, a_x = dnc[r:r + 384][sl], dn[r:r + 384][sl]
                if d == 1:
                    a_c, a_x = a_c[:, ::-1], a_x[:, ::-1]
                parts.append(np.concatenate([a_c, a_x], axis=1))
            gs = []
            for w_ in range(2):
                g_c, g_x = gbc[w_, d * 6 + h], gb[w_, d * 6 + h]
                if d == 1:
                    g_c, g_x = g_c[::-1], g_x[::-1]
                gs.append(np.concatenate([g_c, g_x]))
            units.append((parts[0], parts[1], parts[2], gs[0], gs[1]))
    outs = run_C2(units)
    of = np.zeros((384, SEQ), np.float32)
    ob = np.zeros((384, SEQ), np.float32)
    ofc = np.zeros((384, CTX), np.float32)
    obc = np.zeros((384, CTX), np.float32)
    for h in range(6):
        sl = slice(h * 64, (h + 1) * 64)
        f, b = outs[2 * h], outs[2 * h + 1]
        of[sl] = f[:, CTX:]
        ofc[sl] = f[:, :CTX]
        ob[sl] = b[:, CTX:][:, ::-1]
        obc[sl] = b[:, :CTX][:, ::-1]
    return of, ob, ofc, obc


def stage_E1(xT, hcT, c1, of, ob, ofc, obc, px, pc, w_out_l, modT_l, dng_l, need_ctx):
    ntl = SEQ // NCORES
    ntc = CTX if need_ctx else 0
    nc = build_E1(ntl, ntc)
    m = modT_l.reshape(128, 2, 6, 8)
    mod2 = c_(m[:, :, 2, :])
    dng = c_(np.tile(dng_l.reshape(64), 2).reshape(128, 1))
    bd = c_(np.kron(np.eye(2), np.ones((64, 64))))
    zx, zc = px[R_DZ:R_DZ + 384], pc[R_DZ:R_DZ + 384]

    def cat(a, b, i):
        sl = a[:, i * ntl:(i + 1) * ntl]
        return np.concatenate([sl, b], axis=1) if need_ctx else sl
    ims = []
    for i in range(NCORES):
        nt = ntl + ntc
        ims.append({
            "xT": c_(cat(xT, hcT, i).reshape(8, 128, nt)),
            "ya": c_(cat(c1["yaT"], c1.get("yaTc"), i).reshape(2, 128, nt)),
            "yc": c_(cat(c1["ycT"], c1.get("ycTc"), i).reshape(3, 128, nt)),
            "of": c_(cat(of, ofc, i).reshape(3, 128, nt)),
            "ob": c_(cat(ob, obc, i).reshape(3, 128, nt)),
            "z": c_(cat(zx, zc, i).reshape(3, 128, nt)),
            "w": c_(w_out_l), "mod2": mod2, "dng": dng, "bd": bd,
        })
    res = run(nc, ims)
    x1T = np.concatenate([r["x1T"][:, :, :ntl].reshape(1024, ntl) for r in res], axis=1)
    hc1T = res[0]["x1T"][:, :, ntl:].reshape(1024, CTX) if need_ctx else None
    return x1T, hc1T


def stage_E2(x1T, hc1T, modT_l, wg, wu, wd, wr, gn, need_ctx):
    ntl = SEQ // NCORES
    ntc = CTX if need_ctx else 0
    E, _, F = wg.shape
    final = gn is not None
    nc = build_E2(ntl, ntc, E, F, final)
    m = modT_l.reshape(128, 2, 6, 8)
    mod = c_(m[:, :, 3:5, :])
    m5 = np.stack([np.broadcast_to(m[:, j, 5, :].T.reshape(1, 1024), (128, 1024)) for j in range(2)], axis=1)
    wg, wu, wd = c_(wg), c_(wu), c_(wd)
    ims = []
    for i in range(NCORES):
        sl = x1T[:, i * ntl:(i + 1) * ntl]
        blk = np.concatenate([sl, hc1T], axis=1) if need_ctx else sl
        nt = ntl + ntc
        im = {"x1T": c_(blk.reshape(8, 128, nt)), "x1": c_(blk.T), "mod": mod, "m5": c_(m5),
              "wg": wg, "wu": wu, "wd": wd}
        if E > 1:
            im["wr"] = c_(wr.reshape(8, 128, E).transpose(1, 0, 2))
        if final:
            im["gn"] = c_(np.broadcast_to(gn.reshape(1, 1024), (128, 1024)))
        ims.append(im)
    res = run(nc, ims)
    x2 = np.concatenate([r["x2"][:ntl] for r in res], axis=0)
    hc2 = res[0]["x2"][ntl:] if need_ctx else None
    return x2, hc2


def kernel(x, c, ctx, c_ctx, w_mod, b_mod, w_in, w_out, conv_w, dn_conv_w, dn_a_log, dn_dt_bias, dn_norm_g,
           attn_sink, ffn_w_gate, ffn_w_up, ffn_w_down, moe_router, moe_w_gate, moe_w_up, moe_w_down,
           final_norm_g):
    f = lambda a: np.asarray(a, dtype=np.float32)
    x, c, ctx, c_ctx, w_mod, b_mod, w_in, w_out = map(f, (x, c, ctx, c_ctx, w_mod, b_mod, w_in, w_out))
    modT = stage_A(c, c_ctx, w_mod, b_mod)
    xT = np.ascontiguousarray(x[0].T)
    hcT = np.ascontiguousarray(ctx[0].T)
    x2 = None
    for l in range(2):
        need_ctx = l == 0
        px, pc = stage_B(xT, hcT, w_in[l], modT[l])
        c1 = stage_C1(px, pc, f(conv_w)[l], f(attn_sink)[l], f(dn_conv_w)[l], f(dn_a_log)[l], f(dn_dt_bias)[l],
                      need_ctx)
        of, ob, ofc, obc = stage_C2(c1, need_ctx)
        x1T, hc1T = stage_E1(xT, hcT, c1, of, ob, ofc, obc, px, pc, w_out[l], modT[l], f(dn_norm_g)[l], need_ctx)
        if l == 0:
            x2, hc2 = stage_E2(x1T, hc1T, modT[l], f(ffn_w_gate), f(ffn_w_up), f(ffn_w_down), None, None, True)
            xT = np.ascontiguousarray(x2.T)
            hcT = np.ascontiguousarray(hc2.T)
        else:
            x2, _ = stage_E2(x1T, None, modT[l], f(moe_w_gate)[0], f(moe_w_up)[0], f(moe_w_down)[0],
                             f(moe_router)[0], f(final_norm_g), False)
    return x2.reshape(1, SEQ, D_MODEL).astype(np.float32)
```
